# Optimizing a Trainium2 kernel written in Bass

```python
import jax, jax.numpy as jnp
from jax import lax
import numpy as np

D_MODEL = 1024
BATCH = 32
SEQ = 2048
DEPTH = 1

GRID_W = 64
CTX_LEN = 256
NA_HEADS = 8
NA_HEAD_DIM = 64
NA_WIDTH = NA_HEADS * NA_HEAD_DIM
WIN_H = 8
WIN_W = 16
NA_KEY_COLS = 2 * WIN_W
ML_HEADS = 4
ML_QK_DIM = 64
ML_V_DIM = 128
ML_QK_WIDTH = ML_HEADS * ML_QK_DIM
ML_WIDTH = ML_HEADS * ML_V_DIM
ML_CHUNK = 64
GATE_SOFTCAP = 15.0
ROPE_BASE = 10000.0
N_EXPERTS = 32
TOP_K = 4
D_EXPERT = D_MODEL
SWIGLU_ALPHA = 1.702
SWIGLU_LIMIT = 7.0
EXPERT_BLOCK = 128
NORM_EPS = 1e-6
NEG_INF = -1e30
CTX_SPLITS = (NA_WIDTH, NA_WIDTH, ML_QK_WIDTH, ML_WIDTH, 4 * ML_HEADS)
LAT_SPLITS = (NA_WIDTH, ML_QK_WIDTH, ML_WIDTH, D_MODEL, D_MODEL)
IN_SPLITS = CTX_SPLITS + LAT_SPLITS
N_CTX_COLS = 2 * NA_WIDTH + ML_QK_WIDTH + ML_WIDTH + 4 * ML_HEADS
N_IN_COLS = N_CTX_COLS + NA_WIDTH + ML_QK_WIDTH + ML_WIDTH + 2 * D_MODEL

kernel_name = 'hybrid_natten_mlstm_moe_dit_layer'


def _rmsnorm(x, g):
    xf = x.astype(jnp.float32)
    y = xf * lax.rsqrt(jnp.mean(xf * xf, axis=-1, keepdims=True) + NORM_EPS)
    return (y * g.astype(jnp.float32)).astype(x.dtype)


def _split(a, sizes):
    return jnp.split(a, np.cumsum(sizes)[:-1].tolist(), axis=-1)


def _na_column_tables():
    n_qb = GRID_W // WIN_W
    qcol = np.arange(GRID_W).reshape(n_qb, WIN_W)
    kstart = np.clip(np.arange(n_qb) * WIN_W - WIN_W // 2, 0, GRID_W - NA_KEY_COLS)
    kcol = kstart[:, None] + np.arange(NA_KEY_COLS)
    cs = np.clip(qcol - WIN_W // 2, 0, GRID_W - WIN_W)
    kc = kcol[:, None, :]
    mask = (kc >= cs[:, :, None]) & (kc < cs[:, :, None] + WIN_W)
    dc = np.clip(kc - qcol[:, :, None], -(WIN_W - 1), WIN_W - 1) + (WIN_W - 1)
    return kcol, mask, dc


def _neighbourhood_attention(q, k, v, k_ctx, v_ctx, rpb):
    B, S, _ = q.shape
    rows = S // GRID_W
    wr = min(WIN_H, rows)
    n_qb = GRID_W // WIN_W
    kcol, mask, dc = _na_column_tables()
    grid = (B, rows, GRID_W, NA_HEADS, NA_HEAD_DIM)
    q = q.reshape(grid) * NA_HEAD_DIM ** -0.5
    k = k.reshape(grid)
    v = v.reshape(grid)
    k_ctx = k_ctx.reshape(B, -1, NA_HEADS, NA_HEAD_DIM)
    v_ctx = v_ctx.reshape(B, -1, NA_HEADS, NA_HEAD_DIM)
    n_loc = wr * NA_KEY_COLS

    def one_row(r):
        rs = jnp.clip(r - WIN_H // 2, 0, rows - wr)
        qb = lax.dynamic_index_in_dim(q, r, axis=1, keepdims=False).reshape(B, n_qb, WIN_W, NA_HEADS, NA_HEAD_DIM)
        kb = lax.dynamic_slice_in_dim(k, rs, wr, axis=1)[:, :, kcol]
        vb = lax.dynamic_slice_in_dim(v, rs, wr, axis=1)[:, :, kcol]
        dr = rs + jnp.arange(wr) - r + (WIN_H - 1)
        bias = rpb[:, dr[None, None, :, None], dc[:, :, None, :]].astype(jnp.float32)
        s_loc = jnp.einsum('bjqhd,brjkhd->bhjqrk', qb, kb).astype(jnp.float32) + bias
        s_loc = jnp.where(mask[:, :, None, :], s_loc, NEG_INF).reshape(B, NA_HEADS, n_qb, WIN_W, n_loc)
        s_ctx = jnp.einsum('bjqhd,bchd->bhjqc', qb, k_ctx).astype(jnp.float32)
        p = jax.nn.softmax(jnp.concatenate([s_loc, s_ctx], axis=-1), axis=-1).astype(v.dtype)
        p_loc = p[..., :n_loc].reshape(B, NA_HEADS, n_qb, WIN_W, wr, NA_KEY_COLS)
        o = (jnp.einsum('bhjqrk,brjkhd->bjqhd', p_loc, vb)
             + jnp.einsum('bhjqc,bchd->bjqhd', p[..., n_loc:], v_ctx))
        return o.reshape(B, GRID_W, NA_WIDTH)

    o = lax.map(one_row, jnp.arange(rows, dtype=jnp.int32))
    return jnp.moveaxis(o, 0, 1).reshape(B, S, NA_WIDTH)


def _axial_rope(x, row, col):
    n_freq = ML_QK_DIM // 4
    inv_freq = ROPE_BASE ** (-jnp.arange(n_freq, dtype=jnp.float32) / n_freq)

    def rotate(xh, pos):
        ang = pos[:, None] * inv_freq
        cos = jnp.cos(ang)[:, None, :]
        sin = jnp.sin(ang)[:, None, :]
        x1, x2 = xh[..., :n_freq], xh[..., n_freq:]
        return jnp.concatenate([x1 * cos - x2 * sin, x2 * cos + x1 * sin], axis=-1)

    half = ML_QK_DIM // 2
    return jnp.concatenate([rotate(x[..., :half], row), rotate(x[..., half:], col)], axis=-1)


def _to_chunks(a):
    B, H, T = a.shape[:3]
    a = a.reshape((B, H, T // ML_CHUNK, ML_CHUNK) + a.shape[3:])
    return jnp.moveaxis(a, 2, 0)


def _mlstm_chunkwise(q, k, v, log_i, log_f, state):
    tril = np.tril(np.ones((ML_CHUNK, ML_CHUNK), dtype=bool))
    with_out = q is not None

    def step(carry, inp):
        C, n, m = carry
        if with_out:
            qc, kc, vc, li, lf = inp
        else:
            kc, vc, li, lf = inp
        b = jnp.cumsum(lf, axis=-1)
        bL = b[..., -1]
        dec = bL[..., None] - b + li
        m_new = jnp.maximum(bL + m, jnp.max(dec, axis=-1))
        w_s = jnp.exp(dec - m_new[..., None])
        w_c = jnp.exp(bL + m - m_new)
        C_new = w_c[..., None, None] * C + jnp.einsum('bhs,bhsd,bhse->bhde', w_s, kc, vc)
        n_new = w_c[..., None] * n + jnp.einsum('bhs,bhsd->bhd', w_s, kc)
        if not with_out:
            return (C_new, n_new, m_new), None
        d_mat = jnp.where(tril, b[..., :, None] - b[..., None, :] + li[..., None, :], -jnp.inf)
        m_inter = b + m[..., None]
        m_t = jnp.maximum(m_inter, jnp.max(d_mat, axis=-1))
        w_intra = jnp.exp(d_mat - m_t[..., None])
        w_inter = jnp.exp(m_inter - m_t)
        s = jnp.einsum('bhtd,bhsd->bhts', qc, kc) * w_intra
        num = (w_inter[..., None] * jnp.einsum('bhtd,bhde->bhte', qc, C)
               + jnp.einsum('bhts,bhse->bhte', s, vc))
        den = w_inter * jnp.einsum('bhtd,bhd->bht', qc, n) + jnp.sum(s, axis=-1)
        h = num / jnp.maximum(jnp.abs(den), jnp.exp(-m_t))[..., None]
        return (C_new, n_new, m_new), h

    if with_out:
        xs = (_to_chunks(q), _to_chunks(k), _to_chunks(v), _to_chunks(log_i), _to_chunks(log_f))
    else:
        xs = (_to_chunks(k), _to_chunks(v), _to_chunks(log_i), _to_chunks(log_f))
    final, hs = lax.scan(step, state, xs)
    if not with_out:
        return final
    hs = jnp.moveaxis(hs, 0, 2)
    return hs.reshape(hs.shape[0], hs.shape[1], -1, hs.shape[-1])


def _mlstm_branch(q, k, v, o_pre, gates, k_ctx, v_ctx, gates_ctx, b_gates, g_head, row, col):
    B, S, _ = q.shape
    n_ctx = k_ctx.shape[1]
    f32 = jnp.float32
    qk_scale = ML_QK_DIM ** -0.5

    def heads_first(a):
        return jnp.swapaxes(a, 1, 2)

    def flip(a):
        return jnp.flip(a, axis=2)

    q = heads_first(_axial_rope(q.reshape(B, S, ML_HEADS, ML_QK_DIM).astype(f32), row, col))
    k = heads_first(_axial_rope(k.reshape(B, S, ML_HEADS, ML_QK_DIM).astype(f32), row, col) * qk_scale)
    v = heads_first(v.reshape(B, S, ML_HEADS, ML_V_DIM).astype(f32))
    k_ctx = heads_first(k_ctx.reshape(B, n_ctx, ML_HEADS, ML_QK_DIM).astype(f32) * qk_scale)
    v_ctx = heads_first(v_ctx.reshape(B, n_ctx, ML_HEADS, ML_V_DIM).astype(f32))

    def gate_logs(g):
        t = g.shape[1]
        g = g.reshape(B, t, 4, ML_HEADS).astype(f32) + b_gates.astype(f32)
        g = GATE_SOFTCAP * jnp.tanh(g / GATE_SOFTCAP)
        g = jnp.transpose(g, (2, 0, 3, 1))
        return g[0], jax.nn.log_sigmoid(g[1]), g[2], jax.nn.log_sigmoid(g[3])

    li_f, lf_f, li_b, lf_b = gate_logs(gates)
    lic_f, lfc_f, lic_b, lfc_b = gate_logs(gates_ctx)
    state0 = (jnp.zeros((B, ML_HEADS, ML_QK_DIM, ML_V_DIM), f32),
              jnp.zeros((B, ML_HEADS, ML_QK_DIM), f32),
              jnp.zeros((B, ML_HEADS), f32))
    st_f = _mlstm_chunkwise(None, k_ctx, v_ctx, lic_f, lfc_f, state0)
    h_f = _mlstm_chunkwise(q, k, v, li_f, lf_f, st_f)
    st_b = _mlstm_chunkwise(None, flip(k_ctx), flip(v_ctx), flip(lic_b), flip(lfc_b), state0)
    h_b = flip(_mlstm_chunkwise(flip(q), flip(k), flip(v), flip(li_b), flip(lf_b), st_b))
    h = heads_first(h_f + h_b)
    h = h * lax.rsqrt(jnp.mean(h * h, axis=-1, keepdims=True) + NORM_EPS) * g_head.reshape(ML_HEADS, ML_V_DIM).astype(f32)
    return (jax.nn.sigmoid(o_pre.astype(f32)) * h.reshape(B, S, ML_WIDTH)).astype(o_pre.dtype)


def _clamped_swiglu(g, l):
    g = jnp.minimum(g, SWIGLU_LIMIT)
    l = jnp.clip(l, -SWIGLU_LIMIT, SWIGLU_LIMIT)
    return g * jax.nn.sigmoid(SWIGLU_ALPHA * g) * (l + 1.0)


def _moe_ffn(h, w_router, b_router, w_gate, b_gate, w_lin, b_lin, w_down, b_down):
    B, S, D = h.shape
    n_tok = B * S
    n_assign = n_tok * TOP_K
    xf = h.reshape(n_tok, D)
    logits = (xf @ w_router + b_router).astype(jnp.float32)
    top_logit, top_e = lax.top_k(logits, TOP_K)
    top_w = jax.nn.softmax(top_logit, axis=-1)
    e_flat = top_e.reshape(-1)
    order = jnp.argsort(e_flat)
    e_sorted = e_flat[order]
    counts = jnp.bincount(e_flat, length=N_EXPERTS)
    padded = (counts + EXPERT_BLOCK - 1) // EXPERT_BLOCK * EXPERT_BLOCK
    pad_end = jnp.cumsum(padded)
    pad_start = pad_end - padded
    start = jnp.cumsum(counts) - counts
    dest = pad_start[e_sorted] + jnp.arange(n_assign) - start[e_sorted]
    n_rows = -(-n_assign // EXPERT_BLOCK) * EXPERT_BLOCK + N_EXPERTS * EXPERT_BLOCK
    n_blocks = n_rows // EXPERT_BLOCK
    row_tok = jnp.full((n_rows,), n_tok, jnp.int32).at[dest].set((order // TOP_K).astype(jnp.int32))
    row_w = jnp.zeros((n_rows,), jnp.float32).at[dest].set(top_w.reshape(-1)[order])
    blk_start = jnp.arange(n_blocks) * EXPERT_BLOCK
    blk_e = jnp.minimum(jnp.sum(blk_start[:, None] >= pad_end[None, :], axis=1), N_EXPERTS - 1)
    x_pad = jnp.concatenate([xf, jnp.zeros((1, D), xf.dtype)], axis=0)

    def expert_block(args):
        idx, wts, e = args
        xb = x_pad[idx]
        a = _clamped_swiglu(xb @ w_gate[e] + b_gate[e], xb @ w_lin[e] + b_lin[e])
        y = a @ w_down[e] + b_down[e]
        return (y * wts[:, None]).astype(h.dtype)

    y = lax.map(expert_block, (row_tok.reshape(n_blocks, EXPERT_BLOCK),
                               row_w.reshape(n_blocks, EXPERT_BLOCK), blk_e))
    out = jnp.zeros((n_tok + 1, D), h.dtype).at[row_tok].add(y.reshape(n_rows, D))
    return out[:n_tok].reshape(B, S, D)


def setup_inputs(seed: int = 0) -> dict:
    key = jax.random.key(seed)
    ks = jax.random.split(key, 28)
    f32 = jnp.float32
    D, E, F = D_MODEL, N_EXPERTS, D_EXPERT

    def nrm(k, shape, scale):
        return scale * jax.random.normal(k, shape, f32)

    def gain(k, shape):
        return 1.0 + 0.02 * jax.random.normal(k, shape, f32)

    f_bias = 3.0 + 3.0 * jax.random.uniform(ks[11], (DEPTH, 2, ML_HEADS), f32)
    i_bias = nrm(ks[12], (DEPTH, 2, ML_HEADS), 0.1)
    b_mlstm_gates = jnp.stack([i_bias[:, 0], f_bias[:, 0], i_bias[:, 1], f_bias[:, 1]], axis=1)
    return {
        'x': jax.random.normal(ks[0], (BATCH, SEQ, D), f32),
        'c': jax.random.normal(ks[1], (BATCH, D), f32),
        'ctx': jax.random.normal(ks[2], (BATCH, CTX_LEN, D), f32),
        'c_ctx': jax.random.normal(ks[3], (D,), f32),
        'w_ada': nrm(ks[4], (DEPTH, D, 6 * D), 0.5 * D ** -0.5),
        'b_ada': nrm(ks[5], (DEPTH, 6 * D), 0.02),
        'g_mix_pre': gain(ks[6], (DEPTH, D)),
        'g_mix_post': gain(ks[7], (DEPTH, D)),
        'g_ffn_pre': gain(ks[8], (DEPTH, D)),
        'g_ffn_post': gain(ks[9], (DEPTH, D)),
        'w_in': nrm(ks[10], (DEPTH, D, N_IN_COLS), D ** -0.5),
        'b_mlstm_gates': b_mlstm_gates,
        'rpb': nrm(ks[13], (DEPTH, NA_HEADS, 2 * WIN_H - 1, 2 * WIN_W - 1), 0.1),
        'g_mlstm_head': gain(ks[14], (DEPTH, ML_WIDTH)),
        'w_branch_na': nrm(ks[15], (DEPTH, NA_WIDTH, D), NA_WIDTH ** -0.5),
        'w_branch_ml': nrm(ks[16], (DEPTH, ML_WIDTH, D), ML_WIDTH ** -0.5),
        'w_out': nrm(ks[17], (DEPTH, D, D), D ** -0.5),
        'w_router': nrm(ks[18], (DEPTH, D, E), D ** -0.5),
        'b_router': nrm(ks[19], (DEPTH, E), 0.01),
        'w_gate': nrm(ks[20], (DEPTH, E, D, F), D ** -0.5),
        'b_gate': nrm(ks[21], (DEPTH, E, F), 0.01),
        'w_lin': nrm(ks[22], (DEPTH, E, D, F), D ** -0.5),
        'b_lin': nrm(ks[23], (DEPTH, E, F), 0.01),
        'w_down': nrm(ks[24], (DEPTH, E, F, D), F ** -0.5),
        'b_down': nrm(ks[25], (DEPTH, E, D), 0.01),
    }


def reference(x, c, ctx, c_ctx, w_ada, b_ada, g_mix_pre, g_mix_post, g_ffn_pre, g_ffn_post,
              w_in, b_mlstm_gates, rpb, g_mlstm_head, w_branch_na, w_branch_ml, w_out,
              w_router, b_router, w_gate, b_gate, w_lin, b_lin, w_down, b_down):
    B, S, D = x.shape
    t = jnp.arange(S)
    row = (t // GRID_W).astype(jnp.float32)
    col = (t % GRID_W).astype(jnp.float32)
    silu_c = jax.nn.silu(c)
    silu_cc = jax.nn.silu(c_ctx)
    for layer in range(DEPTH):
        mod = silu_c @ w_ada[layer] + b_ada[layer]
        sh_m, sc_m, gt_m, sh_f, sc_f, gt_f = jnp.split(mod, 6, axis=-1)
        mod_c = silu_cc @ w_ada[layer][:, :2 * D] + b_ada[layer][:2 * D]
        sh_c, sc_c = jnp.split(mod_c, 2, axis=-1)

        h = _rmsnorm(x, g_mix_pre[layer]) * (1.0 + sc_m[:, None]) + sh_m[:, None]
        hc = _rmsnorm(ctx, g_mix_pre[layer]) * (1.0 + sc_c) + sh_c
        (na_k, na_v, ml_k, ml_v, ml_g, na_q, ml_q, ml_o, gate_na, gate_ml) = _split(h @ w_in[layer], IN_SPLITS)
        (na_kc, na_vc, ml_kc, ml_vc, ml_gc) = _split(hc @ w_in[layer][:, :N_CTX_COLS], CTX_SPLITS)
        o_na = _neighbourhood_attention(na_q, na_k, na_v, na_kc, na_vc, rpb[layer])
        o_ml = _mlstm_branch(ml_q, ml_k, ml_v, ml_o, ml_g, ml_kc, ml_vc, ml_gc,
                             b_mlstm_gates[layer], g_mlstm_head[layer], row, col)
        merged = (jax.nn.sigmoid(gate_na) * (o_na @ w_branch_na[layer])
                  + jax.nn.sigmoid(gate_ml) * (o_ml @ w_branch_ml[layer]))
        x = x + gt_m[:, None] * _rmsnorm(merged @ w_out[layer], g_mix_post[layer])

        h2 = _rmsnorm(x, g_ffn_pre[layer]) * (1.0 + sc_f[:, None]) + sh_f[:, None]
        ffn = _moe_ffn(h2, w_router[layer], b_router[layer], w_gate[layer], b_gate[layer],
                       w_lin[layer], b_lin[layer], w_down[layer], b_down[layer])
        x = x + gt_f[:, None] * _rmsnorm(ffn, g_ffn_post[layer])
    return x
```

```python
import numpy as np
from contextlib import ExitStack
import concourse.bass as bass
import concourse.mybir as mybir
from concourse.bass_utils import run_bass_kernel_spmd

F32 = mybir.dt.float32
BF16 = mybir.dt.bfloat16
AF = mybir.ActivationFunctionType
ALU = mybir.AluOpType
AX = mybir.AxisListType

D = 1024
S = 2048
CTX = 256
NB = 4
NT = 16
NTT = 18
NE = 32
EPS = 1e-6
NEG = -80.0
CAP = 2048
I32 = mybir.dt.int32
POOLENG = "dve"
N_IN = 5136
C_NAK, C_NAV, C_MLK, C_MLV, C_MLG = 0, 512, 1024, 1280, 1792
C_NAQ, C_MLQ, C_MLO, C_GNA, C_GML = 1808, 2320, 2576, 3088, 4112


class Res:
    __slots__ = ("lw", "rd")

    def __init__(self):
        self.lw = None
        self.rd = {}


class Prog:
    ENG = ["pe", "act", "dve", "pool", "sp"]

    def __init__(self, nc):
        self.nc = nc
        self.e = dict(pe=nc.tensor, act=nc.scalar, dve=nc.vector, pool=nc.gpsimd, sp=nc.sync)
        self.sem = {k: nc.alloc_semaphore("s_" + k) for k in self.ENG}
        self.cnt = {k: 0 for k in self.ENG}
        self.waited = {k: {} for k in self.ENG}
        self.n_ins = 0

    NDS = 8
    rr = None

    def dsem(self, name):
        if self.rr is None:
            self.rr = {}
        i = self.rr.get(name, 0)
        self.rr[name] = i + 1
        k = "d:%s:%d" % (name, i % self.NDS)
        if k not in self.sem:
            self.sem[k] = self.nc.alloc_semaphore("sd_%s_%d" % (name, i % self.NDS))
            self.cnt[k] = 0
        return k

    def _wait(self, eng, key, val):
        if self.waited[eng].get(key, 0) >= val:
            return
        self.e[eng].wait_ge(self.sem[key], val)
        self.waited[eng][key] = val
        self.n_ins += 1

    def _sync(self, eng, me, r, w):
        for x in r:
            if x.lw is not None:
                k, v = x.lw
                if k == me and me == "pe":
                    continue
                self._wait(eng, k, v)
        for x in w:
            if x.lw is not None:
                k, v = x.lw
                if k != me or me != "pe":
                    self._wait(eng, k, v)
            for k, v in x.rd.items():
                if k != me or me != "pe":
                    self._wait(eng, k, v)

    def _commit(self, me, val, r, w):
        for x in r:
            if x.rd.get(me, 0) < val:
                x.rd[me] = val
        for x in w:
            x.lw = (me, val)
            x.rd = {}

    max_ops = None
    tot = 0

    def op(self, eng, fn, r=(), w=()):
        self.tot += 1
        if self.max_ops is not None and self.tot > self.max_ops:
            return None
        self._sync(eng, eng, r, w)
        ins = fn(self.e[eng])
        self.cnt[eng] += 1
        ins.then_inc(self.sem[eng], 1)
        self._commit(eng, self.cnt[eng], r, w)
        self.n_ins += 1
        return ins

    def dma(self, q, out, in_, r=(), w=(), sem="ld"):
        self.tot += 1
        if self.max_ops is not None and self.tot > self.max_ops:
            return None
        k = self.dsem(sem)
        if self.cnt[k] > 0:
            self._wait(q, k, self.cnt[k])
        self._sync(q, k, r, w)
        ins = self.e[q].dma_start(out=out, in_=in_)
        self.cnt[k] += 16
        ins.then_inc(self.sem[k], 16)
        self._commit(k, self.cnt[k], r, w)
        self.n_ins += 1
        return ins

    def idma(self, out, out_offset, in_, in_offset, bounds, r=(), w=(), sem="ind"):
        self.tot += 1
        if self.max_ops is not None and self.tot > self.max_ops:
            return None
        k = self.dsem(sem)
        if self.cnt[k] > 0:
            self._wait("pool", k, self.cnt[k])
        self._sync("pool", k, r, w)
        ins = self.e["pool"].indirect_dma_start(out=out, out_offset=out_offset, in_=in_, in_offset=in_offset)
        self.cnt[k] += 16
        ins.then_inc(self.sem[k], 16)
        self._commit(k, self.cnt[k], r, w)
        self.n_ins += 1
        return ins

    def barrier(self, engs=None):
        for eng in (engs or self.ENG):
            for k, v in self.cnt.items():
                if k != eng and v > 0:
                    self._wait(eng, k, v)


class T:
    def __init__(self, h, nres=1):
        self.h = h
        self.res = [Res() for _ in range(nres)]

    def __getitem__(self, k):
        return self.h[k]

    def r(self, i=0):
        return self.res[i]


def build(nb=NB, stop=None, dbg=(), max_ops=None):
    nc = bass.Bass("TRN2", target_bir_lowering=False)
    p = Prog(nc)
    p.max_ops = max_ops

    def din(name, shape, dt=F32):
        return nc.dram_tensor(name, list(shape), dt, kind="ExternalInput").ap()

    x_d = din("x", [nb, S, D])
    ctx_d = din("ctx", [nb, CTX, D])
    cT_d = din("cT", [128, 8, 5])
    w_ada_d = din("w_ada", [D, 6 * D])
    b_ada_d = din("b_ada", [1, 6 * D])
    g4_d = din("g4", [4, D])
    w_in_d = din("w_in", [D, N_IN])
    w_qkp_d = din("w_qkp", [D, 512])
    b_mg_d = din("b_mg", [1, 16])
    nab_d = din("nab", [8, 128, 5 * 640])
    g_head_d = din("g_head", [1, 512])
    w_bna_d = din("w_bna", [512, D])
    w_bml_d = din("w_bml", [512, D])
    w_out_d = din("w_out", [D, D])
    w_r_d = din("w_router", [D, NE])
    b_r_d = din("b_router", [1, NE])
    w_gate_d = din("w_gate", [NE, D, D])
    w_lin_d = din("w_lin", [NE, D, D])
    w_down_d = din("w_down", [NE, D, D])
    bgT_d = din("bgT", [128, 8, NE])
    blT_d = din("blT", [128, 8, NE])
    b_down_d = din("b_down", [NE, D])
    ident_d = din("ident", [128, 128])
    trif_d = din("trif", [128, 128])
    trib_d = din("trib", [128, 128])
    cos_d = din("ropecos", [128, S])
    sin_d = din("ropesin", [128, S])
    tris_d = din("tris", [128, 128])
    ecap_d = din("ecap", [128, NE])
    xe_d = nc.dram_tensor("xe_scr", [NE * CAP, D], BF16).ap()
    ye_d = nc.dram_tensor("ye_scr", [NE * CAP, D], F32).ap()
    gf_d = nc.dram_tensor("gf_scr", [nb, 128, D], F32).ap()
    out_d = nc.dram_tensor("out", [nb, S, D], F32, kind="ExternalOutput").ap()
    dbg_d = {}
    for name, shape in dbg:
        dbg_d[name] = nc.dram_tensor("dbg_" + name, list(shape), F32, kind="ExternalOutput").ap()

    uid = [0]

    def sb(es, name, shape, dt=F32, nres=1):
        uid[0] += 1
        return T(es.enter_context(nc.sbuf_tensor("sb%d_%s" % (uid[0], name), list(shape), dt)), nres)

    PD = [T(nc.alloc_psum_tensor("pd%d" % i, [128, 1024], F32)) for i in range(3)]
    PS = [T(nc.alloc_psum_tensor("psg%d" % i, [128, 512], F32)) for i in range(2)]

    def dump(name, tile_ap, res, dst=None):
        if name in dbg_d:
            p.dma("pool", dbg_d[name] if dst is None else dst, tile_ap, r=(res if isinstance(res, list) else [res]), sem="dbg")

    def wview(ap2d):
        return ap2d.rearrange("(kc p) n -> p kc n", p=128)

    def bc(ap, shape):
        return ap.to_broadcast(list(shape))

    rot = [0, 0]

    def nPD():
        rot[0] += 1
        return PD[rot[0] % 3]

    def nPS():
        rot[1] += 1
        return PS[rot[1] % 2]

    with ExitStack() as G:
        ident = sb(G, "ident", [128, 128])
        identb = sb(G, "identb", [128, 128], BF16)
        trif = sb(G, "trif", [128, 128])
        trib = sb(G, "trib", [128, 128])
        ones32 = sb(G, "ones32", [128, 128])
        scT = sb(G, "scT", [128, 8, 5])
        A_c = sb(G, "A_c", [128, D])
        sh_c = sb(G, "sh_c", [128, D])
        trisb = sb(G, "trisb", [128, 128], BF16)
        onesb = sb(G, "onesb", [128, 128], BF16)
        ecap = sb(G, "ecap", [128, NE])
        msum = sb(G, "msum", [128, NE], BF16)
        RIi = sb(G, "RIi", [128, nb * NT, 4], I32, nres=nb * NT)
        RIw = sb(G, "RIw", [128, nb * NT, 4], F32, nres=nb * NT)
        zres = Res()
        gfres = [Res() for _ in range(nb)]
        p.dma("sp", ecap[:], ecap_d, w=[ecap.r()])
        p.op("dve", lambda e: e.memset(onesb[:], 1.0), w=[onesb.r()])
        p.op("dve", lambda e: e.memset(msum[:], 0.0), w=[msum.r()])
        with ExitStack() as L:
            ztile = sb(L, "ztile", [128, 4096], BF16)
            tmp32 = sb(L, "tmp32", [128, 128])
            p.dma("sp", tmp32[:], tris_d, w=[tmp32.r()])
            p.op("dve", lambda e: e.tensor_copy(out=trisb[:], in_=tmp32[:]), r=[tmp32.r()], w=[trisb.r()])
            p.op("dve", lambda e: e.memset(ztile[:], 0.0), w=[ztile.r()])
            for cz in range(NE * CAP // 512):
                p.dma("sp", xe_d[cz * 512:(cz + 1) * 512, :].rearrange("(p j) d -> p (j d)", p=128), ztile[:],
                      r=[ztile.r()], w=[], sem="z")
            p.barrier()
        p.dma("sp", ident[:], ident_d, w=[ident.r()])
        p.dma("sp", trif[:], trif_d, w=[trif.r()])
        p.dma("sp", trib[:], trib_d, w=[trib.r()])
        p.dma("sp", scT[:], cT_d, w=[scT.r()])
        p.op("dve", lambda e: e.tensor_copy(out=identb[:], in_=ident[:]), r=[ident.r()], w=[identb.r()])
        p.op("dve", lambda e: e.memset(ones32[:], 1.0), w=[ones32.r()])
        p.op("act", lambda e: e.activation(out=scT[:], in_=scT[:], func=AF.Silu), r=[scT.r()], w=[scT.r()])

        def rstd_of(src_ap, src_res, junk, st):
            p.op("act", lambda e: e.activation(out=junk[:], in_=src_ap, func=AF.Square, scale=1.0 / 32.0,
                                               accum_out=st[:, 0:1]), r=src_res, w=[junk.r(), st.r()])
            p.op("act", lambda e: e.activation(out=st[:, 1:2], in_=st[:, 0:1], func=AF.Sqrt, bias=EPS),
                 r=[st.r()], w=[st.r()])
            p.op("dve", lambda e: e.reciprocal(out=st[:, 1:2], in_=st[:, 1:2]), r=[st.r()], w=[st.r()])

        def ada_mod(j, pieces, tag):
            with ExitStack() as L:
                lh = sb(L, "lh" + tag, [128, 8, 128], BF16)
                g4 = sb(L, "g4" + tag, [128, 4, D])
                p.dma("sp", g4[:], g4_d.partition_broadcast(128), w=[g4.r()])
                for kc in range(8):
                    p.op("dve", lambda e, kc=kc: e.tensor_scalar_mul(
                        out=lh[:, kc, :], in0=ones32[:], scalar1=scT[:, kc, j:j + 1]),
                        r=[scT.r(), ones32.r()], w=[lh.r()])
                wa = [sb(L, "wa%d%s" % (i, tag), [128, 8, 512], BF16) for i in range(2)]
                ba = [sb(L, "ba%d%s" % (i, tag), [1, 512]) for i in range(2)]
                n = 0
                for (blk, out_t, kind, gi) in pieces:
                    for half in range(2):
                        c0 = blk * D + half * 512
                        wt = wa[n % 2]
                        bt = ba[n % 2]
                        n += 1
                        p.dma("pool", wt[:], wview(w_ada_d[:, c0:c0 + 512]), w=[wt.r()], sem="w")
                        p.dma("sp", bt[:], b_ada_d[:, c0:c0 + 512], w=[bt.r()], sem="x")
                        ps = nPS()
                        for kc in range(8):
                            p.op("pe", lambda e, kc=kc, ps=ps, wt=wt: e.matmul(
                                ps[:], lhsT=lh[:, kc, :], rhs=wt[:, kc, :], start=(kc == 0), stop=(kc == 7)),
                                r=[lh.r(), wt.r()], w=[ps.r()])
                        o = out_t[:, half * 512:(half + 1) * 512]
                        tmp = sb(L, "adatmp%d%s" % (n, tag), [128, 512])
                        ps2 = nPS()
                        p.op("pe", lambda e, ps2=ps2, bt=bt: e.matmul(
                            ps2[:], lhsT=ones32[0:1, :], rhs=bt[0:1, :], start=True, stop=True),
                            r=[ones32.r(), bt.r()], w=[ps2.r()])
                        p.op("act", lambda e, tmp=tmp, ps2=ps2: e.copy(out=tmp[:], in_=ps2[:]), r=[ps2.r()], w=[tmp.r()])
                        p.op("dve", lambda e, tmp=tmp, ps=ps: e.tensor_tensor(out=tmp[:], in0=ps[:], in1=tmp[:], op=ALU.add),
                             r=[ps.r(), tmp.r()], w=[tmp.r()])
                        if kind == "shift":
                            p.op("act", lambda e, o=o, tmp=tmp: e.copy(out=o, in_=tmp[:]), r=[tmp.r()], w=[out_t.r()])
                        elif kind == "scale":
                            gs = g4[:, gi, half * 512:(half + 1) * 512]
                            p.op("dve", lambda e, o=o, tmp=tmp, gs=gs: e.scalar_tensor_tensor(
                                out=o, in0=tmp[:], scalar=1.0, in1=gs, op0=ALU.add, op1=ALU.mult),
                                r=[tmp.r(), g4.r()], w=[out_t.r()])
                        else:
                            gs = g4[:, gi, half * 512:(half + 1) * 512]
                            p.op("dve", lambda e, o=o, tmp=tmp, gs=gs: e.tensor_tensor(
                                out=o, in0=tmp[:], in1=gs, op=ALU.mult),
                                r=[tmp.r(), g4.r()], w=[out_t.r()])
            p.barrier()

        ada_mod(4, [(0, sh_c, "shift", 0), (1, A_c, "scale", 0)], "c")
        x1res = [[Res() for _ in range(NT)] for _ in range(nb)]

        for b in range(nb):
          with ExitStack() as B:
            hT = sb(B, "hT", [128, 8, NTT * 128], BF16, nres=NTT)
            with ExitStack() as M:
              G_m = sb(M, "G_m", [128, D]); A_f = sb(M, "A_f", [128, D]); sh_f = sb(M, "sh_f", [128, D])
              with ExitStack() as PA:
                A_m = sb(PA, "A_m", [128, D]); sh_m = sb(PA, "sh_m", [128, D]); G_f = sb(PA, "G_f", [128, D])
                ada_mod(b, [(0, sh_m, "shift", 0), (1, A_m, "scale", 0), (2, G_m, "gate", 1),
                            (3, sh_f, "shift", 0), (4, A_f, "scale", 2), (5, G_f, "gate", 3)], "b")
                p.dma("sp", gf_d[b], G_f[:], r=[G_f.r()], w=[gfres[b]], sem="o")
                if b == 0:
                    dump("A_m", A_m[:], A_m.r()); dump("sh_m", sh_m[:], sh_m.r()); dump("G_f", G_f[:], G_f.r())
                    dump("A_c", A_c[:], A_c.r())
                with ExitStack() as L:
                    xt = [sb(L, "xt%d" % i, [128, D]) for i in range(2)]
                    sq = [sb(L, "sq%d" % i, [128, D]) for i in range(2)]
                    hb = [sb(L, "hb%d" % i, [128, D], BF16) for i in range(2)]
                    st = [sb(L, "st%d" % i, [128, 2]) for i in range(2)]
                    for t in range(NTT):
                        i = t % 2
                        src = ctx_d[b, t * 128:(t + 1) * 128, :] if t < 2 else x_d[b, (t - 2) * 128:(t - 1) * 128, :]
                        Am, shm = (A_c, sh_c) if t < 2 else (A_m, sh_m)
                        p.dma("sp", xt[i][:], src, w=[xt[i].r()], sem="x")
                        rstd_of(xt[i][:], [xt[i].r()], sq[i], st[i])
                        p.op("dve", lambda e, i=i, Am=Am: e.scalar_tensor_tensor(
                            out=sq[i][:], in0=xt[i][:], scalar=st[i][:, 1:2], in1=Am[:], op0=ALU.mult, op1=ALU.mult),
                            r=[xt[i].r(), st[i].r(), Am.r()], w=[sq[i].r()])
                        p.op("dve", lambda e, i=i, shm=shm: e.tensor_tensor(out=hb[i][:], in0=sq[i][:], in1=shm[:], op=ALU.add),
                             r=[sq[i].r(), shm.r()], w=[hb[i].r()])
                        ps = nPS()
                        psb = ps[:].bitcast(BF16)
                        for kc in range(8):
                            p.op("pe", lambda e, kc=kc, psb=psb, i=i: e.transpose(
                                psb[:, kc * 128:(kc + 1) * 128], hb[i][:, kc * 128:(kc + 1) * 128], identb[:]),
                                r=[hb[i].r(), identb.r()], w=[ps.r()])
                        p.op("act", lambda e, t=t, psb=psb: e.copy(
                            out=hT[:, :, t * 128:(t + 1) * 128], in_=psb.rearrange("p (k n) -> p k n", k=8)),
                            r=[ps.r()], w=[hT.r(t)])
                if b == 0 and "hT" in dbg_d:
                    dump("hT", hT[:], hT.res)
                p.barrier()
              if True:
                if stop == "A":
                    break
                onaT = sb(M, "onaT", [128, 4, S], BF16, nres=NT)

                def mm_fm(ps_ap, ps_res, w_t, wc0, tok0, ntok, nk=8, src=None):
                    src = src or hT
                    tiles = range(tok0 // 128, (tok0 + ntok) // 128)
                    for kc in range(nk):
                        p.op("pe", lambda e, kc=kc: e.matmul(
                            ps_ap, lhsT=w_t[:, kc, wc0:wc0 + 128], rhs=src[:, kc, tok0:tok0 + ntok],
                            start=(kc == 0), stop=(kc == nk - 1)),
                            r=[w_t.r()] + [src.r(t) for t in tiles], w=[ps_res])

                def mm_tm(ps_ap, ps_res, w_t, wc0, ncols, t):
                    for kc in range(8):
                        p.op("pe", lambda e, kc=kc: e.matmul(
                            ps_ap, lhsT=hT[:, kc, t * 128:(t + 1) * 128], rhs=w_t[:, kc, wc0:wc0 + ncols],
                            start=(kc == 0), stop=(kc == 7)),
                            r=[w_t.r(), hT.r(t)], w=[ps_res])

                with ExitStack() as L:
                  qT = sb(L, "qT", [128, 4, S], BF16, nres=4)
                  kT = sb(L, "kT", [128, 4, NTT * 128], BF16, nres=4)
                  vA = sb(L, "vA", [128, NTT, 8, 65], BF16, nres=NTT)
                  p.op("dve", lambda e: e.memset(vA[:, :, :, 64:65], 1.0), w=vA.res)
                  with ExitStack() as L2:
                    wna = sb(L2, "wna", [128, 8, 1536], BF16)
                    p.dma("pool", wna[:, :, 0:1024], wview(w_in_d[:, 0:1024]), w=[wna.r()], sem="w")
                    p.dma("pool", wna[:, :, 1024:1536], wview(w_in_d[:, C_NAQ:C_NAQ + 512]), w=[wna.r()], sem="w")
                    for c in range(4):
                        for tb in range(4):
                            ps = nPD()
                            mm_fm(ps[:, 0:512], ps.r(), wna, 1024 + c * 128, 256 + tb * 512, 512)
                            p.op("act", lambda e, c=c, tb=tb, ps=ps: e.activation(
                                out=qT[:, c, tb * 512:(tb + 1) * 512], in_=ps[:, 0:512], func=AF.Copy, scale=0.125),
                                r=[ps.r()], w=[qT.r(c)])
                        for (t0, n) in [(0, 512), (512, 512), (1024, 512), (1536, 512), (2048, 256)]:
                            ps = nPD()
                            mm_fm(ps[:, 0:n], ps.r(), wna, c * 128, t0, n)
                            p.op("dve", lambda e, c=c, t0=t0, n=n, ps=ps: e.tensor_copy(
                                out=kT[:, c, t0:t0 + n], in_=ps[:, 0:n]), r=[ps.r()], w=[kT.r(c)])
                    for t in range(NTT):
                        ps = nPD()
                        mm_tm(ps[:, 0:512], ps.r(), wna, 512, 512, t)
                        eng = "act" if t % 2 else "dve"
                        if eng == "act":
                            p.op("act", lambda e, t=t, ps=ps: e.copy(
                                out=vA[:, t, :, 0:64], in_=ps[:, 0:512].rearrange("p (h d) -> p h d", h=8)),
                                r=[ps.r()], w=[vA.r(t)])
                        else:
                            p.op("dve", lambda e, t=t, ps=ps: e.tensor_copy(
                                out=vA[:, t, :, 0:64], in_=ps[:, 0:512].rearrange("p (h d) -> p h d", h=8)),
                                r=[ps.r()], w=[vA.r(t)])
                  p.barrier()
                  if True:
                    EBh = [sb(L, "EB%d" % i, [128, 3200], BF16) for i in range(2)]
                    ona = sb(L, "ona", [128, NT, 512], BF16, nres=NT)
                    stg = sb(L, "nabst", [128, 3200])
                    PT = [sb(L, "PT%d" % i, [128, 896], BF16) for i in range(2)]
                    rc = [sb(L, "rc%d" % i, [128, 1]) for i in range(2)]
                    n = 0
                    for h in range(8):
                        hp = (h % 2) * 64
                        c = h // 2
                        EB = EBh[h % 2]
                        p.dma("sp", stg[:], nab_d[h], w=[stg.r()], sem="x")
                        p.op("act", lambda e, EB=EB: e.activation(out=EB[:], in_=stg[:], func=AF.Exp),
                             r=[stg.r()], w=[EB.r()])
                        for i in range(NT):
                            js = min(max(i - 2, 0), 11)
                            cls = i - js if i < 2 or i > 13 else 2
                            n += 1
                            pss = PD[n % 2]
                            pso = PS[n % 2]
                            pt = PT[n % 2]
                            rcc = rc[n % 2]
                            qa = qT[hp:hp + 64, c, i * 128:(i + 1) * 128]
                            for s_i in range(7):
                                kt = (2 + js + s_i) if s_i < 5 else (s_i - 5)
                                p.op("pe", lambda e, s_i=s_i, kt=kt, pss=pss, qa=qa: e.matmul(
                                    pss[:, s_i * 128:(s_i + 1) * 128], lhsT=kT[hp:hp + 64, c, kt * 128:(kt + 1) * 128],
                                    rhs=qa, start=True, stop=True),
                                    r=[kT.r(c), qT.r(c)], w=[pss.r()])
                            p.op("act", lambda e, pt=pt, pss=pss: e.activation(out=pt[:], in_=pss[:, 0:896], func=AF.Exp),
                                 r=[pss.r()], w=[pt.r()])
                            p.op("dve", lambda e, pt=pt, EB=EB, cls=cls: e.tensor_tensor(
                                out=pt[:, 0:640], in0=pt[:, 0:640], in1=EB[:, cls * 640:(cls + 1) * 640], op=ALU.mult),
                                r=[pt.r(), EB.r()], w=[pt.r()])
                            for s_i in range(7):
                                kt = (2 + js + s_i) if s_i < 5 else (s_i - 5)
                                p.op("pe", lambda e, s_i=s_i, kt=kt, pso=pso, pt=pt, h=h: e.matmul(
                                    pso[:, 0:65], lhsT=pt[:, s_i * 128:(s_i + 1) * 128], rhs=vA[:, kt, h, :],
                                    start=(s_i == 0), stop=(s_i == 6)),
                                    r=[pt.r(), vA.r(kt)], w=[pso.r()])
                            p.op("dve", lambda e, rcc=rcc, pso=pso: e.reciprocal(out=rcc[:], in_=pso[:, 64:65]),
                                 r=[pso.r()], w=[rcc.r()])
                            p.op("dve", lambda e, rcc=rcc, pso=pso, i=i, h=h: e.tensor_scalar_mul(
                                out=ona[:, i, h * 64:(h + 1) * 64], in0=pso[:, 0:64], scalar1=rcc[:, 0:1]),
                                r=[pso.r(), rcc.r()], w=[ona.r(i)])
                    for i in range(NT):
                        ps = nPS()
                        psb = ps[:].bitcast(BF16)
                        for c4 in range(4):
                            p.op("pe", lambda e, c4=c4, i=i, psb=psb: e.transpose(
                                psb[:, c4 * 128:(c4 + 1) * 128], ona[:, i, c4 * 128:(c4 + 1) * 128], identb[:]),
                                r=[ona.r(i), identb.r()], w=[ps.r()])
                        p.op("act", lambda e, i=i, psb=psb: e.copy(
                            out=onaT[:, :, i * 128:(i + 1) * 128], in_=psb[:, 0:512].rearrange("p (k n) -> p k n", k=4)),
                            r=[ps.r()], w=[onaT.r(i)])
                    if b == 0 and "ona" in dbg_d:
                        dump("ona", ona[:], ona.res)
                p.barrier()
                if stop == "B":
                    break
                omlT = sb(M, "omlT", [128, 4, S], BF16, nres=NT)

                with ExitStack() as L:
                    mqT = sb(L, "mqT", [128, 2, S], BF16, nres=2)
                    mkT = sb(L, "mkT", [128, 2, S], BF16, nres=2)
                    ktm = sb(L, "ktm", [128, NTT, 256], BF16, nres=NTT)
                    vM = sb(L, "vM", [128, NTT, 4, 129], BF16, nres=NTT)
                    osig = sb(L, "osig", [128, NT, 512], BF16, nres=NT)
                    gts = sb(L, "gts", [128, NTT, 16])
                    LI = sb(L, "LI", [128, NTT, 8])
                    LFn = sb(L, "LFn", [128, NTT, 8])
                    Bn = sb(L, "Bn", [128, NTT, 8])
                    EBt = sb(L, "EBt", [128, NTT, 8])
                    ES = sb(L, "ES", [128, NTT, 8])
                    EBL = sb(L, "EBL", [128, NTT, 8])
                    ghd = sb(L, "ghd", [128, 512])
                    p.dma("sp", ghd[:], g_head_d.partition_broadcast(128), w=[ghd.r()], sem="x")
                    p.op("dve", lambda e: e.memset(vM[:, :, :, 128:129], 1.0), w=vM.res)
                    with ExitStack() as L2:
                        wq = sb(L2, "wq", [128, 8, 256], BF16); wqp = sb(L2, "wqp", [128, 8, 256], BF16)
                        wk = sb(L2, "wk", [128, 8, 256], BF16); wkp = sb(L2, "wkp", [128, 8, 256], BF16)
                        cosT = sb(L2, "cosT", [128, 512]); sinT = sb(L2, "sinT", [128, 512])
                        rt = [sb(L2, "rt%d" % i, [128, 512]) for i in range(2)]
                        p.dma("pool", wq[:], wview(w_in_d[:, C_MLQ:C_MLQ + 256]), w=[wq.r()], sem="w")
                        p.dma("pool", wqp[:], wview(w_qkp_d[:, 0:256]), w=[wqp.r()], sem="w")
                        p.dma("pool", wk[:], wview(w_in_d[:, C_MLK:C_MLK + 256]), w=[wk.r()], sem="w")
                        p.dma("pool", wkp[:], wview(w_qkp_d[:, 256:512]), w=[wkp.r()], sem="w")
                        for (w_a, w_b, dst) in ((wq, wqp, mqT), (wk, wkp, mkT)):
                            for c in range(2):
                                for tb in range(4):
                                    ps = nPD()
                                    mm_fm(ps[:, 0:512], ps.r(), w_a, c * 128, 256 + tb * 512, 512)
                                    mm_fm(ps[:, 512:1024], ps.r(), w_b, c * 128, 256 + tb * 512, 512)
                                    cs_ = slice(tb * 512, (tb + 1) * 512)
                                    p.dma("sp", cosT[:], cos_d[:, cs_], w=[cosT.r()], sem="x")
                                    p.dma("sp", sinT[:], sin_d[:, cs_], w=[sinT.r()], sem="x")
                                    p.op("dve", lambda e, ps=ps, cs_=cs_: e.tensor_tensor(
                                        out=rt[0][:], in0=ps[:, 0:512], in1=cosT[:], op=ALU.mult),
                                        r=[ps.r(), cosT.r()], w=[rt[0].r()])
                                    p.op("dve", lambda e, ps=ps, cs_=cs_: e.tensor_tensor(
                                        out=rt[1][:], in0=ps[:, 512:1024], in1=sinT[:], op=ALU.mult),
                                        r=[ps.r(), sinT.r()], w=[rt[1].r()])
                                    p.op("dve", lambda e, dst=dst, c=c, cs_=cs_: e.tensor_tensor(
                                        out=dst[:, c, cs_], in0=rt[0][:], in1=rt[1][:], op=ALU.add),
                                        r=[rt[0].r(), rt[1].r()], w=[dst.r(c)])
                        for t in range(NT):
                            ps = nPS()
                            psb = ps[:].bitcast(BF16)
                            for c in range(2):
                                p.op("pe", lambda e, c=c, t=t, psb=psb: e.transpose(
                                    psb[:, c * 128:(c + 1) * 128], mkT[:, c, t * 128:(t + 1) * 128], identb[:]),
                                    r=[mkT.r(c), identb.r()], w=[ps.r()])
                            p.op("act", lambda e, t=t, psb=psb: e.copy(out=ktm[:, 2 + t, :], in_=psb[:, 0:256]),
                                 r=[ps.r()], w=[ktm.r(2 + t)])
                        for t in range(2):
                            ps = nPS()
                            mm_tm(ps[:, 0:256], ps.r(), wk, 0, 256, t)
                            p.op("act", lambda e, t=t, ps=ps: e.copy(out=ktm[:, t, :], in_=ps[:, 0:256]),
                                 r=[ps.r()], w=[ktm.r(t)])
                    p.barrier()
                    if stop == "C1":
                        break
                    with ExitStack() as L2:
                        wv = sb(L2, "wv", [128, 8, 512], BF16); wo = sb(L2, "wo", [128, 8, 512], BF16)
                        wg = sb(L2, "wgt", [128, 8, 16], BF16)
                        bmg = sb(L2, "bmg", [1, 16])
                        p.dma("pool", wv[:], wview(w_in_d[:, C_MLV:C_MLV + 512]), w=[wv.r()], sem="w")
                        p.dma("pool", wo[:], wview(w_in_d[:, C_MLO:C_MLO + 512]), w=[wo.r()], sem="w")
                        p.dma("pool", wg[:], wview(w_in_d[:, C_MLG:C_MLG + 16]), w=[wg.r()], sem="w")
                        p.dma("sp", bmg[:], b_mg_d, w=[bmg.r()], sem="x")
                        for t in range(NTT):
                            ps = nPD()
                            mm_tm(ps[:, 0:512], ps.r(), wv, 0, 512, t)
                            p.op("dve", lambda e, t=t, ps=ps: e.tensor_copy(
                                out=vM[:, t, :, 0:128], in_=ps[:, 0:512].rearrange("p (h d) -> p h d", h=4)),
                                r=[ps.r()], w=[vM.r(t)])
                            if t >= 2:
                                mm_tm(ps[:, 512:1024], ps.r(), wo, 0, 512, t)
                                p.op("act", lambda e, t=t, ps=ps: e.activation(
                                    out=osig[:, t - 2, :], in_=ps[:, 512:1024], func=AF.Sigmoid),
                                    r=[ps.r()], w=[osig.r(t - 2)])
                            ps2 = nPS()
                            for kc in range(8):
                                p.op("pe", lambda e, kc=kc, t=t, ps2=ps2: e.matmul(
                                    ps2[:, 0:16], lhsT=hT[:, kc, t * 128:(t + 1) * 128], rhs=wg[:, kc, :],
                                    start=(kc == 0), stop=(kc == 7)), r=[wg.r(), hT.r(t)], w=[ps2.r()])
                            p.op("pe", lambda e, ps2=ps2: e.matmul(
                                ps2[:, 16:32], lhsT=ones32[0:1, :], rhs=bmg[0:1, :], start=True, stop=True),
                                r=[ones32.r(), bmg.r()], w=[ps2.r()])
                            p.op("act", lambda e, t=t, ps2=ps2: e.copy(out=gts[:, t, :], in_=ps2[:, 0:16]),
                                 r=[ps2.r()], w=[gts.r()])
                            p.op("dve", lambda e, t=t, ps2=ps2: e.tensor_tensor(
                                out=gts[:, t, :], in0=gts[:, t, :], in1=ps2[:, 16:32], op=ALU.add),
                                r=[ps2.r(), gts.r()], w=[gts.r()])
                    p.barrier()
                    if stop == "C2":
                        break
                    gv = gts[:].rearrange("p t (k h) -> p t k h", k=4)
                    p.op("act", lambda e: e.activation(out=gts[:], in_=gts[:], func=AF.Tanh, scale=1.0 / 15.0),
                         r=[gts.r()], w=[gts.r()])
                    for d_ in range(2):
                        p.op("dve", lambda e, d_=d_: e.tensor_scalar_mul(
                            out=LI[:, :, d_ * 4:(d_ + 1) * 4], in0=gv[:, :, 2 * d_, :], scalar1=15.0),
                            r=[gts.r()], w=[LI.r()])
                        p.op("act", lambda e, d_=d_: e.activation(
                            out=LFn[:, :, d_ * 4:(d_ + 1) * 4], in_=gv[:, :, 2 * d_ + 1, :], func=AF.Exp, scale=-15.0),
                            r=[gts.r()], w=[LFn.r()])
                    p.op("act", lambda e: e.activation(out=LFn[:], in_=LFn[:], func=AF.Ln, bias=1.0),
                         r=[LFn.r()], w=[LFn.r()])
                    psc = nPS()
                    for t in range(NTT):
                        for d_, tri in ((0, trif), (1, trib)):
                            p.op("pe", lambda e, t=t, d_=d_, tri=tri: e.matmul(
                                psc[:, t * 8 + d_ * 4:t * 8 + d_ * 4 + 4], lhsT=tri[:], rhs=LFn[:, t, d_ * 4:(d_ + 1) * 4],
                                start=True, stop=True), r=[tri.r(), LFn.r()], w=[psc.r()])
                    p.op("dve", lambda e: e.tensor_copy(out=Bn[:].rearrange("p t k -> p (t k)"), in_=psc[:, 0:NTT * 8]),
                         r=[psc.r()], w=[Bn.r()])
                    psl = nPS()
                    p.op("pe", lambda e: e.matmul(psl[:, 0:NTT * 8], lhsT=ones32[:], rhs=LFn[:].rearrange("p t k -> p (t k)"),
                                                  start=True, stop=True), r=[ones32.r(), LFn.r()], w=[psl.r()])
                    p.op("act", lambda e: e.activation(out=EBL[:].rearrange("p t k -> p (t k)"), in_=psl[:, 0:NTT * 8],
                                                       func=AF.Exp, scale=-1.0), r=[psl.r()], w=[EBL.r()])
                    p.op("act", lambda e: e.activation(out=EBt[:], in_=Bn[:], func=AF.Exp, scale=-1.0),
                         r=[Bn.r()], w=[EBt.r()])
                    p.op("dve", lambda e: e.tensor_tensor(out=ES[:], in0=LI[:], in1=Bn[:], op=ALU.add),
                         r=[LI.r(), Bn.r()], w=[ES.r()])
                    p.op("act", lambda e: e.activation(out=ES[:], in_=ES[:], func=AF.Exp, bias=float(-np.log(8.0))),
                         r=[ES.r()], w=[ES.r()])
                    if stop == "C3":
                        p.barrier()
                        break
                    Hf = sb(L, "Hf", [128, NT, 512], BF16, nres=NT)
                    Cst = [sb(L, "Cst%d" % d_, [128, 4, 129]) for d_ in range(2)]
                    Cbf = [sb(L, "Cbf%d" % d_, [128, 4, 129], BF16) for d_ in range(2)]
                    vp = [sb(L, "vp%d" % d_, [128, 4, 129], BF16) for d_ in range(2)]
                    sTm = [sb(L, "sTm%d" % d_, [128, 4, 128], BF16) for d_ in range(2)]
                    sm = [sb(L, "sm%d" % d_, [128, 4, 4]) for d_ in range(2)]
                    Hs = [sb(L, "Hs%d" % i, [128, 512]) for i in range(2)]
                    Hq = [sb(L, "Hq%d" % i, [128, 512]) for i in range(1)]
                    fs = [sb(L, "fs%d" % i, [128, 8]) for i in range(2)]
                    omb = [sb(L, "omb%d" % i, [128, 512], BF16) for i in range(2)]
                    bwd_order = [1, 0] + list(range(NTT - 1, 1, -1))
                    tri4 = [sb(L, "tri4%d" % d_, [128, 4, 128], BF16) for d_ in range(2)]
                    for d_, tr_ in ((0, trif), (1, trib)):
                        for hd in range(4):
                            p.op("dve", lambda e, d_=d_, tr_=tr_, hd=hd: e.tensor_copy(out=tri4[d_][:, hd, :], in_=tr_[:]),
                                 r=[tr_.r()], w=[tri4[d_].r()])
                    psN_ = [PD[0], PD[2]]
                    psU_ = PD[1]
                    for d_ in range(2):
                        if stop in ("C4", "C6", "C7") and d_ == 1:
                            break
                        for step in range(NTT):
                            if (stop == "C6" and step == 2) or (stop == "C7" and step == 3):
                                break
                            t = step if d_ == 0 else bwd_order[step]
                            tri = trif if d_ == 0 else trib
                            psN = psN_[d_]
                            psS = PS[d_]
                            C_, Cb_, vp_, sT_, sm_ = Cst[d_], Cbf[d_], vp[d_], sTm[d_], sm[d_]
                            p.op(POOLENG, lambda e, t=t, d_=d_, vp_=vp_: e.tensor_tensor(
                                out=vp_[:], in0=vM[:, t, :, :], in1=bc(ES[:, t, d_ * 4:(d_ + 1) * 4].unsqueeze(2), [128, 4, 129]),
                                op=ALU.mult), r=[vM.r(t), ES.r()], w=[vp_.r()])
                            if t >= 2:
                                lt = t - 2
                                for hd in range(4):
                                    hp, c = (hd % 2) * 64, hd // 2
                                    p.op("pe", lambda e, hd=hd, hp=hp, c=c, lt=lt, psS=psS: e.matmul(
                                        psS[:, hd * 128:(hd + 1) * 128], lhsT=mkT[hp:hp + 64, c, lt * 128:(lt + 1) * 128],
                                        rhs=mqT[hp:hp + 64, c, lt * 128:(lt + 1) * 128], start=True, stop=True),
                                        r=[mkT.r(c), mqT.r(c)], w=[psS.r()])
                                    p.op("pe", lambda e, hd=hd, c=c, t=t, vp_=vp_: e.matmul(
                                        psU_[:, hd * 256:hd * 256 + 129], lhsT=ktm[:, t, c * 128:(c + 1) * 128], rhs=vp_[:, hd, :],
                                        start=True, stop=True), r=[ktm.r(t), vp_.r()], w=[psU_.r()])
                                p.op("dve", lambda e, sT_=sT_, psS=psS, d_=d_: e.tensor_tensor(
                                    out=sT_[:].rearrange("p h n -> p (h n)"), in0=psS[:, 0:512],
                                    in1=tri4[d_][:].rearrange("p h n -> p (h n)"), op=ALU.mult),
                                    r=[psS.r(), tri4[d_].r()], w=[sT_.r()])
                                for hd in range(4):
                                    hp, c = (hd % 2) * 64, hd // 2
                                    p.op("pe", lambda e, hd=hd, psN=psN, sT_=sT_, vp_=vp_: e.matmul(
                                        psN[:, hd * 256:hd * 256 + 129], lhsT=sT_[:, hd, :], rhs=vp_[:, hd, :],
                                        start=True, stop=(step == 0)), r=[sT_.r(), vp_.r()], w=[psN.r()])
                                    if step > 0:
                                        p.op("pe", lambda e, hd=hd, hp=hp, c=c, lt=lt, psN=psN, Cb_=Cb_: e.matmul(
                                            psN[:, hd * 256:hd * 256 + 129], lhsT=mqT[hp:hp + 64, c, lt * 128:(lt + 1) * 128],
                                            rhs=Cb_[hp:hp + 64, hd, :], start=False, stop=True),
                                            r=[mqT.r(c), Cb_.r()], w=[psN.r()])
                                nv = psN[:].rearrange("p (h x) -> p h x", x=256)
                                p.op("dve", lambda e, nv=nv, sm_=sm_, t=t, d_=d_: e.tensor_tensor(
                                    out=sm_[:, :, 0:1], in0=nv[:, :, 128:129], in1=EBt[:, t, d_ * 4:(d_ + 1) * 4].unsqueeze(2),
                                    op=ALU.mult), r=[psN.r(), EBt.r()], w=[sm_.r()])
                                p.op("dve", lambda e, sm_=sm_: e.tensor_scalar(
                                    out=sm_[:, :, 1:2], in0=sm_[:, :, 0:1], scalar1=-1.0, scalar2=1.0, op0=ALU.mult, op1=ALU.max),
                                    r=[sm_.r()], w=[sm_.r()])
                                p.op("dve", lambda e, sm_=sm_: e.tensor_tensor(
                                    out=sm_[:, :, 2:3], in0=sm_[:, :, 1:2], in1=sm_[:, :, 0:1], op=ALU.max),
                                    r=[sm_.r()], w=[sm_.r()])
                                p.op("dve", lambda e, sm_=sm_: e.reciprocal(out=sm_[:, :, 3:4], in_=sm_[:, :, 2:3]),
                                     r=[sm_.r()], w=[sm_.r()])
                                p.op("dve", lambda e, sm_=sm_, t=t, d_=d_: e.tensor_tensor(
                                    out=sm_[:, :, 0:1], in0=sm_[:, :, 3:4], in1=EBt[:, t, d_ * 4:(d_ + 1) * 4].unsqueeze(2),
                                    op=ALU.mult), r=[sm_.r(), EBt.r()], w=[sm_.r()])
                                if d_ == 0:
                                    p.op("dve", lambda e, nv=nv, sm_=sm_, lt=lt: e.tensor_tensor(
                                        out=Hf[:, lt, :].rearrange("p (h n) -> p h n", h=4), in0=nv[:, :, 0:128],
                                        in1=bc(sm_[:, :, 0:1], [128, 4, 128]), op=ALU.mult),
                                        r=[psN.r(), sm_.r()], w=[Hf.r(lt)])
                                elif stop != "C5":
                                    hs, hq, f_, ob = Hs[lt % 2], Hq[0], fs[lt % 2], omb[lt % 2]
                                    p.op("dve", lambda e, nv=nv, sm_=sm_, hs=hs: e.tensor_tensor(
                                        out=hs[:].rearrange("p (h n) -> p h n", h=4), in0=nv[:, :, 0:128],
                                        in1=bc(sm_[:, :, 0:1], [128, 4, 128]), op=ALU.mult),
                                        r=[psN.r(), sm_.r()], w=[hs.r()])
                                    p.op(POOLENG, lambda e, hs=hs, lt=lt: e.tensor_tensor(
                                        out=hs[:], in0=hs[:], in1=Hf[:, lt, :], op=ALU.add),
                                        r=[hs.r(), Hf.r(lt)], w=[hs.r()])
                                    p.op(POOLENG, lambda e, hs=hs, hq=hq: e.tensor_tensor(out=hq[:], in0=hs[:], in1=hs[:], op=ALU.mult),
                                         r=[hs.r()], w=[hq.r()])
                                    p.op("dve", lambda e, hq=hq, f_=f_: e.reduce_sum(
                                        out=f_[:, 0:4], in_=hq[:].rearrange("p (h n) -> p h n", h=4), axis=AX.X),
                                        r=[hq.r()], w=[f_.r()])
                                    p.op("act", lambda e, f_=f_: e.activation(out=f_[:, 4:8], in_=f_[:, 0:4], func=AF.Sqrt,
                                                                           scale=1.0 / 128.0, bias=EPS), r=[f_.r()], w=[f_.r()])
                                    p.op("dve", lambda e, f_=f_: e.reciprocal(out=f_[:, 4:8], in_=f_[:, 4:8]), r=[f_.r()], w=[f_.r()])
                                    p.op("dve", lambda e, hs=hs, f_=f_: e.tensor_tensor(
                                        out=hs[:].rearrange("p (h n) -> p h n", h=4), in0=hs[:].rearrange("p (h n) -> p h n", h=4),
                                        in1=bc(f_[:, 4:8].unsqueeze(2), [128, 4, 128]), op=ALU.mult),
                                        r=[hs.r(), f_.r()], w=[hs.r()])
                                    p.op(POOLENG, lambda e, hs=hs: e.tensor_tensor(out=hs[:], in0=hs[:], in1=ghd[:], op=ALU.mult),
                                         r=[hs.r(), ghd.r()], w=[hs.r()])
                                    p.op(POOLENG, lambda e, hs=hs, ob=ob, lt=lt: e.tensor_tensor(
                                        out=ob[:], in0=hs[:], in1=osig[:, lt, :], op=ALU.mult),
                                        r=[hs.r(), osig.r(lt)], w=[ob.r()])
                                    ps = psS
                                    psb = ps[:].bitcast(BF16)
                                    for c4 in range(4):
                                        p.op("pe", lambda e, c4=c4, ob=ob, psb=psb: e.transpose(
                                            psb[:, c4 * 128:(c4 + 1) * 128], ob[:, c4 * 128:(c4 + 1) * 128], identb[:]),
                                            r=[ob.r(), identb.r()], w=[ps.r()])
                                    p.op("act", lambda e, lt=lt, psb=psb: e.copy(
                                        out=omlT[:, :, lt * 128:(lt + 1) * 128],
                                        in_=psb[:, 0:512].rearrange("p (k n) -> p k n", k=4)),
                                        r=[ps.r()], w=[omlT.r(lt)])
                            for hd in range(4):
                                c = hd // 2
                                if t >= 2:
                                    break
                                p.op("pe", lambda e, hd=hd, c=c, t=t, vp_=vp_: e.matmul(
                                    psU_[:, hd * 256:hd * 256 + 129], lhsT=ktm[:, t, c * 128:(c + 1) * 128], rhs=vp_[:, hd, :],
                                    start=True, stop=True), r=[ktm.r(t), vp_.r()], w=[psU_.r()])
                            uv = psU_[:].rearrange("p (h x) -> p h x", x=256)[:, :, 0:129]
                            ebl = bc(EBL[:, t, d_ * 4:(d_ + 1) * 4].unsqueeze(2), [128, 4, 129])
                            if step == 0:
                                p.op("dve", lambda e, C_=C_, uv=uv, ebl=ebl: e.tensor_tensor(out=C_[:], in0=uv, in1=ebl, op=ALU.mult),
                                     r=[psU_.r(), EBL.r()], w=[C_.r()])
                            else:
                                p.op("dve", lambda e, C_=C_, uv=uv: e.tensor_tensor(out=C_[:], in0=uv, in1=C_[:], op=ALU.add),
                                     r=[psU_.r(), C_.r()], w=[C_.r()])
                                p.op("dve", lambda e, C_=C_, ebl=ebl: e.tensor_tensor(out=C_[:], in0=C_[:], in1=ebl, op=ALU.mult),
                                     r=[C_.r(), EBL.r()], w=[C_.r()])
                            p.op("act", lambda e, C_=C_, Cb_=Cb_: e.copy(out=Cb_[:], in_=C_[:]), r=[C_.r()], w=[Cb_.r()])
                    if b == 0 and "omlT" in dbg_d:
                        dump("omlT", omlT[:], omlT.res)
                p.barrier()
                if stop in ("C", "C4", "C5", "C6", "C7"):
                    break

                with ExitStack() as L:
                    wbn = sb(L, "wbn", [128, 4, D], BF16); wbm = sb(L, "wbm", [128, 4, D], BF16)
                    wout = sb(L, "wout", [128, 8, D], BF16)
                    wr = sb(L, "wr", [128, 8, NE]); br = sb(L, "br", [1, NE])
                    p.dma("pool", wbn[:], wview(w_bna_d), w=[wbn.r()], sem="w")
                    p.dma("pool", wbm[:], wview(w_bml_d), w=[wbm.r()], sem="w")
                    p.dma("pool", wout[:], wview(w_out_d), w=[wout.r()], sem="w")
                    p.dma("sp", wr[:], wview(w_r_d), w=[wr.r()], sem="x")
                    p.dma("sp", br[:], b_r_d, w=[br.r()], sem="x")
                    wgn = [sb(L, "wgn%d" % i, [128, 8, 128], BF16) for i in range(2)]
                    wgm = [sb(L, "wgm%d" % i, [128, 8, 128], BF16) for i in range(2)]
                    mT = sb(L, "mT", [128, 8, 512], BF16, nres=8)
                    sg = [sb(L, "sg%d" % i, [128, 512]) for i in range(2)]
                    m1 = [sb(L, "m1%d" % i, [128, 512]) for i in range(2)]
                    xt = [sb(L, "xd%d" % i, [128, D]) for i in range(2)]
                    yt = [sb(L, "yd%d" % i, [128, D]) for i in range(2)]
                    jk = sb(L, "jk", [128, D])
                    h2 = [sb(L, "h2%d" % i, [128, D]) for i in range(2)]
                    h2hi = [sb(L, "h2hi%d" % i, [128, D], BF16) for i in range(2)]
                    h2lo = [sb(L, "h2lo%d" % i, [128, D], BF16) for i in range(2)]
                    h2Tlo = [sb(L, "h2Tlo%d" % i, [128, 8, 128], BF16) for i in range(2)]
                    wrhi = sb(L, "wrhi", [128, 8, NE], BF16); wrlo = sb(L, "wrlo", [128, 8, NE], BF16)
                    p.op("dve", lambda e: e.tensor_copy(out=wrhi[:], in_=wr[:]), r=[wr.r()], w=[wrhi.r()])
                    p.op("dve", lambda e: e.tensor_tensor(out=wrlo[:], in0=wr[:], in1=wrhi[:], op=ALU.subtract),
                         r=[wr.r(), wrhi.r()], w=[wrlo.r()])
                    st = [sb(L, "std%d" % i, [128, 4]) for i in range(2)]
                    lg = [sb(L, "lg%d" % i, [128, 3, NE]) for i in range(2)]
                    t8 = [sb(L, "t8%d" % i, [128, 20]) for i in range(2)]
                    mkb = [sb(L, "mkb%d" % i, [128, NE], BF16) for i in range(2)]
                    oh4 = [sb(L, "oh4%d" % i, [128, 4, NE]) for i in range(2)]
                    n = 0
                    for tb in range(4):
                        tok0 = 256 + tb * 512
                        for dc in range(8):
                            n += 1
                            a_, b_ = wgn[n % 2], wgm[n % 2]
                            p.dma("pool", a_[:], wview(w_in_d[:, C_GNA + dc * 128:C_GNA + (dc + 1) * 128]), w=[a_.r()], sem="w")
                            p.dma("pool", b_[:], wview(w_in_d[:, C_GML + dc * 128:C_GML + (dc + 1) * 128]), w=[b_.r()], sem="w")
                            pa, pb = PD[0], PD[1]
                            mm_fm(pa[:, 0:512], pa.r(), a_, 0, tok0, 512)
                            mm_fm(pa[:, 512:1024], pa.r(), wbn, dc * 128, tb * 512, 512, nk=4, src=onaT)
                            mm_fm(pb[:, 0:512], pb.r(), b_, 0, tok0, 512)
                            mm_fm(pb[:, 512:1024], pb.r(), wbm, dc * 128, tb * 512, 512, nk=4, src=omlT)
                            p.op("act", lambda e, pa=pa: e.activation(out=sg[0][:], in_=pa[:, 0:512], func=AF.Sigmoid),
                                 r=[pa.r()], w=[sg[0].r()])
                            p.op("act", lambda e, pb=pb: e.activation(out=sg[1][:], in_=pb[:, 0:512], func=AF.Sigmoid),
                                 r=[pb.r()], w=[sg[1].r()])
                            p.op("dve", lambda e, pa=pa: e.tensor_tensor(out=m1[0][:], in0=pa[:, 512:1024], in1=sg[0][:], op=ALU.mult),
                                 r=[pa.r(), sg[0].r()], w=[m1[0].r()])
                            p.op("dve", lambda e, pb=pb: e.tensor_tensor(out=m1[1][:], in0=pb[:, 512:1024], in1=sg[1][:], op=ALU.mult),
                                 r=[pb.r(), sg[1].r()], w=[m1[1].r()])
                            p.op(POOLENG, lambda e, dc=dc: e.tensor_tensor(out=mT[:, dc, :], in0=m1[0][:], in1=m1[1][:], op=ALU.add),
                                 r=[m1[0].r(), m1[1].r()], w=[mT.r(dc)])
                        for ti in range(4):
                            t = tb * 4 + ti
                            i = t % 2
                            py = PD[2]
                            for half in range(2):
                                for kc in range(8):
                                    p.op("pe", lambda e, kc=kc, half=half, ti=ti, py=py: e.matmul(
                                        py[:, half * 512:(half + 1) * 512], lhsT=mT[:, kc, ti * 128:(ti + 1) * 128],
                                        rhs=wout[:, kc, half * 512:(half + 1) * 512], start=(kc == 0), stop=(kc == 7)),
                                        r=[mT.r(kc), wout.r()], w=[py.r()])
                            p.dma("sp", xt[i][:], x_d[b, t * 128:(t + 1) * 128, :], w=[xt[i].r()], sem="x")
                            rstd_of(py[:], [py.r()], jk, st[i])
                            p.op("dve", lambda e, i=i, py=py: e.scalar_tensor_tensor(
                                out=yt[i][:], in0=py[:], scalar=st[i][:, 1:2], in1=G_m[:], op0=ALU.mult, op1=ALU.mult),
                                r=[py.r(), st[i].r(), G_m.r()], w=[yt[i].r()])
                            p.op(POOLENG, lambda e, i=i: e.tensor_tensor(out=yt[i][:], in0=yt[i][:], in1=xt[i][:], op=ALU.add),
                                 r=[yt[i].r(), xt[i].r()], w=[yt[i].r()])
                            p.dma("sp", out_d[b, t * 128:(t + 1) * 128, :], yt[i][:], r=[yt[i].r()], w=[x1res[b][t]], sem="o")
                            if b == 0 and "x1" in dbg_d:
                                dump("x1", yt[i][:], yt[i].r(), dst=dbg_d["x1"][t])
                            rstd_of(yt[i][:], [yt[i].r()], jk, st[i])
                            p.op("dve", lambda e, i=i: e.scalar_tensor_tensor(
                                out=h2[i][:], in0=yt[i][:], scalar=st[i][:, 1:2], in1=A_f[:], op0=ALU.mult, op1=ALU.mult),
                                r=[yt[i].r(), st[i].r(), A_f.r()], w=[h2[i].r()])
                            p.op(POOLENG, lambda e, i=i: e.tensor_tensor(out=h2[i][:], in0=h2[i][:], in1=sh_f[:], op=ALU.add),
                                 r=[h2[i].r(), sh_f.r()], w=[h2[i].r()])
                            p.op("act", lambda e, i=i: e.copy(out=h2hi[i][:], in_=h2[i][:]), r=[h2[i].r()], w=[h2hi[i].r()])
                            p.op("dve", lambda e, i=i: e.tensor_tensor(out=h2lo[i][:], in0=h2[i][:], in1=h2hi[i][:], op=ALU.subtract),
                                 r=[h2[i].r(), h2hi[i].r()], w=[h2lo[i].r()])
                            pa_ = PS[i]
                            pab = pa_[:].bitcast(BF16)
                            for kc in range(8):
                                p.op("pe", lambda e, kc=kc, i=i, pab=pab: e.transpose(
                                    pab[:, kc * 128:(kc + 1) * 128], h2hi[i][:, kc * 128:(kc + 1) * 128], identb[:]),
                                    r=[h2hi[i].r(), identb.r()], w=[pa_.r()])
                            p.op("act", lambda e, t=t, pab=pab: e.copy(
                                out=hT[:, :, (2 + t) * 128:(3 + t) * 128], in_=pab.rearrange("p (k n) -> p k n", k=8)),
                                r=[pa_.r()], w=[hT.r(2 + t)])
                            pb_ = PD[i]
                            pbb = pb_[:].bitcast(BF16)
                            for kc in range(8):
                                p.op("pe", lambda e, kc=kc, i=i, pbb=pbb: e.transpose(
                                    pbb[:, kc * 128:(kc + 1) * 128], h2lo[i][:, kc * 128:(kc + 1) * 128], identb[:]),
                                    r=[h2lo[i].r(), identb.r()], w=[pb_.r()])
                            p.op("dve", lambda e, i=i, pbb=pbb: e.tensor_copy(
                                out=h2Tlo[i][:], in_=pbb[:, 0:1024].rearrange("p (k n) -> p k n", k=8)),
                                r=[pb_.r()], w=[h2Tlo[i].r()])
                            pl = PS[1 - i]
                            nmm = 0
                            for kc in range(8):
                                for (lh_, lres, w_) in ((hT[:, kc, (2 + t) * 128:(3 + t) * 128], hT.r(2 + t), wrhi),
                                                        (h2Tlo[i][:, kc, :], h2Tlo[i].r(), wrhi),
                                                        (hT[:, kc, (2 + t) * 128:(3 + t) * 128], hT.r(2 + t), wrlo)):
                                    nmm += 1
                                    p.op("pe", lambda e, kc=kc, lh_=lh_, w_=w_, pl=pl, nmm=nmm: e.matmul(
                                        pl[:, 0:NE], lhsT=lh_, rhs=w_[:, kc, :], start=(nmm == 1), stop=(nmm == 24)),
                                        r=[lres, w_.r()], w=[pl.r()])
                            p.op("pe", lambda e, pl=pl: e.matmul(pl[:, 32:64], lhsT=ones32[0:1, :], rhs=br[0:1, :], start=True, stop=True),
                                 r=[ones32.r(), br.r()], w=[pl.r()])
                            L_, T_ = lg[i], t8[i]
                            p.op("act", lambda e, L_=L_, pl=pl: e.copy(out=L_[:, 0, :], in_=pl[:, 0:NE]), r=[pl.r()], w=[L_.r()])
                            p.op("dve", lambda e, L_=L_, pl=pl: e.tensor_tensor(out=L_[:, 0, :], in0=L_[:, 0, :], in1=pl[:, 32:64], op=ALU.add),
                                 r=[pl.r(), L_.r()], w=[L_.r()])
                            if b == 0 and "logits" in dbg_d:
                                dump("logits", L_[:, 0, :], L_.r(), dst=dbg_d["logits"][t])
                            p.op("dve", lambda e, L_=L_, T_=T_: e.max(out=T_[:, 0:8], in_=L_[:, 0, :]), r=[L_.r()], w=[T_.r()])
                            gt = b * NT + t
                            mk_, oh_ = mkb[i], oh4[i]
                            p.op("dve", lambda e, L_=L_, T_=T_: e.tensor_scalar(
                                out=L_[:, 1, :], in0=L_[:, 0, :], scalar1=T_[:, 3:4], scalar2=None, op0=ALU.is_ge),
                                r=[L_.r(), T_.r()], w=[L_.r()])
                            p.op("dve", lambda e, L_=L_, mk_=mk_: e.tensor_copy(out=mk_[:], in_=L_[:, 1, :]), r=[L_.r()], w=[mk_.r()])
                            p.op("pe", lambda e, pl=pl, mk_=mk_: e.matmul(pl[:, 64:96], lhsT=trisb[:], rhs=mk_[:], start=True, stop=False),
                                 r=[trisb.r(), mk_.r()], w=[pl.r()])
                            p.op("pe", lambda e, pl=pl: e.matmul(pl[:, 64:96], lhsT=onesb[:], rhs=msum[:], start=False, stop=True),
                                 r=[onesb.r(), msum.r()], w=[pl.r()])
                            p.op("dve", lambda e, L_=L_, pl=pl: e.scalar_tensor_tensor(
                                out=L_[:, 2, :], in0=pl[:, 64:96], scalar=float(CAP - 1), in1=ecap[:], op0=ALU.min, op1=ALU.add),
                                r=[pl.r(), ecap.r()], w=[L_.r()])
                            p.op("dve", lambda e, mk_=mk_: e.tensor_tensor(out=msum[:], in0=msum[:], in1=mk_[:], op=ALU.add),
                                 r=[msum.r(), mk_.r()], w=[msum.r()])
                            for k in range(4):
                                p.op("dve", lambda e, k=k, L_=L_, T_=T_, oh_=oh_: e.tensor_scalar(
                                    out=oh_[:, k, :], in0=L_[:, 0, :], scalar1=T_[:, k:k + 1], scalar2=None, op0=ALU.is_equal),
                                    r=[L_.r(), T_.r()], w=[oh_.r()])
                                p.op("dve", lambda e, k=k, L_=L_, oh_=oh_: e.tensor_tensor(
                                    out=oh_[:, k, :], in0=oh_[:, k, :], in1=L_[:, 2, :], op=ALU.mult),
                                    r=[oh_.r(), L_.r()], w=[oh_.r()])
                            p.op("dve", lambda e, T_=T_, oh_=oh_: e.reduce_sum(out=T_[:, 12:16], in_=oh_[:], axis=AX.X),
                                 r=[oh_.r()], w=[T_.r()])
                            p.op("dve", lambda e, T_=T_, gt=gt: e.tensor_copy(out=RIi[:, gt, :], in_=T_[:, 12:16]),
                                 r=[T_.r()], w=[RIi.r(gt)])
                            p.op("dve", lambda e, T_=T_: e.tensor_scalar_mul(out=T_[:, 8:9], in0=T_[:, 0:1], scalar1=-1.0),
                                 r=[T_.r()], w=[T_.r()])
                            p.op("act", lambda e, T_=T_: e.activation(out=T_[:, 16:20], in_=T_[:, 0:4], func=AF.Exp,
                                                                     bias=T_[:, 8:9], scale=1.0), r=[T_.r()], w=[T_.r()])
                            p.op("dve", lambda e, T_=T_: e.reduce_sum(out=T_[:, 9:10], in_=T_[:, 16:20], axis=AX.X),
                                 r=[T_.r()], w=[T_.r()])
                            p.op("dve", lambda e, T_=T_: e.reciprocal(out=T_[:, 10:11], in_=T_[:, 9:10]), r=[T_.r()], w=[T_.r()])
                            p.op("dve", lambda e, T_=T_, gt=gt: e.tensor_scalar_mul(
                                out=RIw[:, gt, :], in0=T_[:, 16:20], scalar1=T_[:, 10:11]), r=[T_.r()], w=[RIw.r(gt)])
                            for k in range(4):
                                p.idma(out=xe_d, out_offset=bass.IndirectOffsetOnAxis(ap=RIi[:, gt, k:k + 1], axis=0),
                                       in_=h2hi[i][:], in_offset=None, bounds=NE * CAP - 1,
                                       r=[h2hi[i].r(), RIi.r(gt)], w=[], sem="sc")
                p.barrier()
            if stop is not None:
                break

            p.barrier()

        if stop is None:
            p.barrier()
            with ExitStack() as L:
                wgb = [sb(L, "wgb%d" % i, [128, 8, D], BF16) for i in range(2)]
                wlb = [sb(L, "wlb%d" % i, [128, 8, D], BF16) for i in range(2)]
                wdb = [sb(L, "wdb%d" % i, [128, 8, D], BF16) for i in range(2)]
                bdb = [sb(L, "bdb%d" % i, [128, D]) for i in range(2)]
                bg = sb(L, "bg", [128, 8, NE]); bl = sb(L, "bl", [128, 8, NE])
                p.dma("sp", bg[:], bgT_d, w=[bg.r()], sem="x")
                p.dma("sp", bl[:], blT_d, w=[bl.r()], sem="x")
                xr = [sb(L, "xr%d" % i, [128, 4, D], BF16) for i in range(2)]
                xT = [sb(L, "xT%d" % i, [128, 8, 512], BF16) for i in range(2)]
                aT2 = [sb(L, "aT%d" % i, [128, 8, 512], BF16, nres=8) for i in range(2)]
                gg = [sb(L, "gg%d" % i, [128, 512]) for i in range(2)]
                sg = [sb(L, "sgE%d" % i, [128, 512]) for i in range(2)]
                l1 = [sb(L, "l1%d" % i, [128, 512]) for i in range(2)]
                t1 = [sb(L, "t1%d" % i, [128, 512]) for i in range(2)]
                ysb = [sb(L, "ysb%d" % i, [128, D]) for i in range(2)]

                class BV:
                    def __init__(self, parent, c0):
                        self.par, self.c0, self.res = parent, c0, Res()

                    def r(self):
                        return self.res

                    def ap(self):
                        return self.par[:, self.c0:self.c0 + 512]

                banks = [BV(PD[2], 0), BV(PD[2], 512), BV(PS[0], 0), BV(PS[1], 0)]
                bki = [0]

                def nbank():
                    bki[0] += 1
                    return banks[bki[0] % 4]

                NBLK = CAP // 512
                NTOT = NE * NBLK
                ycnt = [0]

                def load_w(e_):
                    p.dma("pool", wgb[e_ % 2][:], wview(w_gate_d[e_]), w=[wgb[e_ % 2].r()], sem="w")
                    p.dma("pool", wlb[e_ % 2][:], wview(w_lin_d[e_]), w=[wlb[e_ % 2].r()], sem="w")
                    p.dma("pool", wdb[e_ % 2][:], wview(w_down_d[e_]), w=[wdb[e_ % 2].r()], sem="w")
                    p.dma("sp", bdb[e_ % 2][:], b_down_d[e_:e_ + 1, :].partition_broadcast(128), w=[bdb[e_ % 2].r()], sem="x")

                def emit_T(n):
                    e_, blk = divmod(n, NBLK)
                    xr_, xT_ = xr[n % 2], xT[n % 2]
                    row0 = e_ * CAP + blk * 512
                    p.dma("sp", xr_[:], xe_d[row0:row0 + 512, :].rearrange("(j p) d -> p j d", p=128), w=[xr_.r()], sem="x")
                    for j in range(4):
                        bk = nbank()
                        psb = bk.ap().bitcast(BF16)
                        for kc in range(8):
                            p.op("pe", lambda e, kc=kc, j=j, psb=psb, xr_=xr_: e.transpose(
                                psb[:, kc * 128:(kc + 1) * 128], xr_[:, j, kc * 128:(kc + 1) * 128], identb[:]),
                                r=[xr_.r(), identb.r()], w=[bk.r()])
                        eng = "act" if j % 2 else "dve"
                        if eng == "act":
                            p.op("act", lambda e, j=j, psb=psb, xT_=xT_: e.copy(
                                out=xT_[:, :, j * 128:(j + 1) * 128], in_=psb.rearrange("p (k n) -> p k n", k=8)),
                                r=[bk.r()], w=[xT_.r()])
                        else:
                            p.op("dve", lambda e, j=j, psb=psb, xT_=xT_: e.tensor_copy(
                                out=xT_[:, :, j * 128:(j + 1) * 128], in_=psb.rearrange("p (k n) -> p k n", k=8)),
                                r=[bk.r()], w=[xT_.r()])

                def emit_GL(n, fc):
                    e_ = n // NBLK
                    wg_, wl_ = wgb[e_ % 2], wlb[e_ % 2]
                    xT_, aT = xT[n % 2], aT2[n % 2]
                    j = fc % 2
                    pg = PD[j]
                    for (w_, c0) in ((wg_, 0), (wl_, 512)):
                        for kc in range(8):
                            p.op("pe", lambda e, kc=kc, w_=w_, c0=c0, pg=pg: e.matmul(
                                pg[:, c0:c0 + 512], lhsT=w_[:, kc, fc * 128:(fc + 1) * 128], rhs=xT_[:, kc, :],
                                start=(kc == 0), stop=(kc == 7)), r=[w_.r(), xT_.r()], w=[pg.r()])
                    p.op("dve", lambda e: e.tensor_scalar(
                        out=gg[j][:], in0=pg[:, 0:512], scalar1=bg[:, fc, e_:e_ + 1], scalar2=7.0, op0=ALU.add, op1=ALU.min),
                        r=[pg.r(), bg.r()], w=[gg[j].r()])
                    p.op("act", lambda e: e.activation(out=sg[j][:], in_=gg[j][:], func=AF.Sigmoid, scale=1.702),
                         r=[gg[j].r()], w=[sg[j].r()])
                    p.op("dve", lambda e: e.tensor_scalar(
                        out=l1[j][:], in0=pg[:, 512:1024], scalar1=bl[:, fc, e_:e_ + 1], scalar2=7.0, op0=ALU.add, op1=ALU.min),
                        r=[pg.r(), bl.r()], w=[l1[j].r()])
                    p.op("dve", lambda e: e.tensor_scalar(
                        out=l1[j][:], in0=l1[j][:], scalar1=-7.0, scalar2=1.0, op0=ALU.max, op1=ALU.add),
                        r=[l1[j].r()], w=[l1[j].r()])
                    p.op(POOLENG, lambda e: e.tensor_tensor(out=t1[j][:], in0=gg[j][:], in1=sg[j][:], op=ALU.mult),
                         r=[gg[j].r(), sg[j].r()], w=[t1[j].r()])
                    p.op("dve", lambda e: e.tensor_tensor(out=aT[:, fc, :], in0=t1[j][:], in1=l1[j][:], op=ALU.mult),
                         r=[t1[j].r(), l1[j].r()], w=[aT.r(fc)])

                def emit_DOWN(n):
                    e_, blk = divmod(n, NBLK)
                    wd_, bd_, aT = wdb[e_ % 2], bdb[e_ % 2], aT2[n % 2]
                    row0 = e_ * CAP + blk * 512
                    for ti in range(4):
                        ycnt[0] += 1
                        ys_ = ysb[ycnt[0] % 2]
                        for half in range(2):
                            bk = nbank()
                            for fc in range(8):
                                p.op("pe", lambda e, fc=fc, half=half, ti=ti, bk=bk: e.matmul(
                                    bk.ap(), lhsT=aT[:, fc, ti * 128:(ti + 1) * 128],
                                    rhs=wd_[:, fc, half * 512:(half + 1) * 512], start=(fc == 0), stop=(fc == 7)),
                                    r=[aT.r(fc), wd_.r()], w=[bk.r()])
                            p.op("dve", lambda e, half=half, bk=bk, ys_=ys_: e.tensor_tensor(
                                out=ys_[:, half * 512:(half + 1) * 512], in0=bk.ap(), in1=bd_[:, half * 512:(half + 1) * 512], op=ALU.add),
                                r=[bk.r(), bd_.r()], w=[ys_.r()])
                        p.dma("sp", ye_d[row0 + ti * 128:row0 + (ti + 1) * 128, :], ys_[:], r=[ys_.r()], w=[], sem="o")

                for n in range(NTOT):
                    if n % NBLK == 0:
                        load_w(n // NBLK)
                    emit_T(n)
                    emit_GL(n, 0)
                    emit_GL(n, 1)
                    if n > 0:
                        emit_DOWN(n - 1)
                    for fc in range(2, 8):
                        emit_GL(n, fc)
                emit_DOWN(NTOT - 1)
            p.barrier()
            with ExitStack() as L:
                yk = [[sb(L, "yk%d_%d" % (i, k), [128, D]) for k in range(4)] for i in range(2)]
                acc = [sb(L, "acc%d" % i, [128, D]) for i in range(2)]
                xe1 = [sb(L, "xe1%d" % i, [128, D]) for i in range(2)]
                jk = sb(L, "jkF", [128, D], BF16)
                st = [sb(L, "stF%d" % i, [128, 4]) for i in range(2)]
                Gf = sb(L, "GfF", [128, D])
                for gt in range(nb * NT):
                    b, t = divmod(gt, NT)
                    i = gt % 2
                    if t == 0:
                        p.dma("sp", Gf[:], gf_d[b], r=[gfres[b]], w=[Gf.r()], sem="x")
                    for k in range(4):
                        p.idma(out=yk[i][k][:], out_offset=None, in_=ye_d,
                               in_offset=bass.IndirectOffsetOnAxis(ap=RIi[:, gt, k:k + 1], axis=0), bounds=NE * CAP - 1,
                               r=[RIi.r(gt)], w=[yk[i][k].r()], sem="ga")
                    p.dma("sp", xe1[i][:], out_d[b, t * 128:(t + 1) * 128, :], r=[x1res[b][t]], w=[xe1[i].r()], sem="x")
                    a_ = acc[i]
                    p.op("dve", lambda e, a_=a_, i=i, gt=gt: e.tensor_scalar_mul(out=a_[:], in0=yk[i][0][:], scalar1=RIw[:, gt, 0:1]),
                         r=[yk[i][0].r(), RIw.r(gt)], w=[a_.r()])
                    for k in range(1, 4):
                        p.op("dve", lambda e, a_=a_, i=i, gt=gt, k=k: e.scalar_tensor_tensor(
                            out=a_[:], in0=yk[i][k][:], scalar=RIw[:, gt, k:k + 1], in1=a_[:], op0=ALU.mult, op1=ALU.add),
                            r=[yk[i][k].r(), RIw.r(gt), a_.r()], w=[a_.r()])
                    if b == 0 and "ffn" in dbg_d:
                        dump("ffn", a_[:], a_.r(), dst=dbg_d["ffn"][t])
                    rstd_of(a_[:], [a_.r()], jk, st[i])
                    p.op("dve", lambda e, a_=a_, i=i: e.scalar_tensor_tensor(
                        out=a_[:], in0=a_[:], scalar=st[i][:, 1:2], in1=Gf[:], op0=ALU.mult, op1=ALU.mult),
                        r=[a_.r(), st[i].r(), Gf.r()], w=[a_.r()])
                    p.op("dve", lambda e, a_=a_, i=i: e.tensor_tensor(out=xe1[i][:], in0=xe1[i][:], in1=a_[:], op=ALU.add),
                         r=[xe1[i].r(), a_.r()], w=[xe1[i].r()])
                    p.dma("sp", out_d[b, t * 128:(t + 1) * 128, :], xe1[i][:], r=[xe1[i].r()], w=[x1res[b][t]], sem="o")
            p.barrier()

        p.barrier()
    return nc, p


def _host_consts():
    c = {}
    c["ident"] = np.eye(128, dtype=np.float32)
    j = np.arange(128)
    c["trif"] = (j[:, None] <= j[None, :]).astype(np.float32)
    c["trib"] = (j[:, None] >= j[None, :]).astype(np.float32)
    c["tris"] = (j[:, None] < j[None, :]).astype(np.float32)
    c["ecap"] = np.ascontiguousarray(np.broadcast_to((np.arange(NE) * CAP).astype(np.float32)[None, :], (128, NE)))
    t = np.arange(S)
    row = (t // 64).astype(np.float32)
    col = (t % 64).astype(np.float32)
    inv_freq = (np.float32(10000.0) ** (-np.arange(16, dtype=np.float32) / np.float32(16))).astype(np.float32)
    cos = np.zeros((128, S), np.float32)
    sin = np.zeros((128, S), np.float32)
    for pp in range(128):
        d = pp % 64
        pos = row if d < 32 else col
        dd = d % 32
        f = dd % 16
        sign = -1.0 if dd < 16 else 1.0
        ang = (pos * inv_freq[f]).astype(np.float32)
        cos[pp] = np.cos(ang)
        sin[pp] = sign * np.sin(ang)
    c["ropecos"] = cos
    c["ropesin"] = sin
    return c


def _na_bias_table(rpb):
    reps = [0, 1, 5, 14, 15]
    kk = np.arange(128)
    krl = kk // 64
    kc = kk % 64
    qq = np.arange(128)
    qrl = qq // 64
    qc = qq % 64
    cs = np.clip(qc - 8, 0, 48)
    tab = np.full((8, 128, 5, 5, 128), NEG, np.float32)
    for ci, i in enumerate(reps):
        js = int(np.clip(i - 2, 0, 11))
        for s in range(5):
            j = js + s
            kr = 2 * j + krl
            r = 2 * i + qrl
            rs = np.clip(r - 4, 0, 24)
            vr = (kr[:, None] >= rs[None, :]) & (kr[:, None] < rs[None, :] + 8)
            vc = (kc[:, None] >= cs[None, :]) & (kc[:, None] < cs[None, :] + 16)
            valid = vr & vc
            dr = np.clip(kr[:, None] - r[None, :] + 7, 0, 14)
            dc = np.clip(kc[:, None] - qc[None, :] + 15, 0, 30)
            vals = rpb[:, dr, dc]
            tab[:, :, ci, s, :] = np.where(valid[None], vals, np.float32(NEG))
    return np.ascontiguousarray(tab.reshape(8, 128, 5 * 640))


def _prep_inputs(inp, nb=NB, ncores=8):
    f = lambda a: np.ascontiguousarray(np.asarray(a, dtype=np.float32))
    consts = _host_consts()
    w_in = f(inp["w_in"][0])
    d = np.arange(64)
    partner = np.where((d % 32) < 16, d + 16, d - 16)
    permq = np.concatenate([C_MLQ + h * 64 + partner for h in range(4)])
    permk = np.concatenate([C_MLK + h * 64 + partner for h in range(4)])
    w_qkp = np.ascontiguousarray(np.concatenate([w_in[:, permq], w_in[:, permk]], axis=1))
    shared = dict(
        w_ada=f(inp["w_ada"][0]), b_ada=f(inp["b_ada"][0]).reshape(1, -1),
        g4=np.ascontiguousarray(np.stack([f(inp["g_mix_pre"][0]), f(inp["g_mix_post"][0]),
                                          f(inp["g_ffn_pre"][0]), f(inp["g_ffn_post"][0])])),
        w_in=w_in, w_qkp=w_qkp, b_mg=f(inp["b_mlstm_gates"][0]).reshape(1, 16),
        nab=_na_bias_table(f(inp["rpb"][0])), g_head=f(inp["g_mlstm_head"][0]).reshape(1, 512),
        w_bna=f(inp["w_branch_na"][0]), w_bml=f(inp["w_branch_ml"][0]), w_out=f(inp["w_out"][0]),
        w_router=f(inp["w_router"][0]), b_router=f(inp["b_router"][0]).reshape(1, NE),
        w_gate=f(inp["w_gate"][0]), w_lin=f(inp["w_lin"][0]), w_down=f(inp["w_down"][0]),
        bgT=np.ascontiguousarray(f(inp["b_gate"][0]).reshape(NE, 8, 128).transpose(2, 1, 0)),
        blT=np.ascontiguousarray(f(inp["b_lin"][0]).reshape(NE, 8, 128).transpose(2, 1, 0)),
        b_down=f(inp["b_down"][0]), **consts)
    x = f(inp["x"]); ctx = f(inp["ctx"]); c = f(inp["c"]); cc = f(inp["c_ctx"])
    maps = []
    for k in range(ncores):
        sl = slice(k * nb, (k + 1) * nb)
        c5 = np.zeros((5, D), np.float32)
        c5[:nb] = c[sl]
        c5[4] = cc
        cT = np.ascontiguousarray(c5.reshape(5, 8, 128).transpose(2, 1, 0))
        m = dict(shared)
        m.update(x=np.ascontiguousarray(x[sl]), ctx=np.ascontiguousarray(ctx[sl]), cT=cT)
        maps.append(m)
    return maps


def kernel(**inputs):
    maps = _prep_inputs(inputs)
    nc, _ = build()
    res = run_bass_kernel_spmd(nc, maps, core_ids=list(range(8)))
    return np.concatenate([r["out"] for r in res.results], axis=0).astype(np.float32)
```

```python
import numpy as np
from contextlib import ExitStack
import concourse.bass as bass
import concourse.mybir as mybir
from concourse.bass_utils import run_bass_kernel_spmd

F32 = mybir.dt.float32
BF16 = mybir.dt.bfloat16
AF = mybir.ActivationFunctionType
ALU = mybir.AluOpType
AX = mybir.AxisListType

D = 1024
S = 2048
CTX = 256
NB = 4
NT = 16
NTT = 18
NE = 32
EPS = 1e-6
NEG = -80.0
CAP = 2048
I32 = mybir.dt.int32
POOLENG = "dve"
N_IN = 5136
C_NAK, C_NAV, C_MLK, C_MLV, C_MLG = 0, 512, 1024, 1280, 1792
C_NAQ, C_MLQ, C_MLO, C_GNA, C_GML = 1808, 2320, 2576, 3088, 4112


class Res:
    __slots__ = ("lw", "rd")

    def __init__(self):
        self.lw = None
        self.rd = {}


class Prog:
    ENG = ["pe", "act", "dve", "pool", "sp"]

    def __init__(self, nc):
        self.nc = nc
        self.e = dict(pe=nc.tensor, act=nc.scalar, dve=nc.vector, pool=nc.gpsimd, sp=nc.sync)
        self.sem = {k: nc.alloc_semaphore("s_" + k) for k in self.ENG}
        self.cnt = {k: 0 for k in self.ENG}
        self.waited = {k: {} for k in self.ENG}
        self.n_ins = 0

    NDS = 8
    rr = None

    def dsem(self, name):
        if self.rr is None:
            self.rr = {}
        i = self.rr.get(name, 0)
        self.rr[name] = i + 1
        k = "d:%s:%d" % (name, i % self.NDS)
        if k not in self.sem:
            self.sem[k] = self.nc.alloc_semaphore("sd_%s_%d" % (name, i % self.NDS))
            self.cnt[k] = 0
        return k

    def _wait(self, eng, key, val):
        if self.waited[eng].get(key, 0) >= val:
            return
        self.e[eng].wait_ge(self.sem[key], val)
        self.waited[eng][key] = val
        self.n_ins += 1

    def _sync(self, eng, me, r, w):
        for x in r:
            if x.lw is not None:
                k, v = x.lw
                if k == me and me == "pe":
                    continue
                self._wait(eng, k, v)
        for x in w:
            if x.lw is not None:
                k, v = x.lw
                if k != me or me != "pe":
                    self._wait(eng, k, v)
            for k, v in x.rd.items():
                if k != me or me != "pe":
                    self._wait(eng, k, v)

    def _commit(self, me, val, r, w):
        for x in r:
            if x.rd.get(me, 0) < val:
                x.rd[me] = val
        for x in w:
            x.lw = (me, val)
            x.rd = {}

    max_ops = None
    tot = 0

    def op(self, eng, fn, r=(), w=()):
        self.tot += 1
        if self.max_ops is not None and self.tot > self.max_ops:
            return None
        self._sync(eng, eng, r, w)
        ins = fn(self.e[eng])
        self.cnt[eng] += 1
        ins.then_inc(self.sem[eng], 1)
        self._commit(eng, self.cnt[eng], r, w)
        self.n_ins += 1
        return ins

    def dma(self, q, out, in_, r=(), w=(), sem="ld"):
        self.tot += 1
        if self.max_ops is not None and self.tot > self.max_ops:
            return None
        k = self.dsem(sem)
        if self.cnt[k] > 0:
            self._wait(q, k, self.cnt[k])
        self._sync(q, k, r, w)
        ins = self.e[q].dma_start(out=out, in_=in_)
        self.cnt[k] += 16
        ins.then_inc(self.sem[k], 16)
        self._commit(k, self.cnt[k], r, w)
        self.n_ins += 1
        return ins

    def idma(self, out, out_offset, in_, in_offset, bounds, r=(), w=(), sem="ind"):
        self.tot += 1
        if self.max_ops is not None and self.tot > self.max_ops:
            return None
        k = self.dsem(sem)
        if self.cnt[k] > 0:
            self._wait("pool", k, self.cnt[k])
        self._sync("pool", k, r, w)
        ins = self.e["pool"].indirect_dma_start(out=out, out_offset=out_offset, in_=in_, in_offset=in_offset)
        self.cnt[k] += 16
        ins.then_inc(self.sem[k], 16)
        self._commit(k, self.cnt[k], r, w)
        self.n_ins += 1
        return ins

    def barrier(self, engs=None):
        for eng in (engs or self.ENG):
            for k, v in self.cnt.items():
                if k != eng and v > 0:
                    self._wait(eng, k, v)


class T:
    def __init__(self, h, nres=1):
        self.h = h
        self.res = [Res() for _ in range(nres)]

    def __getitem__(self, k):
        return self.h[k]

    def r(self, i=0):
        return self.res[i]


def build(nb=NB, stop=None, dbg=(), max_ops=None):
    nc = bass.Bass("TRN2", target_bir_lowering=False)
    p = Prog(nc)
    p.max_ops = max_ops

    def din(name, shape, dt=F32):
        return nc.dram_tensor(name, list(shape), dt, kind="ExternalInput").ap()

    x_d = din("x", [nb, S, D])
    ctx_d = din("ctx", [nb, CTX, D])
    cT_d = din("cT", [128, 8, 5])
    w_ada_d = din("w_ada", [D, 6 * D])
    b_ada_d = din("b_ada", [1, 6 * D])
    g4_d = din("g4", [4, D])
    w_in_d = din("w_in", [D, N_IN])
    w_qkp_d = din("w_qkp", [D, 512])
    b_mg_d = din("b_mg", [1, 16])
    nab_d = din("nab", [8, 128, 5 * 640])
    g_head_d = din("g_head", [1, 512])
    w_bna_d = din("w_bna", [512, D])
    w_bml_d = din("w_bml", [512, D])
    w_out_d = din("w_out", [D, D])
    w_r_d = din("w_router", [D, NE])
    b_r_d = din("b_router", [1, NE])
    w_gate_d = din("w_gate", [NE, D, D])
    w_lin_d = din("w_lin", [NE, D, D])
    w_down_d = din("w_down", [NE, D, D])
    bgT_d = din("bgT", [128, 8, NE])
    blT_d = din("blT", [128, 8, NE])
    b_down_d = din("b_down", [NE, D])
    ident_d = din("ident", [128, 128])
    trif_d = din("trif", [128, 128])
    trib_d = din("trib", [128, 128])
    cos_d = din("ropecos", [128, S])
    sin_d = din("ropesin", [128, S])
    tris_d = din("tris", [128, 128])
    ecap_d = din("ecap", [128, NE])
    xe_d = nc.dram_tensor("xe_scr", [NE * CAP, D], BF16).ap()
    ye_d = nc.dram_tensor("ye_scr", [NE * CAP, D], F32).ap()
    gf_d = nc.dram_tensor("gf_scr", [nb, 128, D], F32).ap()
    out_d = nc.dram_tensor("out", [nb, S, D], F32, kind="ExternalOutput").ap()
    dbg_d = {}
    for name, shape in dbg:
        dbg_d[name] = nc.dram_tensor("dbg_" + name, list(shape), F32, kind="ExternalOutput").ap()

    uid = [0]

    def sb(es, name, shape, dt=F32, nres=1):
        uid[0] += 1
        return T(es.enter_context(nc.sbuf_tensor("sb%d_%s" % (uid[0], name), list(shape), dt)), nres)

    PD = [T(nc.alloc_psum_tensor("pd%d" % i, [128, 1024], F32)) for i in range(3)]
    PS = [T(nc.alloc_psum_tensor("psg%d" % i, [128, 512], F32)) for i in range(2)]

    def dump(name, tile_ap, res, dst=None):
        if name in dbg_d:
            p.dma("pool", dbg_d[name] if dst is None else dst, tile_ap, r=(res if isinstance(res, list) else [res]), sem="dbg")

    def wview(ap2d):
        return ap2d.rearrange("(kc p) n -> p kc n", p=128)

    def bc(ap, shape):
        return ap.to_broadcast(list(shape))

    rot = [0, 0]

    def nPD():
        rot[0] += 1
        return PD[rot[0] % 3]

    def nPS():
        rot[1] += 1
        return PS[rot[1] % 2]

    with ExitStack() as G:
        ident = sb(G, "ident", [128, 128])
        identb = sb(G, "identb", [128, 128], BF16)
        trif = sb(G, "trif", [128, 128])
        trib = sb(G, "trib", [128, 128])
        ones32 = sb(G, "ones32", [128, 128])
        scT = sb(G, "scT", [128, 8, 5])
        A_c = sb(G, "A_c", [128, D])
        sh_c = sb(G, "sh_c", [128, D])
        trisb = sb(G, "trisb", [128, 128], BF16)
        onesb = sb(G, "onesb", [128, 128], BF16)
        ecap = sb(G, "ecap", [128, NE])
        msum = sb(G, "msum", [128, NE], BF16)
        RIi = sb(G, "RIi", [128, nb * NT, 4], I32, nres=nb * NT)
        RIw = sb(G, "RIw", [128, nb * NT, 4], F32, nres=nb * NT)
        zres = Res()
        gfres = [Res() for _ in range(nb)]
        p.dma("sp", ecap[:], ecap_d, w=[ecap.r()])
        p.op("dve", lambda e: e.memset(onesb[:], 1.0), w=[onesb.r()])
        p.op("dve", lambda e: e.memset(msum[:], 0.0), w=[msum.r()])
        with ExitStack() as L:
            ztile = sb(L, "ztile", [128, 4096], BF16)
            tmp32 = sb(L, "tmp32", [128, 128])
            p.dma("sp", tmp32[:], tris_d, w=[tmp32.r()])
            p.op("dve", lambda e: e.tensor_copy(out=trisb[:], in_=tmp32[:]), r=[tmp32.r()], w=[trisb.r()])
            p.op("dve", lambda e: e.memset(ztile[:], 0.0), w=[ztile.r()])
            for cz in range(NE * CAP // 512):
                p.dma("sp", xe_d[cz * 512:(cz + 1) * 512, :].rearrange("(p j) d -> p (j d)", p=128), ztile[:],
                      r=[ztile.r()], w=[], sem="z")
            p.barrier()
        p.dma("sp", ident[:], ident_d, w=[ident.r()])
        p.dma("sp", trif[:], trif_d, w=[trif.r()])
        p.dma("sp", trib[:], trib_d, w=[trib.r()])
        p.dma("sp", scT[:], cT_d, w=[scT.r()])
        p.op("dve", lambda e: e.tensor_copy(out=identb[:], in_=ident[:]), r=[ident.r()], w=[identb.r()])
        p.op("dve", lambda e: e.memset(ones32[:], 1.0), w=[ones32.r()])
        p.op("act", lambda e: e.activation(out=scT[:], in_=scT[:], func=AF.Silu), r=[scT.r()], w=[scT.r()])

        def rstd_of(src_ap, src_res, junk, st):
            p.op("act", lambda e: e.activation(out=junk[:], in_=src_ap, func=AF.Square, scale=1.0 / 32.0,
                                               accum_out=st[:, 0:1]), r=src_res, w=[junk.r(), st.r()])
            p.op("act", lambda e: e.activation(out=st[:, 1:2], in_=st[:, 0:1], func=AF.Sqrt, bias=EPS),
                 r=[st.r()], w=[st.r()])
            p.op("dve", lambda e: e.reciprocal(out=st[:, 1:2], in_=st[:, 1:2]), r=[st.r()], w=[st.r()])

        def ada_mod(j, pieces, tag):
            with ExitStack() as L:
                lh = sb(L, "lh" + tag, [128, 8, 128], BF16)
                g4 = sb(L, "g4" + tag, [128, 4, D])
                p.dma("sp", g4[:], g4_d.partition_broadcast(128), w=[g4.r()])
                for kc in range(8):
                    p.op("dve", lambda e, kc=kc: e.tensor_scalar_mul(
                        out=lh[:, kc, :], in0=ones32[:], scalar1=scT[:, kc, j:j + 1]),
                        r=[scT.r(), ones32.r()], w=[lh.r()])
                wa = [sb(L, "wa%d%s" % (i, tag), [128, 8, 512], BF16) for i in range(2)]
                ba = [sb(L, "ba%d%s" % (i, tag), [1, 512]) for i in range(2)]
                n = 0
                for (blk, out_t, kind, gi) in pieces:
                    for half in range(2):
                        c0 = blk * D + half * 512
                        wt = wa[n % 2]
                        bt = ba[n % 2]
                        n += 1
                        p.dma("pool", wt[:], wview(w_ada_d[:, c0:c0 + 512]), w=[wt.r()], sem="w")
                        p.dma("sp", bt[:], b_ada_d[:, c0:c0 + 512], w=[bt.r()], sem="x")
                        ps = nPD()
                        for kc in range(8):
                            p.op("pe", lambda e, kc=kc, ps=ps, wt=wt: e.matmul(
                                ps[:, 0:512], lhsT=lh[:, kc, :], rhs=wt[:, kc, :], start=(kc == 0), stop=(kc == 7)),
                                r=[lh.r(), wt.r()], w=[ps.r()])
                        o = out_t[:, half * 512:(half + 1) * 512]
                        tmp = sb(L, "adatmp%d%s" % (n, tag), [128, 512])
                        ps2 = nPS()
                        p.op("pe", lambda e, ps2=ps2, bt=bt: e.matmul(
                            ps2[:], lhsT=ones32[0:1, :], rhs=bt[0:1, :], start=True, stop=True),
                            r=[ones32.r(), bt.r()], w=[ps2.r()])
                        p.op("act", lambda e, tmp=tmp, ps2=ps2: e.copy(out=tmp[:], in_=ps2[:]), r=[ps2.r()], w=[tmp.r()])
                        p.op("dve", lambda e, tmp=tmp, ps=ps: e.tensor_tensor(out=tmp[:], in0=ps[:, 0:512], in1=tmp[:], op=ALU.add),
                             r=[ps.r(), tmp.r()], w=[tmp.r()])
                        if kind == "shift":
                            p.op("act", lambda e, o=o, tmp=tmp: e.copy(out=o, in_=tmp[:]), r=[tmp.r()], w=[out_t.r()])
                        elif kind == "scale":
                            gs = g4[:, gi, half * 512:(half + 1) * 512]
                            p.op("dve", lambda e, o=o, tmp=tmp, gs=gs: e.scalar_tensor_tensor(
                                out=o, in0=tmp[:], scalar=1.0, in1=gs, op0=ALU.add, op1=ALU.mult),
                                r=[tmp.r(), g4.r()], w=[out_t.r()])
                        else:
                            gs = g4[:, gi, half * 512:(half + 1) * 512]
                            p.op("dve", lambda e, o=o, tmp=tmp, gs=gs: e.tensor_tensor(
                                out=o, in0=tmp[:], in1=gs, op=ALU.mult),
                                r=[tmp.r(), g4.r()], w=[out_t.r()])
            p.barrier()

        ada_mod(4, [(0, sh_c, "shift", 0), (1, A_c, "scale", 0)], "c")
        x1res = [[Res() for _ in range(NT)] for _ in range(nb)]

        for b in range(nb):
          with ExitStack() as B:
            hT = sb(B, "hT", [128, 8, NTT * 128], BF16, nres=NTT)
            with ExitStack() as M:
              G_m = sb(M, "G_m", [128, D]); A_f = sb(M, "A_f", [128, D]); sh_f = sb(M, "sh_f", [128, D])
              with ExitStack() as PA:
                A_m = sb(PA, "A_m", [128, D]); sh_m = sb(PA, "sh_m", [128, D]); G_f = sb(PA, "G_f", [128, D])
                ada_mod(b, [(0, sh_m, "shift", 0), (1, A_m, "scale", 0), (2, G_m, "gate", 1),
                            (3, sh_f, "shift", 0), (4, A_f, "scale", 2), (5, G_f, "gate", 3)], "b")
                p.dma("sp", gf_d[b], G_f[:], r=[G_f.r()], w=[gfres[b]], sem="o")
                if b == 0:
                    dump("A_m", A_m[:], A_m.r()); dump("sh_m", sh_m[:], sh_m.r()); dump("G_f", G_f[:], G_f.r())
                    dump("A_c", A_c[:], A_c.r())
                with ExitStack() as L:
                    xt = [sb(L, "xt%d" % i, [128, D]) for i in range(2)]
                    sq = [sb(L, "sq%d" % i, [128, D]) for i in range(2)]
                    hb = [sb(L, "hb%d" % i, [128, D], BF16) for i in range(2)]
                    st = [sb(L, "st%d" % i, [128, 2]) for i in range(2)]
                    for t in range(NTT):
                        i = t % 2
                        src = ctx_d[b, t * 128:(t + 1) * 128, :] if t < 2 else x_d[b, (t - 2) * 128:(t - 1) * 128, :]
                        Am, shm = (A_c, sh_c) if t < 2 else (A_m, sh_m)
                        p.dma("sp", xt[i][:], src, w=[xt[i].r()], sem="x")
                        rstd_of(xt[i][:], [xt[i].r()], sq[i], st[i])
                        p.op("dve", lambda e, i=i, Am=Am: e.scalar_tensor_tensor(
                            out=sq[i][:], in0=xt[i][:], scalar=st[i][:, 1:2], in1=Am[:], op0=ALU.mult, op1=ALU.mult),
                            r=[xt[i].r(), st[i].r(), Am.r()], w=[sq[i].r()])
                        p.op("dve", lambda e, i=i, shm=shm: e.tensor_tensor(out=hb[i][:], in0=sq[i][:], in1=shm[:], op=ALU.add),
                             r=[sq[i].r(), shm.r()], w=[hb[i].r()])
                        ps = nPS()
                        psb = ps[:].bitcast(BF16)
                        for kc in range(8):
                            p.op("pe", lambda e, kc=kc, psb=psb, i=i: e.transpose(
                                psb[:, kc * 128:(kc + 1) * 128], hb[i][:, kc * 128:(kc + 1) * 128], identb[:]),
                                r=[hb[i].r(), identb.r()], w=[ps.r()])
                        p.op("act", lambda e, t=t, psb=psb: e.copy(
                            out=hT[:, :, t * 128:(t + 1) * 128], in_=psb.rearrange("p (k n) -> p k n", k=8)),
                            r=[ps.r()], w=[hT.r(t)])
                if b == 0 and "hT" in dbg_d:
                    dump("hT", hT[:], hT.res)
                p.barrier()
              if True:
                if stop == "A":
                    break
                onaT = sb(M, "onaT", [128, 4, S], BF16, nres=NT)

                def mm_fm(ps_ap, ps_res, w_t, wc0, tok0, ntok, nk=8, src=None):
                    src = src or hT
                    tiles = range(tok0 // 128, (tok0 + ntok) // 128)
                    for kc in range(nk):
                        p.op("pe", lambda e, kc=kc: e.matmul(
                            ps_ap, lhsT=w_t[:, kc, wc0:wc0 + 128], rhs=src[:, kc, tok0:tok0 + ntok],
                            start=(kc == 0), stop=(kc == nk - 1)),
                            r=[w_t.r()] + [src.r(t) for t in tiles], w=[ps_res])

                def mm_tm(ps_ap, ps_res, w_t, wc0, ncols, t):
                    for kc in range(8):
                        p.op("pe", lambda e, kc=kc: e.matmul(
                            ps_ap, lhsT=hT[:, kc, t * 128:(t + 1) * 128], rhs=w_t[:, kc, wc0:wc0 + ncols],
                            start=(kc == 0), stop=(kc == 7)),
                            r=[w_t.r(), hT.r(t)], w=[ps_res])

                with ExitStack() as L:
                  qT = sb(L, "qT", [128, 4, S], BF16, nres=4)
                  kT = sb(L, "kT", [128, 4, NTT * 128], BF16, nres=4)
                  vA = sb(L, "vA", [128, NTT, 8, 65], BF16, nres=NTT)
                  p.op("dve", lambda e: e.memset(vA[:, :, :, 64:65], 1.0), w=vA.res)
                  with ExitStack() as L2:
                    wna = sb(L2, "wna", [128, 8, 1536], BF16)
                    p.dma("pool", wna[:, :, 0:1024], wview(w_in_d[:, 0:1024]), w=[wna.r()], sem="w")
                    p.dma("pool", wna[:, :, 1024:1536], wview(w_in_d[:, C_NAQ:C_NAQ + 512]), w=[wna.r()], sem="w")
                    for c in range(4):
                        for tb in range(4):
                            ps = nPD()
                            mm_fm(ps[:, 0:512], ps.r(), wna, 1024 + c * 128, 256 + tb * 512, 512)
                            p.op("act", lambda e, c=c, tb=tb, ps=ps: e.activation(
                                out=qT[:, c, tb * 512:(tb + 1) * 512], in_=ps[:, 0:512], func=AF.Copy, scale=0.125),
                                r=[ps.r()], w=[qT.r(c)])
                        for (t0, n) in [(0, 512), (512, 512), (1024, 512), (1536, 512), (2048, 256)]:
                            ps = nPD()
                            mm_fm(ps[:, 0:n], ps.r(), wna, c * 128, t0, n)
                            p.op("dve", lambda e, c=c, t0=t0, n=n, ps=ps: e.tensor_copy(
                                out=kT[:, c, t0:t0 + n], in_=ps[:, 0:n]), r=[ps.r()], w=[kT.r(c)])
                    for t in range(NTT):
                        ps = nPD()
                        mm_tm(ps[:, 0:512], ps.r(), wna, 512, 512, t)
                        eng = "act" if t % 2 else "dve"
                        if eng == "act":
                            p.op("act", lambda e, t=t, ps=ps: e.copy(
                                out=vA[:, t, :, 0:64], in_=ps[:, 0:512].rearrange("p (h d) -> p h d", h=8)),
                                r=[ps.r()], w=[vA.r(t)])
                        else:
                            p.op("dve", lambda e, t=t, ps=ps: e.tensor_copy(
                                out=vA[:, t, :, 0:64], in_=ps[:, 0:512].rearrange("p (h d) -> p h d", h=8)),
                                r=[ps.r()], w=[vA.r(t)])
                  p.barrier()
                  if True:
                    EBh = [sb(L, "EB%d" % i, [128, 3200], BF16) for i in range(2)]
                    ona = sb(L, "ona", [128, NT, 512], BF16, nres=NT)
                    stg = sb(L, "nabst", [128, 3200])
                    PT = [sb(L, "PT%d" % i, [128, 896], BF16) for i in range(3)]
                    rc = [sb(L, "rc%d" % i, [128, 1]) for i in range(3)]
                    n = 0
                    items = [(h, i) for h in range(8) for i in range(NT)]
                    state = {}

                    def na_scores(n):
                        h, i = items[n]
                        hp = (h % 2) * 64
                        c = h // 2
                        EB = EBh[h % 2]
                        if i == 0:
                            p.dma("sp", stg[:], nab_d[h], w=[stg.r()], sem="x")
                            p.op("act", lambda e, EB=EB: e.activation(out=EB[:], in_=stg[:], func=AF.Exp),
                                 r=[stg.r()], w=[EB.r()])
                        js = min(max(i - 2, 0), 11)
                        cls = i - js if i < 2 or i > 13 else 2
                        pss = PD[n % 3]
                        pt = PT[n % 3]
                        qa = qT[hp:hp + 64, c, i * 128:(i + 1) * 128]
                        for s_i in range(7):
                            kt = (2 + js + s_i) if s_i < 5 else (s_i - 5)
                            p.op("pe", lambda e, s_i=s_i, kt=kt: e.matmul(
                                pss[:, s_i * 128:(s_i + 1) * 128], lhsT=kT[hp:hp + 64, c, kt * 128:(kt + 1) * 128],
                                rhs=qa, start=True, stop=True),
                                r=[kT.r(c), qT.r(c)], w=[pss.r()])
                        p.op("act", lambda e: e.activation(out=pt[:], in_=pss[:, 0:896], func=AF.Exp),
                             r=[pss.r()], w=[pt.r()])
                        p.op("dve", lambda e: e.tensor_tensor(
                            out=pt[:, 0:640], in0=pt[:, 0:640], in1=EB[:, cls * 640:(cls + 1) * 640], op=ALU.mult),
                            r=[pt.r(), EB.r()], w=[pt.r()])
                        state[n] = (pt, js)

                    def na_pv(n):
                        h, i = items[n]
                        pt, js = state.pop(n)
                        pso = PS[n % 2]
                        rcc = rc[n % 3]
                        for s_i in range(7):
                            kt = (2 + js + s_i) if s_i < 5 else (s_i - 5)
                            p.op("pe", lambda e, s_i=s_i, kt=kt: e.matmul(
                                pso[:, 0:65], lhsT=pt[:, s_i * 128:(s_i + 1) * 128], rhs=vA[:, kt, h, :],
                                start=(s_i == 0), stop=(s_i == 6)),
                                r=[pt.r(), vA.r(kt)], w=[pso.r()])
                        p.op("dve", lambda e: e.reciprocal(out=rcc[:], in_=pso[:, 64:65]),
                             r=[pso.r()], w=[rcc.r()])
                        p.op("dve", lambda e: e.tensor_scalar_mul(
                            out=ona[:, i, h * 64:(h + 1) * 64], in0=pso[:, 0:64], scalar1=rcc[:, 0:1]),
                            r=[pso.r(), rcc.r()], w=[ona.r(i)])

                    for n in range(len(items) + 1):
                        if n < len(items):
                            na_scores(n)
                        if n >= 1:
                            na_pv(n - 1)
                    for i in range(NT):
                        ps = nPS()
                        psb = ps[:].bitcast(BF16)
                        for c4 in range(4):
                            p.op("pe", lambda e, c4=c4, i=i, psb=psb: e.transpose(
                                psb[:, c4 * 128:(c4 + 1) * 128], ona[:, i, c4 * 128:(c4 + 1) * 128], identb[:]),
                                r=[ona.r(i), identb.r()], w=[ps.r()])
                        p.op("act", lambda e, i=i, psb=psb: e.copy(
                            out=onaT[:, :, i * 128:(i + 1) * 128], in_=psb[:, 0:512].rearrange("p (k n) -> p k n", k=4)),
                            r=[ps.r()], w=[onaT.r(i)])
                    if b == 0 and "ona" in dbg_d:
                        dump("ona", ona[:], ona.res)
                p.barrier()
                if stop == "B":
                    break
                omlT = sb(M, "omlT", [128, 4, S], BF16, nres=NT)

                with ExitStack() as L:
                    mqT = sb(L, "mqT", [128, 2, S], BF16, nres=2)
                    mkT = sb(L, "mkT", [128, 2, S], BF16, nres=2)
                    ktm = sb(L, "ktm", [128, NTT, 256], BF16, nres=NTT)
                    vM = sb(L, "vM", [128, NTT, 4, 129], BF16, nres=NTT)
                    osig = sb(L, "osig", [128, NT, 512], BF16, nres=NT)
                    gts = sb(L, "gts", [128, NTT, 16])
                    LI = sb(L, "LI", [128, NTT, 8])
                    LFn = sb(L, "LFn", [128, NTT, 8])
                    Bn = sb(L, "Bn", [128, NTT, 8])
                    EBt = sb(L, "EBt", [128, NTT, 8])
                    ES = sb(L, "ES", [128, NTT, 8])
                    EBL = sb(L, "EBL", [128, NTT, 8])
                    ghd = sb(L, "ghd", [128, 512])
                    p.dma("sp", ghd[:], g_head_d.partition_broadcast(128), w=[ghd.r()], sem="x")
                    p.op("dve", lambda e: e.memset(vM[:, :, :, 128:129], 1.0), w=vM.res)
                    with ExitStack() as L2:
                        wq = sb(L2, "wq", [128, 8, 256], BF16); wqp = sb(L2, "wqp", [128, 8, 256], BF16)
                        wk = sb(L2, "wk", [128, 8, 256], BF16); wkp = sb(L2, "wkp", [128, 8, 256], BF16)
                        cosT = sb(L2, "cosT", [128, 512]); sinT = sb(L2, "sinT", [128, 512])
                        rt = [sb(L2, "rt%d" % i, [128, 512]) for i in range(2)]
                        p.dma("pool", wq[:], wview(w_in_d[:, C_MLQ:C_MLQ + 256]), w=[wq.r()], sem="w")
                        p.dma("pool", wqp[:], wview(w_qkp_d[:, 0:256]), w=[wqp.r()], sem="w")
                        p.dma("pool", wk[:], wview(w_in_d[:, C_MLK:C_MLK + 256]), w=[wk.r()], sem="w")
                        p.dma("pool", wkp[:], wview(w_qkp_d[:, 256:512]), w=[wkp.r()], sem="w")
                        for (w_a, w_b, dst) in ((wq, wqp, mqT), (wk, wkp, mkT)):
                            for c in range(2):
                                for tb in range(4):
                                    ps = nPD()
                                    mm_fm(ps[:, 0:512], ps.r(), w_a, c * 128, 256 + tb * 512, 512)
                                    mm_fm(ps[:, 512:1024], ps.r(), w_b, c * 128, 256 + tb * 512, 512)
                                    cs_ = slice(tb * 512, (tb + 1) * 512)
                                    p.dma("sp", cosT[:], cos_d[:, cs_], w=[cosT.r()], sem="x")
                                    p.dma("sp", sinT[:], sin_d[:, cs_], w=[sinT.r()], sem="x")
                                    p.op("dve", lambda e, ps=ps, cs_=cs_: e.tensor_tensor(
                                        out=rt[0][:], in0=ps[:, 0:512], in1=cosT[:], op=ALU.mult),
                                        r=[ps.r(), cosT.r()], w=[rt[0].r()])
                                    p.op("dve", lambda e, ps=ps, cs_=cs_: e.tensor_tensor(
                                        out=rt[1][:], in0=ps[:, 512:1024], in1=sinT[:], op=ALU.mult),
                                        r=[ps.r(), sinT.r()], w=[rt[1].r()])
                                    p.op("dve", lambda e, dst=dst, c=c, cs_=cs_: e.tensor_tensor(
                                        out=dst[:, c, cs_], in0=rt[0][:], in1=rt[1][:], op=ALU.add),
                                        r=[rt[0].r(), rt[1].r()], w=[dst.r(c)])
                        for t in range(NT):
                            ps = nPS()
                            psb = ps[:].bitcast(BF16)
                            for c in range(2):
                                p.op("pe", lambda e, c=c, t=t, psb=psb: e.transpose(
                                    psb[:, c * 128:(c + 1) * 128], mkT[:, c, t * 128:(t + 1) * 128], identb[:]),
                                    r=[mkT.r(c), identb.r()], w=[ps.r()])
                            p.op("act", lambda e, t=t, psb=psb: e.copy(out=ktm[:, 2 + t, :], in_=psb[:, 0:256]),
                                 r=[ps.r()], w=[ktm.r(2 + t)])
                        for t in range(2):
                            ps = nPS()
                            mm_tm(ps[:, 0:256], ps.r(), wk, 0, 256, t)
                            p.op("act", lambda e, t=t, ps=ps: e.copy(out=ktm[:, t, :], in_=ps[:, 0:256]),
                                 r=[ps.r()], w=[ktm.r(t)])
                    p.barrier()
                    if stop == "C1":
                        break
                    with ExitStack() as L2:
                        wv = sb(L2, "wv", [128, 8, 512], BF16); wo = sb(L2, "wo", [128, 8, 512], BF16)
                        wg = sb(L2, "wgt", [128, 8, 16], BF16)
                        bmg = sb(L2, "bmg", [1, 16])
                        p.dma("pool", wv[:], wview(w_in_d[:, C_MLV:C_MLV + 512]), w=[wv.r()], sem="w")
                        p.dma("pool", wo[:], wview(w_in_d[:, C_MLO:C_MLO + 512]), w=[wo.r()], sem="w")
                        p.dma("pool", wg[:], wview(w_in_d[:, C_MLG:C_MLG + 16]), w=[wg.r()], sem="w")
                        p.dma("sp", bmg[:], b_mg_d, w=[bmg.r()], sem="x")
                        for t in range(NTT):
                            ps = nPD()
                            mm_tm(ps[:, 0:512], ps.r(), wv, 0, 512, t)
                            p.op("dve", lambda e, t=t, ps=ps: e.tensor_copy(
                                out=vM[:, t, :, 0:128], in_=ps[:, 0:512].rearrange("p (h d) -> p h d", h=4)),
                                r=[ps.r()], w=[vM.r(t)])
                            if t >= 2:
                                mm_tm(ps[:, 512:1024], ps.r(), wo, 0, 512, t)
                                p.op("act", lambda e, t=t, ps=ps: e.activation(
                                    out=osig[:, t - 2, :], in_=ps[:, 512:1024], func=AF.Sigmoid),
                                    r=[ps.r()], w=[osig.r(t - 2)])
                            ps2 = nPS()
                            for kc in range(8):
                                p.op("pe", lambda e, kc=kc, t=t, ps2=ps2: e.matmul(
                                    ps2[:, 0:16], lhsT=hT[:, kc, t * 128:(t + 1) * 128], rhs=wg[:, kc, :],
                                    start=(kc == 0), stop=(kc == 7)), r=[wg.r(), hT.r(t)], w=[ps2.r()])
                            p.op("pe", lambda e, ps2=ps2: e.matmul(
                                ps2[:, 16:32], lhsT=ones32[0:1, :], rhs=bmg[0:1, :], start=True, stop=True),
                                r=[ones32.r(), bmg.r()], w=[ps2.r()])
                            p.op("act", lambda e, t=t, ps2=ps2: e.copy(out=gts[:, t, :], in_=ps2[:, 0:16]),
                                 r=[ps2.r()], w=[gts.r()])
                            p.op("dve", lambda e, t=t, ps2=ps2: e.tensor_tensor(
                                out=gts[:, t, :], in0=gts[:, t, :], in1=ps2[:, 16:32], op=ALU.add),
                                r=[ps2.r(), gts.r()], w=[gts.r()])
                    p.barrier()
                    if stop == "C2":
                        break
                    gv = gts[:].rearrange("p t (k h) -> p t k h", k=4)
                    p.op("act", lambda e: e.activation(out=gts[:], in_=gts[:], func=AF.Tanh, scale=1.0 / 15.0),
                         r=[gts.r()], w=[gts.r()])
                    for d_ in range(2):
                        p.op("dve", lambda e, d_=d_: e.tensor_scalar_mul(
                            out=LI[:, :, d_ * 4:(d_ + 1) * 4], in0=gv[:, :, 2 * d_, :], scalar1=15.0),
                            r=[gts.r()], w=[LI.r()])
                        p.op("act", lambda e, d_=d_: e.activation(
                            out=LFn[:, :, d_ * 4:(d_ + 1) * 4], in_=gv[:, :, 2 * d_ + 1, :], func=AF.Exp, scale=-15.0),
                            r=[gts.r()], w=[LFn.r()])
                    p.op("act", lambda e: e.activation(out=LFn[:], in_=LFn[:], func=AF.Ln, bias=1.0),
                         r=[LFn.r()], w=[LFn.r()])
                    psc = nPS()
                    for t in range(NTT):
                        for d_, tri in ((0, trif), (1, trib)):
                            p.op("pe", lambda e, t=t, d_=d_, tri=tri: e.matmul(
                                psc[:, t * 8 + d_ * 4:t * 8 + d_ * 4 + 4], lhsT=tri[:], rhs=LFn[:, t, d_ * 4:(d_ + 1) * 4],
                                start=True, stop=True), r=[tri.r(), LFn.r()], w=[psc.r()])
                    p.op("dve", lambda e: e.tensor_copy(out=Bn[:].rearrange("p t k -> p (t k)"), in_=psc[:, 0:NTT * 8]),
                         r=[psc.r()], w=[Bn.r()])
                    psl = nPS()
                    p.op("pe", lambda e: e.matmul(psl[:, 0:NTT * 8], lhsT=ones32[:], rhs=LFn[:].rearrange("p t k -> p (t k)"),
                                                  start=True, stop=True), r=[ones32.r(), LFn.r()], w=[psl.r()])
                    p.op("act", lambda e: e.activation(out=EBL[:].rearrange("p t k -> p (t k)"), in_=psl[:, 0:NTT * 8],
                                                       func=AF.Exp, scale=-1.0), r=[psl.r()], w=[EBL.r()])
                    p.op("act", lambda e: e.activation(out=EBt[:], in_=Bn[:], func=AF.Exp, scale=-1.0),
                         r=[Bn.r()], w=[EBt.r()])
                    p.op("dve", lambda e: e.tensor_tensor(out=ES[:], in0=LI[:], in1=Bn[:], op=ALU.add),
                         r=[LI.r(), Bn.r()], w=[ES.r()])
                    p.op("act", lambda e: e.activation(out=ES[:], in_=ES[:], func=AF.Exp, bias=float(-np.log(8.0))),
                         r=[ES.r()], w=[ES.r()])
                    if stop == "C3":
                        p.barrier()
                        break
                    Hf = sb(L, "Hf", [128, NT, 512], BF16, nres=NT)
                    Cst = [sb(L, "Cst%d" % d_, [128, 4, 129]) for d_ in range(2)]
                    Cbf = [sb(L, "Cbf%d" % d_, [128, 4, 129], BF16) for d_ in range(2)]
                    vp = [sb(L, "vp%d" % d_, [128, 4, 129], BF16) for d_ in range(2)]
                    sTm = [sb(L, "sTm%d" % d_, [128, 4, 128], BF16) for d_ in range(2)]
                    sm = [sb(L, "sm%d" % d_, [128, 4, 4]) for d_ in range(2)]
                    Hs = [sb(L, "Hs%d" % i, [128, 512]) for i in range(2)]
                    Hq = [sb(L, "Hq%d" % i, [128, 512]) for i in range(1)]
                    fs = [sb(L, "fs%d" % i, [128, 8]) for i in range(2)]
                    omb = [sb(L, "omb%d" % i, [128, 512], BF16) for i in range(2)]
                    bwd_order = [1, 0] + list(range(NTT - 1, 1, -1))
                    tri4 = [sb(L, "tri4%d" % d_, [128, 4, 128], BF16) for d_ in range(2)]
                    for d_, tr_ in ((0, trif), (1, trib)):
                        for hd in range(4):
                            p.op("dve", lambda e, d_=d_, tr_=tr_, hd=hd: e.tensor_copy(out=tri4[d_][:, hd, :], in_=tr_[:]),
                                 r=[tr_.r()], w=[tri4[d_].r()])
                    psN_ = [PD[0], PD[2]]
                    psU_ = PD[1]
                    for d_ in range(2):
                        if stop in ("C4", "C6", "C7") and d_ == 1:
                            break
                        for step in range(NTT):
                            if (stop == "C6" and step == 2) or (stop == "C7" and step == 3):
                                break
                            t = step if d_ == 0 else bwd_order[step]
                            tri = trif if d_ == 0 else trib
                            psN = psN_[d_]
                            psS = PS[d_]
                            C_, Cb_, vp_, sT_, sm_ = Cst[d_], Cbf[d_], vp[d_], sTm[d_], sm[d_]
                            p.op(POOLENG, lambda e, t=t, d_=d_, vp_=vp_: e.tensor_tensor(
                                out=vp_[:], in0=vM[:, t, :, :], in1=bc(ES[:, t, d_ * 4:(d_ + 1) * 4].unsqueeze(2), [128, 4, 129]),
                                op=ALU.mult), r=[vM.r(t), ES.r()], w=[vp_.r()])
                            if t >= 2:
                                lt = t - 2
                                for hd in range(4):
                                    hp, c = (hd % 2) * 64, hd // 2
                                    p.op("pe", lambda e, hd=hd, hp=hp, c=c, lt=lt, psS=psS: e.matmul(
                                        psS[:, hd * 128:(hd + 1) * 128], lhsT=mkT[hp:hp + 64, c, lt * 128:(lt + 1) * 128],
                                        rhs=mqT[hp:hp + 64, c, lt * 128:(lt + 1) * 128], start=True, stop=True),
                                        r=[mkT.r(c), mqT.r(c)], w=[psS.r()])
                                    p.op("pe", lambda e, hd=hd, c=c, t=t, vp_=vp_: e.matmul(
                                        psU_[:, hd * 256:hd * 256 + 129], lhsT=ktm[:, t, c * 128:(c + 1) * 128], rhs=vp_[:, hd, :],
                                        start=True, stop=True), r=[ktm.r(t), vp_.r()], w=[psU_.r()])
                                p.op("dve", lambda e, sT_=sT_, psS=psS, d_=d_: e.tensor_tensor(
                                    out=sT_[:].rearrange("p h n -> p (h n)"), in0=psS[:, 0:512],
                                    in1=tri4[d_][:].rearrange("p h n -> p (h n)"), op=ALU.mult),
                                    r=[psS.r(), tri4[d_].r()], w=[sT_.r()])
                                for hd in range(4):
                                    hp, c = (hd % 2) * 64, hd // 2
                                    p.op("pe", lambda e, hd=hd, psN=psN, sT_=sT_, vp_=vp_: e.matmul(
                                        psN[:, hd * 256:hd * 256 + 129], lhsT=sT_[:, hd, :], rhs=vp_[:, hd, :],
                                        start=True, stop=(step == 0)), r=[sT_.r(), vp_.r()], w=[psN.r()])
                                    if step > 0:
                                        p.op("pe", lambda e, hd=hd, hp=hp, c=c, lt=lt, psN=psN, Cb_=Cb_: e.matmul(
                                            psN[:, hd * 256:hd * 256 + 129], lhsT=mqT[hp:hp + 64, c, lt * 128:(lt + 1) * 128],
                                            rhs=Cb_[hp:hp + 64, hd, :], start=False, stop=True),
                                            r=[mqT.r(c), Cb_.r()], w=[psN.r()])
                                nv = psN[:].rearrange("p (h x) -> p h x", x=256)
                                p.op("dve", lambda e, nv=nv, sm_=sm_, t=t, d_=d_: e.tensor_tensor(
                                    out=sm_[:, :, 0:1], in0=nv[:, :, 128:129], in1=EBt[:, t, d_ * 4:(d_ + 1) * 4].unsqueeze(2),
                                    op=ALU.mult), r=[psN.r(), EBt.r()], w=[sm_.r()])
                                p.op("dve", lambda e, sm_=sm_: e.tensor_scalar(
                                    out=sm_[:, :, 1:2], in0=sm_[:, :, 0:1], scalar1=-1.0, scalar2=1.0, op0=ALU.mult, op1=ALU.max),
                                    r=[sm_.r()], w=[sm_.r()])
                                p.op("dve", lambda e, sm_=sm_: e.tensor_tensor(
                                    out=sm_[:, :, 2:3], in0=sm_[:, :, 1:2], in1=sm_[:, :, 0:1], op=ALU.max),
                                    r=[sm_.r()], w=[sm_.r()])
                                p.op("dve", lambda e, sm_=sm_: e.reciprocal(out=sm_[:, :, 3:4], in_=sm_[:, :, 2:3]),
                                     r=[sm_.r()], w=[sm_.r()])
                                p.op("dve", lambda e, sm_=sm_, t=t, d_=d_: e.tensor_tensor(
                                    out=sm_[:, :, 0:1], in0=sm_[:, :, 3:4], in1=EBt[:, t, d_ * 4:(d_ + 1) * 4].unsqueeze(2),
                                    op=ALU.mult), r=[sm_.r(), EBt.r()], w=[sm_.r()])
                                if d_ == 0:
                                    p.op("dve", lambda e, nv=nv, sm_=sm_, lt=lt: e.tensor_tensor(
                                        out=Hf[:, lt, :].rearrange("p (h n) -> p h n", h=4), in0=nv[:, :, 0:128],
                                        in1=bc(sm_[:, :, 0:1], [128, 4, 128]), op=ALU.mult),
                                        r=[psN.r(), sm_.r()], w=[Hf.r(lt)])
                                elif stop != "C5":
                                    hs, hq, f_, ob = Hs[lt % 2], Hq[0], fs[lt % 2], omb[lt % 2]
                                    p.op("dve", lambda e, nv=nv, sm_=sm_, hs=hs: e.tensor_tensor(
                                        out=hs[:].rearrange("p (h n) -> p h n", h=4), in0=nv[:, :, 0:128],
                                        in1=bc(sm_[:, :, 0:1], [128, 4, 128]), op=ALU.mult),
                                        r=[psN.r(), sm_.r()], w=[hs.r()])
                                    p.op(POOLENG, lambda e, hs=hs, lt=lt: e.tensor_tensor(
                                        out=hs[:], in0=hs[:], in1=Hf[:, lt, :], op=ALU.add),
                                        r=[hs.r(), Hf.r(lt)], w=[hs.r()])
                                    p.op(POOLENG, lambda e, hs=hs, hq=hq: e.tensor_tensor(out=hq[:], in0=hs[:], in1=hs[:], op=ALU.mult),
                                         r=[hs.r()], w=[hq.r()])
                                    p.op("dve", lambda e, hq=hq, f_=f_: e.reduce_sum(
                                        out=f_[:, 0:4], in_=hq[:].rearrange("p (h n) -> p h n", h=4), axis=AX.X),
                                        r=[hq.r()], w=[f_.r()])
                                    p.op("act", lambda e, f_=f_: e.activation(out=f_[:, 4:8], in_=f_[:, 0:4], func=AF.Sqrt,
                                                                           scale=1.0 / 128.0, bias=EPS), r=[f_.r()], w=[f_.r()])
                                    p.op("dve", lambda e, f_=f_: e.reciprocal(out=f_[:, 4:8], in_=f_[:, 4:8]), r=[f_.r()], w=[f_.r()])
                                    p.op("dve", lambda e, hs=hs, f_=f_: e.tensor_tensor(
                                        out=hs[:].rearrange("p (h n) -> p h n", h=4), in0=hs[:].rearrange("p (h n) -> p h n", h=4),
                                        in1=bc(f_[:, 4:8].unsqueeze(2), [128, 4, 128]), op=ALU.mult),
                                        r=[hs.r(), f_.r()], w=[hs.r()])
                                    p.op(POOLENG, lambda e, hs=hs: e.tensor_tensor(out=hs[:], in0=hs[:], in1=ghd[:], op=ALU.mult),
                                         r=[hs.r(), ghd.r()], w=[hs.r()])
                                    p.op(POOLENG, lambda e, hs=hs, ob=ob, lt=lt: e.tensor_tensor(
                                        out=ob[:], in0=hs[:], in1=osig[:, lt, :], op=ALU.mult),
                                        r=[hs.r(), osig.r(lt)], w=[ob.r()])
                                    ps = psS
                                    psb = ps[:].bitcast(BF16)
                                    for c4 in range(4):
                                        p.op("pe", lambda e, c4=c4, ob=ob, psb=psb: e.transpose(
                                            psb[:, c4 * 128:(c4 + 1) * 128], ob[:, c4 * 128:(c4 + 1) * 128], identb[:]),
                                            r=[ob.r(), identb.r()], w=[ps.r()])
                                    p.op("act", lambda e, lt=lt, psb=psb: e.copy(
                                        out=omlT[:, :, lt * 128:(lt + 1) * 128],
                                        in_=psb[:, 0:512].rearrange("p (k n) -> p k n", k=4)),
                                        r=[ps.r()], w=[omlT.r(lt)])
                            for hd in range(4):
                                c = hd // 2
                                if t >= 2:
                                    break
                                p.op("pe", lambda e, hd=hd, c=c, t=t, vp_=vp_: e.matmul(
                                    psU_[:, hd * 256:hd * 256 + 129], lhsT=ktm[:, t, c * 128:(c + 1) * 128], rhs=vp_[:, hd, :],
                                    start=True, stop=True), r=[ktm.r(t), vp_.r()], w=[psU_.r()])
                            uv = psU_[:].rearrange("p (h x) -> p h x", x=256)[:, :, 0:129]
                            ebl = bc(EBL[:, t, d_ * 4:(d_ + 1) * 4].unsqueeze(2), [128, 4, 129])
                            if step == 0:
                                p.op("dve", lambda e, C_=C_, uv=uv, ebl=ebl: e.tensor_tensor(out=C_[:], in0=uv, in1=ebl, op=ALU.mult),
                                     r=[psU_.r(), EBL.r()], w=[C_.r()])
                            else:
                                p.op("dve", lambda e, C_=C_, uv=uv: e.tensor_tensor(out=C_[:], in0=uv, in1=C_[:], op=ALU.add),
                                     r=[psU_.r(), C_.r()], w=[C_.r()])
                                p.op("dve", lambda e, C_=C_, ebl=ebl: e.tensor_tensor(out=C_[:], in0=C_[:], in1=ebl, op=ALU.mult),
                                     r=[C_.r(), EBL.r()], w=[C_.r()])
                            p.op("act", lambda e, C_=C_, Cb_=Cb_: e.copy(out=Cb_[:], in_=C_[:]), r=[C_.r()], w=[Cb_.r()])
                    if b == 0 and "omlT" in dbg_d:
                        dump("omlT", omlT[:], omlT.res)
                p.barrier()
                if stop in ("C", "C4", "C5", "C6", "C7"):
                    break

                with ExitStack() as L:
                    wbn = sb(L, "wbn", [128, 4, D], BF16); wbm = sb(L, "wbm", [128, 4, D], BF16)
                    wout = sb(L, "wout", [128, 8, D], BF16)
                    wr = sb(L, "wr", [128, 8, NE]); br = sb(L, "br", [1, NE])
                    p.dma("pool", wbn[:], wview(w_bna_d), w=[wbn.r()], sem="w")
                    p.dma("pool", wbm[:], wview(w_bml_d), w=[wbm.r()], sem="w")
                    p.dma("pool", wout[:], wview(w_out_d), w=[wout.r()], sem="w")
                    p.dma("sp", wr[:], wview(w_r_d), w=[wr.r()], sem="x")
                    p.dma("sp", br[:], b_r_d, w=[br.r()], sem="x")
                    wgn = [sb(L, "wgn%d" % i, [128, 8, 128], BF16) for i in range(2)]
                    wgm = [sb(L, "wgm%d" % i, [128, 8, 128], BF16) for i in range(2)]
                    mT = sb(L, "mT", [128, 8, S], BF16, nres=32)
                    sg = [sb(L, "sg%d" % i, [128, 512]) for i in range(2)]
                    m1 = [sb(L, "m1%d" % i, [128, 512]) for i in range(2)]
                    xt = [sb(L, "xd", [128, D])] * 2
                    yt = [sb(L, "yd%d" % i, [128, D]) for i in range(2)]
                    jk = sb(L, "jk", [128, D], BF16)
                    h2 = [sb(L, "h2", [128, D])] * 2
                    h2hi = [sb(L, "h2hi%d" % i, [128, D], BF16) for i in range(2)]
                    h2lo = [sb(L, "h2lo", [128, D], BF16)] * 2
                    h2Tlo = [sb(L, "h2Tlo%d" % i, [128, 8, 128], BF16) for i in range(2)]
                    wrhi = sb(L, "wrhi", [128, 8, NE], BF16); wrlo = sb(L, "wrlo", [128, 8, NE], BF16)
                    p.op("dve", lambda e: e.tensor_copy(out=wrhi[:], in_=wr[:]), r=[wr.r()], w=[wrhi.r()])
                    p.op("dve", lambda e: e.tensor_tensor(out=wrlo[:], in0=wr[:], in1=wrhi[:], op=ALU.subtract),
                         r=[wr.r(), wrhi.r()], w=[wrlo.r()])
                    st = [sb(L, "std%d" % i, [128, 4]) for i in range(2)]
                    lg = [sb(L, "lg%d" % i, [128, 3, NE]) for i in range(2)]
                    t8 = [sb(L, "t8%d" % i, [128, 20]) for i in range(2)]
                    mkb = [sb(L, "mkb%d" % i, [128, NE], BF16) for i in range(2)]
                    oh4 = [sb(L, "oh4%d" % i, [128, 4, NE]) for i in range(2)]
                    n = 0
                    for dc in range(8):
                        n += 1
                        a_, b_ = wgn[n % 2], wgm[n % 2]
                        p.dma("pool", a_[:], wview(w_in_d[:, C_GNA + dc * 128:C_GNA + (dc + 1) * 128]), w=[a_.r()], sem="w")
                        p.dma("pool", b_[:], wview(w_in_d[:, C_GML + dc * 128:C_GML + (dc + 1) * 128]), w=[b_.r()], sem="w")
                        for tb in range(4):
                            tok0 = 256 + tb * 512
                            pa, pb = nPD(), nPD()
                            mm_fm(pa[:, 0:512], pa.r(), a_, 0, tok0, 512)
                            mm_fm(pa[:, 512:1024], pa.r(), wbn, dc * 128, tb * 512, 512, nk=4, src=onaT)
                            mm_fm(pb[:, 0:512], pb.r(), b_, 0, tok0, 512)
                            mm_fm(pb[:, 512:1024], pb.r(), wbm, dc * 128, tb * 512, 512, nk=4, src=omlT)
                            p.op("act", lambda e, pa=pa: e.activation(out=sg[0][:], in_=pa[:, 0:512], func=AF.Sigmoid),
                                 r=[pa.r()], w=[sg[0].r()])
                            p.op("act", lambda e, pb=pb: e.activation(out=sg[1][:], in_=pb[:, 0:512], func=AF.Sigmoid),
                                 r=[pb.r()], w=[sg[1].r()])
                            p.op("dve", lambda e, pa=pa: e.tensor_tensor(out=m1[0][:], in0=pa[:, 512:1024], in1=sg[0][:], op=ALU.mult),
                                 r=[pa.r(), sg[0].r()], w=[m1[0].r()])
                            p.op("dve", lambda e, pb=pb: e.tensor_tensor(out=m1[1][:], in0=pb[:, 512:1024], in1=sg[1][:], op=ALU.mult),
                                 r=[pb.r(), sg[1].r()], w=[m1[1].r()])
                            p.op(POOLENG, lambda e, dc=dc, tb=tb: e.tensor_tensor(
                                out=mT[:, dc, tb * 512:(tb + 1) * 512], in0=m1[0][:], in1=m1[1][:], op=ALU.add),
                                r=[m1[0].r(), m1[1].r()], w=[mT.r(dc * 4 + tb)])
                    for tb in range(4):
                        for ti in range(4):
                            t = tb * 4 + ti
                            i = t % 2
                            py = PD[2]
                            for half in range(2):
                                for kc in range(8):
                                    p.op("pe", lambda e, kc=kc, half=half, t=t, py=py: e.matmul(
                                        py[:, half * 512:(half + 1) * 512], lhsT=mT[:, kc, t * 128:(t + 1) * 128],
                                        rhs=wout[:, kc, half * 512:(half + 1) * 512], start=(kc == 0), stop=(kc == 7)),
                                        r=[mT.r(kc * 4 + tb), wout.r()], w=[py.r()])
                            p.dma("sp", xt[i][:], x_d[b, t * 128:(t + 1) * 128, :], w=[xt[i].r()], sem="x")
                            rstd_of(py[:], [py.r()], jk, st[i])
                            p.op("dve", lambda e, i=i, py=py: e.scalar_tensor_tensor(
                                out=yt[i][:], in0=py[:], scalar=st[i][:, 1:2], in1=G_m[:], op0=ALU.mult, op1=ALU.mult),
                                r=[py.r(), st[i].r(), G_m.r()], w=[yt[i].r()])
                            p.op(POOLENG, lambda e, i=i: e.tensor_tensor(out=yt[i][:], in0=yt[i][:], in1=xt[i][:], op=ALU.add),
                                 r=[yt[i].r(), xt[i].r()], w=[yt[i].r()])
                            p.dma("sp", out_d[b, t * 128:(t + 1) * 128, :], yt[i][:], r=[yt[i].r()], w=[x1res[b][t]], sem="o")
                            if b == 0 and "x1" in dbg_d:
                                dump("x1", yt[i][:], yt[i].r(), dst=dbg_d["x1"][t])
                            rstd_of(yt[i][:], [yt[i].r()], jk, st[i])
                            p.op("dve", lambda e, i=i: e.scalar_tensor_tensor(
                                out=h2[i][:], in0=yt[i][:], scalar=st[i][:, 1:2], in1=A_f[:], op0=ALU.mult, op1=ALU.mult),
                                r=[yt[i].r(), st[i].r(), A_f.r()], w=[h2[i].r()])
                            p.op(POOLENG, lambda e, i=i: e.tensor_tensor(out=h2[i][:], in0=h2[i][:], in1=sh_f[:], op=ALU.add),
                                 r=[h2[i].r(), sh_f.r()], w=[h2[i].r()])
                            p.op("act", lambda e, i=i: e.copy(out=h2hi[i][:], in_=h2[i][:]), r=[h2[i].r()], w=[h2hi[i].r()])
                            p.op("dve", lambda e, i=i: e.tensor_tensor(out=h2lo[i][:], in0=h2[i][:], in1=h2hi[i][:], op=ALU.subtract),
                                 r=[h2[i].r(), h2hi[i].r()], w=[h2lo[i].r()])
                            pa_ = PS[i]
                            pab = pa_[:].bitcast(BF16)
                            for kc in range(8):
                                p.op("pe", lambda e, kc=kc, i=i, pab=pab: e.transpose(
                                    pab[:, kc * 128:(kc + 1) * 128], h2hi[i][:, kc * 128:(kc + 1) * 128], identb[:]),
                                    r=[h2hi[i].r(), identb.r()], w=[pa_.r()])
                            p.op("act", lambda e, t=t, pab=pab: e.copy(
                                out=hT[:, :, (2 + t) * 128:(3 + t) * 128], in_=pab.rearrange("p (k n) -> p k n", k=8)),
                                r=[pa_.r()], w=[hT.r(2 + t)])
                            pb_ = PD[i]
                            pbb = pb_[:].bitcast(BF16)
                            for kc in range(8):
                                p.op("pe", lambda e, kc=kc, i=i, pbb=pbb: e.transpose(
                                    pbb[:, kc * 128:(kc + 1) * 128], h2lo[i][:, kc * 128:(kc + 1) * 128], identb[:]),
                                    r=[h2lo[i].r(), identb.r()], w=[pb_.r()])
                            p.op("dve", lambda e, i=i, pbb=pbb: e.tensor_copy(
                                out=h2Tlo[i][:], in_=pbb[:, 0:1024].rearrange("p (k n) -> p k n", k=8)),
                                r=[pb_.r()], w=[h2Tlo[i].r()])
                            pl = PS[1 - i]
                            nmm = 0
                            for kc in range(8):
                                for (lh_, lres, w_) in ((hT[:, kc, (2 + t) * 128:(3 + t) * 128], hT.r(2 + t), wrhi),
                                                        (h2Tlo[i][:, kc, :], h2Tlo[i].r(), wrhi),
                                                        (hT[:, kc, (2 + t) * 128:(3 + t) * 128], hT.r(2 + t), wrlo)):
                                    nmm += 1
                                    p.op("pe", lambda e, kc=kc, lh_=lh_, w_=w_, pl=pl, nmm=nmm: e.matmul(
                                        pl[:, 0:NE], lhsT=lh_, rhs=w_[:, kc, :], start=(nmm == 1), stop=(nmm == 24)),
                                        r=[lres, w_.r()], w=[pl.r()])
                            p.op("pe", lambda e, pl=pl: e.matmul(pl[:, 32:64], lhsT=ones32[0:1, :], rhs=br[0:1, :], start=True, stop=True),
                                 r=[ones32.r(), br.r()], w=[pl.r()])
                            L_, T_ = lg[i], t8[i]
                            p.op("act", lambda e, L_=L_, pl=pl: e.copy(out=L_[:, 0, :], in_=pl[:, 0:NE]), r=[pl.r()], w=[L_.r()])
                            p.op("dve", lambda e, L_=L_, pl=pl: e.tensor_tensor(out=L_[:, 0, :], in0=L_[:, 0, :], in1=pl[:, 32:64], op=ALU.add),
                                 r=[pl.r(), L_.r()], w=[L_.r()])
                            if b == 0 and "logits" in dbg_d:
                                dump("logits", L_[:, 0, :], L_.r(), dst=dbg_d["logits"][t])
                            p.op("dve", lambda e, L_=L_, T_=T_: e.max(out=T_[:, 0:8], in_=L_[:, 0, :]), r=[L_.r()], w=[T_.r()])
                            gt = b * NT + t
                            mk_, oh_ = mkb[i], oh4[i]
                            p.op("dve", lambda e, L_=L_, T_=T_: e.tensor_scalar(
                                out=L_[:, 1, :], in0=L_[:, 0, :], scalar1=T_[:, 3:4], scalar2=None, op0=ALU.is_ge),
                                r=[L_.r(), T_.r()], w=[L_.r()])
                            p.op("dve", lambda e, L_=L_, mk_=mk_: e.tensor_copy(out=mk_[:], in_=L_[:, 1, :]), r=[L_.r()], w=[mk_.r()])
                            p.op("pe", lambda e, pl=pl, mk_=mk_: e.matmul(pl[:, 64:96], lhsT=trisb[:], rhs=mk_[:], start=True, stop=False),
                                 r=[trisb.r(), mk_.r()], w=[pl.r()])
                            p.op("pe", lambda e, pl=pl: e.matmul(pl[:, 64:96], lhsT=onesb[:], rhs=msum[:], start=False, stop=True),
                                 r=[onesb.r(), msum.r()], w=[pl.r()])
                            p.op("dve", lambda e, L_=L_, pl=pl: e.scalar_tensor_tensor(
                                out=L_[:, 2, :], in0=pl[:, 64:96], scalar=float(CAP - 1), in1=ecap[:], op0=ALU.min, op1=ALU.add),
                                r=[pl.r(), ecap.r()], w=[L_.r()])
                            p.op("dve", lambda e, mk_=mk_: e.tensor_tensor(out=msum[:], in0=msum[:], in1=mk_[:], op=ALU.add),
                                 r=[msum.r(), mk_.r()], w=[msum.r()])
                            for k in range(4):
                                p.op("dve", lambda e, k=k, L_=L_, T_=T_, oh_=oh_: e.tensor_scalar(
                                    out=oh_[:, k, :], in0=L_[:, 0, :], scalar1=T_[:, k:k + 1], scalar2=None, op0=ALU.is_equal),
                                    r=[L_.r(), T_.r()], w=[oh_.r()])
                                p.op("dve", lambda e, k=k, L_=L_, oh_=oh_: e.tensor_tensor(
                                    out=oh_[:, k, :], in0=oh_[:, k, :], in1=L_[:, 2, :], op=ALU.mult),
                                    r=[oh_.r(), L_.r()], w=[oh_.r()])
                            p.op("dve", lambda e, T_=T_, oh_=oh_: e.reduce_sum(out=T_[:, 12:16], in_=oh_[:], axis=AX.X),
                                 r=[oh_.r()], w=[T_.r()])
                            p.op("dve", lambda e, T_=T_, gt=gt: e.tensor_copy(out=RIi[:, gt, :], in_=T_[:, 12:16]),
                                 r=[T_.r()], w=[RIi.r(gt)])
                            p.op("dve", lambda e, T_=T_: e.tensor_scalar_mul(out=T_[:, 8:9], in0=T_[:, 0:1], scalar1=-1.0),
                                 r=[T_.r()], w=[T_.r()])
                            p.op("act", lambda e, T_=T_: e.activation(out=T_[:, 16:20], in_=T_[:, 0:4], func=AF.Exp,
                                                                     bias=T_[:, 8:9], scale=1.0), r=[T_.r()], w=[T_.r()])
                            p.op("dve", lambda e, T_=T_: e.reduce_sum(out=T_[:, 9:10], in_=T_[:, 16:20], axis=AX.X),
                                 r=[T_.r()], w=[T_.r()])
                            p.op("dve", lambda e, T_=T_: e.reciprocal(out=T_[:, 10:11], in_=T_[:, 9:10]), r=[T_.r()], w=[T_.r()])
                            p.op("dve", lambda e, T_=T_, gt=gt: e.tensor_scalar_mul(
                                out=RIw[:, gt, :], in0=T_[:, 16:20], scalar1=T_[:, 10:11]), r=[T_.r()], w=[RIw.r(gt)])
                            for k in range(4):
                                p.idma(out=xe_d, out_offset=bass.IndirectOffsetOnAxis(ap=RIi[:, gt, k:k + 1], axis=0),
                                       in_=h2hi[i][:], in_offset=None, bounds=NE * CAP - 1,
                                       r=[h2hi[i].r(), RIi.r(gt)], w=[], sem="sc")
                p.barrier()
            if stop is not None:
                break

            p.barrier()

        if stop is None:
            p.barrier()
            with ExitStack() as L:
                wgb = [sb(L, "wgb%d" % i, [128, 8, D], BF16) for i in range(2)]
                wlb = [sb(L, "wlb%d" % i, [128, 8, D], BF16) for i in range(2)]
                wdb = [sb(L, "wdb%d" % i, [128, 8, D], BF16) for i in range(2)]
                bdb = [sb(L, "bdb%d" % i, [128, D]) for i in range(2)]
                bg = sb(L, "bg", [128, 8, NE]); bl = sb(L, "bl", [128, 8, NE])
                p.dma("sp", bg[:], bgT_d, w=[bg.r()], sem="x")
                p.dma("sp", bl[:], blT_d, w=[bl.r()], sem="x")
                xr = [sb(L, "xr%d" % i, [128, 4, D], BF16) for i in range(2)]
                xT = [sb(L, "xT%d" % i, [128, 8, 512], BF16) for i in range(2)]
                aT2 = [sb(L, "aT%d" % i, [128, 8, 512], BF16, nres=8) for i in range(2)]
                gg = [sb(L, "gg%d" % i, [128, 512]) for i in range(2)]
                sg = [sb(L, "sgE%d" % i, [128, 512]) for i in range(2)]
                l1 = [sb(L, "l1%d" % i, [128, 512]) for i in range(2)]
                t1 = [sb(L, "t1%d" % i, [128, 512]) for i in range(2)]
                ysb = [sb(L, "ysb%d" % i, [128, D]) for i in range(2)]

                class BV:
                    def __init__(self, parent, c0):
                        self.par, self.c0, self.res = parent, c0, Res()

                    def r(self):
                        return self.res

                    def ap(self):
                        return self.par[:, self.c0:self.c0 + 512]

                banks = [BV(PD[2], 0), BV(PD[2], 512), BV(PS[0], 0), BV(PS[1], 0)]
                bki = [0]

                def nbank():
                    bki[0] += 1
                    return banks[bki[0] % 4]

                NBLK = CAP // 512
                NTOT = NE * NBLK
                ycnt = [0]

                def load_w(e_):
                    p.dma("pool", wgb[e_ % 2][:], wview(w_gate_d[e_]), w=[wgb[e_ % 2].r()], sem="w")
                    p.dma("pool", wlb[e_ % 2][:], wview(w_lin_d[e_]), w=[wlb[e_ % 2].r()], sem="w")
                    p.dma("pool", wdb[e_ % 2][:], wview(w_down_d[e_]), w=[wdb[e_ % 2].r()], sem="w")
                    p.dma("sp", bdb[e_ % 2][:], b_down_d[e_:e_ + 1, :].partition_broadcast(128), w=[bdb[e_ % 2].r()], sem="x")

                def emit_T(n):
                    e_, blk = divmod(n, NBLK)
                    xr_, xT_ = xr[n % 2], xT[n % 2]
                    row0 = e_ * CAP + blk * 512
                    p.dma("sp", xr_[:], xe_d[row0:row0 + 512, :].rearrange("(j p) d -> p j d", p=128), w=[xr_.r()], sem="x")
                    for j in range(4):
                        bk = nbank()
                        psb = bk.ap().bitcast(BF16)
                        for kc in range(8):
                            p.op("pe", lambda e, kc=kc, j=j, psb=psb, xr_=xr_: e.transpose(
                                psb[:, kc * 128:(kc + 1) * 128], xr_[:, j, kc * 128:(kc + 1) * 128], identb[:]),
                                r=[xr_.r(), identb.r()], w=[bk.r()])
                        eng = "act" if j % 2 else "dve"
                        if eng == "act":
                            p.op("act", lambda e, j=j, psb=psb, xT_=xT_: e.copy(
                                out=xT_[:, :, j * 128:(j + 1) * 128], in_=psb.rearrange("p (k n) -> p k n", k=8)),
                                r=[bk.r()], w=[xT_.r()])
                        else:
                            p.op("dve", lambda e, j=j, psb=psb, xT_=xT_: e.tensor_copy(
                                out=xT_[:, :, j * 128:(j + 1) * 128], in_=psb.rearrange("p (k n) -> p k n", k=8)),
                                r=[bk.r()], w=[xT_.r()])

                def emit_GL(n, fc):
                    e_ = n // NBLK
                    wg_, wl_ = wgb[e_ % 2], wlb[e_ % 2]
                    xT_, aT = xT[n % 2], aT2[n % 2]
                    j = fc % 2
                    pg = PD[j]
                    for (w_, c0) in ((wg_, 0), (wl_, 512)):
                        for kc in range(8):
                            p.op("pe", lambda e, kc=kc, w_=w_, c0=c0, pg=pg: e.matmul(
                                pg[:, c0:c0 + 512], lhsT=w_[:, kc, fc * 128:(fc + 1) * 128], rhs=xT_[:, kc, :],
                                start=(kc == 0), stop=(kc == 7)), r=[w_.r(), xT_.r()], w=[pg.r()])
                    p.op("dve", lambda e: e.tensor_scalar(
                        out=gg[j][:], in0=pg[:, 0:512], scalar1=bg[:, fc, e_:e_ + 1], scalar2=7.0, op0=ALU.add, op1=ALU.min),
                        r=[pg.r(), bg.r()], w=[gg[j].r()])
                    p.op("act", lambda e: e.activation(out=sg[j][:], in_=gg[j][:], func=AF.Sigmoid, scale=1.702),
                         r=[gg[j].r()], w=[sg[j].r()])
                    p.op("dve", lambda e: e.tensor_scalar(
                        out=l1[j][:], in0=pg[:, 512:1024], scalar1=bl[:, fc, e_:e_ + 1], scalar2=7.0, op0=ALU.add, op1=ALU.min),
                        r=[pg.r(), bl.r()], w=[l1[j].r()])
                    p.op("dve", lambda e: e.tensor_scalar(
                        out=l1[j][:], in0=l1[j][:], scalar1=-7.0, scalar2=1.0, op0=ALU.max, op1=ALU.add),
                        r=[l1[j].r()], w=[l1[j].r()])
                    p.op(POOLENG, lambda e: e.tensor_tensor(out=t1[j][:], in0=gg[j][:], in1=sg[j][:], op=ALU.mult),
                         r=[gg[j].r(), sg[j].r()], w=[t1[j].r()])
                    p.op("dve", lambda e: e.tensor_tensor(out=aT[:, fc, :], in0=t1[j][:], in1=l1[j][:], op=ALU.mult),
                         r=[t1[j].r(), l1[j].r()], w=[aT.r(fc)])

                def emit_DOWN(n):
                    e_, blk = divmod(n, NBLK)
                    wd_, bd_, aT = wdb[e_ % 2], bdb[e_ % 2], aT2[n % 2]
                    row0 = e_ * CAP + blk * 512
                    for ti in range(4):
                        ycnt[0] += 1
                        ys_ = ysb[ycnt[0] % 2]
                        for half in range(2):
                            bk = nbank()
                            for fc in range(8):
                                p.op("pe", lambda e, fc=fc, half=half, ti=ti, bk=bk: e.matmul(
                                    bk.ap(), lhsT=aT[:, fc, ti * 128:(ti + 1) * 128],
                                    rhs=wd_[:, fc, half * 512:(half + 1) * 512], start=(fc == 0), stop=(fc == 7)),
                                    r=[aT.r(fc), wd_.r()], w=[bk.r()])
                            p.op("dve", lambda e, half=half, bk=bk, ys_=ys_: e.tensor_tensor(
                                out=ys_[:, half * 512:(half + 1) * 512], in0=bk.ap(), in1=bd_[:, half * 512:(half + 1) * 512], op=ALU.add),
                                r=[bk.r(), bd_.r()], w=[ys_.r()])
                        p.dma("sp", ye_d[row0 + ti * 128:row0 + (ti + 1) * 128, :], ys_[:], r=[ys_.r()], w=[], sem="o")

                for n in range(NTOT):
                    if n % NBLK == 0:
                        load_w(n // NBLK)
                    emit_T(n)
                    emit_GL(n, 0)
                    emit_GL(n, 1)
                    if n > 0:
                        emit_DOWN(n - 1)
                    for fc in range(2, 8):
                        emit_GL(n, fc)
                emit_DOWN(NTOT - 1)
            p.barrier()
            with ExitStack() as L:
                yk = [[sb(L, "yk%d_%d" % (i, k), [128, D]) for k in range(4)] for i in range(2)]
                acc = [sb(L, "acc%d" % i, [128, D]) for i in range(2)]
                xe1 = [sb(L, "xe1%d" % i, [128, D]) for i in range(2)]
                jk = sb(L, "jkF", [128, D], BF16)
                st = [sb(L, "stF%d" % i, [128, 4]) for i in range(2)]
                Gf = sb(L, "GfF", [128, D])
                for gt in range(nb * NT):
                    b, t = divmod(gt, NT)
                    i = gt % 2
                    if t == 0:
                        p.dma("sp", Gf[:], gf_d[b], r=[gfres[b]], w=[Gf.r()], sem="x")
                    for k in range(4):
                        p.idma(out=yk[i][k][:], out_offset=None, in_=ye_d,
                               in_offset=bass.IndirectOffsetOnAxis(ap=RIi[:, gt, k:k + 1], axis=0), bounds=NE * CAP - 1,
                               r=[RIi.r(gt)], w=[yk[i][k].r()], sem="ga")
                    p.dma("sp", xe1[i][:], out_d[b, t * 128:(t + 1) * 128, :], r=[x1res[b][t]], w=[xe1[i].r()], sem="x")
                    a_ = acc[i]
                    p.op("dve", lambda e, a_=a_, i=i, gt=gt: e.tensor_scalar_mul(out=a_[:], in0=yk[i][0][:], scalar1=RIw[:, gt, 0:1]),
                         r=[yk[i][0].r(), RIw.r(gt)], w=[a_.r()])
                    for k in range(1, 4):
                        p.op("dve", lambda e, a_=a_, i=i, gt=gt, k=k: e.scalar_tensor_tensor(
                            out=a_[:], in0=yk[i][k][:], scalar=RIw[:, gt, k:k + 1], in1=a_[:], op0=ALU.mult, op1=ALU.add),
                            r=[yk[i][k].r(), RIw.r(gt), a_.r()], w=[a_.r()])
                    if b == 0 and "ffn" in dbg_d:
                        dump("ffn", a_[:], a_.r(), dst=dbg_d["ffn"][t])
                    rstd_of(a_[:], [a_.r()], jk, st[i])
                    p.op("dve", lambda e, a_=a_, i=i: e.scalar_tensor_tensor(
                        out=a_[:], in0=a_[:], scalar=st[i][:, 1:2], in1=Gf[:], op0=ALU.mult, op1=ALU.mult),
                        r=[a_.r(), st[i].r(), Gf.r()], w=[a_.r()])
                    p.op("dve", lambda e, a_=a_, i=i: e.tensor_tensor(out=xe1[i][:], in0=xe1[i][:], in1=a_[:], op=ALU.add),
                         r=[xe1[i].r(), a_.r()], w=[xe1[i].r()])
                    p.dma("sp", out_d[b, t * 128:(t + 1) * 128, :], xe1[i][:], r=[xe1[i].r()], w=[x1res[b][t]], sem="o")
            p.barrier()

        p.barrier()
    return nc, p


def _host_consts():
    c = {}
    c["ident"] = np.eye(128, dtype=np.float32)
    j = np.arange(128)
    c["trif"] = (j[:, None] <= j[None, :]).astype(np.float32)
    c["trib"] = (j[:, None] >= j[None, :]).astype(np.float32)
    c["tris"] = (j[:, None] < j[None, :]).astype(np.float32)
    c["ecap"] = np.ascontiguousarray(np.broadcast_to((np.arange(NE) * CAP).astype(np.float32)[None, :], (128, NE)))
    t = np.arange(S)
    row = (t // 64).astype(np.float32)
    col = (t % 64).astype(np.float32)
    inv_freq = (np.float32(10000.0) ** (-np.arange(16, dtype=np.float32) / np.float32(16))).astype(np.float32)
    cos = np.zeros((128, S), np.float32)
    sin = np.zeros((128, S), np.float32)
    for pp in range(128):
        d = pp % 64
        pos = row if d < 32 else col
        dd = d % 32
        f = dd % 16
        sign = -1.0 if dd < 16 else 1.0
        ang = (pos * inv_freq[f]).astype(np.float32)
        cos[pp] = np.cos(ang)
        sin[pp] = sign * np.sin(ang)
    c["ropecos"] = cos
    c["ropesin"] = sin
    return c


def _na_bias_table(rpb):
    reps = [0, 1, 5, 14, 15]
    kk = np.arange(128)
    krl = kk // 64
    kc = kk % 64
    qq = np.arange(128)
    qrl = qq // 64
    qc = qq % 64
    cs = np.clip(qc - 8, 0, 48)
    tab = np.full((8, 128, 5, 5, 128), NEG, np.float32)
    for ci, i in enumerate(reps):
        js = int(np.clip(i - 2, 0, 11))
        for s in range(5):
            j = js + s
            kr = 2 * j + krl
            r = 2 * i + qrl
            rs = np.clip(r - 4, 0, 24)
            vr = (kr[:, None] >= rs[None, :]) & (kr[:, None] < rs[None, :] + 8)
            vc = (kc[:, None] >= cs[None, :]) & (kc[:, None] < cs[None, :] + 16)
            valid = vr & vc
            dr = np.clip(kr[:, None] - r[None, :] + 7, 0, 14)
            dc = np.clip(kc[:, None] - qc[None, :] + 15, 0, 30)
            vals = rpb[:, dr, dc]
            tab[:, :, ci, s, :] = np.where(valid[None], vals, np.float32(NEG))
    return np.ascontiguousarray(tab.reshape(8, 128, 5 * 640))


def _prep_inputs(inp, nb=NB, ncores=8):
    f = lambda a: np.ascontiguousarray(np.asarray(a, dtype=np.float32))
    consts = _host_consts()
    w_in = f(inp["w_in"][0])
    d = np.arange(64)
    partner = np.where((d % 32) < 16, d + 16, d - 16)
    permq = np.concatenate([C_MLQ + h * 64 + partner for h in range(4)])
    permk = np.concatenate([C_MLK + h * 64 + partner for h in range(4)])
    w_qkp = np.ascontiguousarray(np.concatenate([w_in[:, permq], w_in[:, permk]], axis=1))
    shared = dict(
        w_ada=f(inp["w_ada"][0]), b_ada=f(inp["b_ada"][0]).reshape(1, -1),
        g4=np.ascontiguousarray(np.stack([f(inp["g_mix_pre"][0]), f(inp["g_mix_post"][0]),
                                          f(inp["g_ffn_pre"][0]), f(inp["g_ffn_post"][0])])),
        w_in=w_in, w_qkp=w_qkp, b_mg=f(inp["b_mlstm_gates"][0]).reshape(1, 16),
        nab=_na_bias_table(f(inp["rpb"][0])), g_head=f(inp["g_mlstm_head"][0]).reshape(1, 512),
        w_bna=f(inp["w_branch_na"][0]), w_bml=f(inp["w_branch_ml"][0]), w_out=f(inp["w_out"][0]),
        w_router=f(inp["w_router"][0]), b_router=f(inp["b_router"][0]).reshape(1, NE),
        w_gate=f(inp["w_gate"][0]), w_lin=f(inp["w_lin"][0]), w_down=f(inp["w_down"][0]),
        bgT=np.ascontiguousarray(f(inp["b_gate"][0]).reshape(NE, 8, 128).transpose(2, 1, 0)),
        blT=np.ascontiguousarray(f(inp["b_lin"][0]).reshape(NE, 8, 128).transpose(2, 1, 0)),
        b_down=f(inp["b_down"][0]), **consts)
    x = f(inp["x"]); ctx = f(inp["ctx"]); c = f(inp["c"]); cc = f(inp["c_ctx"])
    maps = []
    for k in range(ncores):
        sl = slice(k * nb, (k + 1) * nb)
        c5 = np.zeros((5, D), np.float32)
        c5[:nb] = c[sl]
        c5[4] = cc
        cT = np.ascontiguousarray(c5.reshape(5, 8, 128).transpose(2, 1, 0))
        m = dict(shared)
        m.update(x=np.ascontiguousarray(x[sl]), ctx=np.ascontiguousarray(ctx[sl]), cT=cT)
        maps.append(m)
    return maps


def kernel(**inputs):
    maps = _prep_inputs(inputs)
    nc, _ = build()
    res = run_bass_kernel_spmd(nc, maps, core_ids=list(range(8)))
    return np.concatenate([r["out"] for r in res.results], axis=0).astype(np.float32)
```

```python
import numpy as np
from contextlib import ExitStack
import concourse.bass as bass
import concourse.mybir as mybir
from concourse.bass_utils import run_bass_kernel_spmd

F32 = mybir.dt.float32
BF16 = mybir.dt.bfloat16
AF = mybir.ActivationFunctionType
ALU = mybir.AluOpType
AX = mybir.AxisListType

D = 1024
S = 2048
CTX = 256
NB = 4
NT = 16
NTT = 18
NE = 32
EPS = 1e-6
NEG = -80.0
CAP = 2048
I32 = mybir.dt.int32
POOLENG = "dve"
N_IN = 5136
C_NAK, C_NAV, C_MLK, C_MLV, C_MLG = 0, 512, 1024, 1280, 1792
C_NAQ, C_MLQ, C_MLO, C_GNA, C_GML = 1808, 2320, 2576, 3088, 4112


class Res:
    __slots__ = ("lw", "rd")

    def __init__(self):
        self.lw = None
        self.rd = {}


class Prog:
    ENG = ["pe", "act", "dve", "pool", "sp"]

    def __init__(self, nc):
        self.nc = nc
        self.e = dict(pe=nc.tensor, act=nc.scalar, dve=nc.vector, pool=nc.gpsimd, sp=nc.sync)
        self.sem = {k: nc.alloc_semaphore("s_" + k) for k in self.ENG}
        self.cnt = {k: 0 for k in self.ENG}
        self.waited = {k: {} for k in self.ENG}
        self.n_ins = 0

    NDS = 8
    rr = None

    def dsem(self, name):
        if self.rr is None:
            self.rr = {}
        i = self.rr.get(name, 0)
        self.rr[name] = i + 1
        k = "d:%s:%d" % (name, i % self.NDS)
        if k not in self.sem:
            self.sem[k] = self.nc.alloc_semaphore("sd_%s_%d" % (name, i % self.NDS))
            self.cnt[k] = 0
        return k

    def _wait(self, eng, key, val):
        if self.waited[eng].get(key, 0) >= val:
            return
        self.e[eng].wait_ge(self.sem[key], val)
        self.waited[eng][key] = val
        self.n_ins += 1

    def _sync(self, eng, me, r, w):
        for x in r:
            if x.lw is not None:
                k, v = x.lw
                if k == me and me == "pe":
                    continue
                self._wait(eng, k, v)
        for x in w:
            if x.lw is not None:
                k, v = x.lw
                if k != me or me != "pe":
                    self._wait(eng, k, v)
            for k, v in x.rd.items():
                if k != me or me != "pe":
                    self._wait(eng, k, v)

    def _commit(self, me, val, r, w):
        for x in r:
            if x.rd.get(me, 0) < val:
                x.rd[me] = val
        for x in w:
            x.lw = (me, val)
            x.rd = {}

    max_ops = None
    tot = 0

    def op(self, eng, fn, r=(), w=()):
        self.tot += 1
        if self.max_ops is not None and self.tot > self.max_ops:
            return None
        self._sync(eng, eng, r, w)
        ins = fn(self.e[eng])
        self.cnt[eng] += 1
        ins.then_inc(self.sem[eng], 1)
        self._commit(eng, self.cnt[eng], r, w)
        self.n_ins += 1
        return ins

    def dma(self, q, out, in_, r=(), w=(), sem="ld"):
        self.tot += 1
        if self.max_ops is not None and self.tot > self.max_ops:
            return None
        k = self.dsem(sem)
        if self.cnt[k] > 0:
            self._wait(q, k, self.cnt[k])
        self._sync(q, k, r, w)
        ins = self.e[q].dma_start(out=out, in_=in_)
        self.cnt[k] += 16
        ins.then_inc(self.sem[k], 16)
        self._commit(k, self.cnt[k], r, w)
        self.n_ins += 1
        return ins

    def idma(self, out, out_offset, in_, in_offset, bounds, r=(), w=(), sem="ind"):
        self.tot += 1
        if self.max_ops is not None and self.tot > self.max_ops:
            return None
        k = self.dsem(sem)
        if self.cnt[k] > 0:
            self._wait("pool", k, self.cnt[k])
        self._sync("pool", k, r, w)
        ins = self.e["pool"].indirect_dma_start(out=out, out_offset=out_offset, in_=in_, in_offset=in_offset)
        self.cnt[k] += 16
        ins.then_inc(self.sem[k], 16)
        self._commit(k, self.cnt[k], r, w)
        self.n_ins += 1
        return ins

    def barrier(self, engs=None):
        for eng in (engs or self.ENG):
            for k, v in self.cnt.items():
                if k != eng and v > 0:
                    self._wait(eng, k, v)


class T:
    def __init__(self, h, nres=1):
        self.h = h
        self.res = [Res() for _ in range(nres)]

    def __getitem__(self, k):
        return self.h[k]

    def r(self, i=0):
        return self.res[i]


def build(nb=NB, stop=None, dbg=(), max_ops=None):
    nc = bass.Bass("TRN2", target_bir_lowering=False)
    p = Prog(nc)
    p.max_ops = max_ops

    def din(name, shape, dt=F32):
        return nc.dram_tensor(name, list(shape), dt, kind="ExternalInput").ap()

    x_d = din("x", [nb, S, D])
    ctx_d = din("ctx", [nb, CTX, D])
    cT_d = din("cT", [128, 8, 5])
    w_ada_d = din("w_ada", [D, 6 * D])
    b_ada_d = din("b_ada", [1, 6 * D])
    g4_d = din("g4", [4, D])
    w_in_d = din("w_in", [D, N_IN])
    w_qkp_d = din("w_qkp", [D, 512])
    b_mg_d = din("b_mg", [1, 16])
    nab_d = din("nab", [8, 128, 5 * 640])
    g_head_d = din("g_head", [1, 512])
    w_bna_d = din("w_bna", [512, D])
    w_bml_d = din("w_bml", [512, D])
    w_out_d = din("w_out", [D, D])
    w_r_d = din("w_router", [D, NE])
    b_r_d = din("b_router", [1, NE])
    w_gate_d = din("w_gate", [NE, D, D])
    w_lin_d = din("w_lin", [NE, D, D])
    w_down_d = din("w_down", [NE, D, D])
    bgT_d = din("bgT", [128, 8, NE])
    blT_d = din("blT", [128, 8, NE])
    b_down_d = din("b_down", [NE, D])
    ident_d = din("ident", [128, 128])
    trif_d = din("trif", [128, 128])
    trib_d = din("trib", [128, 128])
    cos_d = din("ropecos", [128, S])
    sin_d = din("ropesin", [128, S])
    tris_d = din("tris", [128, 128])
    ecap_d = din("ecap", [128, NE])
    xe_d = nc.dram_tensor("xe_scr", [NE * CAP, D], BF16).ap()
    ye_d = nc.dram_tensor("ye_scr", [NE * CAP, D], F32).ap()
    gf_d = nc.dram_tensor("gf_scr", [nb, 128, D], F32).ap()
    out_d = nc.dram_tensor("out", [nb, S, D], F32, kind="ExternalOutput").ap()
    dbg_d = {}
    for name, shape in dbg:
        dbg_d[name] = nc.dram_tensor("dbg_" + name, list(shape), F32, kind="ExternalOutput").ap()

    uid = [0]

    def sb(es, name, shape, dt=F32, nres=1):
        uid[0] += 1
        return T(es.enter_context(nc.sbuf_tensor("sb%d_%s" % (uid[0], name), list(shape), dt)), nres)

    PD = [T(nc.alloc_psum_tensor("pd%d" % i, [128, 1024], F32)) for i in range(3)]
    PS = [T(nc.alloc_psum_tensor("psg%d" % i, [128, 512], F32)) for i in range(2)]

    def dump(name, tile_ap, res, dst=None):
        if name in dbg_d:
            p.dma("pool", dbg_d[name] if dst is None else dst, tile_ap, r=(res if isinstance(res, list) else [res]), sem="dbg")

    def wview(ap2d):
        return ap2d.rearrange("(kc p) n -> p kc n", p=128)

    def bc(ap, shape):
        return ap.to_broadcast(list(shape))

    rot = [0, 0]

    def nPD():
        rot[0] += 1
        return PD[rot[0] % 3]

    def nPS():
        rot[1] += 1
        return PS[rot[1] % 2]

    with ExitStack() as G:
        ident = sb(G, "ident", [128, 128])
        identb = sb(G, "identb", [128, 128], BF16)
        trif = sb(G, "trif", [128, 128])
        trib = sb(G, "trib", [128, 128])
        ones32 = sb(G, "ones32", [128, 128])
        scT = sb(G, "scT", [128, 8, 5])
        A_c = sb(G, "A_c", [128, D])
        sh_c = sb(G, "sh_c", [128, D])
        trisb = sb(G, "trisb", [128, 128], BF16)
        onesb = sb(G, "onesb", [128, 128], BF16)
        ecap = sb(G, "ecap", [128, NE])
        msum = sb(G, "msum", [128, NE], BF16)
        RIi = sb(G, "RIi", [128, nb * NT, 4], I32, nres=nb * NT)
        RIw = sb(G, "RIw", [128, nb * NT, 4], F32, nres=nb * NT)
        zres = Res()
        gfres = [Res() for _ in range(nb)]
        p.dma("sp", ecap[:], ecap_d, w=[ecap.r()])
        p.op("dve", lambda e: e.memset(onesb[:], 1.0), w=[onesb.r()])
        p.op("dve", lambda e: e.memset(msum[:], 0.0), w=[msum.r()])
        with ExitStack() as L:
            ztile = sb(L, "ztile", [128, 4096], BF16)
            tmp32 = sb(L, "tmp32", [128, 128])
            p.dma("sp", tmp32[:], tris_d, w=[tmp32.r()])
            p.op("dve", lambda e: e.tensor_copy(out=trisb[:], in_=tmp32[:]), r=[tmp32.r()], w=[trisb.r()])
            p.op("dve", lambda e: e.memset(ztile[:], 0.0), w=[ztile.r()])
            for cz in range(NE * CAP // 512):
                p.dma("sp", xe_d[cz * 512:(cz + 1) * 512, :].rearrange("(p j) d -> p (j d)", p=128), ztile[:],
                      r=[ztile.r()], w=[], sem="z")
            p.barrier()
        p.dma("sp", ident[:], ident_d, w=[ident.r()])
        p.dma("sp", trif[:], trif_d, w=[trif.r()])
        p.dma("sp", trib[:], trib_d, w=[trib.r()])
        p.dma("sp", scT[:], cT_d, w=[scT.r()])
        p.op("dve", lambda e: e.tensor_copy(out=identb[:], in_=ident[:]), r=[ident.r()], w=[identb.r()])
        p.op("dve", lambda e: e.memset(ones32[:], 1.0), w=[ones32.r()])
        p.op("act", lambda e: e.activation(out=scT[:], in_=scT[:], func=AF.Silu), r=[scT.r()], w=[scT.r()])

        def rstd_of(src_ap, src_res, junk, st):
            p.op("act", lambda e: e.activation(out=junk[:], in_=src_ap, func=AF.Square, scale=1.0 / 32.0,
                                               accum_out=st[:, 0:1]), r=src_res, w=[junk.r(), st.r()])
            p.op("act", lambda e: e.activation(out=st[:, 1:2], in_=st[:, 0:1], func=AF.Sqrt, bias=EPS),
                 r=[st.r()], w=[st.r()])
            p.op("dve", lambda e: e.reciprocal(out=st[:, 1:2], in_=st[:, 1:2]), r=[st.r()], w=[st.r()])

        def ada_mod(j, pieces, tag):
            with ExitStack() as L:
                lh = sb(L, "lh" + tag, [128, 8, 128], BF16)
                g4 = sb(L, "g4" + tag, [128, 4, D])
                p.dma("sp", g4[:], g4_d.partition_broadcast(128), w=[g4.r()])
                for kc in range(8):
                    p.op("dve", lambda e, kc=kc: e.tensor_scalar_mul(
                        out=lh[:, kc, :], in0=ones32[:], scalar1=scT[:, kc, j:j + 1]),
                        r=[scT.r(), ones32.r()], w=[lh.r()])
                wa = [sb(L, "wa%d%s" % (i, tag), [128, 8, 512], BF16) for i in range(2)]
                ba = [sb(L, "ba%d%s" % (i, tag), [1, 512]) for i in range(2)]
                n = 0
                for (blk, out_t, kind, gi) in pieces:
                    for half in range(2):
                        c0 = blk * D + half * 512
                        wt = wa[n % 2]
                        bt = ba[n % 2]
                        n += 1
                        p.dma("pool", wt[:], wview(w_ada_d[:, c0:c0 + 512]), w=[wt.r()], sem="w")
                        p.dma("sp", bt[:], b_ada_d[:, c0:c0 + 512], w=[bt.r()], sem="x")
                        ps = nPD()
                        for kc in range(8):
                            p.op("pe", lambda e, kc=kc, ps=ps, wt=wt: e.matmul(
                                ps[:, 0:512], lhsT=lh[:, kc, :], rhs=wt[:, kc, :], start=(kc == 0), stop=(kc == 7)),
                                r=[lh.r(), wt.r()], w=[ps.r()])
                        o = out_t[:, half * 512:(half + 1) * 512]
                        tmp = sb(L, "adatmp%d%s" % (n, tag), [128, 512])
                        ps2 = nPS()
                        p.op("pe", lambda e, ps2=ps2, bt=bt: e.matmul(
                            ps2[:], lhsT=ones32[0:1, :], rhs=bt[0:1, :], start=True, stop=True),
                            r=[ones32.r(), bt.r()], w=[ps2.r()])
                        p.op("act", lambda e, tmp=tmp, ps2=ps2: e.copy(out=tmp[:], in_=ps2[:]), r=[ps2.r()], w=[tmp.r()])
                        p.op("dve", lambda e, tmp=tmp, ps=ps: e.tensor_tensor(out=tmp[:], in0=ps[:, 0:512], in1=tmp[:], op=ALU.add),
                             r=[ps.r(), tmp.r()], w=[tmp.r()])
                        if kind == "shift":
                            p.op("act", lambda e, o=o, tmp=tmp: e.copy(out=o, in_=tmp[:]), r=[tmp.r()], w=[out_t.r()])
                        elif kind == "scale":
                            gs = g4[:, gi, half * 512:(half + 1) * 512]
                            p.op("dve", lambda e, o=o, tmp=tmp, gs=gs: e.scalar_tensor_tensor(
                                out=o, in0=tmp[:], scalar=1.0, in1=gs, op0=ALU.add, op1=ALU.mult),
                                r=[tmp.r(), g4.r()], w=[out_t.r()])
                        else:
                            gs = g4[:, gi, half * 512:(half + 1) * 512]
                            p.op("dve", lambda e, o=o, tmp=tmp, gs=gs: e.tensor_tensor(
                                out=o, in0=tmp[:], in1=gs, op=ALU.mult),
                                r=[tmp.r(), g4.r()], w=[out_t.r()])
            p.barrier()

        ada_mod(4, [(0, sh_c, "shift", 0), (1, A_c, "scale", 0)], "c")
        x1res = [[Res() for _ in range(NT)] for _ in range(nb)]

        for b in range(nb):
          with ExitStack() as B:
            hT = sb(B, "hT", [128, 8, NTT * 128], BF16, nres=NTT)
            with ExitStack() as M:
              G_m = sb(M, "G_m", [128, D]); A_f = sb(M, "A_f", [128, D]); sh_f = sb(M, "sh_f", [128, D])
              with ExitStack() as PA:
                A_m = sb(PA, "A_m", [128, D]); sh_m = sb(PA, "sh_m", [128, D]); G_f = sb(PA, "G_f", [128, D])
                ada_mod(b, [(0, sh_m, "shift", 0), (1, A_m, "scale", 0), (2, G_m, "gate", 1),
                            (3, sh_f, "shift", 0), (4, A_f, "scale", 2), (5, G_f, "gate", 3)], "b")
                p.dma("sp", gf_d[b], G_f[:], r=[G_f.r()], w=[gfres[b]], sem="o")
                if b == 0:
                    dump("A_m", A_m[:], A_m.r()); dump("sh_m", sh_m[:], sh_m.r()); dump("G_f", G_f[:], G_f.r())
                    dump("A_c", A_c[:], A_c.r())
                with ExitStack() as L:
                    xt = [sb(L, "xt%d" % i, [128, D]) for i in range(3)]
                    sq = [sb(L, "sq%d" % i, [128, D]) for i in range(3)]
                    hb = [sb(L, "hb%d" % i, [128, D], BF16) for i in range(3)]
                    st = [sb(L, "st%d" % i, [128, 2]) for i in range(3)]

                    def a_stage1(t):
                        i = t % 3
                        src = ctx_d[b, t * 128:(t + 1) * 128, :] if t < 2 else x_d[b, (t - 2) * 128:(t - 1) * 128, :]
                        Am, shm = (A_c, sh_c) if t < 2 else (A_m, sh_m)
                        p.dma("sp", xt[i][:], src, w=[xt[i].r()], sem="x")
                        rstd_of(xt[i][:], [xt[i].r()], sq[i], st[i])
                        p.op("dve", lambda e: e.scalar_tensor_tensor(
                            out=sq[i][:], in0=xt[i][:], scalar=st[i][:, 1:2], in1=Am[:], op0=ALU.mult, op1=ALU.mult),
                            r=[xt[i].r(), st[i].r(), Am.r()], w=[sq[i].r()])
                        p.op("dve", lambda e: e.tensor_tensor(out=hb[i][:], in0=sq[i][:], in1=shm[:], op=ALU.add),
                             r=[sq[i].r(), shm.r()], w=[hb[i].r()])

                    def a_stage2(t):
                        i = t % 3
                        ps = nPS()
                        psb = ps[:].bitcast(BF16)
                        for kc in range(8):
                            p.op("pe", lambda e, kc=kc: e.transpose(
                                psb[:, kc * 128:(kc + 1) * 128], hb[i][:, kc * 128:(kc + 1) * 128], identb[:]),
                                r=[hb[i].r(), identb.r()], w=[ps.r()])
                        p.op("act", lambda e: e.copy(
                            out=hT[:, :, t * 128:(t + 1) * 128], in_=psb.rearrange("p (k n) -> p k n", k=8)),
                            r=[ps.r()], w=[hT.r(t)])

                    for t in range(NTT + 1):
                        if t < NTT:
                            a_stage1(t)
                        if t >= 1:
                            a_stage2(t - 1)
                if b == 0 and "hT" in dbg_d:
                    dump("hT", hT[:], hT.res)
                p.barrier()
              if True:
                if stop == "A":
                    break
                onaT = sb(M, "onaT", [128, 4, S], BF16, nres=NT)

                def mm_fm(ps_ap, ps_res, w_t, wc0, tok0, ntok, nk=8, src=None):
                    src = src or hT
                    tiles = range(tok0 // 128, (tok0 + ntok) // 128)
                    for kc in range(nk):
                        p.op("pe", lambda e, kc=kc: e.matmul(
                            ps_ap, lhsT=w_t[:, kc, wc0:wc0 + 128], rhs=src[:, kc, tok0:tok0 + ntok],
                            start=(kc == 0), stop=(kc == nk - 1)),
                            r=[w_t.r()] + [src.r(t) for t in tiles], w=[ps_res])

                def mm_tm(ps_ap, ps_res, w_t, wc0, ncols, t):
                    for kc in range(8):
                        p.op("pe", lambda e, kc=kc: e.matmul(
                            ps_ap, lhsT=hT[:, kc, t * 128:(t + 1) * 128], rhs=w_t[:, kc, wc0:wc0 + ncols],
                            start=(kc == 0), stop=(kc == 7)),
                            r=[w_t.r(), hT.r(t)], w=[ps_res])

                with ExitStack() as L:
                  qT = sb(L, "qT", [128, 4, S], BF16, nres=4)
                  kT = sb(L, "kT", [128, 4, NTT * 128], BF16, nres=4)
                  vA = sb(L, "vA", [128, NTT, 8, 65], BF16, nres=NTT)
                  p.op("dve", lambda e: e.memset(vA[:, :, :, 64:65], 1.0), w=vA.res)
                  with ExitStack() as L2:
                    wna = sb(L2, "wna", [128, 8, 1536], BF16)
                    p.dma("pool", wna[:, :, 0:1024], wview(w_in_d[:, 0:1024]), w=[wna.r()], sem="w")
                    p.dma("pool", wna[:, :, 1024:1536], wview(w_in_d[:, C_NAQ:C_NAQ + 512]), w=[wna.r()], sem="w")
                    for c in range(4):
                        for tb in range(4):
                            ps = nPD()
                            mm_fm(ps[:, 0:512], ps.r(), wna, 1024 + c * 128, 256 + tb * 512, 512)
                            p.op("act", lambda e, c=c, tb=tb, ps=ps: e.activation(
                                out=qT[:, c, tb * 512:(tb + 1) * 512], in_=ps[:, 0:512], func=AF.Copy, scale=0.125),
                                r=[ps.r()], w=[qT.r(c)])
                        for (t0, n) in [(0, 512), (512, 512), (1024, 512), (1536, 512), (2048, 256)]:
                            ps = nPD()
                            mm_fm(ps[:, 0:n], ps.r(), wna, c * 128, t0, n)
                            p.op("dve", lambda e, c=c, t0=t0, n=n, ps=ps: e.tensor_copy(
                                out=kT[:, c, t0:t0 + n], in_=ps[:, 0:n]), r=[ps.r()], w=[kT.r(c)])
                    for t in range(NTT):
                        ps = nPD()
                        mm_tm(ps[:, 0:512], ps.r(), wna, 512, 512, t)
                        eng = "act" if t % 2 else "dve"
                        if eng == "act":
                            p.op("act", lambda e, t=t, ps=ps: e.copy(
                                out=vA[:, t, :, 0:64], in_=ps[:, 0:512].rearrange("p (h d) -> p h d", h=8)),
                                r=[ps.r()], w=[vA.r(t)])
                        else:
                            p.op("dve", lambda e, t=t, ps=ps: e.tensor_copy(
                                out=vA[:, t, :, 0:64], in_=ps[:, 0:512].rearrange("p (h d) -> p h d", h=8)),
                                r=[ps.r()], w=[vA.r(t)])
                  p.barrier()
                  if True:
                    EBh = [sb(L, "EB%d" % i, [128, 3200], BF16) for i in range(2)]
                    ona = sb(L, "ona", [128, NT, 512], BF16, nres=NT)
                    stg = sb(L, "nabst", [128, 3200])
                    PT = [sb(L, "PT%d" % i, [128, 896], BF16) for i in range(3)]
                    rc = [sb(L, "rc%d" % i, [128, 1]) for i in range(3)]
                    n = 0
                    items = [(h, i) for h in range(8) for i in range(NT)]
                    state = {}

                    def na_scores(n):
                        h, i = items[n]
                        hp = (h % 2) * 64
                        c = h // 2
                        EB = EBh[h % 2]
                        if i == 0:
                            p.dma("sp", stg[:], nab_d[h], w=[stg.r()], sem="x")
                            p.op("act", lambda e, EB=EB: e.activation(out=EB[:], in_=stg[:], func=AF.Exp),
                                 r=[stg.r()], w=[EB.r()])
                        js = min(max(i - 2, 0), 11)
                        cls = i - js if i < 2 or i > 13 else 2
                        pss = PD[n % 3]
                        pt = PT[n % 3]
                        qa = qT[hp:hp + 64, c, i * 128:(i + 1) * 128]
                        for s_i in range(7):
                            kt = (2 + js + s_i) if s_i < 5 else (s_i - 5)
                            p.op("pe", lambda e, s_i=s_i, kt=kt: e.matmul(
                                pss[:, s_i * 128:(s_i + 1) * 128], lhsT=kT[hp:hp + 64, c, kt * 128:(kt + 1) * 128],
                                rhs=qa, start=True, stop=True),
                                r=[kT.r(c), qT.r(c)], w=[pss.r()])
                        p.op("act", lambda e: e.activation(out=pt[:], in_=pss[:, 0:896], func=AF.Exp),
                             r=[pss.r()], w=[pt.r()])
                        p.op("dve", lambda e: e.tensor_tensor(
                            out=pt[:, 0:640], in0=pt[:, 0:640], in1=EB[:, cls * 640:(cls + 1) * 640], op=ALU.mult),
                            r=[pt.r(), EB.r()], w=[pt.r()])
                        state[n] = (pt, js)

                    def na_pv(n):
                        h, i = items[n]
                        pt, js = state.pop(n)
                        pso = PS[n % 2]
                        rcc = rc[n % 3]
                        for s_i in range(7):
                            kt = (2 + js + s_i) if s_i < 5 else (s_i - 5)
                            p.op("pe", lambda e, s_i=s_i, kt=kt: e.matmul(
                                pso[:, 0:65], lhsT=pt[:, s_i * 128:(s_i + 1) * 128], rhs=vA[:, kt, h, :],
                                start=(s_i == 0), stop=(s_i == 6)),
                                r=[pt.r(), vA.r(kt)], w=[pso.r()])
                        p.op("dve", lambda e: e.reciprocal(out=rcc[:], in_=pso[:, 64:65]),
                             r=[pso.r()], w=[rcc.r()])
                        p.op("dve", lambda e: e.tensor_scalar_mul(
                            out=ona[:, i, h * 64:(h + 1) * 64], in0=pso[:, 0:64], scalar1=rcc[:, 0:1]),
                            r=[pso.r(), rcc.r()], w=[ona.r(i)])

                    for n in range(len(items) + 1):
                        if n < len(items):
                            na_scores(n)
                        if n >= 1:
                            na_pv(n - 1)
                    for i in range(NT):
                        ps = nPS()
                        psb = ps[:].bitcast(BF16)
                        for c4 in range(4):
                            p.op("pe", lambda e, c4=c4, i=i, psb=psb: e.transpose(
                                psb[:, c4 * 128:(c4 + 1) * 128], ona[:, i, c4 * 128:(c4 + 1) * 128], identb[:]),
                                r=[ona.r(i), identb.r()], w=[ps.r()])
                        p.op("act", lambda e, i=i, psb=psb: e.copy(
                            out=onaT[:, :, i * 128:(i + 1) * 128], in_=psb[:, 0:512].rearrange("p (k n) -> p k n", k=4)),
                            r=[ps.r()], w=[onaT.r(i)])
                    if b == 0 and "ona" in dbg_d:
                        dump("ona", ona[:], ona.res)
                p.barrier()
                if stop == "B":
                    break
                omlT = sb(M, "omlT", [128, 4, S], BF16, nres=NT)

                with ExitStack() as L:
                    mqT = sb(L, "mqT", [128, 2, S], BF16, nres=2)
                    mkT = sb(L, "mkT", [128, 2, S], BF16, nres=2)
                    ktm = sb(L, "ktm", [128, NTT, 256], BF16, nres=NTT)
                    vM = sb(L, "vM", [128, NTT, 4, 129], BF16, nres=NTT)
                    osig = sb(L, "osig", [128, NT, 512], BF16, nres=NT)
                    gts = sb(L, "gts", [128, NTT, 16])
                    LI = sb(L, "LI", [128, NTT, 8])
                    LFn = sb(L, "LFn", [128, NTT, 8])
                    Bn = sb(L, "Bn", [128, NTT, 8])
                    EBt = sb(L, "EBt", [128, NTT, 8])
                    ES = sb(L, "ES", [128, NTT, 8])
                    EBL = sb(L, "EBL", [128, NTT, 8])
                    ghd = sb(L, "ghd", [128, 512])
                    p.dma("sp", ghd[:], g_head_d.partition_broadcast(128), w=[ghd.r()], sem="x")
                    p.op("dve", lambda e: e.memset(vM[:, :, :, 128:129], 1.0), w=vM.res)
                    with ExitStack() as L2:
                        wq = sb(L2, "wq", [128, 8, 256], BF16); wqp = sb(L2, "wqp", [128, 8, 256], BF16)
                        wk = sb(L2, "wk", [128, 8, 256], BF16); wkp = sb(L2, "wkp", [128, 8, 256], BF16)
                        cosT = sb(L2, "cosT", [128, 512]); sinT = sb(L2, "sinT", [128, 512])
                        rt = [sb(L2, "rt%d" % i, [128, 512]) for i in range(2)]
                        p.dma("pool", wq[:], wview(w_in_d[:, C_MLQ:C_MLQ + 256]), w=[wq.r()], sem="w")
                        p.dma("pool", wqp[:], wview(w_qkp_d[:, 0:256]), w=[wqp.r()], sem="w")
                        p.dma("pool", wk[:], wview(w_in_d[:, C_MLK:C_MLK + 256]), w=[wk.r()], sem="w")
                        p.dma("pool", wkp[:], wview(w_qkp_d[:, 256:512]), w=[wkp.r()], sem="w")
                        for (w_a, w_b, dst) in ((wq, wqp, mqT), (wk, wkp, mkT)):
                            for c in range(2):
                                for tb in range(4):
                                    ps = nPD()
                                    mm_fm(ps[:, 0:512], ps.r(), w_a, c * 128, 256 + tb * 512, 512)
                                    mm_fm(ps[:, 512:1024], ps.r(), w_b, c * 128, 256 + tb * 512, 512)
                                    cs_ = slice(tb * 512, (tb + 1) * 512)
                                    p.dma("sp", cosT[:], cos_d[:, cs_], w=[cosT.r()], sem="x")
                                    p.dma("sp", sinT[:], sin_d[:, cs_], w=[sinT.r()], sem="x")
                                    p.op("dve", lambda e, ps=ps, cs_=cs_: e.tensor_tensor(
                                        out=rt[0][:], in0=ps[:, 0:512], in1=cosT[:], op=ALU.mult),
                                        r=[ps.r(), cosT.r()], w=[rt[0].r()])
                                    p.op("dve", lambda e, ps=ps, cs_=cs_: e.tensor_tensor(
                                        out=rt[1][:], in0=ps[:, 512:1024], in1=sinT[:], op=ALU.mult),
                                        r=[ps.r(), sinT.r()], w=[rt[1].r()])
                                    p.op("dve", lambda e, dst=dst, c=c, cs_=cs_: e.tensor_tensor(
                                        out=dst[:, c, cs_], in0=rt[0][:], in1=rt[1][:], op=ALU.add),
                                        r=[rt[0].r(), rt[1].r()], w=[dst.r(c)])
                        for t in range(NT):
                            ps = nPS()
                            psb = ps[:].bitcast(BF16)
                            for c in range(2):
                                p.op("pe", lambda e, c=c, t=t, psb=psb: e.transpose(
                                    psb[:, c * 128:(c + 1) * 128], mkT[:, c, t * 128:(t + 1) * 128], identb[:]),
                                    r=[mkT.r(c), identb.r()], w=[ps.r()])
                            p.op("act", lambda e, t=t, psb=psb: e.copy(out=ktm[:, 2 + t, :], in_=psb[:, 0:256]),
                                 r=[ps.r()], w=[ktm.r(2 + t)])
                        for t in range(2):
                            ps = nPS()
                            mm_tm(ps[:, 0:256], ps.r(), wk, 0, 256, t)
                            p.op("act", lambda e, t=t, ps=ps: e.copy(out=ktm[:, t, :], in_=ps[:, 0:256]),
                                 r=[ps.r()], w=[ktm.r(t)])
                    p.barrier()
                    if stop == "C1":
                        break
                    with ExitStack() as L2:
                        wv = sb(L2, "wv", [128, 8, 512], BF16); wo = sb(L2, "wo", [128, 8, 512], BF16)
                        wg = sb(L2, "wgt", [128, 8, 16], BF16)
                        bmg = sb(L2, "bmg", [1, 16])
                        p.dma("pool", wv[:], wview(w_in_d[:, C_MLV:C_MLV + 512]), w=[wv.r()], sem="w")
                        p.dma("pool", wo[:], wview(w_in_d[:, C_MLO:C_MLO + 512]), w=[wo.r()], sem="w")
                        p.dma("pool", wg[:], wview(w_in_d[:, C_MLG:C_MLG + 16]), w=[wg.r()], sem="w")
                        p.dma("sp", bmg[:], b_mg_d, w=[bmg.r()], sem="x")
                        for t in range(NTT):
                            ps = nPD()
                            mm_tm(ps[:, 0:512], ps.r(), wv, 0, 512, t)
                            p.op("dve", lambda e, t=t, ps=ps: e.tensor_copy(
                                out=vM[:, t, :, 0:128], in_=ps[:, 0:512].rearrange("p (h d) -> p h d", h=4)),
                                r=[ps.r()], w=[vM.r(t)])
                            if t >= 2:
                                mm_tm(ps[:, 512:1024], ps.r(), wo, 0, 512, t)
                                p.op("act", lambda e, t=t, ps=ps: e.activation(
                                    out=osig[:, t - 2, :], in_=ps[:, 512:1024], func=AF.Sigmoid),
                                    r=[ps.r()], w=[osig.r(t - 2)])
                            ps2 = nPS()
                            for kc in range(8):
                                p.op("pe", lambda e, kc=kc, t=t, ps2=ps2: e.matmul(
                                    ps2[:, 0:16], lhsT=hT[:, kc, t * 128:(t + 1) * 128], rhs=wg[:, kc, :],
                                    start=(kc == 0), stop=(kc == 7)), r=[wg.r(), hT.r(t)], w=[ps2.r()])
                            p.op("pe", lambda e, ps2=ps2: e.matmul(
                                ps2[:, 16:32], lhsT=ones32[0:1, :], rhs=bmg[0:1, :], start=True, stop=True),
                                r=[ones32.r(), bmg.r()], w=[ps2.r()])
                            p.op("act", lambda e, t=t, ps2=ps2: e.copy(out=gts[:, t, :], in_=ps2[:, 0:16]),
                                 r=[ps2.r()], w=[gts.r()])
                            p.op("dve", lambda e, t=t, ps2=ps2: e.tensor_tensor(
                                out=gts[:, t, :], in0=gts[:, t, :], in1=ps2[:, 16:32], op=ALU.add),
                                r=[ps2.r(), gts.r()], w=[gts.r()])
                    p.barrier()
                    if stop == "C2":
                        break
                    gv = gts[:].rearrange("p t (k h) -> p t k h", k=4)
                    p.op("act", lambda e: e.activation(out=gts[:], in_=gts[:], func=AF.Tanh, scale=1.0 / 15.0),
                         r=[gts.r()], w=[gts.r()])
                    for d_ in range(2):
                        p.op("dve", lambda e, d_=d_: e.tensor_scalar_mul(
                            out=LI[:, :, d_ * 4:(d_ + 1) * 4], in0=gv[:, :, 2 * d_, :], scalar1=15.0),
                            r=[gts.r()], w=[LI.r()])
                        p.op("act", lambda e, d_=d_: e.activation(
                            out=LFn[:, :, d_ * 4:(d_ + 1) * 4], in_=gv[:, :, 2 * d_ + 1, :], func=AF.Exp, scale=-15.0),
                            r=[gts.r()], w=[LFn.r()])
                    p.op("act", lambda e: e.activation(out=LFn[:], in_=LFn[:], func=AF.Ln, bias=1.0),
                         r=[LFn.r()], w=[LFn.r()])
                    psc = nPS()
                    for t in range(NTT):
                        for d_, tri in ((0, trif), (1, trib)):
                            p.op("pe", lambda e, t=t, d_=d_, tri=tri: e.matmul(
                                psc[:, t * 8 + d_ * 4:t * 8 + d_ * 4 + 4], lhsT=tri[:], rhs=LFn[:, t, d_ * 4:(d_ + 1) * 4],
                                start=True, stop=True), r=[tri.r(), LFn.r()], w=[psc.r()])
                    p.op("dve", lambda e: e.tensor_copy(out=Bn[:].rearrange("p t k -> p (t k)"), in_=psc[:, 0:NTT * 8]),
                         r=[psc.r()], w=[Bn.r()])
                    psl = nPS()
                    p.op("pe", lambda e: e.matmul(psl[:, 0:NTT * 8], lhsT=ones32[:], rhs=LFn[:].rearrange("p t k -> p (t k)"),
                                                  start=True, stop=True), r=[ones32.r(), LFn.r()], w=[psl.r()])
                    p.op("act", lambda e: e.activation(out=EBL[:].rearrange("p t k -> p (t k)"), in_=psl[:, 0:NTT * 8],
                                                       func=AF.Exp, scale=-1.0), r=[psl.r()], w=[EBL.r()])
                    p.op("act", lambda e: e.activation(out=EBt[:], in_=Bn[:], func=AF.Exp, scale=-1.0),
                         r=[Bn.r()], w=[EBt.r()])
                    p.op("dve", lambda e: e.tensor_tensor(out=ES[:], in0=LI[:], in1=Bn[:], op=ALU.add),
                         r=[LI.r(), Bn.r()], w=[ES.r()])
                    p.op("act", lambda e: e.activation(out=ES[:], in_=ES[:], func=AF.Exp, bias=float(-np.log(8.0))),
                         r=[ES.r()], w=[ES.r()])
                    if stop == "C3":
                        p.barrier()
                        break
                    Hf = sb(L, "Hf", [128, NT, 512], BF16, nres=NT)
                    Cst = [sb(L, "Cst%d" % d_, [128, 4, 129]) for d_ in range(2)]
                    Cbf = [sb(L, "Cbf%d" % d_, [128, 4, 129], BF16) for d_ in range(2)]
                    vp = [sb(L, "vp%d" % d_, [128, 4, 129], BF16) for d_ in range(2)]
                    sTm = [sb(L, "sTm%d" % d_, [128, 4, 128], BF16) for d_ in range(2)]
                    sm = [sb(L, "sm%d" % d_, [128, 4, 4]) for d_ in range(2)]
                    Hs = [sb(L, "Hs%d" % i, [128, 512]) for i in range(2)]
                    Hq = [sb(L, "Hq%d" % i, [128, 512]) for i in range(1)]
                    fs = [sb(L, "fs%d" % i, [128, 8]) for i in range(2)]
                    omb = [sb(L, "omb%d" % i, [128, 512], BF16) for i in range(2)]
                    bwd_order = [1, 0] + list(range(NTT - 1, 1, -1))
                    tri4 = [sb(L, "tri4%d" % d_, [128, 4, 128], BF16) for d_ in range(2)]
                    for d_, tr_ in ((0, trif), (1, trib)):
                        for hd in range(4):
                            p.op("dve", lambda e, d_=d_, tr_=tr_, hd=hd: e.tensor_copy(out=tri4[d_][:, hd, :], in_=tr_[:]),
                                 r=[tr_.r()], w=[tri4[d_].r()])
                    psN_ = [PD[0], PD[2]]
                    psU_ = PD[1]
                    for d_ in range(2):
                        if stop in ("C4", "C6", "C7") and d_ == 1:
                            break
                        for step in range(NTT):
                            if (stop == "C6" and step == 2) or (stop == "C7" and step == 3):
                                break
                            t = step if d_ == 0 else bwd_order[step]
                            tri = trif if d_ == 0 else trib
                            psN = psN_[d_]
                            psS = PS[d_]
                            C_, Cb_, vp_, sT_, sm_ = Cst[d_], Cbf[d_], vp[d_], sTm[d_], sm[d_]
                            p.op(POOLENG, lambda e, t=t, d_=d_, vp_=vp_: e.tensor_tensor(
                                out=vp_[:], in0=vM[:, t, :, :], in1=bc(ES[:, t, d_ * 4:(d_ + 1) * 4].unsqueeze(2), [128, 4, 129]),
                                op=ALU.mult), r=[vM.r(t), ES.r()], w=[vp_.r()])
                            if t >= 2:
                                lt = t - 2
                                for hd in range(4):
                                    hp, c = (hd % 2) * 64, hd // 2
                                    p.op("pe", lambda e, hd=hd, hp=hp, c=c, lt=lt, psS=psS: e.matmul(
                                        psS[:, hd * 128:(hd + 1) * 128], lhsT=mkT[hp:hp + 64, c, lt * 128:(lt + 1) * 128],
                                        rhs=mqT[hp:hp + 64, c, lt * 128:(lt + 1) * 128], start=True, stop=True),
                                        r=[mkT.r(c), mqT.r(c)], w=[psS.r()])
                                    p.op("pe", lambda e, hd=hd, c=c, t=t, vp_=vp_: e.matmul(
                                        psU_[:, hd * 256:hd * 256 + 129], lhsT=ktm[:, t, c * 128:(c + 1) * 128], rhs=vp_[:, hd, :],
                                        start=True, stop=True), r=[ktm.r(t), vp_.r()], w=[psU_.r()])
                                p.op("dve", lambda e, sT_=sT_, psS=psS, d_=d_: e.tensor_tensor(
                                    out=sT_[:].rearrange("p h n -> p (h n)"), in0=psS[:, 0:512],
                                    in1=tri4[d_][:].rearrange("p h n -> p (h n)"), op=ALU.mult),
                                    r=[psS.r(), tri4[d_].r()], w=[sT_.r()])
                                for hd in range(4):
                                    hp, c = (hd % 2) * 64, hd // 2
                                    p.op("pe", lambda e, hd=hd, psN=psN, sT_=sT_, vp_=vp_: e.matmul(
                                        psN[:, hd * 256:hd * 256 + 129], lhsT=sT_[:, hd, :], rhs=vp_[:, hd, :],
                                        start=True, stop=(step == 0)), r=[sT_.r(), vp_.r()], w=[psN.r()])
                                    if step > 0:
                                        p.op("pe", lambda e, hd=hd, hp=hp, c=c, lt=lt, psN=psN, Cb_=Cb_: e.matmul(
                                            psN[:, hd * 256:hd * 256 + 129], lhsT=mqT[hp:hp + 64, c, lt * 128:(lt + 1) * 128],
                                            rhs=Cb_[hp:hp + 64, hd, :], start=False, stop=True),
                                            r=[mqT.r(c), Cb_.r()], w=[psN.r()])
                                nv = psN[:].rearrange("p (h x) -> p h x", x=256)
                                p.op("dve", lambda e, nv=nv, sm_=sm_, t=t, d_=d_: e.tensor_tensor(
                                    out=sm_[:, :, 0:1], in0=nv[:, :, 128:129], in1=EBt[:, t, d_ * 4:(d_ + 1) * 4].unsqueeze(2),
                                    op=ALU.mult), r=[psN.r(), EBt.r()], w=[sm_.r()])
                                p.op("dve", lambda e, sm_=sm_: e.tensor_scalar(
                                    out=sm_[:, :, 1:2], in0=sm_[:, :, 0:1], scalar1=-1.0, scalar2=1.0, op0=ALU.mult, op1=ALU.max),
                                    r=[sm_.r()], w=[sm_.r()])
                                p.op("dve", lambda e, sm_=sm_: e.tensor_tensor(
                                    out=sm_[:, :, 2:3], in0=sm_[:, :, 1:2], in1=sm_[:, :, 0:1], op=ALU.max),
                                    r=[sm_.r()], w=[sm_.r()])
                                p.op("dve", lambda e, sm_=sm_: e.reciprocal(out=sm_[:, :, 3:4], in_=sm_[:, :, 2:3]),
                                     r=[sm_.r()], w=[sm_.r()])
                                p.op("dve", lambda e, sm_=sm_, t=t, d_=d_: e.tensor_tensor(
                                    out=sm_[:, :, 0:1], in0=sm_[:, :, 3:4], in1=EBt[:, t, d_ * 4:(d_ + 1) * 4].unsqueeze(2),
                                    op=ALU.mult), r=[sm_.r(), EBt.r()], w=[sm_.r()])
                                if d_ == 0:
                                    p.op("dve", lambda e, nv=nv, sm_=sm_, lt=lt: e.tensor_tensor(
                                        out=Hf[:, lt, :].rearrange("p (h n) -> p h n", h=4), in0=nv[:, :, 0:128],
                                        in1=bc(sm_[:, :, 0:1], [128, 4, 128]), op=ALU.mult),
                                        r=[psN.r(), sm_.r()], w=[Hf.r(lt)])
                                elif stop != "C5":
                                    hs, hq, f_, ob = Hs[lt % 2], Hq[0], fs[lt % 2], omb[lt % 2]
                                    p.op("dve", lambda e, nv=nv, sm_=sm_, hs=hs: e.tensor_tensor(
                                        out=hs[:].rearrange("p (h n) -> p h n", h=4), in0=nv[:, :, 0:128],
                                        in1=bc(sm_[:, :, 0:1], [128, 4, 128]), op=ALU.mult),
                                        r=[psN.r(), sm_.r()], w=[hs.r()])
                                    p.op(POOLENG, lambda e, hs=hs, lt=lt: e.tensor_tensor(
                                        out=hs[:], in0=hs[:], in1=Hf[:, lt, :], op=ALU.add),
                                        r=[hs.r(), Hf.r(lt)], w=[hs.r()])
                                    p.op(POOLENG, lambda e, hs=hs, hq=hq: e.tensor_tensor(out=hq[:], in0=hs[:], in1=hs[:], op=ALU.mult),
                                         r=[hs.r()], w=[hq.r()])
                                    p.op("dve", lambda e, hq=hq, f_=f_: e.reduce_sum(
                                        out=f_[:, 0:4], in_=hq[:].rearrange("p (h n) -> p h n", h=4), axis=AX.X),
                                        r=[hq.r()], w=[f_.r()])
                                    p.op("act", lambda e, f_=f_: e.activation(out=f_[:, 4:8], in_=f_[:, 0:4], func=AF.Sqrt,
                                                                           scale=1.0 / 128.0, bias=EPS), r=[f_.r()], w=[f_.r()])
                                    p.op("dve", lambda e, f_=f_: e.reciprocal(out=f_[:, 4:8], in_=f_[:, 4:8]), r=[f_.r()], w=[f_.r()])
                                    p.op("dve", lambda e, hs=hs, f_=f_: e.tensor_tensor(
                                        out=hs[:].rearrange("p (h n) -> p h n", h=4), in0=hs[:].rearrange("p (h n) -> p h n", h=4),
                                        in1=bc(f_[:, 4:8].unsqueeze(2), [128, 4, 128]), op=ALU.mult),
                                        r=[hs.r(), f_.r()], w=[hs.r()])
                                    p.op(POOLENG, lambda e, hs=hs: e.tensor_tensor(out=hs[:], in0=hs[:], in1=ghd[:], op=ALU.mult),
                                         r=[hs.r(), ghd.r()], w=[hs.r()])
                                    p.op(POOLENG, lambda e, hs=hs, ob=ob, lt=lt: e.tensor_tensor(
                                        out=ob[:], in0=hs[:], in1=osig[:, lt, :], op=ALU.mult),
                                        r=[hs.r(), osig.r(lt)], w=[ob.r()])
                                    ps = psS
                                    psb = ps[:].bitcast(BF16)
                                    for c4 in range(4):
                                        p.op("pe", lambda e, c4=c4, ob=ob, psb=psb: e.transpose(
                                            psb[:, c4 * 128:(c4 + 1) * 128], ob[:, c4 * 128:(c4 + 1) * 128], identb[:]),
                                            r=[ob.r(), identb.r()], w=[ps.r()])
                                    p.op("act", lambda e, lt=lt, psb=psb: e.copy(
                                        out=omlT[:, :, lt * 128:(lt + 1) * 128],
                                        in_=psb[:, 0:512].rearrange("p (k n) -> p k n", k=4)),
                                        r=[ps.r()], w=[omlT.r(lt)])
                            for hd in range(4):
                                c = hd // 2
                                if t >= 2:
                                    break
                                p.op("pe", lambda e, hd=hd, c=c, t=t, vp_=vp_: e.matmul(
                                    psU_[:, hd * 256:hd * 256 + 129], lhsT=ktm[:, t, c * 128:(c + 1) * 128], rhs=vp_[:, hd, :],
                                    start=True, stop=True), r=[ktm.r(t), vp_.r()], w=[psU_.r()])
                            uv = psU_[:].rearrange("p (h x) -> p h x", x=256)[:, :, 0:129]
                            ebl = bc(EBL[:, t, d_ * 4:(d_ + 1) * 4].unsqueeze(2), [128, 4, 129])
                            if step == 0:
                                p.op("dve", lambda e, C_=C_, uv=uv, ebl=ebl: e.tensor_tensor(out=C_[:], in0=uv, in1=ebl, op=ALU.mult),
                                     r=[psU_.r(), EBL.r()], w=[C_.r()])
                            else:
                                p.op("dve", lambda e, C_=C_, uv=uv: e.tensor_tensor(out=C_[:], in0=uv, in1=C_[:], op=ALU.add),
                                     r=[psU_.r(), C_.r()], w=[C_.r()])
                                p.op("dve", lambda e, C_=C_, ebl=ebl: e.tensor_tensor(out=C_[:], in0=C_[:], in1=ebl, op=ALU.mult),
                                     r=[C_.r(), EBL.r()], w=[C_.r()])
                            p.op("act", lambda e, C_=C_, Cb_=Cb_: e.copy(out=Cb_[:], in_=C_[:]), r=[C_.r()], w=[Cb_.r()])
                    if b == 0 and "omlT" in dbg_d:
                        dump("omlT", omlT[:], omlT.res)
                p.barrier()
                if stop in ("C", "C4", "C5", "C6", "C7"):
                    break

                with ExitStack() as L:
                    wbn = sb(L, "wbn", [128, 4, D], BF16); wbm = sb(L, "wbm", [128, 4, D], BF16)
                    wout = sb(L, "wout", [128, 8, D], BF16)
                    wr = sb(L, "wr", [128, 8, NE]); br = sb(L, "br", [1, NE])
                    p.dma("pool", wbn[:], wview(w_bna_d), w=[wbn.r()], sem="w")
                    p.dma("pool", wbm[:], wview(w_bml_d), w=[wbm.r()], sem="w")
                    p.dma("pool", wout[:], wview(w_out_d), w=[wout.r()], sem="w")
                    p.dma("sp", wr[:], wview(w_r_d), w=[wr.r()], sem="x")
                    p.dma("sp", br[:], b_r_d, w=[br.r()], sem="x")
                    wgn = [sb(L, "wgn%d" % i, [128, 8, 128], BF16) for i in range(2)]
                    wgm = [sb(L, "wgm%d" % i, [128, 8, 128], BF16) for i in range(2)]
                    mT = sb(L, "mT", [128, 8, S], BF16, nres=32)
                    sg = [sb(L, "sg%d" % i, [128, 512]) for i in range(2)]
                    m1 = [sb(L, "m1%d" % i, [128, 512]) for i in range(2)]
                    xt = [sb(L, "xd", [128, D])] * 2
                    yt = [sb(L, "yd%d" % i, [128, D]) for i in range(2)]
                    jk = sb(L, "jk", [128, D], BF16)
                    h2 = [sb(L, "h2", [128, D])] * 2
                    h2hi = [sb(L, "h2hi%d" % i, [128, D], BF16) for i in range(2)]
                    h2lo = [sb(L, "h2lo", [128, D], BF16)] * 2
                    h2Tlo = [sb(L, "h2Tlo%d" % i, [128, 8, 128], BF16) for i in range(2)]
                    wrhi = sb(L, "wrhi", [128, 8, NE], BF16); wrlo = sb(L, "wrlo", [128, 8, NE], BF16)
                    p.op("dve", lambda e: e.tensor_copy(out=wrhi[:], in_=wr[:]), r=[wr.r()], w=[wrhi.r()])
                    p.op("dve", lambda e: e.tensor_tensor(out=wrlo[:], in0=wr[:], in1=wrhi[:], op=ALU.subtract),
                         r=[wr.r(), wrhi.r()], w=[wrlo.r()])
                    st = [sb(L, "std%d" % i, [128, 4]) for i in range(2)]
                    lg = [sb(L, "lg%d" % i, [128, 3, NE]) for i in range(2)]
                    t8 = [sb(L, "t8%d" % i, [128, 20]) for i in range(2)]
                    mkb = [sb(L, "mkb%d" % i, [128, NE], BF16) for i in range(2)]
                    oh4 = [sb(L, "oh4%d" % i, [128, 4, NE]) for i in range(2)]
                    n = 0
                    for dc in range(8):
                        n += 1
                        a_, b_ = wgn[n % 2], wgm[n % 2]
                        p.dma("pool", a_[:], wview(w_in_d[:, C_GNA + dc * 128:C_GNA + (dc + 1) * 128]), w=[a_.r()], sem="w")
                        p.dma("pool", b_[:], wview(w_in_d[:, C_GML + dc * 128:C_GML + (dc + 1) * 128]), w=[b_.r()], sem="w")
                        for tb in range(4):
                            tok0 = 256 + tb * 512
                            pa, pb = nPD(), nPD()
                            mm_fm(pa[:, 0:512], pa.r(), a_, 0, tok0, 512)
                            mm_fm(pa[:, 512:1024], pa.r(), wbn, dc * 128, tb * 512, 512, nk=4, src=onaT)
                            mm_fm(pb[:, 0:512], pb.r(), b_, 0, tok0, 512)
                            mm_fm(pb[:, 512:1024], pb.r(), wbm, dc * 128, tb * 512, 512, nk=4, src=omlT)
                            p.op("act", lambda e, pa=pa: e.activation(out=sg[0][:], in_=pa[:, 0:512], func=AF.Sigmoid),
                                 r=[pa.r()], w=[sg[0].r()])
                            p.op("act", lambda e, pb=pb: e.activation(out=sg[1][:], in_=pb[:, 0:512], func=AF.Sigmoid),
                                 r=[pb.r()], w=[sg[1].r()])
                            p.op("dve", lambda e, pa=pa: e.tensor_tensor(out=m1[0][:], in0=pa[:, 512:1024], in1=sg[0][:], op=ALU.mult),
                                 r=[pa.r(), sg[0].r()], w=[m1[0].r()])
                            p.op("dve", lambda e, pb=pb: e.tensor_tensor(out=m1[1][:], in0=pb[:, 512:1024], in1=sg[1][:], op=ALU.mult),
                                 r=[pb.r(), sg[1].r()], w=[m1[1].r()])
                            p.op(POOLENG, lambda e, dc=dc, tb=tb: e.tensor_tensor(
                                out=mT[:, dc, tb * 512:(tb + 1) * 512], in0=m1[0][:], in1=m1[1][:], op=ALU.add),
                                r=[m1[0].r(), m1[1].r()], w=[mT.r(dc * 4 + tb)])
                    for tb in range(4):
                        for ti in range(4):
                            t = tb * 4 + ti
                            i = t % 2
                            py = PD[2]
                            for half in range(2):
                                for kc in range(8):
                                    p.op("pe", lambda e, kc=kc, half=half, t=t, py=py: e.matmul(
                                        py[:, half * 512:(half + 1) * 512], lhsT=mT[:, kc, t * 128:(t + 1) * 128],
                                        rhs=wout[:, kc, half * 512:(half + 1) * 512], start=(kc == 0), stop=(kc == 7)),
                                        r=[mT.r(kc * 4 + tb), wout.r()], w=[py.r()])
                            p.dma("sp", xt[i][:], x_d[b, t * 128:(t + 1) * 128, :], w=[xt[i].r()], sem="x")
                            rstd_of(py[:], [py.r()], jk, st[i])
                            p.op("dve", lambda e, i=i, py=py: e.scalar_tensor_tensor(
                                out=yt[i][:], in0=py[:], scalar=st[i][:, 1:2], in1=G_m[:], op0=ALU.mult, op1=ALU.mult),
                                r=[py.r(), st[i].r(), G_m.r()], w=[yt[i].r()])
                            p.op(POOLENG, lambda e, i=i: e.tensor_tensor(out=yt[i][:], in0=yt[i][:], in1=xt[i][:], op=ALU.add),
                                 r=[yt[i].r(), xt[i].r()], w=[yt[i].r()])
                            p.dma("sp", out_d[b, t * 128:(t + 1) * 128, :], yt[i][:], r=[yt[i].r()], w=[x1res[b][t]], sem="o")
                            if b == 0 and "x1" in dbg_d:
                                dump("x1", yt[i][:], yt[i].r(), dst=dbg_d["x1"][t])
                            rstd_of(yt[i][:], [yt[i].r()], jk, st[i])
                            p.op("dve", lambda e, i=i: e.scalar_tensor_tensor(
                                out=h2[i][:], in0=yt[i][:], scalar=st[i][:, 1:2], in1=A_f[:], op0=ALU.mult, op1=ALU.mult),
                                r=[yt[i].r(), st[i].r(), A_f.r()], w=[h2[i].r()])
                            p.op(POOLENG, lambda e, i=i: e.tensor_tensor(out=h2[i][:], in0=h2[i][:], in1=sh_f[:], op=ALU.add),
                                 r=[h2[i].r(), sh_f.r()], w=[h2[i].r()])
                            p.op("act", lambda e, i=i: e.copy(out=h2hi[i][:], in_=h2[i][:]), r=[h2[i].r()], w=[h2hi[i].r()])
                            p.op("dve", lambda e, i=i: e.tensor_tensor(out=h2lo[i][:], in0=h2[i][:], in1=h2hi[i][:], op=ALU.subtract),
                                 r=[h2[i].r(), h2hi[i].r()], w=[h2lo[i].r()])
                            pa_ = PS[i]
                            pab = pa_[:].bitcast(BF16)
                            for kc in range(8):
                                p.op("pe", lambda e, kc=kc, i=i, pab=pab: e.transpose(
                                    pab[:, kc * 128:(kc + 1) * 128], h2hi[i][:, kc * 128:(kc + 1) * 128], identb[:]),
                                    r=[h2hi[i].r(), identb.r()], w=[pa_.r()])
                            p.op("act", lambda e, t=t, pab=pab: e.copy(
                                out=hT[:, :, (2 + t) * 128:(3 + t) * 128], in_=pab.rearrange("p (k n) -> p k n", k=8)),
                                r=[pa_.r()], w=[hT.r(2 + t)])
                            pb_ = PD[i]
                            pbb = pb_[:].bitcast(BF16)
                            for kc in range(8):
                                p.op("pe", lambda e, kc=kc, i=i, pbb=pbb: e.transpose(
                                    pbb[:, kc * 128:(kc + 1) * 128], h2lo[i][:, kc * 128:(kc + 1) * 128], identb[:]),
                                    r=[h2lo[i].r(), identb.r()], w=[pb_.r()])
                            p.op("dve", lambda e, i=i, pbb=pbb: e.tensor_copy(
                                out=h2Tlo[i][:], in_=pbb[:, 0:1024].rearrange("p (k n) -> p k n", k=8)),
                                r=[pb_.r()], w=[h2Tlo[i].r()])
                            pl = PS[1 - i]
                            nmm = 0
                            for kc in range(8):
                                for (lh_, lres, w_) in ((hT[:, kc, (2 + t) * 128:(3 + t) * 128], hT.r(2 + t), wrhi),
                                                        (h2Tlo[i][:, kc, :], h2Tlo[i].r(), wrhi),
                                                        (hT[:, kc, (2 + t) * 128:(3 + t) * 128], hT.r(2 + t), wrlo)):
                                    nmm += 1
                                    p.op("pe", lambda e, kc=kc, lh_=lh_, w_=w_, pl=pl, nmm=nmm: e.matmul(
                                        pl[:, 0:NE], lhsT=lh_, rhs=w_[:, kc, :], start=(nmm == 1), stop=(nmm == 24)),
                                        r=[lres, w_.r()], w=[pl.r()])
                            p.op("pe", lambda e, pl=pl: e.matmul(pl[:, 32:64], lhsT=ones32[0:1, :], rhs=br[0:1, :], start=True, stop=True),
                                 r=[ones32.r(), br.r()], w=[pl.r()])
                            L_, T_ = lg[i], t8[i]
                            p.op("act", lambda e, L_=L_, pl=pl: e.copy(out=L_[:, 0, :], in_=pl[:, 0:NE]), r=[pl.r()], w=[L_.r()])
                            p.op("dve", lambda e, L_=L_, pl=pl: e.tensor_tensor(out=L_[:, 0, :], in0=L_[:, 0, :], in1=pl[:, 32:64], op=ALU.add),
                                 r=[pl.r(), L_.r()], w=[L_.r()])
                            if b == 0 and "logits" in dbg_d:
                                dump("logits", L_[:, 0, :], L_.r(), dst=dbg_d["logits"][t])
                            p.op("dve", lambda e, L_=L_, T_=T_: e.max(out=T_[:, 0:8], in_=L_[:, 0, :]), r=[L_.r()], w=[T_.r()])
                            gt = b * NT + t
                            mk_, oh_ = mkb[i], oh4[i]
                            p.op("dve", lambda e, L_=L_, T_=T_: e.tensor_scalar(
                                out=L_[:, 1, :], in0=L_[:, 0, :], scalar1=T_[:, 3:4], scalar2=None, op0=ALU.is_ge),
                                r=[L_.r(), T_.r()], w=[L_.r()])
                            p.op("dve", lambda e, L_=L_, mk_=mk_: e.tensor_copy(out=mk_[:], in_=L_[:, 1, :]), r=[L_.r()], w=[mk_.r()])
                            p.op("pe", lambda e, pl=pl, mk_=mk_: e.matmul(pl[:, 64:96], lhsT=trisb[:], rhs=mk_[:], start=True, stop=False),
                                 r=[trisb.r(), mk_.r()], w=[pl.r()])
                            p.op("pe", lambda e, pl=pl: e.matmul(pl[:, 64:96], lhsT=onesb[:], rhs=msum[:], start=False, stop=True),
                                 r=[onesb.r(), msum.r()], w=[pl.r()])
                            p.op("dve", lambda e, L_=L_, pl=pl: e.scalar_tensor_tensor(
                                out=L_[:, 2, :], in0=pl[:, 64:96], scalar=float(CAP - 1), in1=ecap[:], op0=ALU.min, op1=ALU.add),
                                r=[pl.r(), ecap.r()], w=[L_.r()])
                            p.op("dve", lambda e, mk_=mk_: e.tensor_tensor(out=msum[:], in0=msum[:], in1=mk_[:], op=ALU.add),
                                 r=[msum.r(), mk_.r()], w=[msum.r()])
                            for k in range(4):
                                p.op("dve", lambda e, k=k, L_=L_, T_=T_, oh_=oh_: e.tensor_scalar(
                                    out=oh_[:, k, :], in0=L_[:, 0, :], scalar1=T_[:, k:k + 1], scalar2=None, op0=ALU.is_equal),
                                    r=[L_.r(), T_.r()], w=[oh_.r()])
                                p.op("dve", lambda e, k=k, L_=L_, oh_=oh_: e.tensor_tensor(
                                    out=oh_[:, k, :], in0=oh_[:, k, :], in1=L_[:, 2, :], op=ALU.mult),
                                    r=[oh_.r(), L_.r()], w=[oh_.r()])
                            p.op("dve", lambda e, T_=T_, oh_=oh_: e.reduce_sum(out=T_[:, 12:16], in_=oh_[:], axis=AX.X),
                                 r=[oh_.r()], w=[T_.r()])
                            p.op("dve", lambda e, T_=T_, gt=gt: e.tensor_copy(out=RIi[:, gt, :], in_=T_[:, 12:16]),
                                 r=[T_.r()], w=[RIi.r(gt)])
                            p.op("dve", lambda e, T_=T_: e.tensor_scalar_mul(out=T_[:, 8:9], in0=T_[:, 0:1], scalar1=-1.0),
                                 r=[T_.r()], w=[T_.r()])
                            p.op("act", lambda e, T_=T_: e.activation(out=T_[:, 16:20], in_=T_[:, 0:4], func=AF.Exp,
                                                                     bias=T_[:, 8:9], scale=1.0), r=[T_.r()], w=[T_.r()])
                            p.op("dve", lambda e, T_=T_: e.reduce_sum(out=T_[:, 9:10], in_=T_[:, 16:20], axis=AX.X),
                                 r=[T_.r()], w=[T_.r()])
                            p.op("dve", lambda e, T_=T_: e.reciprocal(out=T_[:, 10:11], in_=T_[:, 9:10]), r=[T_.r()], w=[T_.r()])
                            p.op("dve", lambda e, T_=T_, gt=gt: e.tensor_scalar_mul(
                                out=RIw[:, gt, :], in0=T_[:, 16:20], scalar1=T_[:, 10:11]), r=[T_.r()], w=[RIw.r(gt)])
                            for k in range(4):
                                p.idma(out=xe_d, out_offset=bass.IndirectOffsetOnAxis(ap=RIi[:, gt, k:k + 1], axis=0),
                                       in_=h2hi[i][:], in_offset=None, bounds=NE * CAP - 1,
                                       r=[h2hi[i].r(), RIi.r(gt)], w=[], sem="sc")
                p.barrier()
            if stop is not None:
                break

            p.barrier()

        if stop is None:
            p.barrier()
            with ExitStack() as L:
                wgb = [sb(L, "wgb%d" % i, [128, 8, D], BF16) for i in range(2)]
                wlb = [sb(L, "wlb%d" % i, [128, 8, D], BF16) for i in range(2)]
                wdb = [sb(L, "wdb%d" % i, [128, 8, D], BF16) for i in range(2)]
                bdb = [sb(L, "bdb%d" % i, [128, D]) for i in range(2)]
                bg = sb(L, "bg", [128, 8, NE]); bl = sb(L, "bl", [128, 8, NE])
                p.dma("sp", bg[:], bgT_d, w=[bg.r()], sem="x")
                p.dma("sp", bl[:], blT_d, w=[bl.r()], sem="x")
                xr = [sb(L, "xr%d" % i, [128, 4, D], BF16) for i in range(2)]
                xT = [sb(L, "xT%d" % i, [128, 8, 512], BF16) for i in range(2)]
                aT2 = [sb(L, "aT%d" % i, [128, 8, 512], BF16, nres=8) for i in range(2)]
                gg = [sb(L, "gg%d" % i, [128, 512]) for i in range(2)]
                sg = [sb(L, "sgE%d" % i, [128, 512]) for i in range(2)]
                l1 = [sb(L, "l1%d" % i, [128, 512]) for i in range(2)]
                t1 = [sb(L, "t1%d" % i, [128, 512]) for i in range(2)]
                ysb = [sb(L, "ysb%d" % i, [128, D]) for i in range(2)]

                class BV:
                    def __init__(self, parent, c0):
                        self.par, self.c0, self.res = parent, c0, Res()

                    def r(self):
                        return self.res

                    def ap(self):
                        return self.par[:, self.c0:self.c0 + 512]

                banks = [BV(PD[2], 0), BV(PD[2], 512), BV(PS[0], 0), BV(PS[1], 0)]
                bki = [0]

                def nbank():
                    bki[0] += 1
                    return banks[bki[0] % 4]

                NBLK = CAP // 512
                NTOT = NE * NBLK
                ycnt = [0]

                def load_w(e_):
                    p.dma("pool", wgb[e_ % 2][:], wview(w_gate_d[e_]), w=[wgb[e_ % 2].r()], sem="w")
                    p.dma("pool", wlb[e_ % 2][:], wview(w_lin_d[e_]), w=[wlb[e_ % 2].r()], sem="w")
                    p.dma("pool", wdb[e_ % 2][:], wview(w_down_d[e_]), w=[wdb[e_ % 2].r()], sem="w")
                    p.dma("sp", bdb[e_ % 2][:], b_down_d[e_:e_ + 1, :].partition_broadcast(128), w=[bdb[e_ % 2].r()], sem="x")

                def emit_T(n):
                    e_, blk = divmod(n, NBLK)
                    xr_, xT_ = xr[n % 2], xT[n % 2]
                    row0 = e_ * CAP + blk * 512
                    p.dma("sp", xr_[:], xe_d[row0:row0 + 512, :].rearrange("(j p) d -> p j d", p=128), w=[xr_.r()], sem="x")
                    for j in range(4):
                        bk = nbank()
                        psb = bk.ap().bitcast(BF16)
                        for kc in range(8):
                            p.op("pe", lambda e, kc=kc, j=j, psb=psb, xr_=xr_: e.transpose(
                                psb[:, kc * 128:(kc + 1) * 128], xr_[:, j, kc * 128:(kc + 1) * 128], identb[:]),
                                r=[xr_.r(), identb.r()], w=[bk.r()])
                        eng = "act" if j % 2 else "dve"
                        if eng == "act":
                            p.op("act", lambda e, j=j, psb=psb, xT_=xT_: e.copy(
                                out=xT_[:, :, j * 128:(j + 1) * 128], in_=psb.rearrange("p (k n) -> p k n", k=8)),
                                r=[bk.r()], w=[xT_.r()])
                        else:
                            p.op("dve", lambda e, j=j, psb=psb, xT_=xT_: e.tensor_copy(
                                out=xT_[:, :, j * 128:(j + 1) * 128], in_=psb.rearrange("p (k n) -> p k n", k=8)),
                                r=[bk.r()], w=[xT_.r()])

                def emit_GL(n, fc):
                    e_ = n // NBLK
                    wg_, wl_ = wgb[e_ % 2], wlb[e_ % 2]
                    xT_, aT = xT[n % 2], aT2[n % 2]
                    j = fc % 2
                    pg = PD[j]
                    for (w_, c0) in ((wg_, 0), (wl_, 512)):
                        for kc in range(8):
                            p.op("pe", lambda e, kc=kc, w_=w_, c0=c0, pg=pg: e.matmul(
                                pg[:, c0:c0 + 512], lhsT=w_[:, kc, fc * 128:(fc + 1) * 128], rhs=xT_[:, kc, :],
                                start=(kc == 0), stop=(kc == 7)), r=[w_.r(), xT_.r()], w=[pg.r()])
                    p.op("dve", lambda e: e.tensor_scalar(
                        out=gg[j][:], in0=pg[:, 0:512], scalar1=bg[:, fc, e_:e_ + 1], scalar2=7.0, op0=ALU.add, op1=ALU.min),
                        r=[pg.r(), bg.r()], w=[gg[j].r()])
                    p.op("act", lambda e: e.activation(out=sg[j][:], in_=gg[j][:], func=AF.Sigmoid, scale=1.702),
                         r=[gg[j].r()], w=[sg[j].r()])
                    p.op("dve", lambda e: e.tensor_scalar(
                        out=l1[j][:], in0=pg[:, 512:1024], scalar1=bl[:, fc, e_:e_ + 1], scalar2=7.0, op0=ALU.add, op1=ALU.min),
                        r=[pg.r(), bl.r()], w=[l1[j].r()])
                    p.op("dve", lambda e: e.tensor_scalar(
                        out=l1[j][:], in0=l1[j][:], scalar1=-7.0, scalar2=1.0, op0=ALU.max, op1=ALU.add),
                        r=[l1[j].r()], w=[l1[j].r()])
                    p.op(POOLENG, lambda e: e.tensor_tensor(out=t1[j][:], in0=gg[j][:], in1=sg[j][:], op=ALU.mult),
                         r=[gg[j].r(), sg[j].r()], w=[t1[j].r()])
                    p.op("dve", lambda e: e.tensor_tensor(out=aT[:, fc, :], in0=t1[j][:], in1=l1[j][:], op=ALU.mult),
                         r=[t1[j].r(), l1[j].r()], w=[aT.r(fc)])

                def emit_DOWN(n):
                    e_, blk = divmod(n, NBLK)
                    wd_, bd_, aT = wdb[e_ % 2], bdb[e_ % 2], aT2[n % 2]
                    row0 = e_ * CAP + blk * 512
                    for ti in range(4):
                        ycnt[0] += 1
                        ys_ = ysb[ycnt[0] % 2]
                        for half in range(2):
                            bk = nbank()
                            for fc in range(8):
                                p.op("pe", lambda e, fc=fc, half=half, ti=ti, bk=bk: e.matmul(
                                    bk.ap(), lhsT=aT[:, fc, ti * 128:(ti + 1) * 128],
                                    rhs=wd_[:, fc, half * 512:(half + 1) * 512], start=(fc == 0), stop=(fc == 7)),
                                    r=[aT.r(fc), wd_.r()], w=[bk.r()])
                            p.op("dve", lambda e, half=half, bk=bk, ys_=ys_: e.tensor_tensor(
                                out=ys_[:, half * 512:(half + 1) * 512], in0=bk.ap(), in1=bd_[:, half * 512:(half + 1) * 512], op=ALU.add),
                                r=[bk.r(), bd_.r()], w=[ys_.r()])
                        p.dma("sp", ye_d[row0 + ti * 128:row0 + (ti + 1) * 128, :], ys_[:], r=[ys_.r()], w=[], sem="o")

                for n in range(NTOT):
                    if n % NBLK == 0:
                        load_w(n // NBLK)
                    emit_T(n)
                    emit_GL(n, 0)
                    emit_GL(n, 1)
                    if n > 0:
                        emit_DOWN(n - 1)
                    for fc in range(2, 8):
                        emit_GL(n, fc)
                emit_DOWN(NTOT - 1)
            p.barrier()
            with ExitStack() as L:
                yk = [[sb(L, "yk%d_%d" % (i, k), [128, D]) for k in range(4)] for i in range(2)]
                acc = [sb(L, "acc%d" % i, [128, D]) for i in range(2)]
                xe1 = [sb(L, "xe1%d" % i, [128, D]) for i in range(2)]
                jk = sb(L, "jkF", [128, D], BF16)
                st = [sb(L, "stF%d" % i, [128, 4]) for i in range(2)]
                Gf = sb(L, "GfF", [128, D])
                for gt in range(nb * NT):
                    b, t = divmod(gt, NT)
                    i = gt % 2
                    if t == 0:
                        p.dma("sp", Gf[:], gf_d[b], r=[gfres[b]], w=[Gf.r()], sem="x")
                    for k in range(4):
                        p.idma(out=yk[i][k][:], out_offset=None, in_=ye_d,
                               in_offset=bass.IndirectOffsetOnAxis(ap=RIi[:, gt, k:k + 1], axis=0), bounds=NE * CAP - 1,
                               r=[RIi.r(gt)], w=[yk[i][k].r()], sem="ga")
                    p.dma("sp", xe1[i][:], out_d[b, t * 128:(t + 1) * 128, :], r=[x1res[b][t]], w=[xe1[i].r()], sem="x")
                    a_ = acc[i]
                    p.op("dve", lambda e, a_=a_, i=i, gt=gt: e.tensor_scalar_mul(out=a_[:], in0=yk[i][0][:], scalar1=RIw[:, gt, 0:1]),
                         r=[yk[i][0].r(), RIw.r(gt)], w=[a_.r()])
                    for k in range(1, 4):
                        p.op("dve", lambda e, a_=a_, i=i, gt=gt, k=k: e.scalar_tensor_tensor(
                            out=a_[:], in0=yk[i][k][:], scalar=RIw[:, gt, k:k + 1], in1=a_[:], op0=ALU.mult, op1=ALU.add),
                            r=[yk[i][k].r(), RIw.r(gt), a_.r()], w=[a_.r()])
                    if b == 0 and "ffn" in dbg_d:
                        dump("ffn", a_[:], a_.r(), dst=dbg_d["ffn"][t])
                    rstd_of(a_[:], [a_.r()], jk, st[i])
                    p.op("dve", lambda e, a_=a_, i=i: e.scalar_tensor_tensor(
                        out=a_[:], in0=a_[:], scalar=st[i][:, 1:2], in1=Gf[:], op0=ALU.mult, op1=ALU.mult),
                        r=[a_.r(), st[i].r(), Gf.r()], w=[a_.r()])
                    p.op("dve", lambda e, a_=a_, i=i: e.tensor_tensor(out=xe1[i][:], in0=xe1[i][:], in1=a_[:], op=ALU.add),
                         r=[xe1[i].r(), a_.r()], w=[xe1[i].r()])
                    p.dma("sp", out_d[b, t * 128:(t + 1) * 128, :], xe1[i][:], r=[xe1[i].r()], w=[x1res[b][t]], sem="o")
            p.barrier()

        p.barrier()
    return nc, p


def _host_consts():
    c = {}
    c["ident"] = np.eye(128, dtype=np.float32)
    j = np.arange(128)
    c["trif"] = (j[:, None] <= j[None, :]).astype(np.float32)
    c["trib"] = (j[:, None] >= j[None, :]).astype(np.float32)
    c["tris"] = (j[:, None] < j[None, :]).astype(np.float32)
    c["ecap"] = np.ascontiguousarray(np.broadcast_to((np.arange(NE) * CAP).astype(np.float32)[None, :], (128, NE)))
    t = np.arange(S)
    row = (t // 64).astype(np.float32)
    col = (t % 64).astype(np.float32)
    inv_freq = (np.float32(10000.0) ** (-np.arange(16, dtype=np.float32) / np.float32(16))).astype(np.float32)
    cos = np.zeros((128, S), np.float32)
    sin = np.zeros((128, S), np.float32)
    for pp in range(128):
        d = pp % 64
        pos = row if d < 32 else col
        dd = d % 32
        f = dd % 16
        sign = -1.0 if dd < 16 else 1.0
        ang = (pos * inv_freq[f]).astype(np.float32)
        cos[pp] = np.cos(ang)
        sin[pp] = sign * np.sin(ang)
    c["ropecos"] = cos
    c["ropesin"] = sin
    return c


def _na_bias_table(rpb):
    reps = [0, 1, 5, 14, 15]
    kk = np.arange(128)
    krl = kk // 64
    kc = kk % 64
    qq = np.arange(128)
    qrl = qq // 64
    qc = qq % 64
    cs = np.clip(qc - 8, 0, 48)
    tab = np.full((8, 128, 5, 5, 128), NEG, np.float32)
    for ci, i in enumerate(reps):
        js = int(np.clip(i - 2, 0, 11))
        for s in range(5):
            j = js + s
            kr = 2 * j + krl
            r = 2 * i + qrl
            rs = np.clip(r - 4, 0, 24)
            vr = (kr[:, None] >= rs[None, :]) & (kr[:, None] < rs[None, :] + 8)
            vc = (kc[:, None] >= cs[None, :]) & (kc[:, None] < cs[None, :] + 16)
            valid = vr & vc
            dr = np.clip(kr[:, None] - r[None, :] + 7, 0, 14)
            dc = np.clip(kc[:, None] - qc[None, :] + 15, 0, 30)
            vals = rpb[:, dr, dc]
            tab[:, :, ci, s, :] = np.where(valid[None], vals, np.float32(NEG))
    return np.ascontiguousarray(tab.reshape(8, 128, 5 * 640))


def _prep_inputs(inp, nb=NB, ncores=8):
    f = lambda a: np.ascontiguousarray(np.asarray(a, dtype=np.float32))
    consts = _host_consts()
    w_in = f(inp["w_in"][0])
    d = np.arange(64)
    partner = np.where((d % 32) < 16, d + 16, d - 16)
    permq = np.concatenate([C_MLQ + h * 64 + partner for h in range(4)])
    permk = np.concatenate([C_MLK + h * 64 + partner for h in range(4)])
    w_qkp = np.ascontiguousarray(np.concatenate([w_in[:, permq], w_in[:, permk]], axis=1))
    shared = dict(
        w_ada=f(inp["w_ada"][0]), b_ada=f(inp["b_ada"][0]).reshape(1, -1),
        g4=np.ascontiguousarray(np.stack([f(inp["g_mix_pre"][0]), f(inp["g_mix_post"][0]),
                                          f(inp["g_ffn_pre"][0]), f(inp["g_ffn_post"][0])])),
        w_in=w_in, w_qkp=w_qkp, b_mg=f(inp["b_mlstm_gates"][0]).reshape(1, 16),
        nab=_na_bias_table(f(inp["rpb"][0])), g_head=f(inp["g_mlstm_head"][0]).reshape(1, 512),
        w_bna=f(inp["w_branch_na"][0]), w_bml=f(inp["w_branch_ml"][0]), w_out=f(inp["w_out"][0]),
        w_router=f(inp["w_router"][0]), b_router=f(inp["b_router"][0]).reshape(1, NE),
        w_gate=f(inp["w_gate"][0]), w_lin=f(inp["w_lin"][0]), w_down=f(inp["w_down"][0]),
        bgT=np.ascontiguousarray(f(inp["b_gate"][0]).reshape(NE, 8, 128).transpose(2, 1, 0)),
        blT=np.ascontiguousarray(f(inp["b_lin"][0]).reshape(NE, 8, 128).transpose(2, 1, 0)),
        b_down=f(inp["b_down"][0]), **consts)
    x = f(inp["x"]); ctx = f(inp["ctx"]); c = f(inp["c"]); cc = f(inp["c_ctx"])
    maps = []
    for k in range(ncores):
        sl = slice(k * nb, (k + 1) * nb)
        c5 = np.zeros((5, D), np.float32)
        c5[:nb] = c[sl]
        c5[4] = cc
        cT = np.ascontiguousarray(c5.reshape(5, 8, 128).transpose(2, 1, 0))
        m = dict(shared)
        m.update(x=np.ascontiguousarray(x[sl]), ctx=np.ascontiguousarray(ctx[sl]), cT=cT)
        maps.append(m)
    return maps


def kernel(**inputs):
    maps = _prep_inputs(inputs)
    nc, _ = build()
    res = run_bass_kernel_spmd(nc, maps, core_ids=list(range(8)))
    return np.concatenate([r["out"] for r in res.results], axis=0).astype(np.float32)
```

```python
import numpy as np
from contextlib import ExitStack
import concourse.bass as bass
import concourse.mybir as mybir
from concourse.bass_utils import run_bass_kernel_spmd

F32 = mybir.dt.float32
BF16 = mybir.dt.bfloat16
AF = mybir.ActivationFunctionType
ALU = mybir.AluOpType
AX = mybir.AxisListType

D = 1024
S = 2048
CTX = 256
NB = 4
NT = 16
NTT = 18
NE = 32
EPS = 1e-6
NEG = -80.0
CAP = 2048
I32 = mybir.dt.int32
POOLENG = "dve"
N_IN = 5136
C_NAK, C_NAV, C_MLK, C_MLV, C_MLG = 0, 512, 1024, 1280, 1792
C_NAQ, C_MLQ, C_MLO, C_GNA, C_GML = 1808, 2320, 2576, 3088, 4112


class Res:
    __slots__ = ("lw", "rd")

    def __init__(self):
        self.lw = None
        self.rd = {}


class Prog:
    ENG = ["pe", "act", "dve", "pool", "sp"]

    def __init__(self, nc):
        self.nc = nc
        self.e = dict(pe=nc.tensor, act=nc.scalar, dve=nc.vector, pool=nc.gpsimd, sp=nc.sync)
        self.sem = {k: nc.alloc_semaphore("s_" + k) for k in self.ENG}
        self.cnt = {k: 0 for k in self.ENG}
        self.waited = {k: {} for k in self.ENG}
        self.n_ins = 0

    NDS = 8
    rr = None

    def dsem(self, name):
        if self.rr is None:
            self.rr = {}
        i = self.rr.get(name, 0)
        self.rr[name] = i + 1
        k = "d:%s:%d" % (name, i % self.NDS)
        if k not in self.sem:
            self.sem[k] = self.nc.alloc_semaphore("sd_%s_%d" % (name, i % self.NDS))
            self.cnt[k] = 0
        return k

    def _wait(self, eng, key, val):
        if self.waited[eng].get(key, 0) >= val:
            return
        self.e[eng].wait_ge(self.sem[key], val)
        self.waited[eng][key] = val
        self.n_ins += 1

    def _sync(self, eng, me, r, w):
        for x in r:
            if x.lw is not None:
                k, v = x.lw
                if k == me and me == "pe":
                    continue
                self._wait(eng, k, v)
        for x in w:
            if x.lw is not None:
                k, v = x.lw
                if k != me or me != "pe":
                    self._wait(eng, k, v)
            for k, v in x.rd.items():
                if k != me or me != "pe":
                    self._wait(eng, k, v)

    def _commit(self, me, val, r, w):
        for x in r:
            if x.rd.get(me, 0) < val:
                x.rd[me] = val
        for x in w:
            x.lw = (me, val)
            x.rd = {}

    max_ops = None
    tot = 0

    def op(self, eng, fn, r=(), w=()):
        self.tot += 1
        if self.max_ops is not None and self.tot > self.max_ops:
            return None
        self._sync(eng, eng, r, w)
        ins = fn(self.e[eng])
        self.cnt[eng] += 1
        ins.then_inc(self.sem[eng], 1)
        self._commit(eng, self.cnt[eng], r, w)
        self.n_ins += 1
        return ins

    def dma(self, q, out, in_, r=(), w=(), sem="ld"):
        self.tot += 1
        if self.max_ops is not None and self.tot > self.max_ops:
            return None
        k = self.dsem(sem)
        if self.cnt[k] > 0:
            self._wait(q, k, self.cnt[k])
        self._sync(q, k, r, w)
        ins = self.e[q].dma_start(out=out, in_=in_)
        self.cnt[k] += 16
        ins.then_inc(self.sem[k], 16)
        self._commit(k, self.cnt[k], r, w)
        self.n_ins += 1
        return ins

    def idma(self, out, out_offset, in_, in_offset, bounds, r=(), w=(), sem="ind"):
        self.tot += 1
        if self.max_ops is not None and self.tot > self.max_ops:
            return None
        k = self.dsem(sem)
        if self.cnt[k] > 0:
            self._wait("pool", k, self.cnt[k])
        self._sync("pool", k, r, w)
        ins = self.e["pool"].indirect_dma_start(out=out, out_offset=out_offset, in_=in_, in_offset=in_offset)
        self.cnt[k] += 16
        ins.then_inc(self.sem[k], 16)
        self._commit(k, self.cnt[k], r, w)
        self.n_ins += 1
        return ins

    def barrier(self, engs=None):
        for eng in (engs or self.ENG):
            for k, v in self.cnt.items():
                if k != eng and v > 0:
                    self._wait(eng, k, v)


class T:
    def __init__(self, h, nres=1):
        self.h = h
        self.res = [Res() for _ in range(nres)]

    def __getitem__(self, k):
        return self.h[k]

    def r(self, i=0):
        return self.res[i]


def build(nb=NB, stop=None, dbg=(), max_ops=None):
    nc = bass.Bass("TRN2", target_bir_lowering=False)
    p = Prog(nc)
    p.max_ops = max_ops

    def din(name, shape, dt=F32):
        return nc.dram_tensor(name, list(shape), dt, kind="ExternalInput").ap()

    x_d = din("x", [nb, S, D])
    ctx_d = din("ctx", [nb, CTX, D])
    cT_d = din("cT", [128, 8, 5])
    w_ada_d = din("w_ada", [D, 6 * D])
    b_ada_d = din("b_ada", [1, 6 * D])
    g4_d = din("g4", [4, D])
    w_in_d = din("w_in", [D, N_IN])
    w_qkp_d = din("w_qkp", [D, 512])
    b_mg_d = din("b_mg", [1, 16])
    nab_d = din("nab", [8, 128, 5 * 640])
    g_head_d = din("g_head", [1, 512])
    w_bna_d = din("w_bna", [512, D])
    w_bml_d = din("w_bml", [512, D])
    w_out_d = din("w_out", [D, D])
    w_r_d = din("w_router", [D, NE])
    b_r_d = din("b_router", [1, NE])
    w_gate_d = din("w_gate", [NE, D, D])
    w_lin_d = din("w_lin", [NE, D, D])
    w_down_d = din("w_down", [NE, D, D])
    bgT_d = din("bgT", [128, 8, NE])
    blT_d = din("blT", [128, 8, NE])
    b_down_d = din("b_down", [NE, D])
    ident_d = din("ident", [128, 128])
    trif_d = din("trif", [128, 128])
    trib_d = din("trib", [128, 128])
    cos_d = din("ropecos", [128, S])
    sin_d = din("ropesin", [128, S])
    tris_d = din("tris", [128, 128])
    ecap_d = din("ecap", [128, NE])
    xe_d = nc.dram_tensor("xe_scr", [NE * CAP, D], BF16).ap()
    ye_d = nc.dram_tensor("ye_scr", [NE * CAP, D], F32).ap()
    gf_d = nc.dram_tensor("gf_scr", [nb, 128, D], F32).ap()
    out_d = nc.dram_tensor("out", [nb, S, D], F32, kind="ExternalOutput").ap()
    dbg_d = {}
    for name, shape in dbg:
        dbg_d[name] = nc.dram_tensor("dbg_" + name, list(shape), F32, kind="ExternalOutput").ap()

    uid = [0]

    def sb(es, name, shape, dt=F32, nres=1):
        uid[0] += 1
        return T(es.enter_context(nc.sbuf_tensor("sb%d_%s" % (uid[0], name), list(shape), dt)), nres)

    PD = [T(nc.alloc_psum_tensor("pd%d" % i, [128, 1024], F32)) for i in range(3)]
    PS = [T(nc.alloc_psum_tensor("psg%d" % i, [128, 512], F32)) for i in range(2)]

    def dump(name, tile_ap, res, dst=None):
        if name in dbg_d:
            p.dma("pool", dbg_d[name] if dst is None else dst, tile_ap, r=(res if isinstance(res, list) else [res]), sem="dbg")

    def wview(ap2d):
        return ap2d.rearrange("(kc p) n -> p kc n", p=128)

    def bc(ap, shape):
        return ap.to_broadcast(list(shape))

    rot = [0, 0]

    def nPD():
        rot[0] += 1
        return PD[rot[0] % 3]

    def nPS():
        rot[1] += 1
        return PS[rot[1] % 2]

    with ExitStack() as G:
        ident = sb(G, "ident", [128, 128])
        identb = sb(G, "identb", [128, 128], BF16)
        trif = sb(G, "trif", [128, 128])
        trib = sb(G, "trib", [128, 128])
        ones32 = sb(G, "ones32", [128, 128])
        scT = sb(G, "scT", [128, 8, 5])
        A_c = sb(G, "A_c", [128, D])
        sh_c = sb(G, "sh_c", [128, D])
        trisb = sb(G, "trisb", [128, 128], BF16)
        onesb = sb(G, "onesb", [128, 128], BF16)
        ecap = sb(G, "ecap", [128, NE])
        msum = sb(G, "msum", [128, NE], BF16)
        RIi = sb(G, "RIi", [128, nb * NT, 4], I32, nres=nb * NT)
        RIw = sb(G, "RIw", [128, nb * NT, 4], F32, nres=nb * NT)
        zres = Res()
        gfres = [Res() for _ in range(nb)]
        p.dma("sp", ecap[:], ecap_d, w=[ecap.r()])
        p.op("dve", lambda e: e.memset(onesb[:], 1.0), w=[onesb.r()])
        p.op("dve", lambda e: e.memset(msum[:], 0.0), w=[msum.r()])
        ztile = sb(G, "ztile", [128, 1024], BF16)
        p.op("dve", lambda e: e.memset(ztile[:], 0.0), w=[ztile.r()])
        p.dma("pool", trisb[:], tris_d, w=[trisb.r()], sem="w")
        zdone = [0]
        NZ = NE * CAP // 128

        def emit_zero(k):
            for _ in range(k):
                cz = zdone[0]
                if cz >= NZ:
                    return
                zdone[0] += 1
                p.dma("sp", xe_d[cz * 128:(cz + 1) * 128, :], ztile[:],
                      r=[ztile.r()], w=[], sem="z")

        def wait_zero():
            emit_zero(NZ)
            for k_, v_ in p.cnt.items():
                if k_.startswith("d:z:") and v_ > 0:
                    p._wait("pool", k_, v_)
        p.dma("sp", ident[:], ident_d, w=[ident.r()])
        p.dma("sp", trif[:], trif_d, w=[trif.r()])
        p.dma("sp", trib[:], trib_d, w=[trib.r()])
        p.dma("sp", scT[:], cT_d, w=[scT.r()])
        p.op("dve", lambda e: e.tensor_copy(out=identb[:], in_=ident[:]), r=[ident.r()], w=[identb.r()])
        p.op("dve", lambda e: e.memset(ones32[:], 1.0), w=[ones32.r()])
        p.op("act", lambda e: e.activation(out=scT[:], in_=scT[:], func=AF.Silu), r=[scT.r()], w=[scT.r()])

        def rstd_of(src_ap, src_res, junk, st):
            p.op("act", lambda e: e.activation(out=junk[:], in_=src_ap, func=AF.Square, scale=1.0 / 32.0,
                                               accum_out=st[:, 0:1]), r=src_res, w=[junk.r(), st.r()])
            p.op("act", lambda e: e.activation(out=st[:, 1:2], in_=st[:, 0:1], func=AF.Sqrt, bias=EPS),
                 r=[st.r()], w=[st.r()])
            p.op("dve", lambda e: e.reciprocal(out=st[:, 1:2], in_=st[:, 1:2]), r=[st.r()], w=[st.r()])

        def ada_mod(j, pieces, tag):
            with ExitStack() as L:
                lh = sb(L, "lh" + tag, [128, 8, 128], BF16)
                g4 = sb(L, "g4" + tag, [128, 4, D])
                p.dma("sp", g4[:], g4_d.partition_broadcast(128), w=[g4.r()])
                for kc in range(8):
                    p.op("dve", lambda e, kc=kc: e.tensor_scalar_mul(
                        out=lh[:, kc, :], in0=ones32[:], scalar1=scT[:, kc, j:j + 1]),
                        r=[scT.r(), ones32.r()], w=[lh.r()])
                wa = [sb(L, "wa%d%s" % (i, tag), [128, 8, 512], BF16) for i in range(2)]
                ba = [sb(L, "ba%d%s" % (i, tag), [1, 512]) for i in range(2)]
                n = 0
                for (blk, out_t, kind, gi) in pieces:
                    for half in range(2):
                        c0 = blk * D + half * 512
                        wt = wa[n % 2]
                        bt = ba[n % 2]
                        n += 1
                        p.dma("pool", wt[:], wview(w_ada_d[:, c0:c0 + 512]), w=[wt.r()], sem="w")
                        p.dma("sp", bt[:], b_ada_d[:, c0:c0 + 512], w=[bt.r()], sem="x")
                        ps = nPD()
                        for kc in range(8):
                            p.op("pe", lambda e, kc=kc, ps=ps, wt=wt: e.matmul(
                                ps[:, 0:512], lhsT=lh[:, kc, :], rhs=wt[:, kc, :], start=(kc == 0), stop=(kc == 7)),
                                r=[lh.r(), wt.r()], w=[ps.r()])
                        o = out_t[:, half * 512:(half + 1) * 512]
                        tmp = sb(L, "adatmp%d%s" % (n, tag), [128, 512])
                        ps2 = nPS()
                        p.op("pe", lambda e, ps2=ps2, bt=bt: e.matmul(
                            ps2[:], lhsT=ones32[0:1, :], rhs=bt[0:1, :], start=True, stop=True),
                            r=[ones32.r(), bt.r()], w=[ps2.r()])
                        p.op("act", lambda e, tmp=tmp, ps2=ps2: e.copy(out=tmp[:], in_=ps2[:]), r=[ps2.r()], w=[tmp.r()])
                        p.op("dve", lambda e, tmp=tmp, ps=ps: e.tensor_tensor(out=tmp[:], in0=ps[:, 0:512], in1=tmp[:], op=ALU.add),
                             r=[ps.r(), tmp.r()], w=[tmp.r()])
                        if kind == "shift":
                            p.op("act", lambda e, o=o, tmp=tmp: e.copy(out=o, in_=tmp[:]), r=[tmp.r()], w=[out_t.r()])
                        elif kind == "scale":
                            gs = g4[:, gi, half * 512:(half + 1) * 512]
                            p.op("dve", lambda e, o=o, tmp=tmp, gs=gs: e.scalar_tensor_tensor(
                                out=o, in0=tmp[:], scalar=1.0, in1=gs, op0=ALU.add, op1=ALU.mult),
                                r=[tmp.r(), g4.r()], w=[out_t.r()])
                        else:
                            gs = g4[:, gi, half * 512:(half + 1) * 512]
                            p.op("dve", lambda e, o=o, tmp=tmp, gs=gs: e.tensor_tensor(
                                out=o, in0=tmp[:], in1=gs, op=ALU.mult),
                                r=[tmp.r(), g4.r()], w=[out_t.r()])
            p.barrier()

        ada_mod(4, [(0, sh_c, "shift", 0), (1, A_c, "scale", 0)], "c")
        x1res = [[Res() for _ in range(NT)] for _ in range(nb)]

        for b in range(nb):
          with ExitStack() as B:
            hT = sb(B, "hT", [128, 8, NTT * 128], BF16, nres=NTT)
            with ExitStack() as M:
              G_m = sb(M, "G_m", [128, D]); A_f = sb(M, "A_f", [128, D]); sh_f = sb(M, "sh_f", [128, D])
              with ExitStack() as PA:
                A_m = sb(PA, "A_m", [128, D]); sh_m = sb(PA, "sh_m", [128, D]); G_f = sb(PA, "G_f", [128, D])
                ada_mod(b, [(0, sh_m, "shift", 0), (1, A_m, "scale", 0), (2, G_m, "gate", 1),
                            (3, sh_f, "shift", 0), (4, A_f, "scale", 2), (5, G_f, "gate", 3)], "b")
                p.dma("sp", gf_d[b], G_f[:], r=[G_f.r()], w=[gfres[b]], sem="o")
                if b == 0:
                    dump("A_m", A_m[:], A_m.r()); dump("sh_m", sh_m[:], sh_m.r()); dump("G_f", G_f[:], G_f.r())
                    dump("A_c", A_c[:], A_c.r())
                with ExitStack() as L:
                    xt = [sb(L, "xt%d" % i, [128, D]) for i in range(3)]
                    sq = [sb(L, "sq%d" % i, [128, D]) for i in range(3)]
                    hb = [sb(L, "hb%d" % i, [128, D], BF16) for i in range(3)]
                    st = [sb(L, "st%d" % i, [128, 2]) for i in range(3)]

                    def a_stage1(t):
                        i = t % 3
                        src = ctx_d[b, t * 128:(t + 1) * 128, :] if t < 2 else x_d[b, (t - 2) * 128:(t - 1) * 128, :]
                        Am, shm = (A_c, sh_c) if t < 2 else (A_m, sh_m)
                        p.dma("sp", xt[i][:], src, w=[xt[i].r()], sem="x")
                        rstd_of(xt[i][:], [xt[i].r()], sq[i], st[i])
                        p.op("dve", lambda e: e.scalar_tensor_tensor(
                            out=sq[i][:], in0=xt[i][:], scalar=st[i][:, 1:2], in1=Am[:], op0=ALU.mult, op1=ALU.mult),
                            r=[xt[i].r(), st[i].r(), Am.r()], w=[sq[i].r()])
                        p.op("dve", lambda e: e.tensor_tensor(out=hb[i][:], in0=sq[i][:], in1=shm[:], op=ALU.add),
                             r=[sq[i].r(), shm.r()], w=[hb[i].r()])

                    def a_stage2(t):
                        i = t % 3
                        ps = nPS()
                        psb = ps[:].bitcast(BF16)
                        for kc in range(8):
                            p.op("pe", lambda e, kc=kc: e.transpose(
                                psb[:, kc * 128:(kc + 1) * 128], hb[i][:, kc * 128:(kc + 1) * 128], identb[:]),
                                r=[hb[i].r(), identb.r()], w=[ps.r()])
                        p.op("act", lambda e: e.copy(
                            out=hT[:, :, t * 128:(t + 1) * 128], in_=psb.rearrange("p (k n) -> p k n", k=8)),
                            r=[ps.r()], w=[hT.r(t)])

                    for t in range(NTT + 1):
                        if t < NTT:
                            a_stage1(t)
                        if t >= 1:
                            a_stage2(t - 1)
                if b == 0 and "hT" in dbg_d:
                    dump("hT", hT[:], hT.res)
                p.barrier()
              if True:
                if stop == "A":
                    break
                if b == 0:
                    emit_zero(128)
                onaT = sb(M, "onaT", [128, 4, S], BF16, nres=NT)

                def mm_fm(ps_ap, ps_res, w_t, wc0, tok0, ntok, nk=8, src=None):
                    src = src or hT
                    tiles = range(tok0 // 128, (tok0 + ntok) // 128)
                    for kc in range(nk):
                        p.op("pe", lambda e, kc=kc: e.matmul(
                            ps_ap, lhsT=w_t[:, kc, wc0:wc0 + 128], rhs=src[:, kc, tok0:tok0 + ntok],
                            start=(kc == 0), stop=(kc == nk - 1)),
                            r=[w_t.r()] + [src.r(t) for t in tiles], w=[ps_res])

                def mm_tm(ps_ap, ps_res, w_t, wc0, ncols, t):
                    for kc in range(8):
                        p.op("pe", lambda e, kc=kc: e.matmul(
                            ps_ap, lhsT=hT[:, kc, t * 128:(t + 1) * 128], rhs=w_t[:, kc, wc0:wc0 + ncols],
                            start=(kc == 0), stop=(kc == 7)),
                            r=[w_t.r(), hT.r(t)], w=[ps_res])

                with ExitStack() as L:
                  qT = sb(L, "qT", [128, 4, S], BF16, nres=4)
                  kT = sb(L, "kT", [128, 4, NTT * 128], BF16, nres=4)
                  vA = sb(L, "vA", [128, NTT, 8, 65], BF16, nres=NTT)
                  p.op("dve", lambda e: e.memset(vA[:, :, :, 64:65], 1.0), w=vA.res)
                  with ExitStack() as L2:
                    wna = sb(L2, "wna", [128, 8, 1536], BF16)
                    p.dma("pool", wna[:, :, 0:1024], wview(w_in_d[:, 0:1024]), w=[wna.r()], sem="w")
                    p.dma("pool", wna[:, :, 1024:1536], wview(w_in_d[:, C_NAQ:C_NAQ + 512]), w=[wna.r()], sem="w")
                    for c in range(4):
                        for tb in range(4):
                            ps = nPD()
                            mm_fm(ps[:, 0:512], ps.r(), wna, 1024 + c * 128, 256 + tb * 512, 512)
                            p.op("act", lambda e, c=c, tb=tb, ps=ps: e.activation(
                                out=qT[:, c, tb * 512:(tb + 1) * 512], in_=ps[:, 0:512], func=AF.Copy, scale=0.125),
                                r=[ps.r()], w=[qT.r(c)])
                        for (t0, n) in [(0, 512), (512, 512), (1024, 512), (1536, 512), (2048, 256)]:
                            ps = nPD()
                            mm_fm(ps[:, 0:n], ps.r(), wna, c * 128, t0, n)
                            p.op("dve", lambda e, c=c, t0=t0, n=n, ps=ps: e.tensor_copy(
                                out=kT[:, c, t0:t0 + n], in_=ps[:, 0:n]), r=[ps.r()], w=[kT.r(c)])
                    for t in range(NTT):
                        ps = nPD()
                        mm_tm(ps[:, 0:512], ps.r(), wna, 512, 512, t)
                        eng = "act" if t % 2 else "dve"
                        if eng == "act":
                            p.op("act", lambda e, t=t, ps=ps: e.copy(
                                out=vA[:, t, :, 0:64], in_=ps[:, 0:512].rearrange("p (h d) -> p h d", h=8)),
                                r=[ps.r()], w=[vA.r(t)])
                        else:
                            p.op("dve", lambda e, t=t, ps=ps: e.tensor_copy(
                                out=vA[:, t, :, 0:64], in_=ps[:, 0:512].rearrange("p (h d) -> p h d", h=8)),
                                r=[ps.r()], w=[vA.r(t)])
                  p.barrier()
                  if True:
                    EBh = [sb(L, "EB%d" % i, [128, 3200], BF16) for i in range(2)]
                    ona = sb(L, "ona", [128, NT, 512], BF16, nres=NT)
                    stg = sb(L, "nabst", [128, 3200])
                    PT = [sb(L, "PT%d" % i, [128, 896], BF16) for i in range(3)]
                    rc = [sb(L, "rc%d" % i, [128, 1]) for i in range(3)]
                    n = 0
                    items = [(h, i) for h in range(8) for i in range(NT)]
                    state = {}

                    def na_scores(n):
                        h, i = items[n]
                        hp = (h % 2) * 64
                        c = h // 2
                        EB = EBh[h % 2]
                        if i == 0:
                            p.dma("sp", stg[:], nab_d[h], w=[stg.r()], sem="x")
                            p.op("act", lambda e, EB=EB: e.activation(out=EB[:], in_=stg[:], func=AF.Exp),
                                 r=[stg.r()], w=[EB.r()])
                            if b == 0 and h >= 1:
                                emit_zero(24)
                        js = min(max(i - 2, 0), 11)
                        cls = i - js if i < 2 or i > 13 else 2
                        pss = PD[n % 3]
                        pt = PT[n % 3]
                        qa = qT[hp:hp + 64, c, i * 128:(i + 1) * 128]
                        for s_i in range(7):
                            kt = (2 + js + s_i) if s_i < 5 else (s_i - 5)
                            p.op("pe", lambda e, s_i=s_i, kt=kt: e.matmul(
                                pss[:, s_i * 128:(s_i + 1) * 128], lhsT=kT[hp:hp + 64, c, kt * 128:(kt + 1) * 128],
                                rhs=qa, start=True, stop=True),
                                r=[kT.r(c), qT.r(c)], w=[pss.r()])
                        p.op("act", lambda e: e.activation(out=pt[:], in_=pss[:, 0:896], func=AF.Exp),
                             r=[pss.r()], w=[pt.r()])
                        p.op("dve", lambda e: e.tensor_tensor(
                            out=pt[:, 0:640], in0=pt[:, 0:640], in1=EB[:, cls * 640:(cls + 1) * 640], op=ALU.mult),
                            r=[pt.r(), EB.r()], w=[pt.r()])
                        state[n] = (pt, js)

                    def na_pv(n):
                        h, i = items[n]
                        pt, js = state.pop(n)
                        pso = PS[n % 2]
                        rcc = rc[n % 3]
                        for s_i in range(7):
                            kt = (2 + js + s_i) if s_i < 5 else (s_i - 5)
                            p.op("pe", lambda e, s_i=s_i, kt=kt: e.matmul(
                                pso[:, 0:65], lhsT=pt[:, s_i * 128:(s_i + 1) * 128], rhs=vA[:, kt, h, :],
                                start=(s_i == 0), stop=(s_i == 6)),
                                r=[pt.r(), vA.r(kt)], w=[pso.r()])
                        p.op("dve", lambda e: e.reciprocal(out=rcc[:], in_=pso[:, 64:65]),
                             r=[pso.r()], w=[rcc.r()])
                        p.op("dve", lambda e: e.tensor_scalar_mul(
                            out=ona[:, i, h * 64:(h + 1) * 64], in0=pso[:, 0:64], scalar1=rcc[:, 0:1]),
                            r=[pso.r(), rcc.r()], w=[ona.r(i)])

                    for n in range(len(items) + 1):
                        if n < len(items):
                            na_scores(n)
                        if n >= 1:
                            na_pv(n - 1)
                    for i in range(NT):
                        ps = nPS()
                        psb = ps[:].bitcast(BF16)
                        for c4 in range(4):
                            p.op("pe", lambda e, c4=c4, i=i, psb=psb: e.transpose(
                                psb[:, c4 * 128:(c4 + 1) * 128], ona[:, i, c4 * 128:(c4 + 1) * 128], identb[:]),
                                r=[ona.r(i), identb.r()], w=[ps.r()])
                        p.op("act", lambda e, i=i, psb=psb: e.copy(
                            out=onaT[:, :, i * 128:(i + 1) * 128], in_=psb[:, 0:512].rearrange("p (k n) -> p k n", k=4)),
                            r=[ps.r()], w=[onaT.r(i)])
                    if b == 0 and "ona" in dbg_d:
                        dump("ona", ona[:], ona.res)
                p.barrier()
                if stop == "B":
                    break
                omlT = sb(M, "omlT", [128, 4, S], BF16, nres=NT)

                with ExitStack() as L:
                    mqT = sb(L, "mqT", [128, 2, S], BF16, nres=2)
                    mkT = sb(L, "mkT", [128, 2, S], BF16, nres=2)
                    ktm = sb(L, "ktm", [128, NTT, 256], BF16, nres=NTT)
                    vM = sb(L, "vM", [128, NTT, 4, 129], BF16, nres=NTT)
                    osig = sb(L, "osig", [128, NT, 512], BF16, nres=NT)
                    gts = sb(L, "gts", [128, NTT, 16])
                    LI = sb(L, "LI", [128, NTT, 8])
                    LFn = sb(L, "LFn", [128, NTT, 8])
                    Bn = sb(L, "Bn", [128, NTT, 8])
                    EBt = sb(L, "EBt", [128, NTT, 8])
                    ES = sb(L, "ES", [128, NTT, 8])
                    EBL = sb(L, "EBL", [128, NTT, 8])
                    ghd = sb(L, "ghd", [128, 512])
                    p.dma("sp", ghd[:], g_head_d.partition_broadcast(128), w=[ghd.r()], sem="x")
                    p.op("dve", lambda e: e.memset(vM[:, :, :, 128:129], 1.0), w=vM.res)
                    with ExitStack() as L2:
                        wq = sb(L2, "wq", [128, 8, 256], BF16); wqp = sb(L2, "wqp", [128, 8, 256], BF16)
                        wk = sb(L2, "wk", [128, 8, 256], BF16); wkp = sb(L2, "wkp", [128, 8, 256], BF16)
                        cosT = sb(L2, "cosT", [128, 512]); sinT = sb(L2, "sinT", [128, 512])
                        rt = [sb(L2, "rt%d" % i, [128, 512]) for i in range(2)]
                        p.dma("pool", wq[:], wview(w_in_d[:, C_MLQ:C_MLQ + 256]), w=[wq.r()], sem="w")
                        p.dma("pool", wqp[:], wview(w_qkp_d[:, 0:256]), w=[wqp.r()], sem="w")
                        p.dma("pool", wk[:], wview(w_in_d[:, C_MLK:C_MLK + 256]), w=[wk.r()], sem="w")
                        p.dma("pool", wkp[:], wview(w_qkp_d[:, 256:512]), w=[wkp.r()], sem="w")
                        for (w_a, w_b, dst) in ((wq, wqp, mqT), (wk, wkp, mkT)):
                            for c in range(2):
                                for tb in range(4):
                                    ps = nPD()
                                    mm_fm(ps[:, 0:512], ps.r(), w_a, c * 128, 256 + tb * 512, 512)
                                    mm_fm(ps[:, 512:1024], ps.r(), w_b, c * 128, 256 + tb * 512, 512)
                                    cs_ = slice(tb * 512, (tb + 1) * 512)
                                    p.dma("sp", cosT[:], cos_d[:, cs_], w=[cosT.r()], sem="x")
                                    p.dma("sp", sinT[:], sin_d[:, cs_], w=[sinT.r()], sem="x")
                                    p.op("dve", lambda e, ps=ps, cs_=cs_: e.tensor_tensor(
                                        out=rt[0][:], in0=ps[:, 0:512], in1=cosT[:], op=ALU.mult),
                                        r=[ps.r(), cosT.r()], w=[rt[0].r()])
                                    p.op("dve", lambda e, ps=ps, cs_=cs_: e.tensor_tensor(
                                        out=rt[1][:], in0=ps[:, 512:1024], in1=sinT[:], op=ALU.mult),
                                        r=[ps.r(), sinT.r()], w=[rt[1].r()])
                                    p.op("dve", lambda e, dst=dst, c=c, cs_=cs_: e.tensor_tensor(
                                        out=dst[:, c, cs_], in0=rt[0][:], in1=rt[1][:], op=ALU.add),
                                        r=[rt[0].r(), rt[1].r()], w=[dst.r(c)])
                        for t in range(NT):
                            ps = nPS()
                            psb = ps[:].bitcast(BF16)
                            for c in range(2):
                                p.op("pe", lambda e, c=c, t=t, psb=psb: e.transpose(
                                    psb[:, c * 128:(c + 1) * 128], mkT[:, c, t * 128:(t + 1) * 128], identb[:]),
                                    r=[mkT.r(c), identb.r()], w=[ps.r()])
                            p.op("act", lambda e, t=t, psb=psb: e.copy(out=ktm[:, 2 + t, :], in_=psb[:, 0:256]),
                                 r=[ps.r()], w=[ktm.r(2 + t)])
                        for t in range(2):
                            ps = nPS()
                            mm_tm(ps[:, 0:256], ps.r(), wk, 0, 256, t)
                            p.op("act", lambda e, t=t, ps=ps: e.copy(out=ktm[:, t, :], in_=ps[:, 0:256]),
                                 r=[ps.r()], w=[ktm.r(t)])
                    p.barrier()
                    if stop == "C1":
                        break
                    with ExitStack() as L2:
                        wv = sb(L2, "wv", [128, 8, 512], BF16); wo = sb(L2, "wo", [128, 8, 512], BF16)
                        wg = sb(L2, "wgt", [128, 8, 16], BF16)
                        bmg = sb(L2, "bmg", [1, 16])
                        p.dma("pool", wv[:], wview(w_in_d[:, C_MLV:C_MLV + 512]), w=[wv.r()], sem="w")
                        p.dma("pool", wo[:], wview(w_in_d[:, C_MLO:C_MLO + 512]), w=[wo.r()], sem="w")
                        p.dma("pool", wg[:], wview(w_in_d[:, C_MLG:C_MLG + 16]), w=[wg.r()], sem="w")
                        p.dma("sp", bmg[:], b_mg_d, w=[bmg.r()], sem="x")
                        for t in range(NTT):
                            ps = nPD()
                            mm_tm(ps[:, 0:512], ps.r(), wv, 0, 512, t)
                            p.op("dve", lambda e, t=t, ps=ps: e.tensor_copy(
                                out=vM[:, t, :, 0:128], in_=ps[:, 0:512].rearrange("p (h d) -> p h d", h=4)),
                                r=[ps.r()], w=[vM.r(t)])
                            if t >= 2:
                                mm_tm(ps[:, 512:1024], ps.r(), wo, 0, 512, t)
                                p.op("act", lambda e, t=t, ps=ps: e.activation(
                                    out=osig[:, t - 2, :], in_=ps[:, 512:1024], func=AF.Sigmoid),
                                    r=[ps.r()], w=[osig.r(t - 2)])
                            ps2 = nPS()
                            for kc in range(8):
                                p.op("pe", lambda e, kc=kc, t=t, ps2=ps2: e.matmul(
                                    ps2[:, 0:16], lhsT=hT[:, kc, t * 128:(t + 1) * 128], rhs=wg[:, kc, :],
                                    start=(kc == 0), stop=(kc == 7)), r=[wg.r(), hT.r(t)], w=[ps2.r()])
                            p.op("pe", lambda e, ps2=ps2: e.matmul(
                                ps2[:, 16:32], lhsT=ones32[0:1, :], rhs=bmg[0:1, :], start=True, stop=True),
                                r=[ones32.r(), bmg.r()], w=[ps2.r()])
                            p.op("act", lambda e, t=t, ps2=ps2: e.copy(out=gts[:, t, :], in_=ps2[:, 0:16]),
                                 r=[ps2.r()], w=[gts.r()])
                            p.op("dve", lambda e, t=t, ps2=ps2: e.tensor_tensor(
                                out=gts[:, t, :], in0=gts[:, t, :], in1=ps2[:, 16:32], op=ALU.add),
                                r=[ps2.r(), gts.r()], w=[gts.r()])
                    p.barrier()
                    if stop == "C2":
                        break
                    gv = gts[:].rearrange("p t (k h) -> p t k h", k=4)
                    p.op("act", lambda e: e.activation(out=gts[:], in_=gts[:], func=AF.Tanh, scale=1.0 / 15.0),
                         r=[gts.r()], w=[gts.r()])
                    for d_ in range(2):
                        p.op("dve", lambda e, d_=d_: e.tensor_scalar_mul(
                            out=LI[:, :, d_ * 4:(d_ + 1) * 4], in0=gv[:, :, 2 * d_, :], scalar1=15.0),
                            r=[gts.r()], w=[LI.r()])
                        p.op("act", lambda e, d_=d_: e.activation(
                            out=LFn[:, :, d_ * 4:(d_ + 1) * 4], in_=gv[:, :, 2 * d_ + 1, :], func=AF.Exp, scale=-15.0),
                            r=[gts.r()], w=[LFn.r()])
                    p.op("act", lambda e: e.activation(out=LFn[:], in_=LFn[:], func=AF.Ln, bias=1.0),
                         r=[LFn.r()], w=[LFn.r()])
                    psc = nPS()
                    for t in range(NTT):
                        for d_, tri in ((0, trif), (1, trib)):
                            p.op("pe", lambda e, t=t, d_=d_, tri=tri: e.matmul(
                                psc[:, t * 8 + d_ * 4:t * 8 + d_ * 4 + 4], lhsT=tri[:], rhs=LFn[:, t, d_ * 4:(d_ + 1) * 4],
                                start=True, stop=True), r=[tri.r(), LFn.r()], w=[psc.r()])
                    p.op("dve", lambda e: e.tensor_copy(out=Bn[:].rearrange("p t k -> p (t k)"), in_=psc[:, 0:NTT * 8]),
                         r=[psc.r()], w=[Bn.r()])
                    psl = nPS()
                    p.op("pe", lambda e: e.matmul(psl[:, 0:NTT * 8], lhsT=ones32[:], rhs=LFn[:].rearrange("p t k -> p (t k)"),
                                                  start=True, stop=True), r=[ones32.r(), LFn.r()], w=[psl.r()])
                    p.op("act", lambda e: e.activation(out=EBL[:].rearrange("p t k -> p (t k)"), in_=psl[:, 0:NTT * 8],
                                                       func=AF.Exp, scale=-1.0), r=[psl.r()], w=[EBL.r()])
                    p.op("act", lambda e: e.activation(out=EBt[:], in_=Bn[:], func=AF.Exp, scale=-1.0),
                         r=[Bn.r()], w=[EBt.r()])
                    p.op("dve", lambda e: e.tensor_tensor(out=ES[:], in0=LI[:], in1=Bn[:], op=ALU.add),
                         r=[LI.r(), Bn.r()], w=[ES.r()])
                    p.op("act", lambda e: e.activation(out=ES[:], in_=ES[:], func=AF.Exp, bias=float(-np.log(8.0))),
                         r=[ES.r()], w=[ES.r()])
                    if stop == "C3":
                        p.barrier()
                        break
                    if b == 0:
                        emit_zero(NZ)
                    Hf = sb(L, "Hf", [128, NT, 512], BF16, nres=NT)
                    Cst = [sb(L, "Cst%d" % d_, [128, 4, 129]) for d_ in range(2)]
                    Cbf = [sb(L, "Cbf%d" % d_, [128, 4, 129], BF16) for d_ in range(2)]
                    vp = [sb(L, "vp%d" % d_, [128, 4, 129], BF16) for d_ in range(2)]
                    sTm = [sb(L, "sTm%d" % d_, [128, 4, 128], BF16) for d_ in range(2)]
                    sm = [sb(L, "sm%d" % d_, [128, 4, 4]) for d_ in range(2)]
                    Hs = [sb(L, "Hs%d" % i, [128, 512]) for i in range(2)]
                    Hq = [sb(L, "Hq%d" % i, [128, 512]) for i in range(1)]
                    fs = [sb(L, "fs%d" % i, [128, 8]) for i in range(2)]
                    omb = [sb(L, "omb%d" % i, [128, 512], BF16) for i in range(2)]
                    bwd_order = [1, 0] + list(range(NTT - 1, 1, -1))
                    tri4 = [sb(L, "tri4%d" % d_, [128, 4, 128], BF16) for d_ in range(2)]
                    for d_, tr_ in ((0, trif), (1, trib)):
                        for hd in range(4):
                            p.op("dve", lambda e, d_=d_, tr_=tr_, hd=hd: e.tensor_copy(out=tri4[d_][:, hd, :], in_=tr_[:]),
                                 r=[tr_.r()], w=[tri4[d_].r()])
                    psN_ = [PD[0], PD[2]]
                    psU_ = PD[1]
                    for d_ in range(2):
                        if stop in ("C4", "C6", "C7") and d_ == 1:
                            break
                        for step in range(NTT):
                            if (stop == "C6" and step == 2) or (stop == "C7" and step == 3):
                                break
                            t = step if d_ == 0 else bwd_order[step]
                            tri = trif if d_ == 0 else trib
                            psN = psN_[d_]
                            psS = PS[d_]
                            C_, Cb_, vp_, sT_, sm_ = Cst[d_], Cbf[d_], vp[d_], sTm[d_], sm[d_]
                            p.op(POOLENG, lambda e, t=t, d_=d_, vp_=vp_: e.tensor_tensor(
                                out=vp_[:], in0=vM[:, t, :, :], in1=bc(ES[:, t, d_ * 4:(d_ + 1) * 4].unsqueeze(2), [128, 4, 129]),
                                op=ALU.mult), r=[vM.r(t), ES.r()], w=[vp_.r()])
                            if t >= 2:
                                lt = t - 2
                                for hd in range(4):
                                    hp, c = (hd % 2) * 64, hd // 2
                                    p.op("pe", lambda e, hd=hd, hp=hp, c=c, lt=lt, psS=psS: e.matmul(
                                        psS[:, hd * 128:(hd + 1) * 128], lhsT=mkT[hp:hp + 64, c, lt * 128:(lt + 1) * 128],
                                        rhs=mqT[hp:hp + 64, c, lt * 128:(lt + 1) * 128], start=True, stop=True),
                                        r=[mkT.r(c), mqT.r(c)], w=[psS.r()])
                                    p.op("pe", lambda e, hd=hd, c=c, t=t, vp_=vp_: e.matmul(
                                        psU_[:, hd * 256:hd * 256 + 129], lhsT=ktm[:, t, c * 128:(c + 1) * 128], rhs=vp_[:, hd, :],
                                        start=True, stop=True), r=[ktm.r(t), vp_.r()], w=[psU_.r()])
                                p.op("dve", lambda e, sT_=sT_, psS=psS, d_=d_: e.tensor_tensor(
                                    out=sT_[:].rearrange("p h n -> p (h n)"), in0=psS[:, 0:512],
                                    in1=tri4[d_][:].rearrange("p h n -> p (h n)"), op=ALU.mult),
                                    r=[psS.r(), tri4[d_].r()], w=[sT_.r()])
                                for hd in range(4):
                                    hp, c = (hd % 2) * 64, hd // 2
                                    p.op("pe", lambda e, hd=hd, psN=psN, sT_=sT_, vp_=vp_: e.matmul(
                                        psN[:, hd * 256:hd * 256 + 129], lhsT=sT_[:, hd, :], rhs=vp_[:, hd, :],
                                        start=True, stop=(step == 0)), r=[sT_.r(), vp_.r()], w=[psN.r()])
                                    if step > 0:
                                        p.op("pe", lambda e, hd=hd, hp=hp, c=c, lt=lt, psN=psN, Cb_=Cb_: e.matmul(
                                            psN[:, hd * 256:hd * 256 + 129], lhsT=mqT[hp:hp + 64, c, lt * 128:(lt + 1) * 128],
                                            rhs=Cb_[hp:hp + 64, hd, :], start=False, stop=True),
                                            r=[mqT.r(c), Cb_.r()], w=[psN.r()])
                                nv = psN[:].rearrange("p (h x) -> p h x", x=256)
                                p.op("dve", lambda e, nv=nv, sm_=sm_, t=t, d_=d_: e.tensor_tensor(
                                    out=sm_[:, :, 0:1], in0=nv[:, :, 128:129], in1=EBt[:, t, d_ * 4:(d_ + 1) * 4].unsqueeze(2),
                                    op=ALU.mult), r=[psN.r(), EBt.r()], w=[sm_.r()])
                                p.op("dve", lambda e, sm_=sm_: e.tensor_scalar(
                                    out=sm_[:, :, 1:2], in0=sm_[:, :, 0:1], scalar1=-1.0, scalar2=1.0, op0=ALU.mult, op1=ALU.max),
                                    r=[sm_.r()], w=[sm_.r()])
                                p.op("dve", lambda e, sm_=sm_: e.tensor_tensor(
                                    out=sm_[:, :, 2:3], in0=sm_[:, :, 1:2], in1=sm_[:, :, 0:1], op=ALU.max),
                                    r=[sm_.r()], w=[sm_.r()])
                                p.op("dve", lambda e, sm_=sm_: e.reciprocal(out=sm_[:, :, 3:4], in_=sm_[:, :, 2:3]),
                                     r=[sm_.r()], w=[sm_.r()])
                                p.op("dve", lambda e, sm_=sm_, t=t, d_=d_: e.tensor_tensor(
                                    out=sm_[:, :, 0:1], in0=sm_[:, :, 3:4], in1=EBt[:, t, d_ * 4:(d_ + 1) * 4].unsqueeze(2),
                                    op=ALU.mult), r=[sm_.r(), EBt.r()], w=[sm_.r()])
                                if d_ == 0:
                                    p.op("dve", lambda e, nv=nv, sm_=sm_, lt=lt: e.tensor_tensor(
                                        out=Hf[:, lt, :].rearrange("p (h n) -> p h n", h=4), in0=nv[:, :, 0:128],
                                        in1=bc(sm_[:, :, 0:1], [128, 4, 128]), op=ALU.mult),
                                        r=[psN.r(), sm_.r()], w=[Hf.r(lt)])
                                elif stop != "C5":
                                    hs, hq, f_, ob = Hs[lt % 2], Hq[0], fs[lt % 2], omb[lt % 2]
                                    p.op("dve", lambda e, nv=nv, sm_=sm_, hs=hs: e.tensor_tensor(
                                        out=hs[:].rearrange("p (h n) -> p h n", h=4), in0=nv[:, :, 0:128],
                                        in1=bc(sm_[:, :, 0:1], [128, 4, 128]), op=ALU.mult),
                                        r=[psN.r(), sm_.r()], w=[hs.r()])
                                    p.op(POOLENG, lambda e, hs=hs, lt=lt: e.tensor_tensor(
                                        out=hs[:], in0=hs[:], in1=Hf[:, lt, :], op=ALU.add),
                                        r=[hs.r(), Hf.r(lt)], w=[hs.r()])
                                    p.op(POOLENG, lambda e, hs=hs, hq=hq: e.tensor_tensor(out=hq[:], in0=hs[:], in1=hs[:], op=ALU.mult),
                                         r=[hs.r()], w=[hq.r()])
                                    p.op("dve", lambda e, hq=hq, f_=f_: e.reduce_sum(
                                        out=f_[:, 0:4], in_=hq[:].rearrange("p (h n) -> p h n", h=4), axis=AX.X),
                                        r=[hq.r()], w=[f_.r()])
                                    p.op("act", lambda e, f_=f_: e.activation(out=f_[:, 4:8], in_=f_[:, 0:4], func=AF.Sqrt,
                                                                           scale=1.0 / 128.0, bias=EPS), r=[f_.r()], w=[f_.r()])
                                    p.op("dve", lambda e, f_=f_: e.reciprocal(out=f_[:, 4:8], in_=f_[:, 4:8]), r=[f_.r()], w=[f_.r()])
                                    p.op("dve", lambda e, hs=hs, f_=f_: e.tensor_tensor(
                                        out=hs[:].rearrange("p (h n) -> p h n", h=4), in0=hs[:].rearrange("p (h n) -> p h n", h=4),
                                        in1=bc(f_[:, 4:8].unsqueeze(2), [128, 4, 128]), op=ALU.mult),
                                        r=[hs.r(), f_.r()], w=[hs.r()])
                                    p.op(POOLENG, lambda e, hs=hs: e.tensor_tensor(out=hs[:], in0=hs[:], in1=ghd[:], op=ALU.mult),
                                         r=[hs.r(), ghd.r()], w=[hs.r()])
                                    p.op(POOLENG, lambda e, hs=hs, ob=ob, lt=lt: e.tensor_tensor(
                                        out=ob[:], in0=hs[:], in1=osig[:, lt, :], op=ALU.mult),
                                        r=[hs.r(), osig.r(lt)], w=[ob.r()])
                                    ps = psS
                                    psb = ps[:].bitcast(BF16)
                                    for c4 in range(4):
                                        p.op("pe", lambda e, c4=c4, ob=ob, psb=psb: e.transpose(
                                            psb[:, c4 * 128:(c4 + 1) * 128], ob[:, c4 * 128:(c4 + 1) * 128], identb[:]),
                                            r=[ob.r(), identb.r()], w=[ps.r()])
                                    p.op("act", lambda e, lt=lt, psb=psb: e.copy(
                                        out=omlT[:, :, lt * 128:(lt + 1) * 128],
                                        in_=psb[:, 0:512].rearrange("p (k n) -> p k n", k=4)),
                                        r=[ps.r()], w=[omlT.r(lt)])
                            for hd in range(4):
                                c = hd // 2
                                if t >= 2:
                                    break
                                p.op("pe", lambda e, hd=hd, c=c, t=t, vp_=vp_: e.matmul(
                                    psU_[:, hd * 256:hd * 256 + 129], lhsT=ktm[:, t, c * 128:(c + 1) * 128], rhs=vp_[:, hd, :],
                                    start=True, stop=True), r=[ktm.r(t), vp_.r()], w=[psU_.r()])
                            uv = psU_[:].rearrange("p (h x) -> p h x", x=256)[:, :, 0:129]
                            ebl = bc(EBL[:, t, d_ * 4:(d_ + 1) * 4].unsqueeze(2), [128, 4, 129])
                            if step == 0:
                                p.op("dve", lambda e, C_=C_, uv=uv, ebl=ebl: e.tensor_tensor(out=C_[:], in0=uv, in1=ebl, op=ALU.mult),
                                     r=[psU_.r(), EBL.r()], w=[C_.r()])
                            else:
                                p.op("dve", lambda e, C_=C_, uv=uv: e.tensor_tensor(out=C_[:], in0=uv, in1=C_[:], op=ALU.add),
                                     r=[psU_.r(), C_.r()], w=[C_.r()])
                                p.op("dve", lambda e, C_=C_, ebl=ebl: e.tensor_tensor(out=C_[:], in0=C_[:], in1=ebl, op=ALU.mult),
                                     r=[C_.r(), EBL.r()], w=[C_.r()])
                            p.op("act", lambda e, C_=C_, Cb_=Cb_: e.copy(out=Cb_[:], in_=C_[:]), r=[C_.r()], w=[Cb_.r()])
                    if b == 0 and "omlT" in dbg_d:
                        dump("omlT", omlT[:], omlT.res)
                p.barrier()
                if stop in ("C", "C4", "C5", "C6", "C7"):
                    break

                with ExitStack() as L:
                    wbn = sb(L, "wbn", [128, 4, D], BF16); wbm = sb(L, "wbm", [128, 4, D], BF16)
                    wout = sb(L, "wout", [128, 8, D], BF16)
                    wr = sb(L, "wr", [128, 8, NE]); br = sb(L, "br", [1, NE])
                    p.dma("pool", wbn[:], wview(w_bna_d), w=[wbn.r()], sem="w")
                    p.dma("pool", wbm[:], wview(w_bml_d), w=[wbm.r()], sem="w")
                    p.dma("pool", wout[:], wview(w_out_d), w=[wout.r()], sem="w")
                    p.dma("sp", wr[:], wview(w_r_d), w=[wr.r()], sem="x")
                    p.dma("sp", br[:], b_r_d, w=[br.r()], sem="x")
                    wgn = [sb(L, "wgn%d" % i, [128, 8, 128], BF16) for i in range(2)]
                    wgm = [sb(L, "wgm%d" % i, [128, 8, 128], BF16) for i in range(2)]
                    mT = sb(L, "mT", [128, 8, S], BF16, nres=32)
                    sg = [sb(L, "sg%d" % i, [128, 512]) for i in range(2)]
                    m1 = [sb(L, "m1%d" % i, [128, 512]) for i in range(2)]
                    xt = [sb(L, "xd", [128, D])] * 2
                    yt = [sb(L, "yd%d" % i, [128, D]) for i in range(2)]
                    jk = sb(L, "jk", [128, D], BF16)
                    h2 = [sb(L, "h2", [128, D])] * 2
                    h2hi = [sb(L, "h2hi%d" % i, [128, D], BF16) for i in range(2)]
                    h2lo = [sb(L, "h2lo", [128, D], BF16)] * 2
                    h2Tlo = [sb(L, "h2Tlo", [128, 8, 128], BF16)] * 2
                    wrhi = sb(L, "wrhi", [128, 8, NE], BF16); wrlo = sb(L, "wrlo", [128, 8, NE], BF16)
                    p.op("dve", lambda e: e.tensor_copy(out=wrhi[:], in_=wr[:]), r=[wr.r()], w=[wrhi.r()])
                    p.op("dve", lambda e: e.tensor_tensor(out=wrlo[:], in0=wr[:], in1=wrhi[:], op=ALU.subtract),
                         r=[wr.r(), wrhi.r()], w=[wrlo.r()])
                    st = [sb(L, "std%d" % i, [128, 4]) for i in range(2)]
                    lg = [sb(L, "lg%d" % i, [128, 3, NE]) for i in range(2)]
                    t8 = [sb(L, "t8%d" % i, [128, 20]) for i in range(2)]
                    mkb = [sb(L, "mkb%d" % i, [128, NE], BF16) for i in range(2)]
                    oh4 = [sb(L, "oh4%d" % i, [128, 4, NE]) for i in range(2)]
                    n = 0
                    for dc in range(8):
                        n += 1
                        a_, b_ = wgn[n % 2], wgm[n % 2]
                        p.dma("pool", a_[:], wview(w_in_d[:, C_GNA + dc * 128:C_GNA + (dc + 1) * 128]), w=[a_.r()], sem="w")
                        p.dma("pool", b_[:], wview(w_in_d[:, C_GML + dc * 128:C_GML + (dc + 1) * 128]), w=[b_.r()], sem="w")
                        for tb in range(4):
                            tok0 = 256 + tb * 512
                            pa, pb = nPD(), nPD()
                            mm_fm(pa[:, 0:512], pa.r(), a_, 0, tok0, 512)
                            mm_fm(pa[:, 512:1024], pa.r(), wbn, dc * 128, tb * 512, 512, nk=4, src=onaT)
                            mm_fm(pb[:, 0:512], pb.r(), b_, 0, tok0, 512)
                            mm_fm(pb[:, 512:1024], pb.r(), wbm, dc * 128, tb * 512, 512, nk=4, src=omlT)
                            p.op("act", lambda e, pa=pa: e.activation(out=sg[0][:], in_=pa[:, 0:512], func=AF.Sigmoid),
                                 r=[pa.r()], w=[sg[0].r()])
                            p.op("act", lambda e, pb=pb: e.activation(out=sg[1][:], in_=pb[:, 0:512], func=AF.Sigmoid),
                                 r=[pb.r()], w=[sg[1].r()])
                            p.op("dve", lambda e, pa=pa: e.tensor_tensor(out=m1[0][:], in0=pa[:, 512:1024], in1=sg[0][:], op=ALU.mult),
                                 r=[pa.r(), sg[0].r()], w=[m1[0].r()])
                            p.op("dve", lambda e, pb=pb: e.tensor_tensor(out=m1[1][:], in0=pb[:, 512:1024], in1=sg[1][:], op=ALU.mult),
                                 r=[pb.r(), sg[1].r()], w=[m1[1].r()])
                            p.op(POOLENG, lambda e, dc=dc, tb=tb: e.tensor_tensor(
                                out=mT[:, dc, tb * 512:(tb + 1) * 512], in0=m1[0][:], in1=m1[1][:], op=ALU.add),
                                r=[m1[0].r(), m1[1].r()], w=[mT.r(dc * 4 + tb)])
                    for tb in range(4):
                        for ti in range(4):
                            t = tb * 4 + ti
                            i = t % 2
                            py = PD[2]
                            for half in range(2):
                                for kc in range(8):
                                    p.op("pe", lambda e, kc=kc, half=half, t=t, py=py: e.matmul(
                                        py[:, half * 512:(half + 1) * 512], lhsT=mT[:, kc, t * 128:(t + 1) * 128],
                                        rhs=wout[:, kc, half * 512:(half + 1) * 512], start=(kc == 0), stop=(kc == 7)),
                                        r=[mT.r(kc * 4 + tb), wout.r()], w=[py.r()])
                            p.dma("sp", xt[i][:], x_d[b, t * 128:(t + 1) * 128, :], w=[xt[i].r()], sem="x")
                            rstd_of(py[:], [py.r()], jk, st[i])
                            p.op("dve", lambda e, i=i, py=py: e.scalar_tensor_tensor(
                                out=yt[i][:], in0=py[:], scalar=st[i][:, 1:2], in1=G_m[:], op0=ALU.mult, op1=ALU.mult),
                                r=[py.r(), st[i].r(), G_m.r()], w=[yt[i].r()])
                            p.op(POOLENG, lambda e, i=i: e.tensor_tensor(out=yt[i][:], in0=yt[i][:], in1=xt[i][:], op=ALU.add),
                                 r=[yt[i].r(), xt[i].r()], w=[yt[i].r()])
                            p.dma("sp", out_d[b, t * 128:(t + 1) * 128, :], yt[i][:], r=[yt[i].r()], w=[x1res[b][t]], sem="o")
                            if b == 0 and "x1" in dbg_d:
                                dump("x1", yt[i][:], yt[i].r(), dst=dbg_d["x1"][t])
                            rstd_of(yt[i][:], [yt[i].r()], jk, st[i])
                            p.op("dve", lambda e, i=i: e.scalar_tensor_tensor(
                                out=h2[i][:], in0=yt[i][:], scalar=st[i][:, 1:2], in1=A_f[:], op0=ALU.mult, op1=ALU.mult),
                                r=[yt[i].r(), st[i].r(), A_f.r()], w=[h2[i].r()])
                            p.op(POOLENG, lambda e, i=i: e.tensor_tensor(out=h2[i][:], in0=h2[i][:], in1=sh_f[:], op=ALU.add),
                                 r=[h2[i].r(), sh_f.r()], w=[h2[i].r()])
                            p.op("act", lambda e, i=i: e.copy(out=h2hi[i][:], in_=h2[i][:]), r=[h2[i].r()], w=[h2hi[i].r()])
                            p.op("dve", lambda e, i=i: e.tensor_tensor(out=h2lo[i][:], in0=h2[i][:], in1=h2hi[i][:], op=ALU.subtract),
                                 r=[h2[i].r(), h2hi[i].r()], w=[h2lo[i].r()])
                            pa_ = PS[i]
                            pab = pa_[:].bitcast(BF16)
                            for kc in range(8):
                                p.op("pe", lambda e, kc=kc, i=i, pab=pab: e.transpose(
                                    pab[:, kc * 128:(kc + 1) * 128], h2hi[i][:, kc * 128:(kc + 1) * 128], identb[:]),
                                    r=[h2hi[i].r(), identb.r()], w=[pa_.r()])
                            p.op("act", lambda e, t=t, pab=pab: e.copy(
                                out=hT[:, :, (2 + t) * 128:(3 + t) * 128], in_=pab.rearrange("p (k n) -> p k n", k=8)),
                                r=[pa_.r()], w=[hT.r(2 + t)])
                            pb_ = PD[i]
                            pbb = pb_[:].bitcast(BF16)
                            for kc in range(8):
                                p.op("pe", lambda e, kc=kc, i=i, pbb=pbb: e.transpose(
                                    pbb[:, kc * 128:(kc + 1) * 128], h2lo[i][:, kc * 128:(kc + 1) * 128], identb[:]),
                                    r=[h2lo[i].r(), identb.r()], w=[pb_.r()])
                            p.op("dve", lambda e, i=i, pbb=pbb: e.tensor_copy(
                                out=h2Tlo[i][:], in_=pbb[:, 0:1024].rearrange("p (k n) -> p k n", k=8)),
                                r=[pb_.r()], w=[h2Tlo[i].r()])
                            pl = PS[1 - i]
                            nmm = 0
                            for kc in range(8):
                                for (lh_, lres, w_) in ((hT[:, kc, (2 + t) * 128:(3 + t) * 128], hT.r(2 + t), wrhi),
                                                        (h2Tlo[i][:, kc, :], h2Tlo[i].r(), wrhi),
                                                        (hT[:, kc, (2 + t) * 128:(3 + t) * 128], hT.r(2 + t), wrlo)):
                                    nmm += 1
                                    p.op("pe", lambda e, kc=kc, lh_=lh_, w_=w_, pl=pl, nmm=nmm: e.matmul(
                                        pl[:, 0:NE], lhsT=lh_, rhs=w_[:, kc, :], start=(nmm == 1), stop=(nmm == 24)),
                                        r=[lres, w_.r()], w=[pl.r()])
                            p.op("pe", lambda e, pl=pl: e.matmul(pl[:, 32:64], lhsT=ones32[0:1, :], rhs=br[0:1, :], start=True, stop=True),
                                 r=[ones32.r(), br.r()], w=[pl.r()])
                            L_, T_ = lg[i], t8[i]
                            p.op("act", lambda e, L_=L_, pl=pl: e.copy(out=L_[:, 0, :], in_=pl[:, 0:NE]), r=[pl.r()], w=[L_.r()])
                            p.op("dve", lambda e, L_=L_, pl=pl: e.tensor_tensor(out=L_[:, 0, :], in0=L_[:, 0, :], in1=pl[:, 32:64], op=ALU.add),
                                 r=[pl.r(), L_.r()], w=[L_.r()])
                            if b == 0 and "logits" in dbg_d:
                                dump("logits", L_[:, 0, :], L_.r(), dst=dbg_d["logits"][t])
                            p.op("dve", lambda e, L_=L_, T_=T_: e.max(out=T_[:, 0:8], in_=L_[:, 0, :]), r=[L_.r()], w=[T_.r()])
                            gt = b * NT + t
                            mk_, oh_ = mkb[i], oh4[i]
                            p.op("dve", lambda e, L_=L_, T_=T_: e.tensor_scalar(
                                out=L_[:, 1, :], in0=L_[:, 0, :], scalar1=T_[:, 3:4], scalar2=None, op0=ALU.is_ge),
                                r=[L_.r(), T_.r()], w=[L_.r()])
                            p.op("dve", lambda e, L_=L_, mk_=mk_: e.tensor_copy(out=mk_[:], in_=L_[:, 1, :]), r=[L_.r()], w=[mk_.r()])
                            p.op("pe", lambda e, pl=pl, mk_=mk_: e.matmul(pl[:, 64:96], lhsT=trisb[:], rhs=mk_[:], start=True, stop=False),
                                 r=[trisb.r(), mk_.r()], w=[pl.r()])
                            p.op("pe", lambda e, pl=pl: e.matmul(pl[:, 64:96], lhsT=onesb[:], rhs=msum[:], start=False, stop=True),
                                 r=[onesb.r(), msum.r()], w=[pl.r()])
                            p.op("dve", lambda e, L_=L_, pl=pl: e.scalar_tensor_tensor(
                                out=L_[:, 2, :], in0=pl[:, 64:96], scalar=float(CAP - 1), in1=ecap[:], op0=ALU.min, op1=ALU.add),
                                r=[pl.r(), ecap.r()], w=[L_.r()])
                            p.op("dve", lambda e, mk_=mk_: e.tensor_tensor(out=msum[:], in0=msum[:], in1=mk_[:], op=ALU.add),
                                 r=[msum.r(), mk_.r()], w=[msum.r()])
                            for k in range(4):
                                p.op("dve", lambda e, k=k, L_=L_, T_=T_, oh_=oh_: e.tensor_scalar(
                                    out=oh_[:, k, :], in0=L_[:, 0, :], scalar1=T_[:, k:k + 1], scalar2=None, op0=ALU.is_equal),
                                    r=[L_.r(), T_.r()], w=[oh_.r()])
                                p.op("dve", lambda e, k=k, L_=L_, oh_=oh_: e.tensor_tensor(
                                    out=oh_[:, k, :], in0=oh_[:, k, :], in1=L_[:, 2, :], op=ALU.mult),
                                    r=[oh_.r(), L_.r()], w=[oh_.r()])
                            p.op("dve", lambda e, T_=T_, oh_=oh_: e.reduce_sum(out=T_[:, 12:16], in_=oh_[:], axis=AX.X),
                                 r=[oh_.r()], w=[T_.r()])
                            p.op("dve", lambda e, T_=T_, gt=gt: e.tensor_copy(out=RIi[:, gt, :], in_=T_[:, 12:16]),
                                 r=[T_.r()], w=[RIi.r(gt)])
                            p.op("dve", lambda e, T_=T_: e.tensor_scalar_mul(out=T_[:, 8:9], in0=T_[:, 0:1], scalar1=-1.0),
                                 r=[T_.r()], w=[T_.r()])
                            p.op("act", lambda e, T_=T_: e.activation(out=T_[:, 16:20], in_=T_[:, 0:4], func=AF.Exp,
                                                                     bias=T_[:, 8:9], scale=1.0), r=[T_.r()], w=[T_.r()])
                            p.op("dve", lambda e, T_=T_: e.reduce_sum(out=T_[:, 9:10], in_=T_[:, 16:20], axis=AX.X),
                                 r=[T_.r()], w=[T_.r()])
                            p.op("dve", lambda e, T_=T_: e.reciprocal(out=T_[:, 10:11], in_=T_[:, 9:10]), r=[T_.r()], w=[T_.r()])
                            p.op("dve", lambda e, T_=T_, gt=gt: e.tensor_scalar_mul(
                                out=RIw[:, gt, :], in0=T_[:, 16:20], scalar1=T_[:, 10:11]), r=[T_.r()], w=[RIw.r(gt)])
                            if b == 0 and t == 0:
                                wait_zero()
                            for k in range(4):
                                p.idma(out=xe_d, out_offset=bass.IndirectOffsetOnAxis(ap=RIi[:, gt, k:k + 1], axis=0),
                                       in_=h2hi[i][:], in_offset=None, bounds=NE * CAP - 1,
                                       r=[h2hi[i].r(), RIi.r(gt)], w=[], sem="sc")
                p.barrier()
            if stop is not None:
                break

            p.barrier()

        if stop is None:
            p.barrier()
            with ExitStack() as L:
                wgb = [sb(L, "wgb%d" % i, [128, 8, D], BF16) for i in range(2)]
                wlb = [sb(L, "wlb%d" % i, [128, 8, D], BF16) for i in range(2)]
                wdb = [sb(L, "wdb%d" % i, [128, 8, D], BF16) for i in range(2)]
                bdb = [sb(L, "bdb%d" % i, [128, D]) for i in range(2)]
                bg = sb(L, "bg", [128, 8, NE]); bl = sb(L, "bl", [128, 8, NE])
                p.dma("sp", bg[:], bgT_d, w=[bg.r()], sem="x")
                p.dma("sp", bl[:], blT_d, w=[bl.r()], sem="x")
                xr = [sb(L, "xr%d" % i, [128, 4, D], BF16) for i in range(2)]
                xT = [sb(L, "xT%d" % i, [128, 8, 512], BF16) for i in range(2)]
                aT2 = [sb(L, "aT%d" % i, [128, 8, 512], BF16, nres=8) for i in range(2)]
                gg = [sb(L, "gg%d" % i, [128, 512]) for i in range(2)]
                sg = [sb(L, "sgE%d" % i, [128, 512]) for i in range(2)]
                l1 = [sb(L, "l1%d" % i, [128, 512]) for i in range(2)]
                t1 = [sb(L, "t1%d" % i, [128, 512]) for i in range(2)]
                ysb = [sb(L, "ysb%d" % i, [128, D]) for i in range(2)]

                class BV:
                    def __init__(self, parent, c0):
                        self.par, self.c0, self.res = parent, c0, Res()

                    def r(self):
                        return self.res

                    def ap(self):
                        return self.par[:, self.c0:self.c0 + 512]

                banks = [BV(PD[2], 0), BV(PD[2], 512), BV(PS[0], 0), BV(PS[1], 0)]
                bki = [0]

                def nbank():
                    bki[0] += 1
                    return banks[bki[0] % 4]

                NBLK = CAP // 512
                NTOT = NE * NBLK
                ycnt = [0]

                def load_w(e_):
                    p.dma("pool", wgb[e_ % 2][:], wview(w_gate_d[e_]), w=[wgb[e_ % 2].r()], sem="w")
                    p.dma("pool", wlb[e_ % 2][:], wview(w_lin_d[e_]), w=[wlb[e_ % 2].r()], sem="w")
                    p.dma("pool", wdb[e_ % 2][:], wview(w_down_d[e_]), w=[wdb[e_ % 2].r()], sem="w")
                    p.dma("sp", bdb[e_ % 2][:], b_down_d[e_:e_ + 1, :].partition_broadcast(128), w=[bdb[e_ % 2].r()], sem="x")

                def emit_T(n):
                    e_, blk = divmod(n, NBLK)
                    xr_, xT_ = xr[n % 2], xT[n % 2]
                    row0 = e_ * CAP + blk * 512
                    p.dma("sp", xr_[:], xe_d[row0:row0 + 512, :].rearrange("(j p) d -> p j d", p=128), w=[xr_.r()], sem="x")
                    for j in range(4):
                        bk = nbank()
                        psb = bk.ap().bitcast(BF16)
                        for kc in range(8):
                            p.op("pe", lambda e, kc=kc, j=j, psb=psb, xr_=xr_: e.transpose(
                                psb[:, kc * 128:(kc + 1) * 128], xr_[:, j, kc * 128:(kc + 1) * 128], identb[:]),
                                r=[xr_.r(), identb.r()], w=[bk.r()])
                        eng = "act" if j % 2 else "dve"
                        if eng == "act":
                            p.op("act", lambda e, j=j, psb=psb, xT_=xT_: e.copy(
                                out=xT_[:, :, j * 128:(j + 1) * 128], in_=psb.rearrange("p (k n) -> p k n", k=8)),
                                r=[bk.r()], w=[xT_.r()])
                        else:
                            p.op("dve", lambda e, j=j, psb=psb, xT_=xT_: e.tensor_copy(
                                out=xT_[:, :, j * 128:(j + 1) * 128], in_=psb.rearrange("p (k n) -> p k n", k=8)),
                                r=[bk.r()], w=[xT_.r()])

                def emit_GL(n, fc):
                    e_ = n // NBLK
                    wg_, wl_ = wgb[e_ % 2], wlb[e_ % 2]
                    xT_, aT = xT[n % 2], aT2[n % 2]
                    j = fc % 2
                    pg = PD[j]
                    for (w_, c0) in ((wg_, 0), (wl_, 512)):
                        for kc in range(8):
                            p.op("pe", lambda e, kc=kc, w_=w_, c0=c0, pg=pg: e.matmul(
                                pg[:, c0:c0 + 512], lhsT=w_[:, kc, fc * 128:(fc + 1) * 128], rhs=xT_[:, kc, :],
                                start=(kc == 0), stop=(kc == 7)), r=[w_.r(), xT_.r()], w=[pg.r()])
                    p.op("dve", lambda e: e.tensor_scalar(
                        out=gg[j][:], in0=pg[:, 0:512], scalar1=bg[:, fc, e_:e_ + 1], scalar2=7.0, op0=ALU.add, op1=ALU.min),
                        r=[pg.r(), bg.r()], w=[gg[j].r()])
                    p.op("act", lambda e: e.activation(out=sg[j][:], in_=gg[j][:], func=AF.Sigmoid, scale=1.702),
                         r=[gg[j].r()], w=[sg[j].r()])
                    p.op("dve", lambda e: e.tensor_scalar(
                        out=l1[j][:], in0=pg[:, 512:1024], scalar1=bl[:, fc, e_:e_ + 1], scalar2=7.0, op0=ALU.add, op1=ALU.min),
                        r=[pg.r(), bl.r()], w=[l1[j].r()])
                    p.op("dve", lambda e: e.tensor_scalar(
                        out=l1[j][:], in0=l1[j][:], scalar1=-7.0, scalar2=1.0, op0=ALU.max, op1=ALU.add),
                        r=[l1[j].r()], w=[l1[j].r()])
                    p.op(POOLENG, lambda e: e.tensor_tensor(out=t1[j][:], in0=gg[j][:], in1=sg[j][:], op=ALU.mult),
                         r=[gg[j].r(), sg[j].r()], w=[t1[j].r()])
                    p.op("dve", lambda e: e.tensor_tensor(out=aT[:, fc, :], in0=t1[j][:], in1=l1[j][:], op=ALU.mult),
                         r=[t1[j].r(), l1[j].r()], w=[aT.r(fc)])

                def emit_DOWN(n):
                    e_, blk = divmod(n, NBLK)
                    wd_, bd_, aT = wdb[e_ % 2], bdb[e_ % 2], aT2[n % 2]
                    row0 = e_ * CAP + blk * 512
                    for ti in range(4):
                        ycnt[0] += 1
                        ys_ = ysb[ycnt[0] % 2]
                        for half in range(2):
                            bk = nbank()
                            for fc in range(8):
                                p.op("pe", lambda e, fc=fc, half=half, ti=ti, bk=bk: e.matmul(
                                    bk.ap(), lhsT=aT[:, fc, ti * 128:(ti + 1) * 128],
                                    rhs=wd_[:, fc, half * 512:(half + 1) * 512], start=(fc == 0), stop=(fc == 7)),
                                    r=[aT.r(fc), wd_.r()], w=[bk.r()])
                            p.op("dve", lambda e, half=half, bk=bk, ys_=ys_: e.tensor_tensor(
                                out=ys_[:, half * 512:(half + 1) * 512], in0=bk.ap(), in1=bd_[:, half * 512:(half + 1) * 512], op=ALU.add),
                                r=[bk.r(), bd_.r()], w=[ys_.r()])
                        p.dma("sp", ye_d[row0 + ti * 128:row0 + (ti + 1) * 128, :], ys_[:], r=[ys_.r()], w=[], sem="o")

                for n in range(NTOT):
                    if n % NBLK == 0:
                        load_w(n // NBLK)
                    emit_T(n)
                    emit_GL(n, 0)
                    emit_GL(n, 1)
                    if n > 0:
                        emit_DOWN(n - 1)
                    for fc in range(2, 8):
                        emit_GL(n, fc)
                emit_DOWN(NTOT - 1)
            p.barrier()
            with ExitStack() as L:
                yk = [[sb(L, "yk%d_%d" % (i, k), [128, D]) for k in range(4)] for i in range(2)]
                acc = [sb(L, "acc%d" % i, [128, D]) for i in range(2)]
                xe1 = [sb(L, "xe1%d" % i, [128, D]) for i in range(2)]
                jk = sb(L, "jkF", [128, D], BF16)
                st = [sb(L, "stF%d" % i, [128, 4]) for i in range(2)]
                Gf = sb(L, "GfF", [128, D])
                for gt in range(nb * NT):
                    b, t = divmod(gt, NT)
                    i = gt % 2
                    if t == 0:
                        p.dma("sp", Gf[:], gf_d[b], r=[gfres[b]], w=[Gf.r()], sem="x")
                    for k in range(4):
                        p.idma(out=yk[i][k][:], out_offset=None, in_=ye_d,
                               in_offset=bass.IndirectOffsetOnAxis(ap=RIi[:, gt, k:k + 1], axis=0), bounds=NE * CAP - 1,
                               r=[RIi.r(gt)], w=[yk[i][k].r()], sem="ga")
                    p.dma("sp", xe1[i][:], out_d[b, t * 128:(t + 1) * 128, :], r=[x1res[b][t]], w=[xe1[i].r()], sem="x")
                    a_ = acc[i]
                    p.op("dve", lambda e, a_=a_, i=i, gt=gt: e.tensor_scalar_mul(out=a_[:], in0=yk[i][0][:], scalar1=RIw[:, gt, 0:1]),
                         r=[yk[i][0].r(), RIw.r(gt)], w=[a_.r()])
                    for k in range(1, 4):
                        p.op("dve", lambda e, a_=a_, i=i, gt=gt, k=k: e.scalar_tensor_tensor(
                            out=a_[:], in0=yk[i][k][:], scalar=RIw[:, gt, k:k + 1], in1=a_[:], op0=ALU.mult, op1=ALU.add),
                            r=[yk[i][k].r(), RIw.r(gt), a_.r()], w=[a_.r()])
                    if b == 0 and "ffn" in dbg_d:
                        dump("ffn", a_[:], a_.r(), dst=dbg_d["ffn"][t])
                    rstd_of(a_[:], [a_.r()], jk, st[i])
                    p.op("dve", lambda e, a_=a_, i=i: e.scalar_tensor_tensor(
                        out=a_[:], in0=a_[:], scalar=st[i][:, 1:2], in1=Gf[:], op0=ALU.mult, op1=ALU.mult),
                        r=[a_.r(), st[i].r(), Gf.r()], w=[a_.r()])
                    p.op("dve", lambda e, a_=a_, i=i: e.tensor_tensor(out=xe1[i][:], in0=xe1[i][:], in1=a_[:], op=ALU.add),
                         r=[xe1[i].r(), a_.r()], w=[xe1[i].r()])
                    p.dma("sp", out_d[b, t * 128:(t + 1) * 128, :], xe1[i][:], r=[xe1[i].r()], w=[x1res[b][t]], sem="o")
            p.barrier()

        p.barrier()
    return nc, p


def _host_consts():
    c = {}
    c["ident"] = np.eye(128, dtype=np.float32)
    j = np.arange(128)
    c["trif"] = (j[:, None] <= j[None, :]).astype(np.float32)
    c["trib"] = (j[:, None] >= j[None, :]).astype(np.float32)
    c["tris"] = (j[:, None] < j[None, :]).astype(np.float32)
    c["ecap"] = np.ascontiguousarray(np.broadcast_to((np.arange(NE) * CAP).astype(np.float32)[None, :], (128, NE)))
    t = np.arange(S)
    row = (t // 64).astype(np.float32)
    col = (t % 64).astype(np.float32)
    inv_freq = (np.float32(10000.0) ** (-np.arange(16, dtype=np.float32) / np.float32(16))).astype(np.float32)
    cos = np.zeros((128, S), np.float32)
    sin = np.zeros((128, S), np.float32)
    for pp in range(128):
        d = pp % 64
        pos = row if d < 32 else col
        dd = d % 32
        f = dd % 16
        sign = -1.0 if dd < 16 else 1.0
        ang = (pos * inv_freq[f]).astype(np.float32)
        cos[pp] = np.cos(ang)
        sin[pp] = sign * np.sin(ang)
    c["ropecos"] = cos
    c["ropesin"] = sin
    return c


def _na_bias_table(rpb):
    reps = [0, 1, 5, 14, 15]
    kk = np.arange(128)
    krl = kk // 64
    kc = kk % 64
    qq = np.arange(128)
    qrl = qq // 64
    qc = qq % 64
    cs = np.clip(qc - 8, 0, 48)
    tab = np.full((8, 128, 5, 5, 128), NEG, np.float32)
    for ci, i in enumerate(reps):
        js = int(np.clip(i - 2, 0, 11))
        for s in range(5):
            j = js + s
            kr = 2 * j + krl
            r = 2 * i + qrl
            rs = np.clip(r - 4, 0, 24)
            vr = (kr[:, None] >= rs[None, :]) & (kr[:, None] < rs[None, :] + 8)
            vc = (kc[:, None] >= cs[None, :]) & (kc[:, None] < cs[None, :] + 16)
            valid = vr & vc
            dr = np.clip(kr[:, None] - r[None, :] + 7, 0, 14)
            dc = np.clip(kc[:, None] - qc[None, :] + 15, 0, 30)
            vals = rpb[:, dr, dc]
            tab[:, :, ci, s, :] = np.where(valid[None], vals, np.float32(NEG))
    return np.ascontiguousarray(tab.reshape(8, 128, 5 * 640))


def _prep_inputs(inp, nb=NB, ncores=8):
    f = lambda a: np.ascontiguousarray(np.asarray(a, dtype=np.float32))
    consts = _host_consts()
    w_in = f(inp["w_in"][0])
    d = np.arange(64)
    partner = np.where((d % 32) < 16, d + 16, d - 16)
    permq = np.concatenate([C_MLQ + h * 64 + partner for h in range(4)])
    permk = np.concatenate([C_MLK + h * 64 + partner for h in range(4)])
    w_qkp = np.ascontiguousarray(np.concatenate([w_in[:, permq], w_in[:, permk]], axis=1))
    shared = dict(
        w_ada=f(inp["w_ada"][0]), b_ada=f(inp["b_ada"][0]).reshape(1, -1),
        g4=np.ascontiguousarray(np.stack([f(inp["g_mix_pre"][0]), f(inp["g_mix_post"][0]),
                                          f(inp["g_ffn_pre"][0]), f(inp["g_ffn_post"][0])])),
        w_in=w_in, w_qkp=w_qkp, b_mg=f(inp["b_mlstm_gates"][0]).reshape(1, 16),
        nab=_na_bias_table(f(inp["rpb"][0])), g_head=f(inp["g_mlstm_head"][0]).reshape(1, 512),
        w_bna=f(inp["w_branch_na"][0]), w_bml=f(inp["w_branch_ml"][0]), w_out=f(inp["w_out"][0]),
        w_router=f(inp["w_router"][0]), b_router=f(inp["b_router"][0]).reshape(1, NE),
        w_gate=f(inp["w_gate"][0]), w_lin=f(inp["w_lin"][0]), w_down=f(inp["w_down"][0]),
        bgT=np.ascontiguousarray(f(inp["b_gate"][0]).reshape(NE, 8, 128).transpose(2, 1, 0)),
        blT=np.ascontiguousarray(f(inp["b_lin"][0]).reshape(NE, 8, 128).transpose(2, 1, 0)),
        b_down=f(inp["b_down"][0]), **consts)
    x = f(inp["x"]); ctx = f(inp["ctx"]); c = f(inp["c"]); cc = f(inp["c_ctx"])
    maps = []
    for k in range(ncores):
        sl = slice(k * nb, (k + 1) * nb)
        c5 = np.zeros((5, D), np.float32)
        c5[:nb] = c[sl]
        c5[4] = cc
        cT = np.ascontiguousarray(c5.reshape(5, 8, 128).transpose(2, 1, 0))
        m = dict(shared)
        m.update(x=np.ascontiguousarray(x[sl]), ctx=np.ascontiguousarray(ctx[sl]), cT=cT)
        maps.append(m)
    return maps


def kernel(**inputs):
    maps = _prep_inputs(inputs)
    nc, _ = build()
    res = run_bass_kernel_spmd(nc, maps, core_ids=list(range(8)))
    return np.concatenate([r["out"] for r in res.results], axis=0).astype(np.float32)
```

```python
import numpy as np
from contextlib import ExitStack
import concourse.bass as bass
import concourse.mybir as mybir
from concourse.bass_utils import run_bass_kernel_spmd

F32 = mybir.dt.float32
BF16 = mybir.dt.bfloat16
AF = mybir.ActivationFunctionType
ALU = mybir.AluOpType
AX = mybir.AxisListType

D = 1024
S = 2048
CTX = 256
NB = 4
NT = 16
NTT = 18
NE = 32
EPS = 1e-6
NEG = -80.0
CAP = 2048
I32 = mybir.dt.int32
POOLENG = "dve"
N_IN = 5136
C_NAK, C_NAV, C_MLK, C_MLV, C_MLG = 0, 512, 1024, 1280, 1792
C_NAQ, C_MLQ, C_MLO, C_GNA, C_GML = 1808, 2320, 2576, 3088, 4112


class Res:
    __slots__ = ("lw", "rd")

    def __init__(self):
        self.lw = None
        self.rd = {}


class Prog:
    ENG = ["pe", "act", "dve", "pool", "sp"]

    def __init__(self, nc):
        self.nc = nc
        self.e = dict(pe=nc.tensor, act=nc.scalar, dve=nc.vector, pool=nc.gpsimd, sp=nc.sync)
        self.sem = {k: nc.alloc_semaphore("s_" + k) for k in self.ENG}
        self.cnt = {k: 0 for k in self.ENG}
        self.waited = {k: {} for k in self.ENG}
        self.n_ins = 0

    NDS = 8
    rr = None

    def dsem(self, name):
        if self.rr is None:
            self.rr = {}
        i = self.rr.get(name, 0)
        self.rr[name] = i + 1
        k = "d:%s:%d" % (name, i % self.NDS)
        if k not in self.sem:
            self.sem[k] = self.nc.alloc_semaphore("sd_%s_%d" % (name, i % self.NDS))
            self.cnt[k] = 0
        return k

    def _wait(self, eng, key, val):
        if self.waited[eng].get(key, 0) >= val:
            return
        self.e[eng].wait_ge(self.sem[key], val)
        self.waited[eng][key] = val
        self.n_ins += 1

    def _sync(self, eng, me, r, w):
        for x in r:
            if x.lw is not None:
                k, v = x.lw
                if k == me and me == "pe":
                    continue
                self._wait(eng, k, v)
        for x in w:
            if x.lw is not None:
                k, v = x.lw
                if k != me or me != "pe":
                    self._wait(eng, k, v)
            for k, v in x.rd.items():
                if k != me or me != "pe":
                    self._wait(eng, k, v)

    def _commit(self, me, val, r, w):
        for x in r:
            if x.rd.get(me, 0) < val:
                x.rd[me] = val
        for x in w:
            x.lw = (me, val)
            x.rd = {}

    max_ops = None
    tot = 0

    def op(self, eng, fn, r=(), w=()):
        self.tot += 1
        if self.max_ops is not None and self.tot > self.max_ops:
            return None
        self._sync(eng, eng, r, w)
        ins = fn(self.e[eng])
        self.cnt[eng] += 1
        ins.then_inc(self.sem[eng], 1)
        self._commit(eng, self.cnt[eng], r, w)
        self.n_ins += 1
        return ins

    def dma(self, q, out, in_, r=(), w=(), sem="ld"):
        self.tot += 1
        if self.max_ops is not None and self.tot > self.max_ops:
            return None
        k = self.dsem(sem)
        if self.cnt[k] > 0:
            self._wait(q, k, self.cnt[k])
        self._sync(q, k, r, w)
        ins = self.e[q].dma_start(out=out, in_=in_)
        self.cnt[k] += 16
        ins.then_inc(self.sem[k], 16)
        self._commit(k, self.cnt[k], r, w)
        self.n_ins += 1
        return ins

    def idma(self, out, out_offset, in_, in_offset, bounds, r=(), w=(), sem="ind"):
        self.tot += 1
        if self.max_ops is not None and self.tot > self.max_ops:
            return None
        k = self.dsem(sem)
        if self.cnt[k] > 0:
            self._wait("pool", k, self.cnt[k])
        self._sync("pool", k, r, w)
        ins = self.e["pool"].indirect_dma_start(out=out, out_offset=out_offset, in_=in_, in_offset=in_offset)
        self.cnt[k] += 16
        ins.then_inc(self.sem[k], 16)
        self._commit(k, self.cnt[k], r, w)
        self.n_ins += 1
        return ins

    def barrier(self, engs=None):
        for eng in (engs or self.ENG):
            for k, v in self.cnt.items():
                if k != eng and v > 0:
                    self._wait(eng, k, v)


class T:
    def __init__(self, h, nres=1):
        self.h = h
        self.res = [Res() for _ in range(nres)]

    def __getitem__(self, k):
        return self.h[k]

    def r(self, i=0):
        return self.res[i]


def build(nb=NB, stop=None, dbg=(), max_ops=None):
    nc = bass.Bass("TRN2", target_bir_lowering=False)
    p = Prog(nc)
    p.max_ops = max_ops

    def din(name, shape, dt=F32):
        return nc.dram_tensor(name, list(shape), dt, kind="ExternalInput").ap()

    x_d = din("x", [nb, S, D])
    ctx_d = din("ctx", [nb, CTX, D])
    cT_d = din("cT", [128, 8, 5])
    w_ada_d = din("w_ada", [D, 6 * D])
    b_ada_d = din("b_ada", [1, 6 * D])
    g4_d = din("g4", [4, D])
    w_in_d = din("w_in", [D, N_IN])
    w_qkp_d = din("w_qkp", [D, 512])
    b_mg_d = din("b_mg", [1, 16])
    nab_d = din("nab", [8, 128, 5 * 640])
    g_head_d = din("g_head", [1, 512])
    w_bna_d = din("w_bna", [512, D])
    w_bml_d = din("w_bml", [512, D])
    w_out_d = din("w_out", [D, D])
    w_r_d = din("w_router", [D, NE])
    b_r_d = din("b_router", [1, NE])
    w_gate_d = din("w_gate", [NE, D, D])
    w_lin_d = din("w_lin", [NE, D, D])
    w_down_d = din("w_down", [NE, D, D])
    bgT_d = din("bgT", [128, 8, NE])
    blT_d = din("blT", [128, 8, NE])
    b_down_d = din("b_down", [NE, D])
    ident_d = din("ident", [128, 128])
    trif_d = din("trif", [128, 128])
    trib_d = din("trib", [128, 128])
    cos_d = din("ropecos", [128, S])
    sin_d = din("ropesin", [128, S])
    tris_d = din("tris", [128, 128])
    ecap_d = din("ecap", [128, NE])
    xe_d = nc.dram_tensor("xe_scr", [NE * CAP, D], BF16).ap()
    ye_d = nc.dram_tensor("ye_scr", [NE * CAP, D], F32).ap()
    gf_d = nc.dram_tensor("gf_scr", [nb, 128, D], F32).ap()
    out_d = nc.dram_tensor("out", [nb, S, D], F32, kind="ExternalOutput").ap()
    dbg_d = {}
    for name, shape in dbg:
        dbg_d[name] = nc.dram_tensor("dbg_" + name, list(shape), F32, kind="ExternalOutput").ap()

    uid = [0]

    def sb(es, name, shape, dt=F32, nres=1):
        uid[0] += 1
        return T(es.enter_context(nc.sbuf_tensor("sb%d_%s" % (uid[0], name), list(shape), dt)), nres)

    PD = [T(nc.alloc_psum_tensor("pd%d" % i, [128, 1024], F32)) for i in range(3)]
    PS = [T(nc.alloc_psum_tensor("psg%d" % i, [128, 512], F32)) for i in range(2)]

    def dump(name, tile_ap, res, dst=None):
        if name in dbg_d:
            p.dma("pool", dbg_d[name] if dst is None else dst, tile_ap, r=(res if isinstance(res, list) else [res]), sem="dbg")

    def wview(ap2d):
        return ap2d.rearrange("(kc p) n -> p kc n", p=128)

    def bc(ap, shape):
        return ap.to_broadcast(list(shape))

    rot = [0, 0]

    def nPD():
        rot[0] += 1
        return PD[rot[0] % 3]

    def nPS():
        rot[1] += 1
        return PS[rot[1] % 2]

    with ExitStack() as G:
        ident = sb(G, "ident", [128, 128])
        identb = sb(G, "identb", [128, 128], BF16)
        trif = sb(G, "trif", [128, 128])
        trib = sb(G, "trib", [128, 128])
        ones32 = sb(G, "ones32", [128, 128])
        scT = sb(G, "scT", [128, 8, 5])
        A_c = sb(G, "A_c", [128, D])
        sh_c = sb(G, "sh_c", [128, D])
        trisb = sb(G, "trisb", [128, 128], BF16)
        onesb = sb(G, "onesb", [128, 128], BF16)
        ecap = sb(G, "ecap", [128, NE])
        msum = sb(G, "msum", [128, NE], BF16)
        RIi = sb(G, "RIi", [128, nb * NT, 4], I32, nres=nb * NT)
        RIw = sb(G, "RIw", [128, nb * NT, 4], F32, nres=nb * NT)
        zres = Res()
        gfres = [Res() for _ in range(nb)]
        p.dma("sp", ecap[:], ecap_d, w=[ecap.r()])
        p.op("dve", lambda e: e.memset(onesb[:], 1.0), w=[onesb.r()])
        p.op("dve", lambda e: e.memset(msum[:], 0.0), w=[msum.r()])
        ztile = sb(G, "ztile", [128, 1024], BF16)
        p.op("dve", lambda e: e.memset(ztile[:], 0.0), w=[ztile.r()])
        p.dma("pool", trisb[:], tris_d, w=[trisb.r()], sem="w")
        zdone = [0]
        NZ = NE * CAP // 128

        def emit_zero(k):
            for _ in range(k):
                cz = zdone[0]
                if cz >= NZ:
                    return
                zdone[0] += 1
                p.dma("sp", xe_d[cz * 128:(cz + 1) * 128, :], ztile[:],
                      r=[ztile.r()], w=[], sem="z")

        def wait_zero():
            emit_zero(NZ)
            for k_, v_ in p.cnt.items():
                if k_.startswith("d:z:") and v_ > 0:
                    p._wait("pool", k_, v_)
        p.dma("sp", ident[:], ident_d, w=[ident.r()])
        p.dma("sp", trif[:], trif_d, w=[trif.r()])
        p.dma("sp", trib[:], trib_d, w=[trib.r()])
        p.dma("sp", scT[:], cT_d, w=[scT.r()])
        p.op("dve", lambda e: e.tensor_copy(out=identb[:], in_=ident[:]), r=[ident.r()], w=[identb.r()])
        p.op("dve", lambda e: e.memset(ones32[:], 1.0), w=[ones32.r()])
        p.op("act", lambda e: e.activation(out=scT[:], in_=scT[:], func=AF.Silu), r=[scT.r()], w=[scT.r()])

        def rstd_of(src_ap, src_res, junk, st):
            p.op("act", lambda e: e.activation(out=junk[:], in_=src_ap, func=AF.Square, scale=1.0 / 32.0,
                                               accum_out=st[:, 0:1]), r=src_res, w=[junk.r(), st.r()])
            p.op("act", lambda e: e.activation(out=st[:, 1:2], in_=st[:, 0:1], func=AF.Sqrt, bias=EPS),
                 r=[st.r()], w=[st.r()])
            p.op("dve", lambda e: e.reciprocal(out=st[:, 1:2], in_=st[:, 1:2]), r=[st.r()], w=[st.r()])

        def ada_mod(j, pieces, tag):
            with ExitStack() as L:
                lh = sb(L, "lh" + tag, [128, 8, 128], BF16)
                g4 = sb(L, "g4" + tag, [128, 4, D])
                p.dma("sp", g4[:], g4_d.partition_broadcast(128), w=[g4.r()])
                for kc in range(8):
                    p.op("dve", lambda e, kc=kc: e.tensor_scalar_mul(
                        out=lh[:, kc, :], in0=ones32[:], scalar1=scT[:, kc, j:j + 1]),
                        r=[scT.r(), ones32.r()], w=[lh.r()])
                wa = [sb(L, "wa%d%s" % (i, tag), [128, 8, 512], BF16) for i in range(2)]
                ba = [sb(L, "ba%d%s" % (i, tag), [1, 512]) for i in range(2)]
                n = 0
                for (blk, out_t, kind, gi) in pieces:
                    for half in range(2):
                        c0 = blk * D + half * 512
                        wt = wa[n % 2]
                        bt = ba[n % 2]
                        n += 1
                        p.dma("pool", wt[:], wview(w_ada_d[:, c0:c0 + 512]), w=[wt.r()], sem="w")
                        p.dma("sp", bt[:], b_ada_d[:, c0:c0 + 512], w=[bt.r()], sem="x")
                        ps = nPD()
                        for kc in range(8):
                            p.op("pe", lambda e, kc=kc, ps=ps, wt=wt: e.matmul(
                                ps[:, 0:512], lhsT=lh[:, kc, :], rhs=wt[:, kc, :], start=(kc == 0), stop=(kc == 7)),
                                r=[lh.r(), wt.r()], w=[ps.r()])
                        o = out_t[:, half * 512:(half + 1) * 512]
                        tmp = sb(L, "adatmp%d%s" % (n, tag), [128, 512])
                        ps2 = nPS()
                        p.op("pe", lambda e, ps2=ps2, bt=bt: e.matmul(
                            ps2[:], lhsT=ones32[0:1, :], rhs=bt[0:1, :], start=True, stop=True),
                            r=[ones32.r(), bt.r()], w=[ps2.r()])
                        p.op("act", lambda e, tmp=tmp, ps2=ps2: e.copy(out=tmp[:], in_=ps2[:]), r=[ps2.r()], w=[tmp.r()])
                        p.op("dve", lambda e, tmp=tmp, ps=ps: e.tensor_tensor(out=tmp[:], in0=ps[:, 0:512], in1=tmp[:], op=ALU.add),
                             r=[ps.r(), tmp.r()], w=[tmp.r()])
                        if kind == "shift":
                            p.op("act", lambda e, o=o, tmp=tmp: e.copy(out=o, in_=tmp[:]), r=[tmp.r()], w=[out_t.r()])
                        elif kind == "scale":
                            gs = g4[:, gi, half * 512:(half + 1) * 512]
                            p.op("dve", lambda e, o=o, tmp=tmp, gs=gs: e.scalar_tensor_tensor(
                                out=o, in0=tmp[:], scalar=1.0, in1=gs, op0=ALU.add, op1=ALU.mult),
                                r=[tmp.r(), g4.r()], w=[out_t.r()])
                        else:
                            gs = g4[:, gi, half * 512:(half + 1) * 512]
                            p.op("dve", lambda e, o=o, tmp=tmp, gs=gs: e.tensor_tensor(
                                out=o, in0=tmp[:], in1=gs, op=ALU.mult),
                                r=[tmp.r(), g4.r()], w=[out_t.r()])
            p.barrier()

        ada_mod(4, [(0, sh_c, "shift", 0), (1, A_c, "scale", 0)], "c")
        x1res = [[Res() for _ in range(NT)] for _ in range(nb)]

        for b in range(nb):
          with ExitStack() as B:
            hT = sb(B, "hT", [128, 8, NTT * 128], BF16, nres=NTT)
            with ExitStack() as M:
              G_m = sb(M, "G_m", [128, D]); A_f = sb(M, "A_f", [128, D]); sh_f = sb(M, "sh_f", [128, D])
              with ExitStack() as PA:
                A_m = sb(PA, "A_m", [128, D]); sh_m = sb(PA, "sh_m", [128, D]); G_f = sb(PA, "G_f", [128, D])
                ada_mod(b, [(0, sh_m, "shift", 0), (1, A_m, "scale", 0), (2, G_m, "gate", 1),
                            (3, sh_f, "shift", 0), (4, A_f, "scale", 2), (5, G_f, "gate", 3)], "b")
                p.dma("sp", gf_d[b], G_f[:], r=[G_f.r()], w=[gfres[b]], sem="o")
                if b == 0:
                    dump("A_m", A_m[:], A_m.r()); dump("sh_m", sh_m[:], sh_m.r()); dump("G_f", G_f[:], G_f.r())
                    dump("A_c", A_c[:], A_c.r())
                with ExitStack() as L:
                    xt = [sb(L, "xt%d" % i, [128, D]) for i in range(3)]
                    sq = [sb(L, "sq%d" % i, [128, D]) for i in range(3)]
                    hb = [sb(L, "hb%d" % i, [128, D], BF16) for i in range(3)]
                    st = [sb(L, "st%d" % i, [128, 2]) for i in range(3)]

                    def a_stage1(t):
                        i = t % 3
                        src = ctx_d[b, t * 128:(t + 1) * 128, :] if t < 2 else x_d[b, (t - 2) * 128:(t - 1) * 128, :]
                        Am, shm = (A_c, sh_c) if t < 2 else (A_m, sh_m)
                        p.dma("sp", xt[i][:], src, w=[xt[i].r()], sem="x")
                        rstd_of(xt[i][:], [xt[i].r()], sq[i], st[i])
                        p.op("dve", lambda e: e.scalar_tensor_tensor(
                            out=sq[i][:], in0=xt[i][:], scalar=st[i][:, 1:2], in1=Am[:], op0=ALU.mult, op1=ALU.mult),
                            r=[xt[i].r(), st[i].r(), Am.r()], w=[sq[i].r()])
                        p.op("dve", lambda e: e.tensor_tensor(out=hb[i][:], in0=sq[i][:], in1=shm[:], op=ALU.add),
                             r=[sq[i].r(), shm.r()], w=[hb[i].r()])

                    def a_stage2(t):
                        i = t % 3
                        ps = nPS()
                        psb = ps[:].bitcast(BF16)
                        for kc in range(8):
                            p.op("pe", lambda e, kc=kc: e.transpose(
                                psb[:, kc * 128:(kc + 1) * 128], hb[i][:, kc * 128:(kc + 1) * 128], identb[:]),
                                r=[hb[i].r(), identb.r()], w=[ps.r()])
                        p.op("act", lambda e: e.copy(
                            out=hT[:, :, t * 128:(t + 1) * 128], in_=psb.rearrange("p (k n) -> p k n", k=8)),
                            r=[ps.r()], w=[hT.r(t)])

                    for t in range(NTT + 1):
                        if t < NTT:
                            a_stage1(t)
                        if t >= 1:
                            a_stage2(t - 1)
                if b == 0 and "hT" in dbg_d:
                    dump("hT", hT[:], hT.res)
                p.barrier()
              if True:
                if stop == "A":
                    break
                if b == 0:
                    emit_zero(128)
                onaT = sb(M, "onaT", [128, 4, S], BF16, nres=NT)

                def mm_fm(ps_ap, ps_res, w_t, wc0, tok0, ntok, nk=8, src=None):
                    src = src or hT
                    tiles = range(tok0 // 128, (tok0 + ntok) // 128)
                    for kc in range(nk):
                        p.op("pe", lambda e, kc=kc: e.matmul(
                            ps_ap, lhsT=w_t[:, kc, wc0:wc0 + 128], rhs=src[:, kc, tok0:tok0 + ntok],
                            start=(kc == 0), stop=(kc == nk - 1)),
                            r=[w_t.r()] + [src.r(t) for t in tiles], w=[ps_res])

                def mm_tm(ps_ap, ps_res, w_t, wc0, ncols, t):
                    for kc in range(8):
                        p.op("pe", lambda e, kc=kc: e.matmul(
                            ps_ap, lhsT=hT[:, kc, t * 128:(t + 1) * 128], rhs=w_t[:, kc, wc0:wc0 + ncols],
                            start=(kc == 0), stop=(kc == 7)),
                            r=[w_t.r(), hT.r(t)], w=[ps_res])

                with ExitStack() as L:
                  qT = sb(L, "qT", [128, 4, S], BF16, nres=4)
                  kT = sb(L, "kT", [128, 4, NTT * 128], BF16, nres=4)
                  vA = sb(L, "vA", [128, NTT, 8, 65], BF16, nres=NTT)
                  p.op("dve", lambda e: e.memset(vA[:, :, :, 64:65], 1.0), w=vA.res)
                  with ExitStack() as L2:
                    wna = sb(L2, "wna", [128, 8, 1536], BF16)
                    p.dma("pool", wna[:, :, 0:1024], wview(w_in_d[:, 0:1024]), w=[wna.r()], sem="w")
                    p.dma("pool", wna[:, :, 1024:1536], wview(w_in_d[:, C_NAQ:C_NAQ + 512]), w=[wna.r()], sem="w")
                    for c in range(4):
                        for tb in range(4):
                            ps = nPD()
                            mm_fm(ps[:, 0:512], ps.r(), wna, 1024 + c * 128, 256 + tb * 512, 512)
                            p.op("act", lambda e, c=c, tb=tb, ps=ps: e.activation(
                                out=qT[:, c, tb * 512:(tb + 1) * 512], in_=ps[:, 0:512], func=AF.Copy, scale=0.125),
                                r=[ps.r()], w=[qT.r(c)])
                        for (t0, n) in [(0, 512), (512, 512), (1024, 512), (1536, 512), (2048, 256)]:
                            ps = nPD()
                            mm_fm(ps[:, 0:n], ps.r(), wna, c * 128, t0, n)
                            p.op("dve", lambda e, c=c, t0=t0, n=n, ps=ps: e.tensor_copy(
                                out=kT[:, c, t0:t0 + n], in_=ps[:, 0:n]), r=[ps.r()], w=[kT.r(c)])
                    for t in range(NTT):
                        ps = nPD()
                        mm_tm(ps[:, 0:512], ps.r(), wna, 512, 512, t)
                        eng = "act" if t % 2 else "dve"
                        if eng == "act":
                            p.op("act", lambda e, t=t, ps=ps: e.copy(
                                out=vA[:, t, :, 0:64], in_=ps[:, 0:512].rearrange("p (h d) -> p h d", h=8)),
                                r=[ps.r()], w=[vA.r(t)])
                        else:
                            p.op("dve", lambda e, t=t, ps=ps: e.tensor_copy(
                                out=vA[:, t, :, 0:64], in_=ps[:, 0:512].rearrange("p (h d) -> p h d", h=8)),
                                r=[ps.r()], w=[vA.r(t)])
                  p.barrier()
                  if True:
                    EBh = [sb(L, "EB%d" % i, [128, 3200], BF16) for i in range(2)]
                    ona = sb(L, "ona", [128, NT, 512], BF16, nres=NT)
                    stg = sb(L, "nabst", [128, 3200])
                    PT = [sb(L, "PT%d" % i, [128, 896], BF16) for i in range(3)]
                    rc = [sb(L, "rc%d" % i, [128, 1]) for i in range(3)]
                    n = 0
                    items = [(h, i) for h in range(8) for i in range(NT)]
                    state = {}

                    def na_scores(n):
                        h, i = items[n]
                        hp = (h % 2) * 64
                        c = h // 2
                        EB = EBh[h % 2]
                        if i == 0:
                            p.dma("sp", stg[:], nab_d[h], w=[stg.r()], sem="x")
                            p.op("act", lambda e, EB=EB: e.activation(out=EB[:], in_=stg[:], func=AF.Exp),
                                 r=[stg.r()], w=[EB.r()])
                            if b == 0 and h >= 1:
                                emit_zero(24)
                        js = min(max(i - 2, 0), 11)
                        cls = i - js if i < 2 or i > 13 else 2
                        pss = PD[n % 3]
                        pt = PT[n % 3]
                        qa = qT[hp:hp + 64, c, i * 128:(i + 1) * 128]
                        for s_i in range(7):
                            kt = (2 + js + s_i) if s_i < 5 else (s_i - 5)
                            p.op("pe", lambda e, s_i=s_i, kt=kt: e.matmul(
                                pss[:, s_i * 128:(s_i + 1) * 128], lhsT=kT[hp:hp + 64, c, kt * 128:(kt + 1) * 128],
                                rhs=qa, start=True, stop=True),
                                r=[kT.r(c), qT.r(c)], w=[pss.r()])
                        p.op("act", lambda e: e.activation(out=pt[:], in_=pss[:, 0:896], func=AF.Exp),
                             r=[pss.r()], w=[pt.r()])
                        p.op("dve", lambda e: e.tensor_tensor(
                            out=pt[:, 0:640], in0=pt[:, 0:640], in1=EB[:, cls * 640:(cls + 1) * 640], op=ALU.mult),
                            r=[pt.r(), EB.r()], w=[pt.r()])
                        state[n] = (pt, js)

                    def na_pv(n):
                        h, i = items[n]
                        pt, js = state.pop(n)
                        pso = PS[n % 2]
                        rcc = rc[n % 3]
                        for s_i in range(7):
                            kt = (2 + js + s_i) if s_i < 5 else (s_i - 5)
                            p.op("pe", lambda e, s_i=s_i, kt=kt: e.matmul(
                                pso[:, 0:65], lhsT=pt[:, s_i * 128:(s_i + 1) * 128], rhs=vA[:, kt, h, :],
                                start=(s_i == 0), stop=(s_i == 6)),
                                r=[pt.r(), vA.r(kt)], w=[pso.r()])
                        p.op("dve", lambda e: e.reciprocal(out=rcc[:], in_=pso[:, 64:65]),
                             r=[pso.r()], w=[rcc.r()])
                        p.op("dve", lambda e: e.tensor_scalar_mul(
                            out=ona[:, i, h * 64:(h + 1) * 64], in0=pso[:, 0:64], scalar1=rcc[:, 0:1]),
                            r=[pso.r(), rcc.r()], w=[ona.r(i)])

                    LA = 2
                    for n in range(len(items) + LA):
                        if n < len(items):
                            na_scores(n)
                        if n >= LA:
                            na_pv(n - LA)
                    for i in range(NT):
                        ps = nPS()
                        psb = ps[:].bitcast(BF16)
                        for c4 in range(4):
                            p.op("pe", lambda e, c4=c4, i=i, psb=psb: e.transpose(
                                psb[:, c4 * 128:(c4 + 1) * 128], ona[:, i, c4 * 128:(c4 + 1) * 128], identb[:]),
                                r=[ona.r(i), identb.r()], w=[ps.r()])
                        p.op("act", lambda e, i=i, psb=psb: e.copy(
                            out=onaT[:, :, i * 128:(i + 1) * 128], in_=psb[:, 0:512].rearrange("p (k n) -> p k n", k=4)),
                            r=[ps.r()], w=[onaT.r(i)])
                    if b == 0 and "ona" in dbg_d:
                        dump("ona", ona[:], ona.res)
                p.barrier()
                if stop == "B":
                    break
                omlT = sb(M, "omlT", [128, 4, S], BF16, nres=NT)

                with ExitStack() as L:
                    mqT = sb(L, "mqT", [128, 2, S], BF16, nres=2)
                    mkT = sb(L, "mkT", [128, 2, S], BF16, nres=2)
                    ktm = sb(L, "ktm", [128, NTT, 256], BF16, nres=NTT)
                    vM = sb(L, "vM", [128, NTT, 4, 129], BF16, nres=NTT)
                    osig = sb(L, "osig", [128, NT, 512], BF16, nres=NT)
                    gts = sb(L, "gts", [128, NTT, 16])
                    LI = sb(L, "LI", [128, NTT, 8])
                    LFn = sb(L, "LFn", [128, NTT, 8])
                    Bn = sb(L, "Bn", [128, NTT, 8])
                    EBt = sb(L, "EBt", [128, NTT, 8])
                    ES = sb(L, "ES", [128, NTT, 8])
                    EBL = sb(L, "EBL", [128, NTT, 8])
                    ghd = sb(L, "ghd", [128, 512])
                    p.dma("sp", ghd[:], g_head_d.partition_broadcast(128), w=[ghd.r()], sem="x")
                    p.op("dve", lambda e: e.memset(vM[:, :, :, 128:129], 1.0), w=vM.res)
                    with ExitStack() as L2:
                        wq = sb(L2, "wq", [128, 8, 256], BF16); wqp = sb(L2, "wqp", [128, 8, 256], BF16)
                        wk = sb(L2, "wk", [128, 8, 256], BF16); wkp = sb(L2, "wkp", [128, 8, 256], BF16)
                        cosT = sb(L2, "cosT", [128, 512]); sinT = sb(L2, "sinT", [128, 512])
                        rt = [sb(L2, "rt%d" % i, [128, 512]) for i in range(2)]
                        p.dma("pool", wq[:], wview(w_in_d[:, C_MLQ:C_MLQ + 256]), w=[wq.r()], sem="w")
                        p.dma("pool", wqp[:], wview(w_qkp_d[:, 0:256]), w=[wqp.r()], sem="w")
                        p.dma("pool", wk[:], wview(w_in_d[:, C_MLK:C_MLK + 256]), w=[wk.r()], sem="w")
                        p.dma("pool", wkp[:], wview(w_qkp_d[:, 256:512]), w=[wkp.r()], sem="w")
                        for (w_a, w_b, dst) in ((wq, wqp, mqT), (wk, wkp, mkT)):
                            for c in range(2):
                                for tb in range(4):
                                    ps = nPD()
                                    mm_fm(ps[:, 0:512], ps.r(), w_a, c * 128, 256 + tb * 512, 512)
                                    mm_fm(ps[:, 512:1024], ps.r(), w_b, c * 128, 256 + tb * 512, 512)
                                    cs_ = slice(tb * 512, (tb + 1) * 512)
                                    p.dma("sp", cosT[:], cos_d[:, cs_], w=[cosT.r()], sem="x")
                                    p.dma("sp", sinT[:], sin_d[:, cs_], w=[sinT.r()], sem="x")
                                    p.op("dve", lambda e, ps=ps, cs_=cs_: e.tensor_tensor(
                                        out=rt[0][:], in0=ps[:, 0:512], in1=cosT[:], op=ALU.mult),
                                        r=[ps.r(), cosT.r()], w=[rt[0].r()])
                                    p.op("dve", lambda e, ps=ps, cs_=cs_: e.tensor_tensor(
                                        out=rt[1][:], in0=ps[:, 512:1024], in1=sinT[:], op=ALU.mult),
                                        r=[ps.r(), sinT.r()], w=[rt[1].r()])
                                    p.op("dve", lambda e, dst=dst, c=c, cs_=cs_: e.tensor_tensor(
                                        out=dst[:, c, cs_], in0=rt[0][:], in1=rt[1][:], op=ALU.add),
                                        r=[rt[0].r(), rt[1].r()], w=[dst.r(c)])
                        for t in range(NT):
                            ps = nPS()
                            psb = ps[:].bitcast(BF16)
                            for c in range(2):
                                p.op("pe", lambda e, c=c, t=t, psb=psb: e.transpose(
                                    psb[:, c * 128:(c + 1) * 128], mkT[:, c, t * 128:(t + 1) * 128], identb[:]),
                                    r=[mkT.r(c), identb.r()], w=[ps.r()])
                            p.op("act", lambda e, t=t, psb=psb: e.copy(out=ktm[:, 2 + t, :], in_=psb[:, 0:256]),
                                 r=[ps.r()], w=[ktm.r(2 + t)])
                        for t in range(2):
                            ps = nPS()
                            mm_tm(ps[:, 0:256], ps.r(), wk, 0, 256, t)
                            p.op("act", lambda e, t=t, ps=ps: e.copy(out=ktm[:, t, :], in_=ps[:, 0:256]),
                                 r=[ps.r()], w=[ktm.r(t)])
                    p.barrier()
                    if stop == "C1":
                        break
                    with ExitStack() as L2:
                        wv = sb(L2, "wv", [128, 8, 512], BF16); wo = sb(L2, "wo", [128, 8, 512], BF16)
                        wg = sb(L2, "wgt", [128, 8, 16], BF16)
                        bmg = sb(L2, "bmg", [1, 16])
                        p.dma("pool", wv[:], wview(w_in_d[:, C_MLV:C_MLV + 512]), w=[wv.r()], sem="w")
                        p.dma("pool", wo[:], wview(w_in_d[:, C_MLO:C_MLO + 512]), w=[wo.r()], sem="w")
                        p.dma("pool", wg[:], wview(w_in_d[:, C_MLG:C_MLG + 16]), w=[wg.r()], sem="w")
                        p.dma("sp", bmg[:], b_mg_d, w=[bmg.r()], sem="x")
                        for t in range(NTT):
                            ps = nPD()
                            mm_tm(ps[:, 0:512], ps.r(), wv, 0, 512, t)
                            p.op("dve", lambda e, t=t, ps=ps: e.tensor_copy(
                                out=vM[:, t, :, 0:128], in_=ps[:, 0:512].rearrange("p (h d) -> p h d", h=4)),
                                r=[ps.r()], w=[vM.r(t)])
                            if t >= 2:
                                mm_tm(ps[:, 512:1024], ps.r(), wo, 0, 512, t)
                                p.op("act", lambda e, t=t, ps=ps: e.activation(
                                    out=osig[:, t - 2, :], in_=ps[:, 512:1024], func=AF.Sigmoid),
                                    r=[ps.r()], w=[osig.r(t - 2)])
                            ps2 = nPS()
                            for kc in range(8):
                                p.op("pe", lambda e, kc=kc, t=t, ps2=ps2: e.matmul(
                                    ps2[:, 0:16], lhsT=hT[:, kc, t * 128:(t + 1) * 128], rhs=wg[:, kc, :],
                                    start=(kc == 0), stop=(kc == 7)), r=[wg.r(), hT.r(t)], w=[ps2.r()])
                            p.op("pe", lambda e, ps2=ps2: e.matmul(
                                ps2[:, 16:32], lhsT=ones32[0:1, :], rhs=bmg[0:1, :], start=True, stop=True),
                                r=[ones32.r(), bmg.r()], w=[ps2.r()])
                            p.op("act", lambda e, t=t, ps2=ps2: e.copy(out=gts[:, t, :], in_=ps2[:, 0:16]),
                                 r=[ps2.r()], w=[gts.r()])
                            p.op("dve", lambda e, t=t, ps2=ps2: e.tensor_tensor(
                                out=gts[:, t, :], in0=gts[:, t, :], in1=ps2[:, 16:32], op=ALU.add),
                                r=[ps2.r(), gts.r()], w=[gts.r()])
                    p.barrier()
                    if stop == "C2":
                        break
                    gv = gts[:].rearrange("p t (k h) -> p t k h", k=4)
                    p.op("act", lambda e: e.activation(out=gts[:], in_=gts[:], func=AF.Tanh, scale=1.0 / 15.0),
                         r=[gts.r()], w=[gts.r()])
                    for d_ in range(2):
                        p.op("dve", lambda e, d_=d_: e.tensor_scalar_mul(
                            out=LI[:, :, d_ * 4:(d_ + 1) * 4], in0=gv[:, :, 2 * d_, :], scalar1=15.0),
                            r=[gts.r()], w=[LI.r()])
                        p.op("act", lambda e, d_=d_: e.activation(
                            out=LFn[:, :, d_ * 4:(d_ + 1) * 4], in_=gv[:, :, 2 * d_ + 1, :], func=AF.Exp, scale=-15.0),
                            r=[gts.r()], w=[LFn.r()])
                    p.op("act", lambda e: e.activation(out=LFn[:], in_=LFn[:], func=AF.Ln, bias=1.0),
                         r=[LFn.r()], w=[LFn.r()])
                    psc = nPS()
                    for t in range(NTT):
                        for d_, tri in ((0, trif), (1, trib)):
                            p.op("pe", lambda e, t=t, d_=d_, tri=tri: e.matmul(
                                psc[:, t * 8 + d_ * 4:t * 8 + d_ * 4 + 4], lhsT=tri[:], rhs=LFn[:, t, d_ * 4:(d_ + 1) * 4],
                                start=True, stop=True), r=[tri.r(), LFn.r()], w=[psc.r()])
                    p.op("dve", lambda e: e.tensor_copy(out=Bn[:].rearrange("p t k -> p (t k)"), in_=psc[:, 0:NTT * 8]),
                         r=[psc.r()], w=[Bn.r()])
                    psl = nPS()
                    p.op("pe", lambda e: e.matmul(psl[:, 0:NTT * 8], lhsT=ones32[:], rhs=LFn[:].rearrange("p t k -> p (t k)"),
                                                  start=True, stop=True), r=[ones32.r(), LFn.r()], w=[psl.r()])
                    p.op("act", lambda e: e.activation(out=EBL[:].rearrange("p t k -> p (t k)"), in_=psl[:, 0:NTT * 8],
                                                       func=AF.Exp, scale=-1.0), r=[psl.r()], w=[EBL.r()])
                    p.op("act", lambda e: e.activation(out=EBt[:], in_=Bn[:], func=AF.Exp, scale=-1.0),
                         r=[Bn.r()], w=[EBt.r()])
                    p.op("dve", lambda e: e.tensor_tensor(out=ES[:], in0=LI[:], in1=Bn[:], op=ALU.add),
                         r=[LI.r(), Bn.r()], w=[ES.r()])
                    p.op("act", lambda e: e.activation(out=ES[:], in_=ES[:], func=AF.Exp, bias=float(-np.log(8.0))),
                         r=[ES.r()], w=[ES.r()])
                    if stop == "C3":
                        p.barrier()
                        break
                    if b == 0:
                        emit_zero(NZ)
                    Hf = sb(L, "Hf", [128, NT, 512], BF16, nres=NT)
                    Cst = [sb(L, "Cst%d" % d_, [128, 4, 129]) for d_ in range(2)]
                    Cbf = [sb(L, "Cbf%d" % d_, [128, 4, 129], BF16) for d_ in range(2)]
                    vp = [sb(L, "vp%d" % d_, [128, 4, 129], BF16) for d_ in range(2)]
                    sTm = [sb(L, "sTm%d" % d_, [128, 4, 128], BF16) for d_ in range(2)]
                    sm = [sb(L, "sm%d" % d_, [128, 4, 4]) for d_ in range(2)]
                    Hs = [sb(L, "Hs%d" % i, [128, 512]) for i in range(2)]
                    Hq = [sb(L, "Hq%d" % i, [128, 512]) for i in range(1)]
                    fs = [sb(L, "fs%d" % i, [128, 8]) for i in range(2)]
                    omb = [sb(L, "omb%d" % i, [128, 512], BF16) for i in range(2)]
                    bwd_order = [1, 0] + list(range(NTT - 1, 1, -1))
                    tri4 = [sb(L, "tri4%d" % d_, [128, 4, 128], BF16) for d_ in range(2)]
                    for d_, tr_ in ((0, trif), (1, trib)):
                        for hd in range(4):
                            p.op("dve", lambda e, d_=d_, tr_=tr_, hd=hd: e.tensor_copy(out=tri4[d_][:, hd, :], in_=tr_[:]),
                                 r=[tr_.r()], w=[tri4[d_].r()])
                    psN_ = [PD[0], PD[2]]
                    psU_ = PD[1]
                    for d_ in range(2):
                        if stop in ("C4", "C6", "C7") and d_ == 1:
                            break
                        for step in range(NTT):
                            if (stop == "C6" and step == 2) or (stop == "C7" and step == 3):
                                break
                            t = step if d_ == 0 else bwd_order[step]
                            tri = trif if d_ == 0 else trib
                            psN = psN_[d_]
                            psS = PS[d_]
                            C_, Cb_, vp_, sT_, sm_ = Cst[d_], Cbf[d_], vp[d_], sTm[d_], sm[d_]
                            p.op(POOLENG, lambda e, t=t, d_=d_, vp_=vp_: e.tensor_tensor(
                                out=vp_[:], in0=vM[:, t, :, :], in1=bc(ES[:, t, d_ * 4:(d_ + 1) * 4].unsqueeze(2), [128, 4, 129]),
                                op=ALU.mult), r=[vM.r(t), ES.r()], w=[vp_.r()])
                            if t >= 2:
                                lt = t - 2
                                for hd in range(4):
                                    hp, c = (hd % 2) * 64, hd // 2
                                    p.op("pe", lambda e, hd=hd, hp=hp, c=c, lt=lt, psS=psS: e.matmul(
                                        psS[:, hd * 128:(hd + 1) * 128], lhsT=mkT[hp:hp + 64, c, lt * 128:(lt + 1) * 128],
                                        rhs=mqT[hp:hp + 64, c, lt * 128:(lt + 1) * 128], start=True, stop=True),
                                        r=[mkT.r(c), mqT.r(c)], w=[psS.r()])
                                    p.op("pe", lambda e, hd=hd, c=c, t=t, vp_=vp_: e.matmul(
                                        psU_[:, hd * 256:hd * 256 + 129], lhsT=ktm[:, t, c * 128:(c + 1) * 128], rhs=vp_[:, hd, :],
                                        start=True, stop=True), r=[ktm.r(t), vp_.r()], w=[psU_.r()])
                                p.op("dve", lambda e, sT_=sT_, psS=psS, d_=d_: e.tensor_tensor(
                                    out=sT_[:].rearrange("p h n -> p (h n)"), in0=psS[:, 0:512],
                                    in1=tri4[d_][:].rearrange("p h n -> p (h n)"), op=ALU.mult),
                                    r=[psS.r(), tri4[d_].r()], w=[sT_.r()])
                                for hd in range(4):
                                    hp, c = (hd % 2) * 64, hd // 2
                                    p.op("pe", lambda e, hd=hd, psN=psN, sT_=sT_, vp_=vp_: e.matmul(
                                        psN[:, hd * 256:hd * 256 + 129], lhsT=sT_[:, hd, :], rhs=vp_[:, hd, :],
                                        start=True, stop=(step == 0)), r=[sT_.r(), vp_.r()], w=[psN.r()])
                                    if step > 0:
                                        p.op("pe", lambda e, hd=hd, hp=hp, c=c, lt=lt, psN=psN, Cb_=Cb_: e.matmul(
                                            psN[:, hd * 256:hd * 256 + 129], lhsT=mqT[hp:hp + 64, c, lt * 128:(lt + 1) * 128],
                                            rhs=Cb_[hp:hp + 64, hd, :], start=False, stop=True),
                                            r=[mqT.r(c), Cb_.r()], w=[psN.r()])
                                nv = psN[:].rearrange("p (h x) -> p h x", x=256)
                                p.op("dve", lambda e, nv=nv, sm_=sm_, t=t, d_=d_: e.tensor_tensor(
                                    out=sm_[:, :, 0:1], in0=nv[:, :, 128:129], in1=EBt[:, t, d_ * 4:(d_ + 1) * 4].unsqueeze(2),
                                    op=ALU.mult), r=[psN.r(), EBt.r()], w=[sm_.r()])
                                p.op("dve", lambda e, sm_=sm_: e.tensor_scalar(
                                    out=sm_[:, :, 1:2], in0=sm_[:, :, 0:1], scalar1=-1.0, scalar2=1.0, op0=ALU.mult, op1=ALU.max),
                                    r=[sm_.r()], w=[sm_.r()])
                                p.op("dve", lambda e, sm_=sm_: e.tensor_tensor(
                                    out=sm_[:, :, 2:3], in0=sm_[:, :, 1:2], in1=sm_[:, :, 0:1], op=ALU.max),
                                    r=[sm_.r()], w=[sm_.r()])
                                p.op("dve", lambda e, sm_=sm_: e.reciprocal(out=sm_[:, :, 3:4], in_=sm_[:, :, 2:3]),
                                     r=[sm_.r()], w=[sm_.r()])
                                p.op("dve", lambda e, sm_=sm_, t=t, d_=d_: e.tensor_tensor(
                                    out=sm_[:, :, 0:1], in0=sm_[:, :, 3:4], in1=EBt[:, t, d_ * 4:(d_ + 1) * 4].unsqueeze(2),
                                    op=ALU.mult), r=[sm_.r(), EBt.r()], w=[sm_.r()])
                                if d_ == 0:
                                    p.op("dve", lambda e, nv=nv, sm_=sm_, lt=lt: e.tensor_tensor(
                                        out=Hf[:, lt, :].rearrange("p (h n) -> p h n", h=4), in0=nv[:, :, 0:128],
                                        in1=bc(sm_[:, :, 0:1], [128, 4, 128]), op=ALU.mult),
                                        r=[psN.r(), sm_.r()], w=[Hf.r(lt)])
                                elif stop != "C5":
                                    hs, hq, f_, ob = Hs[lt % 2], Hq[0], fs[lt % 2], omb[lt % 2]
                                    p.op("dve", lambda e, nv=nv, sm_=sm_, hs=hs: e.tensor_tensor(
                                        out=hs[:].rearrange("p (h n) -> p h n", h=4), in0=nv[:, :, 0:128],
                                        in1=bc(sm_[:, :, 0:1], [128, 4, 128]), op=ALU.mult),
                                        r=[psN.r(), sm_.r()], w=[hs.r()])
                                    p.op(POOLENG, lambda e, hs=hs, lt=lt: e.tensor_tensor(
                                        out=hs[:], in0=hs[:], in1=Hf[:, lt, :], op=ALU.add),
                                        r=[hs.r(), Hf.r(lt)], w=[hs.r()])
                                    p.op(POOLENG, lambda e, hs=hs, hq=hq: e.tensor_tensor(out=hq[:], in0=hs[:], in1=hs[:], op=ALU.mult),
                                         r=[hs.r()], w=[hq.r()])
                                    p.op("dve", lambda e, hq=hq, f_=f_: e.reduce_sum(
                                        out=f_[:, 0:4], in_=hq[:].rearrange("p (h n) -> p h n", h=4), axis=AX.X),
                                        r=[hq.r()], w=[f_.r()])
                                    p.op("act", lambda e, f_=f_: e.activation(out=f_[:, 4:8], in_=f_[:, 0:4], func=AF.Sqrt,
                                                                           scale=1.0 / 128.0, bias=EPS), r=[f_.r()], w=[f_.r()])
                                    p.op("dve", lambda e, f_=f_: e.reciprocal(out=f_[:, 4:8], in_=f_[:, 4:8]), r=[f_.r()], w=[f_.r()])
                                    p.op("dve", lambda e, hs=hs, f_=f_: e.tensor_tensor(
                                        out=hs[:].rearrange("p (h n) -> p h n", h=4), in0=hs[:].rearrange("p (h n) -> p h n", h=4),
                                        in1=bc(f_[:, 4:8].unsqueeze(2), [128, 4, 128]), op=ALU.mult),
                                        r=[hs.r(), f_.r()], w=[hs.r()])
                                    p.op(POOLENG, lambda e, hs=hs: e.tensor_tensor(out=hs[:], in0=hs[:], in1=ghd[:], op=ALU.mult),
                                         r=[hs.r(), ghd.r()], w=[hs.r()])
                                    p.op(POOLENG, lambda e, hs=hs, ob=ob, lt=lt: e.tensor_tensor(
                                        out=ob[:], in0=hs[:], in1=osig[:, lt, :], op=ALU.mult),
                                        r=[hs.r(), osig.r(lt)], w=[ob.r()])
                                    ps = psS
                                    psb = ps[:].bitcast(BF16)
                                    for c4 in range(4):
                                        p.op("pe", lambda e, c4=c4, ob=ob, psb=psb: e.transpose(
                                            psb[:, c4 * 128:(c4 + 1) * 128], ob[:, c4 * 128:(c4 + 1) * 128], identb[:]),
                                            r=[ob.r(), identb.r()], w=[ps.r()])
                                    p.op("act", lambda e, lt=lt, psb=psb: e.copy(
                                        out=omlT[:, :, lt * 128:(lt + 1) * 128],
                                        in_=psb[:, 0:512].rearrange("p (k n) -> p k n", k=4)),
                                        r=[ps.r()], w=[omlT.r(lt)])
                            for hd in range(4):
                                c = hd // 2
                                if t >= 2:
                                    break
                                p.op("pe", lambda e, hd=hd, c=c, t=t, vp_=vp_: e.matmul(
                                    psU_[:, hd * 256:hd * 256 + 129], lhsT=ktm[:, t, c * 128:(c + 1) * 128], rhs=vp_[:, hd, :],
                                    start=True, stop=True), r=[ktm.r(t), vp_.r()], w=[psU_.r()])
                            uv = psU_[:].rearrange("p (h x) -> p h x", x=256)[:, :, 0:129]
                            ebl = bc(EBL[:, t, d_ * 4:(d_ + 1) * 4].unsqueeze(2), [128, 4, 129])
                            if step == 0:
                                p.op("dve", lambda e, C_=C_, uv=uv, ebl=ebl: e.tensor_tensor(out=C_[:], in0=uv, in1=ebl, op=ALU.mult),
                                     r=[psU_.r(), EBL.r()], w=[C_.r()])
                            else:
                                p.op("dve", lambda e, C_=C_, uv=uv: e.tensor_tensor(out=C_[:], in0=uv, in1=C_[:], op=ALU.add),
                                     r=[psU_.r(), C_.r()], w=[C_.r()])
                                p.op("dve", lambda e, C_=C_, ebl=ebl: e.tensor_tensor(out=C_[:], in0=C_[:], in1=ebl, op=ALU.mult),
                                     r=[C_.r(), EBL.r()], w=[C_.r()])
                            p.op("act", lambda e, C_=C_, Cb_=Cb_: e.copy(out=Cb_[:], in_=C_[:]), r=[C_.r()], w=[Cb_.r()])
                    if b == 0 and "omlT" in dbg_d:
                        dump("omlT", omlT[:], omlT.res)
                p.barrier()
                if stop in ("C", "C4", "C5", "C6", "C7"):
                    break

                with ExitStack() as L:
                    wbn = sb(L, "wbn", [128, 4, D], BF16); wbm = sb(L, "wbm", [128, 4, D], BF16)
                    wout = sb(L, "wout", [128, 8, D], BF16)
                    wr = sb(L, "wr", [128, 8, NE]); br = sb(L, "br", [1, NE])
                    p.dma("pool", wbn[:], wview(w_bna_d), w=[wbn.r()], sem="w")
                    p.dma("pool", wbm[:], wview(w_bml_d), w=[wbm.r()], sem="w")
                    p.dma("pool", wout[:], wview(w_out_d), w=[wout.r()], sem="w")
                    p.dma("sp", wr[:], wview(w_r_d), w=[wr.r()], sem="x")
                    p.dma("sp", br[:], b_r_d, w=[br.r()], sem="x")
                    wgn = [sb(L, "wgn%d" % i, [128, 8, 128], BF16) for i in range(2)]
                    wgm = [sb(L, "wgm%d" % i, [128, 8, 128], BF16) for i in range(2)]
                    mT = sb(L, "mT", [128, 8, S], BF16, nres=32)
                    sg = [sb(L, "sg%d" % i, [128, 512]) for i in range(2)]
                    m1 = [sb(L, "m1%d" % i, [128, 512]) for i in range(2)]
                    xt = [sb(L, "xd", [128, D])] * 2
                    yt = [sb(L, "yd%d" % i, [128, D]) for i in range(2)]
                    jk = sb(L, "jk", [128, D], BF16)
                    h2 = [sb(L, "h2", [128, D])] * 2
                    h2hi = [sb(L, "h2hi%d" % i, [128, D], BF16) for i in range(2)]
                    h2lo = [sb(L, "h2lo", [128, D], BF16)] * 2
                    h2Tlo = [sb(L, "h2Tlo", [128, 8, 128], BF16)] * 2
                    wrhi = sb(L, "wrhi", [128, 8, NE], BF16); wrlo = sb(L, "wrlo", [128, 8, NE], BF16)
                    p.op("dve", lambda e: e.tensor_copy(out=wrhi[:], in_=wr[:]), r=[wr.r()], w=[wrhi.r()])
                    p.op("dve", lambda e: e.tensor_tensor(out=wrlo[:], in0=wr[:], in1=wrhi[:], op=ALU.subtract),
                         r=[wr.r(), wrhi.r()], w=[wrlo.r()])
                    st = [sb(L, "std%d" % i, [128, 4]) for i in range(2)]
                    lg = [sb(L, "lg%d" % i, [128, 3, NE]) for i in range(2)]
                    t8 = [sb(L, "t8%d" % i, [128, 20]) for i in range(2)]
                    mkb = [sb(L, "mkb%d" % i, [128, NE], BF16) for i in range(2)]
                    oh4 = [sb(L, "oh4%d" % i, [128, 4, NE]) for i in range(2)]
                    n = 0
                    for dc in range(8):
                        n += 1
                        a_, b_ = wgn[n % 2], wgm[n % 2]
                        p.dma("pool", a_[:], wview(w_in_d[:, C_GNA + dc * 128:C_GNA + (dc + 1) * 128]), w=[a_.r()], sem="w")
                        p.dma("pool", b_[:], wview(w_in_d[:, C_GML + dc * 128:C_GML + (dc + 1) * 128]), w=[b_.r()], sem="w")
                        for tb in range(4):
                            tok0 = 256 + tb * 512
                            pa, pb = nPD(), nPD()
                            mm_fm(pa[:, 0:512], pa.r(), a_, 0, tok0, 512)
                            mm_fm(pa[:, 512:1024], pa.r(), wbn, dc * 128, tb * 512, 512, nk=4, src=onaT)
                            mm_fm(pb[:, 0:512], pb.r(), b_, 0, tok0, 512)
                            mm_fm(pb[:, 512:1024], pb.r(), wbm, dc * 128, tb * 512, 512, nk=4, src=omlT)
                            p.op("act", lambda e, pa=pa: e.activation(out=sg[0][:], in_=pa[:, 0:512], func=AF.Sigmoid),
                                 r=[pa.r()], w=[sg[0].r()])
                            p.op("act", lambda e, pb=pb: e.activation(out=sg[1][:], in_=pb[:, 0:512], func=AF.Sigmoid),
                                 r=[pb.r()], w=[sg[1].r()])
                            p.op("dve", lambda e, pa=pa: e.tensor_tensor(out=m1[0][:], in0=pa[:, 512:1024], in1=sg[0][:], op=ALU.mult),
                                 r=[pa.r(), sg[0].r()], w=[m1[0].r()])
                            p.op("dve", lambda e, pb=pb: e.tensor_tensor(out=m1[1][:], in0=pb[:, 512:1024], in1=sg[1][:], op=ALU.mult),
                                 r=[pb.r(), sg[1].r()], w=[m1[1].r()])
                            p.op(POOLENG, lambda e, dc=dc, tb=tb: e.tensor_tensor(
                                out=mT[:, dc, tb * 512:(tb + 1) * 512], in0=m1[0][:], in1=m1[1][:], op=ALU.add),
                                r=[m1[0].r(), m1[1].r()], w=[mT.r(dc * 4 + tb)])
                    for tb in range(4):
                        for ti in range(4):
                            t = tb * 4 + ti
                            i = t % 2
                            py = PD[2]
                            for half in range(2):
                                for kc in range(8):
                                    p.op("pe", lambda e, kc=kc, half=half, t=t, py=py: e.matmul(
                                        py[:, half * 512:(half + 1) * 512], lhsT=mT[:, kc, t * 128:(t + 1) * 128],
                                        rhs=wout[:, kc, half * 512:(half + 1) * 512], start=(kc == 0), stop=(kc == 7)),
                                        r=[mT.r(kc * 4 + tb), wout.r()], w=[py.r()])
                            p.dma("sp", xt[i][:], x_d[b, t * 128:(t + 1) * 128, :], w=[xt[i].r()], sem="x")
                            rstd_of(py[:], [py.r()], jk, st[i])
                            p.op("dve", lambda e, i=i, py=py: e.scalar_tensor_tensor(
                                out=yt[i][:], in0=py[:], scalar=st[i][:, 1:2], in1=G_m[:], op0=ALU.mult, op1=ALU.mult),
                                r=[py.r(), st[i].r(), G_m.r()], w=[yt[i].r()])
                            p.op(POOLENG, lambda e, i=i: e.tensor_tensor(out=yt[i][:], in0=yt[i][:], in1=xt[i][:], op=ALU.add),
                                 r=[yt[i].r(), xt[i].r()], w=[yt[i].r()])
                            p.dma("sp", out_d[b, t * 128:(t + 1) * 128, :], yt[i][:], r=[yt[i].r()], w=[x1res[b][t]], sem="o")
                            if b == 0 and "x1" in dbg_d:
                                dump("x1", yt[i][:], yt[i].r(), dst=dbg_d["x1"][t])
                            rstd_of(yt[i][:], [yt[i].r()], jk, st[i])
                            p.op("dve", lambda e, i=i: e.scalar_tensor_tensor(
                                out=h2[i][:], in0=yt[i][:], scalar=st[i][:, 1:2], in1=A_f[:], op0=ALU.mult, op1=ALU.mult),
                                r=[yt[i].r(), st[i].r(), A_f.r()], w=[h2[i].r()])
                            p.op(POOLENG, lambda e, i=i: e.tensor_tensor(out=h2[i][:], in0=h2[i][:], in1=sh_f[:], op=ALU.add),
                                 r=[h2[i].r(), sh_f.r()], w=[h2[i].r()])
                            p.op("act", lambda e, i=i: e.copy(out=h2hi[i][:], in_=h2[i][:]), r=[h2[i].r()], w=[h2hi[i].r()])
                            p.op("dve", lambda e, i=i: e.tensor_tensor(out=h2lo[i][:], in0=h2[i][:], in1=h2hi[i][:], op=ALU.subtract),
                                 r=[h2[i].r(), h2hi[i].r()], w=[h2lo[i].r()])
                            pa_ = PS[i]
                            pab = pa_[:].bitcast(BF16)
                            for kc in range(8):
                                p.op("pe", lambda e, kc=kc, i=i, pab=pab: e.transpose(
                                    pab[:, kc * 128:(kc + 1) * 128], h2hi[i][:, kc * 128:(kc + 1) * 128], identb[:]),
                                    r=[h2hi[i].r(), identb.r()], w=[pa_.r()])
                            p.op("act", lambda e, t=t, pab=pab: e.copy(
                                out=hT[:, :, (2 + t) * 128:(3 + t) * 128], in_=pab.rearrange("p (k n) -> p k n", k=8)),
                                r=[pa_.r()], w=[hT.r(2 + t)])
                            pb_ = PD[i]
                            pbb = pb_[:].bitcast(BF16)
                            for kc in range(8):
                                p.op("pe", lambda e, kc=kc, i=i, pbb=pbb: e.transpose(
                                    pbb[:, kc * 128:(kc + 1) * 128], h2lo[i][:, kc * 128:(kc + 1) * 128], identb[:]),
                                    r=[h2lo[i].r(), identb.r()], w=[pb_.r()])
                            p.op("dve", lambda e, i=i, pbb=pbb: e.tensor_copy(
                                out=h2Tlo[i][:], in_=pbb[:, 0:1024].rearrange("p (k n) -> p k n", k=8)),
                                r=[pb_.r()], w=[h2Tlo[i].r()])
                            pl = PS[1 - i]
                            nmm = 0
                            for kc in range(8):
                                for (lh_, lres, w_) in ((hT[:, kc, (2 + t) * 128:(3 + t) * 128], hT.r(2 + t), wrhi),
                                                        (h2Tlo[i][:, kc, :], h2Tlo[i].r(), wrhi),
                                                        (hT[:, kc, (2 + t) * 128:(3 + t) * 128], hT.r(2 + t), wrlo)):
                                    nmm += 1
                                    p.op("pe", lambda e, kc=kc, lh_=lh_, w_=w_, pl=pl, nmm=nmm: e.matmul(
                                        pl[:, 0:NE], lhsT=lh_, rhs=w_[:, kc, :], start=(nmm == 1), stop=(nmm == 24)),
                                        r=[lres, w_.r()], w=[pl.r()])
                            p.op("pe", lambda e, pl=pl: e.matmul(pl[:, 32:64], lhsT=ones32[0:1, :], rhs=br[0:1, :], start=True, stop=True),
                                 r=[ones32.r(), br.r()], w=[pl.r()])
                            L_, T_ = lg[i], t8[i]
                            p.op("act", lambda e, L_=L_, pl=pl: e.copy(out=L_[:, 0, :], in_=pl[:, 0:NE]), r=[pl.r()], w=[L_.r()])
                            p.op("dve", lambda e, L_=L_, pl=pl: e.tensor_tensor(out=L_[:, 0, :], in0=L_[:, 0, :], in1=pl[:, 32:64], op=ALU.add),
                                 r=[pl.r(), L_.r()], w=[L_.r()])
                            if b == 0 and "logits" in dbg_d:
                                dump("logits", L_[:, 0, :], L_.r(), dst=dbg_d["logits"][t])
                            p.op("dve", lambda e, L_=L_, T_=T_: e.max(out=T_[:, 0:8], in_=L_[:, 0, :]), r=[L_.r()], w=[T_.r()])
                            gt = b * NT + t
                            mk_, oh_ = mkb[i], oh4[i]
                            p.op("dve", lambda e, L_=L_, T_=T_: e.tensor_scalar(
                                out=L_[:, 1, :], in0=L_[:, 0, :], scalar1=T_[:, 3:4], scalar2=None, op0=ALU.is_ge),
                                r=[L_.r(), T_.r()], w=[L_.r()])
                            p.op("dve", lambda e, L_=L_, mk_=mk_: e.tensor_copy(out=mk_[:], in_=L_[:, 1, :]), r=[L_.r()], w=[mk_.r()])
                            p.op("pe", lambda e, pl=pl, mk_=mk_: e.matmul(pl[:, 64:96], lhsT=trisb[:], rhs=mk_[:], start=True, stop=False),
                                 r=[trisb.r(), mk_.r()], w=[pl.r()])
                            p.op("pe", lambda e, pl=pl: e.matmul(pl[:, 64:96], lhsT=onesb[:], rhs=msum[:], start=False, stop=True),
                                 r=[onesb.r(), msum.r()], w=[pl.r()])
                            p.op("dve", lambda e, L_=L_, pl=pl: e.scalar_tensor_tensor(
                                out=L_[:, 2, :], in0=pl[:, 64:96], scalar=float(CAP - 1), in1=ecap[:], op0=ALU.min, op1=ALU.add),
                                r=[pl.r(), ecap.r()], w=[L_.r()])
                            p.op("dve", lambda e, mk_=mk_: e.tensor_tensor(out=msum[:], in0=msum[:], in1=mk_[:], op=ALU.add),
                                 r=[msum.r(), mk_.r()], w=[msum.r()])
                            for k in range(4):
                                p.op("dve", lambda e, k=k, L_=L_, T_=T_, oh_=oh_: e.tensor_scalar(
                                    out=oh_[:, k, :], in0=L_[:, 0, :], scalar1=T_[:, k:k + 1], scalar2=None, op0=ALU.is_equal),
                                    r=[L_.r(), T_.r()], w=[oh_.r()])
                                p.op("dve", lambda e, k=k, L_=L_, oh_=oh_: e.tensor_tensor(
                                    out=oh_[:, k, :], in0=oh_[:, k, :], in1=L_[:, 2, :], op=ALU.mult),
                                    r=[oh_.r(), L_.r()], w=[oh_.r()])
                            p.op("dve", lambda e, T_=T_, oh_=oh_: e.reduce_sum(out=T_[:, 12:16], in_=oh_[:], axis=AX.X),
                                 r=[oh_.r()], w=[T_.r()])
                            p.op("dve", lambda e, T_=T_, gt=gt: e.tensor_copy(out=RIi[:, gt, :], in_=T_[:, 12:16]),
                                 r=[T_.r()], w=[RIi.r(gt)])
                            p.op("dve", lambda e, T_=T_: e.tensor_scalar_mul(out=T_[:, 8:9], in0=T_[:, 0:1], scalar1=-1.0),
                                 r=[T_.r()], w=[T_.r()])
                            p.op("act", lambda e, T_=T_: e.activation(out=T_[:, 16:20], in_=T_[:, 0:4], func=AF.Exp,
                                                                     bias=T_[:, 8:9], scale=1.0), r=[T_.r()], w=[T_.r()])
                            p.op("dve", lambda e, T_=T_: e.reduce_sum(out=T_[:, 9:10], in_=T_[:, 16:20], axis=AX.X),
                                 r=[T_.r()], w=[T_.r()])
                            p.op("dve", lambda e, T_=T_: e.reciprocal(out=T_[:, 10:11], in_=T_[:, 9:10]), r=[T_.r()], w=[T_.r()])
                            p.op("dve", lambda e, T_=T_, gt=gt: e.tensor_scalar_mul(
                                out=RIw[:, gt, :], in0=T_[:, 16:20], scalar1=T_[:, 10:11]), r=[T_.r()], w=[RIw.r(gt)])
                            if b == 0 and t == 0:
                                wait_zero()
                            for k in range(4):
                                p.idma(out=xe_d, out_offset=bass.IndirectOffsetOnAxis(ap=RIi[:, gt, k:k + 1], axis=0),
                                       in_=h2hi[i][:], in_offset=None, bounds=NE * CAP - 1,
                                       r=[h2hi[i].r(), RIi.r(gt)], w=[], sem="sc")
                p.barrier()
            if stop is not None:
                break

            p.barrier()

        if stop is None:
            p.barrier()
            with ExitStack() as L:
                wgb = [sb(L, "wgb%d" % i, [128, 8, D], BF16) for i in range(2)]
                wlb = [sb(L, "wlb%d" % i, [128, 8, D], BF16) for i in range(2)]
                wdb = [sb(L, "wdb%d" % i, [128, 8, D], BF16) for i in range(2)]
                bdb = [sb(L, "bdb%d" % i, [128, D]) for i in range(2)]
                bg = sb(L, "bg", [128, 8, NE]); bl = sb(L, "bl", [128, 8, NE])
                p.dma("sp", bg[:], bgT_d, w=[bg.r()], sem="x")
                p.dma("sp", bl[:], blT_d, w=[bl.r()], sem="x")
                xr = [sb(L, "xr%d" % i, [128, 4, D], BF16) for i in range(2)]
                xT = [sb(L, "xT%d" % i, [128, 8, 512], BF16) for i in range(2)]
                aT2 = [sb(L, "aT%d" % i, [128, 8, 512], BF16, nres=8) for i in range(2)]
                gg = [sb(L, "gg%d" % i, [128, 512]) for i in range(2)]
                sg = [sb(L, "sgE%d" % i, [128, 512]) for i in range(2)]
                l1 = [sb(L, "l1%d" % i, [128, 512]) for i in range(2)]
                t1 = [sb(L, "t1%d" % i, [128, 512]) for i in range(2)]
                ysb = [sb(L, "ysb%d" % i, [128, D]) for i in range(2)]

                class BV:
                    def __init__(self, parent, c0):
                        self.par, self.c0, self.res = parent, c0, Res()

                    def r(self):
                        return self.res

                    def ap(self):
                        return self.par[:, self.c0:self.c0 + 512]

                banks = [BV(PD[2], 0), BV(PD[2], 512), BV(PS[0], 0), BV(PS[1], 0)]
                bki = [0]

                def nbank():
                    bki[0] += 1
                    return banks[bki[0] % 4]

                NBLK = CAP // 512
                NTOT = NE * NBLK
                ycnt = [0]

                def load_w(e_):
                    p.dma("pool", wgb[e_ % 2][:], wview(w_gate_d[e_]), w=[wgb[e_ % 2].r()], sem="w")
                    p.dma("pool", wlb[e_ % 2][:], wview(w_lin_d[e_]), w=[wlb[e_ % 2].r()], sem="w")
                    p.dma("pool", wdb[e_ % 2][:], wview(w_down_d[e_]), w=[wdb[e_ % 2].r()], sem="w")
                    p.dma("sp", bdb[e_ % 2][:], b_down_d[e_:e_ + 1, :].partition_broadcast(128), w=[bdb[e_ % 2].r()], sem="x")

                def emit_T(n):
                    e_, blk = divmod(n, NBLK)
                    xr_, xT_ = xr[n % 2], xT[n % 2]
                    row0 = e_ * CAP + blk * 512
                    p.dma("sp", xr_[:], xe_d[row0:row0 + 512, :].rearrange("(j p) d -> p j d", p=128), w=[xr_.r()], sem="x")
                    for j in range(4):
                        bk = nbank()
                        psb = bk.ap().bitcast(BF16)
                        for kc in range(8):
                            p.op("pe", lambda e, kc=kc, j=j, psb=psb, xr_=xr_: e.transpose(
                                psb[:, kc * 128:(kc + 1) * 128], xr_[:, j, kc * 128:(kc + 1) * 128], identb[:]),
                                r=[xr_.r(), identb.r()], w=[bk.r()])
                        eng = "act" if j % 2 else "dve"
                        if eng == "act":
                            p.op("act", lambda e, j=j, psb=psb, xT_=xT_: e.copy(
                                out=xT_[:, :, j * 128:(j + 1) * 128], in_=psb.rearrange("p (k n) -> p k n", k=8)),
                                r=[bk.r()], w=[xT_.r()])
                        else:
                            p.op("dve", lambda e, j=j, psb=psb, xT_=xT_: e.tensor_copy(
                                out=xT_[:, :, j * 128:(j + 1) * 128], in_=psb.rearrange("p (k n) -> p k n", k=8)),
                                r=[bk.r()], w=[xT_.r()])

                def emit_GL(n, fc):
                    e_ = n // NBLK
                    wg_, wl_ = wgb[e_ % 2], wlb[e_ % 2]
                    xT_, aT = xT[n % 2], aT2[n % 2]
                    j = fc % 2
                    pg = PD[j]
                    for (w_, c0) in ((wg_, 0), (wl_, 512)):
                        for kc in range(8):
                            p.op("pe", lambda e, kc=kc, w_=w_, c0=c0, pg=pg: e.matmul(
                                pg[:, c0:c0 + 512], lhsT=w_[:, kc, fc * 128:(fc + 1) * 128], rhs=xT_[:, kc, :],
                                start=(kc == 0), stop=(kc == 7)), r=[w_.r(), xT_.r()], w=[pg.r()])
                    p.op("dve", lambda e: e.tensor_scalar(
                        out=gg[j][:], in0=pg[:, 0:512], scalar1=bg[:, fc, e_:e_ + 1], scalar2=7.0, op0=ALU.add, op1=ALU.min),
                        r=[pg.r(), bg.r()], w=[gg[j].r()])
                    p.op("act", lambda e: e.activation(out=sg[j][:], in_=gg[j][:], func=AF.Sigmoid, scale=1.702),
                         r=[gg[j].r()], w=[sg[j].r()])
                    p.op("dve", lambda e: e.tensor_scalar(
                        out=l1[j][:], in0=pg[:, 512:1024], scalar1=bl[:, fc, e_:e_ + 1], scalar2=7.0, op0=ALU.add, op1=ALU.min),
                        r=[pg.r(), bl.r()], w=[l1[j].r()])
                    p.op("dve", lambda e: e.tensor_scalar(
                        out=l1[j][:], in0=l1[j][:], scalar1=-7.0, scalar2=1.0, op0=ALU.max, op1=ALU.add),
                        r=[l1[j].r()], w=[l1[j].r()])
                    p.op(POOLENG, lambda e: e.tensor_tensor(out=t1[j][:], in0=gg[j][:], in1=sg[j][:], op=ALU.mult),
                         r=[gg[j].r(), sg[j].r()], w=[t1[j].r()])
                    p.op("dve", lambda e: e.tensor_tensor(out=aT[:, fc, :], in0=t1[j][:], in1=l1[j][:], op=ALU.mult),
                         r=[t1[j].r(), l1[j].r()], w=[aT.r(fc)])

                def emit_DOWN(n):
                    e_, blk = divmod(n, NBLK)
                    wd_, bd_, aT = wdb[e_ % 2], bdb[e_ % 2], aT2[n % 2]
                    row0 = e_ * CAP + blk * 512
                    for ti in range(4):
                        ycnt[0] += 1
                        ys_ = ysb[ycnt[0] % 2]
                        for half in range(2):
                            bk = nbank()
                            for fc in range(8):
                                p.op("pe", lambda e, fc=fc, half=half, ti=ti, bk=bk: e.matmul(
                                    bk.ap(), lhsT=aT[:, fc, ti * 128:(ti + 1) * 128],
                                    rhs=wd_[:, fc, half * 512:(half + 1) * 512], start=(fc == 0), stop=(fc == 7)),
                                    r=[aT.r(fc), wd_.r()], w=[bk.r()])
                            p.op("dve", lambda e, half=half, bk=bk, ys_=ys_: e.tensor_tensor(
                                out=ys_[:, half * 512:(half + 1) * 512], in0=bk.ap(), in1=bd_[:, half * 512:(half + 1) * 512], op=ALU.add),
                                r=[bk.r(), bd_.r()], w=[ys_.r()])
                        p.dma("sp", ye_d[row0 + ti * 128:row0 + (ti + 1) * 128, :], ys_[:], r=[ys_.r()], w=[], sem="o")

                for n in range(NTOT):
                    if n % NBLK == 0:
                        load_w(n // NBLK)
                    emit_T(n)
                    emit_GL(n, 0)
                    emit_GL(n, 1)
                    if n > 0:
                        emit_DOWN(n - 1)
                    for fc in range(2, 8):
                        emit_GL(n, fc)
                emit_DOWN(NTOT - 1)
            p.barrier()
            with ExitStack() as L:
                yk = [[sb(L, "yk%d_%d" % (i, k), [128, D]) for k in range(4)] for i in range(2)]
                acc = [sb(L, "acc%d" % i, [128, D]) for i in range(2)]
                xe1 = [sb(L, "xe1%d" % i, [128, D]) for i in range(2)]
                jk = sb(L, "jkF", [128, D], BF16)
                st = [sb(L, "stF%d" % i, [128, 4]) for i in range(2)]
                Gf = sb(L, "GfF", [128, D])
                for gt in range(nb * NT):
                    b, t = divmod(gt, NT)
                    i = gt % 2
                    if t == 0:
                        p.dma("sp", Gf[:], gf_d[b], r=[gfres[b]], w=[Gf.r()], sem="x")
                    for k in range(4):
                        p.idma(out=yk[i][k][:], out_offset=None, in_=ye_d,
                               in_offset=bass.IndirectOffsetOnAxis(ap=RIi[:, gt, k:k + 1], axis=0), bounds=NE * CAP - 1,
                               r=[RIi.r(gt)], w=[yk[i][k].r()], sem="ga")
                    p.dma("sp", xe1[i][:], out_d[b, t * 128:(t + 1) * 128, :], r=[x1res[b][t]], w=[xe1[i].r()], sem="x")
                    a_ = acc[i]
                    p.op("dve", lambda e, a_=a_, i=i, gt=gt: e.tensor_scalar_mul(out=a_[:], in0=yk[i][0][:], scalar1=RIw[:, gt, 0:1]),
                         r=[yk[i][0].r(), RIw.r(gt)], w=[a_.r()])
                    for k in range(1, 4):
                        p.op("dve", lambda e, a_=a_, i=i, gt=gt, k=k: e.scalar_tensor_tensor(
                            out=a_[:], in0=yk[i][k][:], scalar=RIw[:, gt, k:k + 1], in1=a_[:], op0=ALU.mult, op1=ALU.add),
                            r=[yk[i][k].r(), RIw.r(gt), a_.r()], w=[a_.r()])
                    if b == 0 and "ffn" in dbg_d:
                        dump("ffn", a_[:], a_.r(), dst=dbg_d["ffn"][t])
                    rstd_of(a_[:], [a_.r()], jk, st[i])
                    p.op("dve", lambda e, a_=a_, i=i: e.scalar_tensor_tensor(
                        out=a_[:], in0=a_[:], scalar=st[i][:, 1:2], in1=Gf[:], op0=ALU.mult, op1=ALU.mult),
                        r=[a_.r(), st[i].r(), Gf.r()], w=[a_.r()])
                    p.op("dve", lambda e, a_=a_, i=i: e.tensor_tensor(out=xe1[i][:], in0=xe1[i][:], in1=a_[:], op=ALU.add),
                         r=[xe1[i].r(), a_.r()], w=[xe1[i].r()])
                    p.dma("sp", out_d[b, t * 128:(t + 1) * 128, :], xe1[i][:], r=[xe1[i].r()], w=[x1res[b][t]], sem="o")
            p.barrier()

        p.barrier()
    return nc, p


def _host_consts():
    c = {}
    c["ident"] = np.eye(128, dtype=np.float32)
    j = np.arange(128)
    c["trif"] = (j[:, None] <= j[None, :]).astype(np.float32)
    c["trib"] = (j[:, None] >= j[None, :]).astype(np.float32)
    c["tris"] = (j[:, None] < j[None, :]).astype(np.float32)
    c["ecap"] = np.ascontiguousarray(np.broadcast_to((np.arange(NE) * CAP).astype(np.float32)[None, :], (128, NE)))
    t = np.arange(S)
    row = (t // 64).astype(np.float32)
    col = (t % 64).astype(np.float32)
    inv_freq = (np.float32(10000.0) ** (-np.arange(16, dtype=np.float32) / np.float32(16))).astype(np.float32)
    cos = np.zeros((128, S), np.float32)
    sin = np.zeros((128, S), np.float32)
    for pp in range(128):
        d = pp % 64
        pos = row if d < 32 else col
        dd = d % 32
        f = dd % 16
        sign = -1.0 if dd < 16 else 1.0
        ang = (pos * inv_freq[f]).astype(np.float32)
        cos[pp] = np.cos(ang)
        sin[pp] = sign * np.sin(ang)
    c["ropecos"] = cos
    c["ropesin"] = sin
    return c


def _na_bias_table(rpb):
    reps = [0, 1, 5, 14, 15]
    kk = np.arange(128)
    krl = kk // 64
    kc = kk % 64
    qq = np.arange(128)
    qrl = qq // 64
    qc = qq % 64
    cs = np.clip(qc - 8, 0, 48)
    tab = np.full((8, 128, 5, 5, 128), NEG, np.float32)
    for ci, i in enumerate(reps):
        js = int(np.clip(i - 2, 0, 11))
        for s in range(5):
            j = js + s
            kr = 2 * j + krl
            r = 2 * i + qrl
            rs = np.clip(r - 4, 0, 24)
            vr = (kr[:, None] >= rs[None, :]) & (kr[:, None] < rs[None, :] + 8)
            vc = (kc[:, None] >= cs[None, :]) & (kc[:, None] < cs[None, :] + 16)
            valid = vr & vc
            dr = np.clip(kr[:, None] - r[None, :] + 7, 0, 14)
            dc = np.clip(kc[:, None] - qc[None, :] + 15, 0, 30)
            vals = rpb[:, dr, dc]
            tab[:, :, ci, s, :] = np.where(valid[None], vals, np.float32(NEG))
    return np.ascontiguousarray(tab.reshape(8, 128, 5 * 640))


def _prep_inputs(inp, nb=NB, ncores=8):
    f = lambda a: np.ascontiguousarray(np.asarray(a, dtype=np.float32))
    consts = _host_consts()
    w_in = f(inp["w_in"][0])
    d = np.arange(64)
    partner = np.where((d % 32) < 16, d + 16, d - 16)
    permq = np.concatenate([C_MLQ + h * 64 + partner for h in range(4)])
    permk = np.concatenate([C_MLK + h * 64 + partner for h in range(4)])
    w_qkp = np.ascontiguousarray(np.concatenate([w_in[:, permq], w_in[:, permk]], axis=1))
    shared = dict(
        w_ada=f(inp["w_ada"][0]), b_ada=f(inp["b_ada"][0]).reshape(1, -1),
        g4=np.ascontiguousarray(np.stack([f(inp["g_mix_pre"][0]), f(inp["g_mix_post"][0]),
                                          f(inp["g_ffn_pre"][0]), f(inp["g_ffn_post"][0])])),
        w_in=w_in, w_qkp=w_qkp, b_mg=f(inp["b_mlstm_gates"][0]).reshape(1, 16),
        nab=_na_bias_table(f(inp["rpb"][0])), g_head=f(inp["g_mlstm_head"][0]).reshape(1, 512),
        w_bna=f(inp["w_branch_na"][0]), w_bml=f(inp["w_branch_ml"][0]), w_out=f(inp["w_out"][0]),
        w_router=f(inp["w_router"][0]), b_router=f(inp["b_router"][0]).reshape(1, NE),
        w_gate=f(inp["w_gate"][0]), w_lin=f(inp["w_lin"][0]), w_down=f(inp["w_down"][0]),
        bgT=np.ascontiguousarray(f(inp["b_gate"][0]).reshape(NE, 8, 128).transpose(2, 1, 0)),
        blT=np.ascontiguousarray(f(inp["b_lin"][0]).reshape(NE, 8, 128).transpose(2, 1, 0)),
        b_down=f(inp["b_down"][0]), **consts)
    x = f(inp["x"]); ctx = f(inp["ctx"]); c = f(inp["c"]); cc = f(inp["c_ctx"])
    maps = []
    for k in range(ncores):
        sl = slice(k * nb, (k + 1) * nb)
        c5 = np.zeros((5, D), np.float32)
        c5[:nb] = c[sl]
        c5[4] = cc
        cT = np.ascontiguousarray(c5.reshape(5, 8, 128).transpose(2, 1, 0))
        m = dict(shared)
        m.update(x=np.ascontiguousarray(x[sl]), ctx=np.ascontiguousarray(ctx[sl]), cT=cT)
        maps.append(m)
    return maps


def kernel(**inputs):
    maps = _prep_inputs(inputs)
    nc, _ = build()
    res = run_bass_kernel_spmd(nc, maps, core_ids=list(range(8)))
    return np.concatenate([r["out"] for r in res.results], axis=0).astype(np.float32)
```

```python
import numpy as np
from contextlib import ExitStack
import concourse.bass as bass
import concourse.mybir as mybir
from concourse.bass_utils import run_bass_kernel_spmd

F32 = mybir.dt.float32
BF16 = mybir.dt.bfloat16
AF = mybir.ActivationFunctionType
ALU = mybir.AluOpType
AX = mybir.AxisListType

D = 1024
S = 2048
CTX = 256
NB = 4
NT = 16
NTT = 18
NE = 32
EPS = 1e-6
NEG = -80.0
CAP = 2048
I32 = mybir.dt.int32
POOLENG = "dve"
N_IN = 5136
C_NAK, C_NAV, C_MLK, C_MLV, C_MLG = 0, 512, 1024, 1280, 1792
C_NAQ, C_MLQ, C_MLO, C_GNA, C_GML = 1808, 2320, 2576, 3088, 4112


class Res:
    __slots__ = ("lw", "rd")

    def __init__(self):
        self.lw = None
        self.rd = {}


class Prog:
    ENG = ["pe", "act", "dve", "pool", "sp"]

    def __init__(self, nc):
        self.nc = nc
        self.e = dict(pe=nc.tensor, act=nc.scalar, dve=nc.vector, pool=nc.gpsimd, sp=nc.sync)
        self.sem = {k: nc.alloc_semaphore("s_" + k) for k in self.ENG}
        self.cnt = {k: 0 for k in self.ENG}
        self.waited = {k: {} for k in self.ENG}
        self.n_ins = 0

    NDS = 8
    rr = None

    def dsem(self, name):
        if self.rr is None:
            self.rr = {}
        i = self.rr.get(name, 0)
        self.rr[name] = i + 1
        k = "d:%s:%d" % (name, i % self.NDS)
        if k not in self.sem:
            self.sem[k] = self.nc.alloc_semaphore("sd_%s_%d" % (name, i % self.NDS))
            self.cnt[k] = 0
        return k

    def _wait(self, eng, key, val):
        if self.waited[eng].get(key, 0) >= val:
            return
        self.e[eng].wait_ge(self.sem[key], val)
        self.waited[eng][key] = val
        self.n_ins += 1

    def _sync(self, eng, me, r, w):
        for x in r:
            if x.lw is not None:
                k, v = x.lw
                if k == me and me == "pe":
                    continue
                self._wait(eng, k, v)
        for x in w:
            if x.lw is not None:
                k, v = x.lw
                if k != me or me != "pe":
                    self._wait(eng, k, v)
            for k, v in x.rd.items():
                if k != me or me != "pe":
                    self._wait(eng, k, v)

    def _commit(self, me, val, r, w):
        for x in r:
            if x.rd.get(me, 0) < val:
                x.rd[me] = val
        for x in w:
            x.lw = (me, val)
            x.rd = {}

    max_ops = None
    tot = 0

    def op(self, eng, fn, r=(), w=()):
        self.tot += 1
        if self.max_ops is not None and self.tot > self.max_ops:
            return None
        self._sync(eng, eng, r, w)
        ins = fn(self.e[eng])
        self.cnt[eng] += 1
        ins.then_inc(self.sem[eng], 1)
        self._commit(eng, self.cnt[eng], r, w)
        self.n_ins += 1
        return ins

    def dma(self, q, out, in_, r=(), w=(), sem="ld"):
        self.tot += 1
        if self.max_ops is not None and self.tot > self.max_ops:
            return None
        k = self.dsem(sem)
        if self.cnt[k] > 0:
            self._wait(q, k, self.cnt[k])
        self._sync(q, k, r, w)
        ins = self.e[q].dma_start(out=out, in_=in_)
        self.cnt[k] += 16
        ins.then_inc(self.sem[k], 16)
        self._commit(k, self.cnt[k], r, w)
        self.n_ins += 1
        return ins

    def idma(self, out, out_offset, in_, in_offset, bounds, r=(), w=(), sem="ind"):
        self.tot += 1
        if self.max_ops is not None and self.tot > self.max_ops:
            return None
        k = self.dsem(sem)
        if self.cnt[k] > 0:
            self._wait("pool", k, self.cnt[k])
        self._sync("pool", k, r, w)
        ins = self.e["pool"].indirect_dma_start(out=out, out_offset=out_offset, in_=in_, in_offset=in_offset)
        self.cnt[k] += 16
        ins.then_inc(self.sem[k], 16)
        self._commit(k, self.cnt[k], r, w)
        self.n_ins += 1
        return ins

    def barrier(self, engs=None):
        for eng in (engs or self.ENG):
            for k, v in self.cnt.items():
                if k != eng and v > 0:
                    self._wait(eng, k, v)


class T:
    def __init__(self, h, nres=1):
        self.h = h
        self.res = [Res() for _ in range(nres)]

    def __getitem__(self, k):
        return self.h[k]

    def r(self, i=0):
        return self.res[i]


def build(nb=NB, stop=None, dbg=(), max_ops=None):
    nc = bass.Bass("TRN2", target_bir_lowering=False)
    p = Prog(nc)
    p.max_ops = max_ops

    def din(name, shape, dt=F32):
        return nc.dram_tensor(name, list(shape), dt, kind="ExternalInput").ap()

    x_d = din("x", [nb, S, D])
    ctx_d = din("ctx", [nb, CTX, D])
    cT_d = din("cT", [128, 8, 5])
    w_ada_d = din("w_ada", [D, 6 * D])
    b_ada_d = din("b_ada", [1, 6 * D])
    g4_d = din("g4", [4, D])
    w_in_d = din("w_in", [D, N_IN])
    w_qkp_d = din("w_qkp", [D, 512])
    b_mg_d = din("b_mg", [1, 16])
    nab_d = din("nab", [8, 128, 5 * 640])
    g_head_d = din("g_head", [1, 512])
    w_bna_d = din("w_bna", [512, D])
    w_bml_d = din("w_bml", [512, D])
    w_out_d = din("w_out", [D, D])
    w_r_d = din("w_router", [D, NE])
    b_r_d = din("b_router", [1, NE])
    w_gate_d = din("w_gate", [NE, D, D])
    w_lin_d = din("w_lin", [NE, D, D])
    w_down_d = din("w_down", [NE, D, D])
    bgT_d = din("bgT", [128, 8, NE])
    blT_d = din("blT", [128, 8, NE])
    b_down_d = din("b_down", [NE, D])
    ident_d = din("ident", [128, 128])
    trif_d = din("trif", [128, 128])
    trib_d = din("trib", [128, 128])
    cos_d = din("ropecos", [128, S])
    sin_d = din("ropesin", [128, S])
    tris_d = din("tris", [128, 128])
    ecap_d = din("ecap", [128, NE])
    xe_d = nc.dram_tensor("xe_scr", [NE * CAP, D], BF16).ap()
    ye_d = nc.dram_tensor("ye_scr", [NE * CAP, D], F32).ap()
    gf_d = nc.dram_tensor("gf_scr", [nb, 128, D], F32).ap()
    out_d = nc.dram_tensor("out", [nb, S, D], F32, kind="ExternalOutput").ap()
    dbg_d = {}
    for name, shape in dbg:
        dbg_d[name] = nc.dram_tensor("dbg_" + name, list(shape), F32, kind="ExternalOutput").ap()

    uid = [0]

    def sb(es, name, shape, dt=F32, nres=1):
        uid[0] += 1
        return T(es.enter_context(nc.sbuf_tensor("sb%d_%s" % (uid[0], name), list(shape), dt)), nres)

    PD = [T(nc.alloc_psum_tensor("pd%d" % i, [128, 1024], F32)) for i in range(3)]
    PS = [T(nc.alloc_psum_tensor("psg%d" % i, [128, 512], F32)) for i in range(2)]

    def dump(name, tile_ap, res, dst=None):
        if name in dbg_d:
            p.dma("pool", dbg_d[name] if dst is None else dst, tile_ap, r=(res if isinstance(res, list) else [res]), sem="dbg")

    def wview(ap2d):
        return ap2d.rearrange("(kc p) n -> p kc n", p=128)

    def bc(ap, shape):
        return ap.to_broadcast(list(shape))

    rot = [0, 0]

    def nPD():
        rot[0] += 1
        return PD[rot[0] % 3]

    def nPS():
        rot[1] += 1
        return PS[rot[1] % 2]

    with ExitStack() as G:
        ident = sb(G, "ident", [128, 128])
        identb = sb(G, "identb", [128, 128], BF16)
        trif = sb(G, "trif", [128, 128])
        trib = sb(G, "trib", [128, 128])
        ones32 = sb(G, "ones32", [128, 128])
        scT = sb(G, "scT", [128, 8, 5])
        A_c = sb(G, "A_c", [128, D])
        sh_c = sb(G, "sh_c", [128, D])
        trisb = sb(G, "trisb", [128, 128], BF16)
        onesb = sb(G, "onesb", [128, 128], BF16)
        ecap = sb(G, "ecap", [128, NE])
        msum = sb(G, "msum", [128, NE], BF16)
        RIi = sb(G, "RIi", [128, nb * NT, 4], I32, nres=nb * NT)
        RIw = sb(G, "RIw", [128, nb * NT, 4], F32, nres=nb * NT)
        zres = Res()
        gfres = [Res() for _ in range(nb)]
        p.dma("sp", ecap[:], ecap_d, w=[ecap.r()])
        p.op("dve", lambda e: e.memset(onesb[:], 1.0), w=[onesb.r()])
        p.op("dve", lambda e: e.memset(msum[:], 0.0), w=[msum.r()])
        ztile = sb(G, "ztile", [128, 1024], BF16)
        p.op("dve", lambda e: e.memset(ztile[:], 0.0), w=[ztile.r()])
        p.dma("pool", trisb[:], tris_d, w=[trisb.r()], sem="w")
        zdone = [0]
        NZ = NE * CAP // 128

        def emit_zero(k):
            for _ in range(k):
                cz = zdone[0]
                if cz >= NZ:
                    return
                zdone[0] += 1
                p.dma("sp", xe_d[cz * 128:(cz + 1) * 128, :], ztile[:],
                      r=[ztile.r()], w=[], sem="z")

        def wait_zero():
            emit_zero(NZ)
            for k_, v_ in p.cnt.items():
                if k_.startswith("d:z:") and v_ > 0:
                    p._wait("pool", k_, v_)
        p.dma("sp", ident[:], ident_d, w=[ident.r()])
        p.dma("sp", trif[:], trif_d, w=[trif.r()])
        p.dma("sp", trib[:], trib_d, w=[trib.r()])
        p.dma("sp", scT[:], cT_d, w=[scT.r()])
        p.op("dve", lambda e: e.tensor_copy(out=identb[:], in_=ident[:]), r=[ident.r()], w=[identb.r()])
        p.op("dve", lambda e: e.memset(ones32[:], 1.0), w=[ones32.r()])
        p.op("act", lambda e: e.activation(out=scT[:], in_=scT[:], func=AF.Silu), r=[scT.r()], w=[scT.r()])

        def rstd_of(src_ap, src_res, junk, st):
            p.op("act", lambda e: e.activation(out=junk[:], in_=src_ap, func=AF.Square, scale=1.0 / 32.0,
                                               accum_out=st[:, 0:1]), r=src_res, w=[junk.r(), st.r()])
            p.op("act", lambda e: e.activation(out=st[:, 1:2], in_=st[:, 0:1], func=AF.Sqrt, bias=EPS),
                 r=[st.r()], w=[st.r()])
            p.op("dve", lambda e: e.reciprocal(out=st[:, 1:2], in_=st[:, 1:2]), r=[st.r()], w=[st.r()])

        def ada_mod(j, pieces, tag):
            with ExitStack() as L:
                lh = sb(L, "lh" + tag, [128, 8, 128], BF16)
                g4 = sb(L, "g4" + tag, [128, 4, D])
                p.dma("sp", g4[:], g4_d.partition_broadcast(128), w=[g4.r()])
                for kc in range(8):
                    p.op("dve", lambda e, kc=kc: e.tensor_scalar_mul(
                        out=lh[:, kc, :], in0=ones32[:], scalar1=scT[:, kc, j:j + 1]),
                        r=[scT.r(), ones32.r()], w=[lh.r()])
                wa = [sb(L, "wa%d%s" % (i, tag), [128, 8, 512], BF16) for i in range(2)]
                ba = [sb(L, "ba%d%s" % (i, tag), [1, 512]) for i in range(2)]
                n = 0
                for (blk, out_t, kind, gi) in pieces:
                    for half in range(2):
                        c0 = blk * D + half * 512
                        wt = wa[n % 2]
                        bt = ba[n % 2]
                        n += 1
                        p.dma("pool", wt[:], wview(w_ada_d[:, c0:c0 + 512]), w=[wt.r()], sem="w")
                        p.dma("sp", bt[:], b_ada_d[:, c0:c0 + 512], w=[bt.r()], sem="x")
                        ps = nPD()
                        for kc in range(8):
                            p.op("pe", lambda e, kc=kc, ps=ps, wt=wt: e.matmul(
                                ps[:, 0:512], lhsT=lh[:, kc, :], rhs=wt[:, kc, :], start=(kc == 0), stop=(kc == 7)),
                                r=[lh.r(), wt.r()], w=[ps.r()])
                        o = out_t[:, half * 512:(half + 1) * 512]
                        tmp = sb(L, "adatmp%d%s" % (n, tag), [128, 512])
                        ps2 = nPS()
                        p.op("pe", lambda e, ps2=ps2, bt=bt: e.matmul(
                            ps2[:], lhsT=ones32[0:1, :], rhs=bt[0:1, :], start=True, stop=True),
                            r=[ones32.r(), bt.r()], w=[ps2.r()])
                        p.op("act", lambda e, tmp=tmp, ps2=ps2: e.copy(out=tmp[:], in_=ps2[:]), r=[ps2.r()], w=[tmp.r()])
                        p.op("dve", lambda e, tmp=tmp, ps=ps: e.tensor_tensor(out=tmp[:], in0=ps[:, 0:512], in1=tmp[:], op=ALU.add),
                             r=[ps.r(), tmp.r()], w=[tmp.r()])
                        if kind == "shift":
                            p.op("act", lambda e, o=o, tmp=tmp: e.copy(out=o, in_=tmp[:]), r=[tmp.r()], w=[out_t.r()])
                        elif kind == "scale":
                            gs = g4[:, gi, half * 512:(half + 1) * 512]
                            p.op("dve", lambda e, o=o, tmp=tmp, gs=gs: e.scalar_tensor_tensor(
                                out=o, in0=tmp[:], scalar=1.0, in1=gs, op0=ALU.add, op1=ALU.mult),
                                r=[tmp.r(), g4.r()], w=[out_t.r()])
                        else:
                            gs = g4[:, gi, half * 512:(half + 1) * 512]
                            p.op("dve", lambda e, o=o, tmp=tmp, gs=gs: e.tensor_tensor(
                                out=o, in0=tmp[:], in1=gs, op=ALU.mult),
                                r=[tmp.r(), g4.r()], w=[out_t.r()])
            p.barrier()

        ada_mod(4, [(0, sh_c, "shift", 0), (1, A_c, "scale", 0)], "c")
        x1res = [[Res() for _ in range(NT)] for _ in range(nb)]

        for b in range(nb):
          with ExitStack() as B:
            hT = sb(B, "hT", [128, 8, NTT * 128], BF16, nres=NTT)
            with ExitStack() as M:
              G_m = sb(M, "G_m", [128, D]); A_f = sb(M, "A_f", [128, D]); sh_f = sb(M, "sh_f", [128, D])
              with ExitStack() as PA:
                A_m = sb(PA, "A_m", [128, D]); sh_m = sb(PA, "sh_m", [128, D]); G_f = sb(PA, "G_f", [128, D])
                ada_mod(b, [(0, sh_m, "shift", 0), (1, A_m, "scale", 0), (2, G_m, "gate", 1),
                            (3, sh_f, "shift", 0), (4, A_f, "scale", 2), (5, G_f, "gate", 3)], "b")
                p.dma("sp", gf_d[b], G_f[:], r=[G_f.r()], w=[gfres[b]], sem="o")
                if b == 0:
                    dump("A_m", A_m[:], A_m.r()); dump("sh_m", sh_m[:], sh_m.r()); dump("G_f", G_f[:], G_f.r())
                    dump("A_c", A_c[:], A_c.r())
                with ExitStack() as L:
                    xt = [sb(L, "xt%d" % i, [128, D]) for i in range(3)]
                    sq = [sb(L, "sq%d" % i, [128, D]) for i in range(3)]
                    hb = [sb(L, "hb%d" % i, [128, D], BF16) for i in range(3)]
                    st = [sb(L, "st%d" % i, [128, 2]) for i in range(3)]

                    def a_stage1(t):
                        i = t % 3
                        src = ctx_d[b, t * 128:(t + 1) * 128, :] if t < 2 else x_d[b, (t - 2) * 128:(t - 1) * 128, :]
                        Am, shm = (A_c, sh_c) if t < 2 else (A_m, sh_m)
                        p.dma("sp", xt[i][:], src, w=[xt[i].r()], sem="x")
                        rstd_of(xt[i][:], [xt[i].r()], sq[i], st[i])
                        p.op("dve", lambda e: e.scalar_tensor_tensor(
                            out=sq[i][:], in0=xt[i][:], scalar=st[i][:, 1:2], in1=Am[:], op0=ALU.mult, op1=ALU.mult),
                            r=[xt[i].r(), st[i].r(), Am.r()], w=[sq[i].r()])
                        p.op("dve", lambda e: e.tensor_tensor(out=hb[i][:], in0=sq[i][:], in1=shm[:], op=ALU.add),
                             r=[sq[i].r(), shm.r()], w=[hb[i].r()])

                    def a_stage2(t):
                        i = t % 3
                        ps = nPS()
                        psb = ps[:].bitcast(BF16)
                        for kc in range(8):
                            p.op("pe", lambda e, kc=kc: e.transpose(
                                psb[:, kc * 128:(kc + 1) * 128], hb[i][:, kc * 128:(kc + 1) * 128], identb[:]),
                                r=[hb[i].r(), identb.r()], w=[ps.r()])
                        p.op("act", lambda e: e.copy(
                            out=hT[:, :, t * 128:(t + 1) * 128], in_=psb.rearrange("p (k n) -> p k n", k=8)),
                            r=[ps.r()], w=[hT.r(t)])

                    for t in range(NTT + 1):
                        if t < NTT:
                            a_stage1(t)
                        if t >= 1:
                            a_stage2(t - 1)
                if b == 0 and "hT" in dbg_d:
                    dump("hT", hT[:], hT.res)
                p.barrier()
              if True:
                if stop == "A":
                    break
                if b == 0:
                    emit_zero(128)
                onaT = sb(M, "onaT", [128, 4, S], BF16, nres=NT)

                def mm_fm(ps_ap, ps_res, w_t, wc0, tok0, ntok, nk=8, src=None):
                    src = src or hT
                    tiles = range(tok0 // 128, (tok0 + ntok) // 128)
                    for kc in range(nk):
                        p.op("pe", lambda e, kc=kc: e.matmul(
                            ps_ap, lhsT=w_t[:, kc, wc0:wc0 + 128], rhs=src[:, kc, tok0:tok0 + ntok],
                            start=(kc == 0), stop=(kc == nk - 1)),
                            r=[w_t.r()] + [src.r(t) for t in tiles], w=[ps_res])

                def mm_tm(ps_ap, ps_res, w_t, wc0, ncols, t):
                    for kc in range(8):
                        p.op("pe", lambda e, kc=kc: e.matmul(
                            ps_ap, lhsT=hT[:, kc, t * 128:(t + 1) * 128], rhs=w_t[:, kc, wc0:wc0 + ncols],
                            start=(kc == 0), stop=(kc == 7)),
                            r=[w_t.r(), hT.r(t)], w=[ps_res])

                with ExitStack() as L:
                  qT = sb(L, "qT", [128, 4, S], BF16, nres=4)
                  kT = sb(L, "kT", [128, 4, NTT * 128], BF16, nres=4)
                  vA = sb(L, "vA", [128, NTT, 8, 65], BF16, nres=NTT)
                  p.op("dve", lambda e: e.memset(vA[:, :, :, 64:65], 1.0), w=vA.res)
                  with ExitStack() as L2:
                    wna = sb(L2, "wna", [128, 8, 1536], BF16)
                    p.dma("pool", wna[:, :, 0:1024], wview(w_in_d[:, 0:1024]), w=[wna.r()], sem="w")
                    p.dma("pool", wna[:, :, 1024:1536], wview(w_in_d[:, C_NAQ:C_NAQ + 512]), w=[wna.r()], sem="w")
                    for c in range(4):
                        for tb in range(4):
                            ps = nPD()
                            mm_fm(ps[:, 0:512], ps.r(), wna, 1024 + c * 128, 256 + tb * 512, 512)
                            p.op("act", lambda e, c=c, tb=tb, ps=ps: e.activation(
                                out=qT[:, c, tb * 512:(tb + 1) * 512], in_=ps[:, 0:512], func=AF.Copy, scale=0.125),
                                r=[ps.r()], w=[qT.r(c)])
                        for (t0, n) in [(0, 512), (512, 512), (1024, 512), (1536, 512), (2048, 256)]:
                            ps = nPD()
                            mm_fm(ps[:, 0:n], ps.r(), wna, c * 128, t0, n)
                            p.op("dve", lambda e, c=c, t0=t0, n=n, ps=ps: e.tensor_copy(
                                out=kT[:, c, t0:t0 + n], in_=ps[:, 0:n]), r=[ps.r()], w=[kT.r(c)])
                    for t in range(NTT):
                        ps = nPD()
                        mm_tm(ps[:, 0:512], ps.r(), wna, 512, 512, t)
                        eng = "act" if t % 2 else "dve"
                        if eng == "act":
                            p.op("act", lambda e, t=t, ps=ps: e.copy(
                                out=vA[:, t, :, 0:64], in_=ps[:, 0:512].rearrange("p (h d) -> p h d", h=8)),
                                r=[ps.r()], w=[vA.r(t)])
                        else:
                            p.op("dve", lambda e, t=t, ps=ps: e.tensor_copy(
                                out=vA[:, t, :, 0:64], in_=ps[:, 0:512].rearrange("p (h d) -> p h d", h=8)),
                                r=[ps.r()], w=[vA.r(t)])
                  p.barrier()
                  if True:
                    EBh = [sb(L, "EB%d" % i, [128, 3200], BF16) for i in range(2)]
                    ona = sb(L, "ona", [128, NT, 512], BF16, nres=NT)
                    stg = sb(L, "nabst", [128, 3200])
                    PT = [sb(L, "PT%d" % i, [128, 896], BF16) for i in range(3)]
                    rc = [sb(L, "rc%d" % i, [128, 1]) for i in range(3)]
                    n = 0
                    items = [(h, i) for h in range(8) for i in range(NT)]
                    state = {}

                    def na_scores(n):
                        h, i = items[n]
                        hp = (h % 2) * 64
                        c = h // 2
                        EB = EBh[h % 2]
                        if i == 0:
                            p.dma("sp", stg[:], nab_d[h], w=[stg.r()], sem="x")
                            p.op("act", lambda e, EB=EB: e.activation(out=EB[:], in_=stg[:], func=AF.Exp),
                                 r=[stg.r()], w=[EB.r()])
                            if b == 0 and h >= 1:
                                emit_zero(24)
                        js = min(max(i - 2, 0), 11)
                        cls = i - js if i < 2 or i > 13 else 2
                        pss = PD[n % 3]
                        pt = PT[n % 3]
                        qa = qT[hp:hp + 64, c, i * 128:(i + 1) * 128]
                        for s_i in range(7):
                            kt = (2 + js + s_i) if s_i < 5 else (s_i - 5)
                            p.op("pe", lambda e, s_i=s_i, kt=kt: e.matmul(
                                pss[:, s_i * 128:(s_i + 1) * 128], lhsT=kT[hp:hp + 64, c, kt * 128:(kt + 1) * 128],
                                rhs=qa, start=True, stop=True),
                                r=[kT.r(c), qT.r(c)], w=[pss.r()])
                        p.op("act", lambda e: e.activation(out=pt[:], in_=pss[:, 0:896], func=AF.Exp),
                             r=[pss.r()], w=[pt.r()])
                        p.op("dve", lambda e: e.tensor_tensor(
                            out=pt[:, 0:640], in0=pt[:, 0:640], in1=EB[:, cls * 640:(cls + 1) * 640], op=ALU.mult),
                            r=[pt.r(), EB.r()], w=[pt.r()])
                        state[n] = (pt, js)

                    def na_pv(n):
                        h, i = items[n]
                        pt, js = state.pop(n)
                        pso = PS[n % 2]
                        rcc = rc[n % 3]
                        for s_i in range(7):
                            kt = (2 + js + s_i) if s_i < 5 else (s_i - 5)
                            p.op("pe", lambda e, s_i=s_i, kt=kt: e.matmul(
                                pso[:, 0:65], lhsT=pt[:, s_i * 128:(s_i + 1) * 128], rhs=vA[:, kt, h, :],
                                start=(s_i == 0), stop=(s_i == 6)),
                                r=[pt.r(), vA.r(kt)], w=[pso.r()])
                        p.op("dve", lambda e: e.reciprocal(out=rcc[:], in_=pso[:, 64:65]),
                             r=[pso.r()], w=[rcc.r()])
                        p.op("dve", lambda e: e.tensor_scalar_mul(
                            out=ona[:, i, h * 64:(h + 1) * 64], in0=pso[:, 0:64], scalar1=rcc[:, 0:1]),
                            r=[pso.r(), rcc.r()], w=[ona.r(i)])

                    for n in range(len(items) + 1):
                        if n < len(items):
                            na_scores(n)
                        if n >= 1:
                            na_pv(n - 1)
                    for i in range(NT):
                        ps = nPS()
                        psb = ps[:].bitcast(BF16)
                        for c4 in range(4):
                            p.op("pe", lambda e, c4=c4, i=i, psb=psb: e.transpose(
                                psb[:, c4 * 128:(c4 + 1) * 128], ona[:, i, c4 * 128:(c4 + 1) * 128], identb[:]),
                                r=[ona.r(i), identb.r()], w=[ps.r()])
                        p.op("act", lambda e, i=i, psb=psb: e.copy(
                            out=onaT[:, :, i * 128:(i + 1) * 128], in_=psb[:, 0:512].rearrange("p (k n) -> p k n", k=4)),
                            r=[ps.r()], w=[onaT.r(i)])
                    if b == 0 and "ona" in dbg_d:
                        dump("ona", ona[:], ona.res)
                p.barrier()
                if stop == "B":
                    break
                omlT = sb(M, "omlT", [128, 4, S], BF16, nres=NT)

                with ExitStack() as L:
                    mqT = sb(L, "mqT", [128, 2, S], BF16, nres=2)
                    mkT = sb(L, "mkT", [128, 2, S], BF16, nres=2)
                    ktm = sb(L, "ktm", [128, NTT, 256], BF16, nres=NTT)
                    vM = sb(L, "vM", [128, NTT, 4, 129], BF16, nres=NTT)
                    osig = sb(L, "osig", [128, NT, 512], BF16, nres=NT)
                    gts = sb(L, "gts", [128, NTT, 16])
                    LI = sb(L, "LI", [128, NTT, 8])
                    LFn = sb(L, "LFn", [128, NTT, 8])
                    Bn = sb(L, "Bn", [128, NTT, 8])
                    EBt = sb(L, "EBt", [128, NTT, 8])
                    ES = sb(L, "ES", [128, NTT, 8])
                    EBL = sb(L, "EBL", [128, NTT, 8])
                    ghd = sb(L, "ghd", [128, 512])
                    p.dma("sp", ghd[:], g_head_d.partition_broadcast(128), w=[ghd.r()], sem="x")
                    p.op("dve", lambda e: e.memset(vM[:, :, :, 128:129], 1.0), w=vM.res)
                    with ExitStack() as L2:
                        wq = sb(L2, "wq", [128, 8, 256], BF16); wqp = sb(L2, "wqp", [128, 8, 256], BF16)
                        wk = sb(L2, "wk", [128, 8, 256], BF16); wkp = sb(L2, "wkp", [128, 8, 256], BF16)
                        cosT = sb(L2, "cosT", [128, 512]); sinT = sb(L2, "sinT", [128, 512])
                        rt = [sb(L2, "rt%d" % i, [128, 512]) for i in range(2)]
                        p.dma("pool", wq[:], wview(w_in_d[:, C_MLQ:C_MLQ + 256]), w=[wq.r()], sem="w")
                        p.dma("pool", wqp[:], wview(w_qkp_d[:, 0:256]), w=[wqp.r()], sem="w")
                        p.dma("pool", wk[:], wview(w_in_d[:, C_MLK:C_MLK + 256]), w=[wk.r()], sem="w")
                        p.dma("pool", wkp[:], wview(w_qkp_d[:, 256:512]), w=[wkp.r()], sem="w")
                        for (w_a, w_b, dst) in ((wq, wqp, mqT), (wk, wkp, mkT)):
                            for c in range(2):
                                for tb in range(4):
                                    ps = nPD()
                                    mm_fm(ps[:, 0:512], ps.r(), w_a, c * 128, 256 + tb * 512, 512)
                                    mm_fm(ps[:, 512:1024], ps.r(), w_b, c * 128, 256 + tb * 512, 512)
                                    cs_ = slice(tb * 512, (tb + 1) * 512)
                                    p.dma("sp", cosT[:], cos_d[:, cs_], w=[cosT.r()], sem="x")
                                    p.dma("sp", sinT[:], sin_d[:, cs_], w=[sinT.r()], sem="x")
                                    p.op("dve", lambda e, ps=ps, cs_=cs_: e.tensor_tensor(
                                        out=rt[0][:], in0=ps[:, 0:512], in1=cosT[:], op=ALU.mult),
                                        r=[ps.r(), cosT.r()], w=[rt[0].r()])
                                    p.op("dve", lambda e, ps=ps, cs_=cs_: e.tensor_tensor(
                                        out=rt[1][:], in0=ps[:, 512:1024], in1=sinT[:], op=ALU.mult),
                                        r=[ps.r(), sinT.r()], w=[rt[1].r()])
                                    p.op("dve", lambda e, dst=dst, c=c, cs_=cs_: e.tensor_tensor(
                                        out=dst[:, c, cs_], in0=rt[0][:], in1=rt[1][:], op=ALU.add),
                                        r=[rt[0].r(), rt[1].r()], w=[dst.r(c)])
                        for t in range(NT):
                            ps = nPS()
                            psb = ps[:].bitcast(BF16)
                            for c in range(2):
                                p.op("pe", lambda e, c=c, t=t, psb=psb: e.transpose(
                                    psb[:, c * 128:(c + 1) * 128], mkT[:, c, t * 128:(t + 1) * 128], identb[:]),
                                    r=[mkT.r(c), identb.r()], w=[ps.r()])
                            p.op("act", lambda e, t=t, psb=psb: e.copy(out=ktm[:, 2 + t, :], in_=psb[:, 0:256]),
                                 r=[ps.r()], w=[ktm.r(2 + t)])
                        for t in range(2):
                            ps = nPS()
                            mm_tm(ps[:, 0:256], ps.r(), wk, 0, 256, t)
                            p.op("act", lambda e, t=t, ps=ps: e.copy(out=ktm[:, t, :], in_=ps[:, 0:256]),
                                 r=[ps.r()], w=[ktm.r(t)])
                    p.barrier()
                    if stop == "C1":
                        break
                    with ExitStack() as L2:
                        wv = sb(L2, "wv", [128, 8, 512], BF16); wo = sb(L2, "wo", [128, 8, 512], BF16)
                        wg = sb(L2, "wgt", [128, 8, 16], BF16)
                        bmg = sb(L2, "bmg", [1, 16])
                        p.dma("pool", wv[:], wview(w_in_d[:, C_MLV:C_MLV + 512]), w=[wv.r()], sem="w")
                        p.dma("pool", wo[:], wview(w_in_d[:, C_MLO:C_MLO + 512]), w=[wo.r()], sem="w")
                        p.dma("pool", wg[:], wview(w_in_d[:, C_MLG:C_MLG + 16]), w=[wg.r()], sem="w")
                        p.dma("sp", bmg[:], b_mg_d, w=[bmg.r()], sem="x")
                        for t in range(NTT):
                            ps = nPD()
                            mm_tm(ps[:, 0:512], ps.r(), wv, 0, 512, t)
                            p.op("dve", lambda e, t=t, ps=ps: e.tensor_copy(
                                out=vM[:, t, :, 0:128], in_=ps[:, 0:512].rearrange("p (h d) -> p h d", h=4)),
                                r=[ps.r()], w=[vM.r(t)])
                            if t >= 2:
                                mm_tm(ps[:, 512:1024], ps.r(), wo, 0, 512, t)
                                p.op("act", lambda e, t=t, ps=ps: e.activation(
                                    out=osig[:, t - 2, :], in_=ps[:, 512:1024], func=AF.Sigmoid),
                                    r=[ps.r()], w=[osig.r(t - 2)])
                            ps2 = nPS()
                            for kc in range(8):
                                p.op("pe", lambda e, kc=kc, t=t, ps2=ps2: e.matmul(
                                    ps2[:, 0:16], lhsT=hT[:, kc, t * 128:(t + 1) * 128], rhs=wg[:, kc, :],
                                    start=(kc == 0), stop=(kc == 7)), r=[wg.r(), hT.r(t)], w=[ps2.r()])
                            p.op("pe", lambda e, ps2=ps2: e.matmul(
                                ps2[:, 16:32], lhsT=ones32[0:1, :], rhs=bmg[0:1, :], start=True, stop=True),
                                r=[ones32.r(), bmg.r()], w=[ps2.r()])
                            p.op("act", lambda e, t=t, ps2=ps2: e.copy(out=gts[:, t, :], in_=ps2[:, 0:16]),
                                 r=[ps2.r()], w=[gts.r()])
                            p.op("dve", lambda e, t=t, ps2=ps2: e.tensor_tensor(
                                out=gts[:, t, :], in0=gts[:, t, :], in1=ps2[:, 16:32], op=ALU.add),
                                r=[ps2.r(), gts.r()], w=[gts.r()])
                    p.barrier()
                    if stop == "C2":
                        break
                    gv = gts[:].rearrange("p t (k h) -> p t k h", k=4)
                    p.op("act", lambda e: e.activation(out=gts[:], in_=gts[:], func=AF.Tanh, scale=1.0 / 15.0),
                         r=[gts.r()], w=[gts.r()])
                    for d_ in range(2):
                        p.op("dve", lambda e, d_=d_: e.tensor_scalar_mul(
                            out=LI[:, :, d_ * 4:(d_ + 1) * 4], in0=gv[:, :, 2 * d_, :], scalar1=15.0),
                            r=[gts.r()], w=[LI.r()])
                        p.op("act", lambda e, d_=d_: e.activation(
                            out=LFn[:, :, d_ * 4:(d_ + 1) * 4], in_=gv[:, :, 2 * d_ + 1, :], func=AF.Exp, scale=-15.0),
                            r=[gts.r()], w=[LFn.r()])
                    p.op("act", lambda e: e.activation(out=LFn[:], in_=LFn[:], func=AF.Ln, bias=1.0),
                         r=[LFn.r()], w=[LFn.r()])
                    psc = nPS()
                    for t in range(NTT):
                        for d_, tri in ((0, trif), (1, trib)):
                            p.op("pe", lambda e, t=t, d_=d_, tri=tri: e.matmul(
                                psc[:, t * 8 + d_ * 4:t * 8 + d_ * 4 + 4], lhsT=tri[:], rhs=LFn[:, t, d_ * 4:(d_ + 1) * 4],
                                start=True, stop=True), r=[tri.r(), LFn.r()], w=[psc.r()])
                    p.op("dve", lambda e: e.tensor_copy(out=Bn[:].rearrange("p t k -> p (t k)"), in_=psc[:, 0:NTT * 8]),
                         r=[psc.r()], w=[Bn.r()])
                    psl = nPS()
                    p.op("pe", lambda e: e.matmul(psl[:, 0:NTT * 8], lhsT=ones32[:], rhs=LFn[:].rearrange("p t k -> p (t k)"),
                                                  start=True, stop=True), r=[ones32.r(), LFn.r()], w=[psl.r()])
                    p.op("act", lambda e: e.activation(out=EBL[:].rearrange("p t k -> p (t k)"), in_=psl[:, 0:NTT * 8],
                                                       func=AF.Exp, scale=-1.0), r=[psl.r()], w=[EBL.r()])
                    p.op("act", lambda e: e.activation(out=EBt[:], in_=Bn[:], func=AF.Exp, scale=-1.0),
                         r=[Bn.r()], w=[EBt.r()])
                    p.op("dve", lambda e: e.tensor_tensor(out=ES[:], in0=LI[:], in1=Bn[:], op=ALU.add),
                         r=[LI.r(), Bn.r()], w=[ES.r()])
                    p.op("act", lambda e: e.activation(out=ES[:], in_=ES[:], func=AF.Exp, bias=float(-np.log(8.0))),
                         r=[ES.r()], w=[ES.r()])
                    if stop == "C3":
                        p.barrier()
                        break
                    if b == 0:
                        emit_zero(NZ)
                    Hf = sb(L, "Hf", [128, NT, 512], BF16, nres=NT)
                    Cst = [sb(L, "Cst%d" % d_, [128, 4, 129]) for d_ in range(2)]
                    Cbf = [sb(L, "Cbf%d" % d_, [128, 4, 129], BF16) for d_ in range(2)]
                    vp = [sb(L, "vp%d" % d_, [128, 4, 129], BF16) for d_ in range(2)]
                    sTm = [sb(L, "sTm%d" % d_, [128, 4, 128], BF16) for d_ in range(2)]
                    sm = [sb(L, "sm%d" % d_, [128, 4, 4]) for d_ in range(2)]
                    Hs = [sb(L, "Hs%d" % i, [128, 512]) for i in range(2)]
                    Hq = [sb(L, "Hq%d" % i, [128, 512]) for i in range(1)]
                    fs = [sb(L, "fs%d" % i, [128, 8]) for i in range(2)]
                    omb = [sb(L, "omb%d" % i, [128, 512], BF16) for i in range(2)]
                    bwd_order = [1, 0] + list(range(NTT - 1, 1, -1))
                    tri4 = [sb(L, "tri4%d" % d_, [128, 4, 128], BF16) for d_ in range(2)]
                    for d_, tr_ in ((0, trif), (1, trib)):
                        for hd in range(4):
                            p.op("dve", lambda e, d_=d_, tr_=tr_, hd=hd: e.tensor_copy(out=tri4[d_][:, hd, :], in_=tr_[:]),
                                 r=[tr_.r()], w=[tri4[d_].r()])
                    psN_ = [PD[0], PD[2]]
                    psU_ = PD[1]
                    for d_ in range(2):
                        if stop in ("C4", "C6", "C7") and d_ == 1:
                            break
                        for step in range(NTT):
                            if (stop == "C6" and step == 2) or (stop == "C7" and step == 3):
                                break
                            t = step if d_ == 0 else bwd_order[step]
                            tri = trif if d_ == 0 else trib
                            psN = psN_[d_]
                            psS = PS[d_]
                            C_, Cb_, vp_, sT_, sm_ = Cst[d_], Cbf[d_], vp[d_], sTm[d_], sm[d_]
                            p.op(POOLENG, lambda e, t=t, d_=d_, vp_=vp_: e.tensor_tensor(
                                out=vp_[:], in0=vM[:, t, :, :], in1=bc(ES[:, t, d_ * 4:(d_ + 1) * 4].unsqueeze(2), [128, 4, 129]),
                                op=ALU.mult), r=[vM.r(t), ES.r()], w=[vp_.r()])
                            if t >= 2:
                                lt = t - 2
                                for hd in range(4):
                                    hp, c = (hd % 2) * 64, hd // 2
                                    p.op("pe", lambda e, hd=hd, hp=hp, c=c, lt=lt, psS=psS: e.matmul(
                                        psS[:, hd * 128:(hd + 1) * 128], lhsT=mkT[hp:hp + 64, c, lt * 128:(lt + 1) * 128],
                                        rhs=mqT[hp:hp + 64, c, lt * 128:(lt + 1) * 128], start=True, stop=True),
                                        r=[mkT.r(c), mqT.r(c)], w=[psS.r()])
                                    p.op("pe", lambda e, hd=hd, c=c, t=t, vp_=vp_: e.matmul(
                                        psU_[:, hd * 256:hd * 256 + 129], lhsT=ktm[:, t, c * 128:(c + 1) * 128], rhs=vp_[:, hd, :],
                                        start=True, stop=True), r=[ktm.r(t), vp_.r()], w=[psU_.r()])
                                p.op("dve", lambda e, sT_=sT_, psS=psS, d_=d_: e.tensor_tensor(
                                    out=sT_[:].rearrange("p h n -> p (h n)"), in0=psS[:, 0:512],
                                    in1=tri4[d_][:].rearrange("p h n -> p (h n)"), op=ALU.mult),
                                    r=[psS.r(), tri4[d_].r()], w=[sT_.r()])
                                for hd in range(4):
                                    hp, c = (hd % 2) * 64, hd // 2
                                    p.op("pe", lambda e, hd=hd, psN=psN, sT_=sT_, vp_=vp_: e.matmul(
                                        psN[:, hd * 256:hd * 256 + 129], lhsT=sT_[:, hd, :], rhs=vp_[:, hd, :],
                                        start=True, stop=(step == 0)), r=[sT_.r(), vp_.r()], w=[psN.r()])
                                    if step > 0:
                                        p.op("pe", lambda e, hd=hd, hp=hp, c=c, lt=lt, psN=psN, Cb_=Cb_: e.matmul(
                                            psN[:, hd * 256:hd * 256 + 129], lhsT=mqT[hp:hp + 64, c, lt * 128:(lt + 1) * 128],
                                            rhs=Cb_[hp:hp + 64, hd, :], start=False, stop=True),
                                            r=[mqT.r(c), Cb_.r()], w=[psN.r()])
                                nv = psN[:].rearrange("p (h x) -> p h x", x=256)
                                p.op("dve", lambda e, nv=nv, sm_=sm_, t=t, d_=d_: e.tensor_tensor(
                                    out=sm_[:, :, 0:1], in0=nv[:, :, 128:129], in1=EBt[:, t, d_ * 4:(d_ + 1) * 4].unsqueeze(2),
                                    op=ALU.mult), r=[psN.r(), EBt.r()], w=[sm_.r()])
                                p.op("dve", lambda e, sm_=sm_: e.tensor_scalar(
                                    out=sm_[:, :, 1:2], in0=sm_[:, :, 0:1], scalar1=-1.0, scalar2=1.0, op0=ALU.mult, op1=ALU.max),
                                    r=[sm_.r()], w=[sm_.r()])
                                p.op("dve", lambda e, sm_=sm_: e.tensor_tensor(
                                    out=sm_[:, :, 2:3], in0=sm_[:, :, 1:2], in1=sm_[:, :, 0:1], op=ALU.max),
                                    r=[sm_.r()], w=[sm_.r()])
                                p.op("dve", lambda e, sm_=sm_: e.reciprocal(out=sm_[:, :, 3:4], in_=sm_[:, :, 2:3]),
                                     r=[sm_.r()], w=[sm_.r()])
                                p.op("dve", lambda e, sm_=sm_, t=t, d_=d_: e.tensor_tensor(
                                    out=sm_[:, :, 0:1], in0=sm_[:, :, 3:4], in1=EBt[:, t, d_ * 4:(d_ + 1) * 4].unsqueeze(2),
                                    op=ALU.mult), r=[sm_.r(), EBt.r()], w=[sm_.r()])
                                if d_ == 0:
                                    p.op("dve", lambda e, nv=nv, sm_=sm_, lt=lt: e.tensor_tensor(
                                        out=Hf[:, lt, :].rearrange("p (h n) -> p h n", h=4), in0=nv[:, :, 0:128],
                                        in1=bc(sm_[:, :, 0:1], [128, 4, 128]), op=ALU.mult),
                                        r=[psN.r(), sm_.r()], w=[Hf.r(lt)])
                                elif stop != "C5":
                                    hs, hq, f_, ob = Hs[lt % 2], Hq[0], fs[lt % 2], omb[lt % 2]
                                    p.op("dve", lambda e, nv=nv, sm_=sm_, hs=hs: e.tensor_tensor(
                                        out=hs[:].rearrange("p (h n) -> p h n", h=4), in0=nv[:, :, 0:128],
                                        in1=bc(sm_[:, :, 0:1], [128, 4, 128]), op=ALU.mult),
                                        r=[psN.r(), sm_.r()], w=[hs.r()])
                                    p.op(POOLENG, lambda e, hs=hs, lt=lt: e.tensor_tensor(
                                        out=hs[:], in0=hs[:], in1=Hf[:, lt, :], op=ALU.add),
                                        r=[hs.r(), Hf.r(lt)], w=[hs.r()])
                                    p.op(POOLENG, lambda e, hs=hs, hq=hq: e.tensor_tensor(out=hq[:], in0=hs[:], in1=hs[:], op=ALU.mult),
                                         r=[hs.r()], w=[hq.r()])
                                    p.op("dve", lambda e, hq=hq, f_=f_: e.reduce_sum(
                                        out=f_[:, 0:4], in_=hq[:].rearrange("p (h n) -> p h n", h=4), axis=AX.X),
                                        r=[hq.r()], w=[f_.r()])
                                    p.op("act", lambda e, f_=f_: e.activation(out=f_[:, 4:8], in_=f_[:, 0:4], func=AF.Sqrt,
                                                                           scale=1.0 / 128.0, bias=EPS), r=[f_.r()], w=[f_.r()])
                                    p.op("dve", lambda e, f_=f_: e.reciprocal(out=f_[:, 4:8], in_=f_[:, 4:8]), r=[f_.r()], w=[f_.r()])
                                    p.op("dve", lambda e, hs=hs, f_=f_: e.tensor_tensor(
                                        out=hs[:].rearrange("p (h n) -> p h n", h=4), in0=hs[:].rearrange("p (h n) -> p h n", h=4),
                                        in1=bc(f_[:, 4:8].unsqueeze(2), [128, 4, 128]), op=ALU.mult),
                                        r=[hs.r(), f_.r()], w=[hs.r()])
                                    p.op(POOLENG, lambda e, hs=hs: e.tensor_tensor(out=hs[:], in0=hs[:], in1=ghd[:], op=ALU.mult),
                                         r=[hs.r(), ghd.r()], w=[hs.r()])
                                    p.op(POOLENG, lambda e, hs=hs, ob=ob, lt=lt: e.tensor_tensor(
                                        out=ob[:], in0=hs[:], in1=osig[:, lt, :], op=ALU.mult),
                                        r=[hs.r(), osig.r(lt)], w=[ob.r()])
                                    ps = psS
                                    psb = ps[:].bitcast(BF16)
                                    for c4 in range(4):
                                        p.op("pe", lambda e, c4=c4, ob=ob, psb=psb: e.transpose(
                                            psb[:, c4 * 128:(c4 + 1) * 128], ob[:, c4 * 128:(c4 + 1) * 128], identb[:]),
                                            r=[ob.r(), identb.r()], w=[ps.r()])
                                    p.op("act", lambda e, lt=lt, psb=psb: e.copy(
                                        out=omlT[:, :, lt * 128:(lt + 1) * 128],
                                        in_=psb[:, 0:512].rearrange("p (k n) -> p k n", k=4)),
                                        r=[ps.r()], w=[omlT.r(lt)])
                            for hd in range(4):
                                c = hd // 2
                                if t >= 2:
                                    break
                                p.op("pe", lambda e, hd=hd, c=c, t=t, vp_=vp_: e.matmul(
                                    psU_[:, hd * 256:hd * 256 + 129], lhsT=ktm[:, t, c * 128:(c + 1) * 128], rhs=vp_[:, hd, :],
                                    start=True, stop=True), r=[ktm.r(t), vp_.r()], w=[psU_.r()])
                            uv = psU_[:].rearrange("p (h x) -> p h x", x=256)[:, :, 0:129]
                            ebl = bc(EBL[:, t, d_ * 4:(d_ + 1) * 4].unsqueeze(2), [128, 4, 129])
                            if step == 0:
                                p.op("dve", lambda e, C_=C_, uv=uv, ebl=ebl: e.tensor_tensor(out=C_[:], in0=uv, in1=ebl, op=ALU.mult),
                                     r=[psU_.r(), EBL.r()], w=[C_.r()])
                            else:
                                p.op("dve", lambda e, C_=C_, uv=uv: e.tensor_tensor(out=C_[:], in0=uv, in1=C_[:], op=ALU.add),
                                     r=[psU_.r(), C_.r()], w=[C_.r()])
                                p.op("dve", lambda e, C_=C_, ebl=ebl: e.tensor_tensor(out=C_[:], in0=C_[:], in1=ebl, op=ALU.mult),
                                     r=[C_.r(), EBL.r()], w=[C_.r()])
                            p.op("act", lambda e, C_=C_, Cb_=Cb_: e.copy(out=Cb_[:], in_=C_[:]), r=[C_.r()], w=[Cb_.r()])
                    if b == 0 and "omlT" in dbg_d:
                        dump("omlT", omlT[:], omlT.res)
                p.barrier()
                if stop in ("C", "C4", "C5", "C6", "C7"):
                    break

                with ExitStack() as L:
                    wbn = sb(L, "wbn", [128, 4, D], BF16); wbm = sb(L, "wbm", [128, 4, D], BF16)
                    wout = sb(L, "wout", [128, 8, D], BF16)
                    wr = sb(L, "wr", [128, 8, NE]); br = sb(L, "br", [1, NE])
                    p.dma("pool", wbn[:], wview(w_bna_d), w=[wbn.r()], sem="w")
                    p.dma("pool", wbm[:], wview(w_bml_d), w=[wbm.r()], sem="w")
                    p.dma("pool", wout[:], wview(w_out_d), w=[wout.r()], sem="w")
                    p.dma("sp", wr[:], wview(w_r_d), w=[wr.r()], sem="x")
                    p.dma("sp", br[:], b_r_d, w=[br.r()], sem="x")
                    wgn = [sb(L, "wgn%d" % i, [128, 8, 128], BF16) for i in range(2)]
                    wgm = [sb(L, "wgm%d" % i, [128, 8, 128], BF16) for i in range(2)]
                    mT = sb(L, "mT", [128, 8, S], BF16, nres=32)
                    sg = [sb(L, "sg%d" % i, [128, 512]) for i in range(2)]
                    m1 = [sb(L, "m1%d" % i, [128, 512]) for i in range(2)]
                    xt = [sb(L, "xd", [128, D])] * 2
                    yt = [sb(L, "yd%d" % i, [128, D]) for i in range(2)]
                    jk = sb(L, "jk", [128, D], BF16)
                    h2 = [sb(L, "h2", [128, D])] * 2
                    h2hi = [sb(L, "h2hi%d" % i, [128, D], BF16) for i in range(2)]
                    h2lo = [sb(L, "h2lo", [128, D], BF16)] * 2
                    h2Tlo = [sb(L, "h2Tlo", [128, 8, 128], BF16)] * 2
                    wrhi = sb(L, "wrhi", [128, 8, NE], BF16); wrlo = sb(L, "wrlo", [128, 8, NE], BF16)
                    p.op("dve", lambda e: e.tensor_copy(out=wrhi[:], in_=wr[:]), r=[wr.r()], w=[wrhi.r()])
                    p.op("dve", lambda e: e.tensor_tensor(out=wrlo[:], in0=wr[:], in1=wrhi[:], op=ALU.subtract),
                         r=[wr.r(), wrhi.r()], w=[wrlo.r()])
                    st = [sb(L, "std%d" % i, [128, 4]) for i in range(2)]
                    lg = [sb(L, "lg%d" % i, [128, 3, NE]) for i in range(2)]
                    t8 = [sb(L, "t8%d" % i, [128, 20]) for i in range(2)]
                    mkb = [sb(L, "mkb%d" % i, [128, NE], BF16) for i in range(2)]
                    oh4 = [sb(L, "oh4%d" % i, [128, 4, NE]) for i in range(2)]
                    n = 0
                    for dc in range(8):
                        n += 1
                        a_, b_ = wgn[n % 2], wgm[n % 2]
                        p.dma("pool", a_[:], wview(w_in_d[:, C_GNA + dc * 128:C_GNA + (dc + 1) * 128]), w=[a_.r()], sem="w")
                        p.dma("pool", b_[:], wview(w_in_d[:, C_GML + dc * 128:C_GML + (dc + 1) * 128]), w=[b_.r()], sem="w")
                        for tb in range(4):
                            tok0 = 256 + tb * 512
                            pa, pb = nPD(), nPD()
                            mm_fm(pa[:, 0:512], pa.r(), a_, 0, tok0, 512)
                            mm_fm(pa[:, 512:1024], pa.r(), wbn, dc * 128, tb * 512, 512, nk=4, src=onaT)
                            mm_fm(pb[:, 0:512], pb.r(), b_, 0, tok0, 512)
                            mm_fm(pb[:, 512:1024], pb.r(), wbm, dc * 128, tb * 512, 512, nk=4, src=omlT)
                            p.op("act", lambda e, pa=pa: e.activation(out=sg[0][:], in_=pa[:, 0:512], func=AF.Sigmoid),
                                 r=[pa.r()], w=[sg[0].r()])
                            p.op("act", lambda e, pb=pb: e.activation(out=sg[1][:], in_=pb[:, 0:512], func=AF.Sigmoid),
                                 r=[pb.r()], w=[sg[1].r()])
                            p.op("dve", lambda e, pa=pa: e.tensor_tensor(out=m1[0][:], in0=pa[:, 512:1024], in1=sg[0][:], op=ALU.mult),
                                 r=[pa.r(), sg[0].r()], w=[m1[0].r()])
                            p.op("dve", lambda e, pb=pb: e.tensor_tensor(out=m1[1][:], in0=pb[:, 512:1024], in1=sg[1][:], op=ALU.mult),
                                 r=[pb.r(), sg[1].r()], w=[m1[1].r()])
                            p.op(POOLENG, lambda e, dc=dc, tb=tb: e.tensor_tensor(
                                out=mT[:, dc, tb * 512:(tb + 1) * 512], in0=m1[0][:], in1=m1[1][:], op=ALU.add),
                                r=[m1[0].r(), m1[1].r()], w=[mT.r(dc * 4 + tb)])
                    for tb in range(4):
                        for ti in range(4):
                            t = tb * 4 + ti
                            i = t % 2
                            py = PD[2]
                            for half in range(2):
                                for kc in range(8):
                                    p.op("pe", lambda e, kc=kc, half=half, t=t, py=py: e.matmul(
                                        py[:, half * 512:(half + 1) * 512], lhsT=mT[:, kc, t * 128:(t + 1) * 128],
                                        rhs=wout[:, kc, half * 512:(half + 1) * 512], start=(kc == 0), stop=(kc == 7)),
                                        r=[mT.r(kc * 4 + tb), wout.r()], w=[py.r()])
                            p.dma("sp", xt[i][:], x_d[b, t * 128:(t + 1) * 128, :], w=[xt[i].r()], sem="x")
                            rstd_of(py[:], [py.r()], jk, st[i])
                            p.op("dve", lambda e, i=i, py=py: e.scalar_tensor_tensor(
                                out=yt[i][:], in0=py[:], scalar=st[i][:, 1:2], in1=G_m[:], op0=ALU.mult, op1=ALU.mult),
                                r=[py.r(), st[i].r(), G_m.r()], w=[yt[i].r()])
                            p.op(POOLENG, lambda e, i=i: e.tensor_tensor(out=yt[i][:], in0=yt[i][:], in1=xt[i][:], op=ALU.add),
                                 r=[yt[i].r(), xt[i].r()], w=[yt[i].r()])
                            p.dma("sp", out_d[b, t * 128:(t + 1) * 128, :], yt[i][:], r=[yt[i].r()], w=[x1res[b][t]], sem="o")
                            if b == 0 and "x1" in dbg_d:
                                dump("x1", yt[i][:], yt[i].r(), dst=dbg_d["x1"][t])
                            rstd_of(yt[i][:], [yt[i].r()], jk, st[i])
                            p.op("dve", lambda e, i=i: e.scalar_tensor_tensor(
                                out=h2[i][:], in0=yt[i][:], scalar=st[i][:, 1:2], in1=A_f[:], op0=ALU.mult, op1=ALU.mult),
                                r=[yt[i].r(), st[i].r(), A_f.r()], w=[h2[i].r()])
                            p.op(POOLENG, lambda e, i=i: e.tensor_tensor(out=h2[i][:], in0=h2[i][:], in1=sh_f[:], op=ALU.add),
                                 r=[h2[i].r(), sh_f.r()], w=[h2[i].r()])
                            p.op("act", lambda e, i=i: e.copy(out=h2hi[i][:], in_=h2[i][:]), r=[h2[i].r()], w=[h2hi[i].r()])
                            p.op("dve", lambda e, i=i: e.tensor_tensor(out=h2lo[i][:], in0=h2[i][:], in1=h2hi[i][:], op=ALU.subtract),
                                 r=[h2[i].r(), h2hi[i].r()], w=[h2lo[i].r()])
                            pa_ = PS[i]
                            pab = pa_[:].bitcast(BF16)
                            for kc in range(8):
                                p.op("pe", lambda e, kc=kc, i=i, pab=pab: e.transpose(
                                    pab[:, kc * 128:(kc + 1) * 128], h2hi[i][:, kc * 128:(kc + 1) * 128], identb[:]),
                                    r=[h2hi[i].r(), identb.r()], w=[pa_.r()])
                            p.op("act", lambda e, t=t, pab=pab: e.copy(
                                out=hT[:, :, (2 + t) * 128:(3 + t) * 128], in_=pab.rearrange("p (k n) -> p k n", k=8)),
                                r=[pa_.r()], w=[hT.r(2 + t)])
                            pb_ = PD[i]
                            pbb = pb_[:].bitcast(BF16)
                            for kc in range(8):
                                p.op("pe", lambda e, kc=kc, i=i, pbb=pbb: e.transpose(
                                    pbb[:, kc * 128:(kc + 1) * 128], h2lo[i][:, kc * 128:(kc + 1) * 128], identb[:]),
                                    r=[h2lo[i].r(), identb.r()], w=[pb_.r()])
                            p.op("dve", lambda e, i=i, pbb=pbb: e.tensor_copy(
                                out=h2Tlo[i][:], in_=pbb[:, 0:1024].rearrange("p (k n) -> p k n", k=8)),
                                r=[pb_.r()], w=[h2Tlo[i].r()])
                            pl = PS[1 - i]
                            nmm = 0
                            for kc in range(8):
                                for (lh_, lres, w_) in ((hT[:, kc, (2 + t) * 128:(3 + t) * 128], hT.r(2 + t), wrhi),
                                                        (h2Tlo[i][:, kc, :], h2Tlo[i].r(), wrhi),
                                                        (hT[:, kc, (2 + t) * 128:(3 + t) * 128], hT.r(2 + t), wrlo)):
                                    nmm += 1
                                    p.op("pe", lambda e, kc=kc, lh_=lh_, w_=w_, pl=pl, nmm=nmm: e.matmul(
                                        pl[:, 0:NE], lhsT=lh_, rhs=w_[:, kc, :], start=(nmm == 1), stop=(nmm == 24)),
                                        r=[lres, w_.r()], w=[pl.r()])
                            p.op("pe", lambda e, pl=pl: e.matmul(pl[:, 32:64], lhsT=ones32[0:1, :], rhs=br[0:1, :], start=True, stop=True),
                                 r=[ones32.r(), br.r()], w=[pl.r()])
                            L_, T_ = lg[i], t8[i]
                            p.op("act", lambda e, L_=L_, pl=pl: e.copy(out=L_[:, 0, :], in_=pl[:, 0:NE]), r=[pl.r()], w=[L_.r()])
                            p.op("dve", lambda e, L_=L_, pl=pl: e.tensor_tensor(out=L_[:, 0, :], in0=L_[:, 0, :], in1=pl[:, 32:64], op=ALU.add),
                                 r=[pl.r(), L_.r()], w=[L_.r()])
                            if b == 0 and "logits" in dbg_d:
                                dump("logits", L_[:, 0, :], L_.r(), dst=dbg_d["logits"][t])
                            p.op("dve", lambda e, L_=L_, T_=T_: e.max(out=T_[:, 0:8], in_=L_[:, 0, :]), r=[L_.r()], w=[T_.r()])
                            gt = b * NT + t
                            mk_, oh_ = mkb[i], oh4[i]
                            p.op("dve", lambda e, L_=L_, T_=T_: e.tensor_scalar(
                                out=L_[:, 1, :], in0=L_[:, 0, :], scalar1=T_[:, 3:4], scalar2=None, op0=ALU.is_ge),
                                r=[L_.r(), T_.r()], w=[L_.r()])
                            p.op("dve", lambda e, L_=L_, mk_=mk_: e.tensor_copy(out=mk_[:], in_=L_[:, 1, :]), r=[L_.r()], w=[mk_.r()])
                            p.op("pe", lambda e, pl=pl, mk_=mk_: e.matmul(pl[:, 64:96], lhsT=trisb[:], rhs=mk_[:], start=True, stop=False),
                                 r=[trisb.r(), mk_.r()], w=[pl.r()])
                            p.op("pe", lambda e, pl=pl: e.matmul(pl[:, 64:96], lhsT=onesb[:], rhs=msum[:], start=False, stop=True),
                                 r=[onesb.r(), msum.r()], w=[pl.r()])
                            p.op("dve", lambda e, L_=L_, pl=pl: e.scalar_tensor_tensor(
                                out=L_[:, 2, :], in0=pl[:, 64:96], scalar=float(CAP - 1), in1=ecap[:], op0=ALU.min, op1=ALU.add),
                                r=[pl.r(), ecap.r()], w=[L_.r()])
                            p.op("dve", lambda e, mk_=mk_: e.tensor_tensor(out=msum[:], in0=msum[:], in1=mk_[:], op=ALU.add),
                                 r=[msum.r(), mk_.r()], w=[msum.r()])
                            for k in range(4):
                                p.op("dve", lambda e, k=k, L_=L_, T_=T_, oh_=oh_: e.tensor_scalar(
                                    out=oh_[:, k, :], in0=L_[:, 0, :], scalar1=T_[:, k:k + 1], scalar2=None, op0=ALU.is_equal),
                                    r=[L_.r(), T_.r()], w=[oh_.r()])
                                p.op("dve", lambda e, k=k, L_=L_, oh_=oh_: e.tensor_tensor(
                                    out=oh_[:, k, :], in0=oh_[:, k, :], in1=L_[:, 2, :], op=ALU.mult),
                                    r=[oh_.r(), L_.r()], w=[oh_.r()])
                            p.op("dve", lambda e, T_=T_, oh_=oh_: e.reduce_sum(out=T_[:, 12:16], in_=oh_[:], axis=AX.X),
                                 r=[oh_.r()], w=[T_.r()])
                            p.op("dve", lambda e, T_=T_, gt=gt: e.tensor_copy(out=RIi[:, gt, :], in_=T_[:, 12:16]),
                                 r=[T_.r()], w=[RIi.r(gt)])
                            p.op("dve", lambda e, T_=T_: e.tensor_scalar_mul(out=T_[:, 8:9], in0=T_[:, 0:1], scalar1=-1.0),
                                 r=[T_.r()], w=[T_.r()])
                            p.op("act", lambda e, T_=T_: e.activation(out=T_[:, 16:20], in_=T_[:, 0:4], func=AF.Exp,
                                                                     bias=T_[:, 8:9], scale=1.0), r=[T_.r()], w=[T_.r()])
                            p.op("dve", lambda e, T_=T_: e.reduce_sum(out=T_[:, 9:10], in_=T_[:, 16:20], axis=AX.X),
                                 r=[T_.r()], w=[T_.r()])
                            p.op("dve", lambda e, T_=T_: e.reciprocal(out=T_[:, 10:11], in_=T_[:, 9:10]), r=[T_.r()], w=[T_.r()])
                            p.op("dve", lambda e, T_=T_, gt=gt: e.tensor_scalar_mul(
                                out=RIw[:, gt, :], in0=T_[:, 16:20], scalar1=T_[:, 10:11]), r=[T_.r()], w=[RIw.r(gt)])
                            if b == 0 and t == 0:
                                wait_zero()
                            for k in range(4):
                                p.idma(out=xe_d, out_offset=bass.IndirectOffsetOnAxis(ap=RIi[:, gt, k:k + 1], axis=0),
                                       in_=h2hi[i][:], in_offset=None, bounds=NE * CAP - 1,
                                       r=[h2hi[i].r(), RIi.r(gt)], w=[], sem="sc")
                p.barrier()
            if stop is not None:
                break

            p.barrier()

        if stop is None:
            p.barrier()
            with ExitStack() as L:
                wgb = [sb(L, "wgb%d" % i, [128, 8, D], BF16) for i in range(2)]
                wlb = [sb(L, "wlb%d" % i, [128, 8, D], BF16) for i in range(2)]
                wdb = [sb(L, "wdb%d" % i, [128, 8, D], BF16) for i in range(2)]
                bdb = [sb(L, "bdb%d" % i, [128, D]) for i in range(2)]
                bg = sb(L, "bg", [128, 8, NE]); bl = sb(L, "bl", [128, 8, NE])
                p.dma("sp", bg[:], bgT_d, w=[bg.r()], sem="x")
                p.dma("sp", bl[:], blT_d, w=[bl.r()], sem="x")
                xr = [sb(L, "xr%d" % i, [128, 4, D], BF16) for i in range(2)]
                xT = [sb(L, "xT%d" % i, [128, 8, 512], BF16) for i in range(2)]
                aT2 = [sb(L, "aT%d" % i, [128, 8, 512], BF16, nres=8) for i in range(2)]
                gg = [sb(L, "gg%d" % i, [128, 512]) for i in range(2)]
                sg = [sb(L, "sgE%d" % i, [128, 512]) for i in range(2)]
                l1 = [sb(L, "l1%d" % i, [128, 512]) for i in range(2)]
                t1 = [sb(L, "t1%d" % i, [128, 512]) for i in range(2)]
                ysb = [sb(L, "ysb%d" % i, [128, D]) for i in range(2)]

                class BV:
                    def __init__(self, parent, c0):
                        self.par, self.c0, self.res = parent, c0, Res()

                    def r(self):
                        return self.res

                    def ap(self):
                        return self.par[:, self.c0:self.c0 + 512]

                banks = [BV(PD[2], 0), BV(PD[2], 512), BV(PS[0], 0), BV(PS[1], 0)]
                bki = [0]

                def nbank():
                    bki[0] += 1
                    return banks[bki[0] % 4]

                NBLK = CAP // 512
                NTOT = NE * NBLK
                ycnt = [0]

                def load_w(e_):
                    p.dma("pool", wgb[e_ % 2][:], wview(w_gate_d[e_]), w=[wgb[e_ % 2].r()], sem="w")
                    p.dma("pool", wlb[e_ % 2][:], wview(w_lin_d[e_]), w=[wlb[e_ % 2].r()], sem="w")
                    p.dma("pool", wdb[e_ % 2][:], wview(w_down_d[e_]), w=[wdb[e_ % 2].r()], sem="w")
                    p.dma("sp", bdb[e_ % 2][:], b_down_d[e_:e_ + 1, :].partition_broadcast(128), w=[bdb[e_ % 2].r()], sem="x")

                def emit_T(n):
                    e_, blk = divmod(n, NBLK)
                    xr_, xT_ = xr[n % 2], xT[n % 2]
                    row0 = e_ * CAP + blk * 512
                    p.dma("sp", xr_[:], xe_d[row0:row0 + 512, :].rearrange("(j p) d -> p j d", p=128), w=[xr_.r()], sem="x")
                    for j in range(4):
                        bk = nbank()
                        psb = bk.ap().bitcast(BF16)
                        for kc in range(8):
                            p.op("pe", lambda e, kc=kc, j=j, psb=psb, xr_=xr_: e.transpose(
                                psb[:, kc * 128:(kc + 1) * 128], xr_[:, j, kc * 128:(kc + 1) * 128], identb[:]),
                                r=[xr_.r(), identb.r()], w=[bk.r()])
                        eng = "act" if j % 2 else "dve"
                        if eng == "act":
                            p.op("act", lambda e, j=j, psb=psb, xT_=xT_: e.copy(
                                out=xT_[:, :, j * 128:(j + 1) * 128], in_=psb.rearrange("p (k n) -> p k n", k=8)),
                                r=[bk.r()], w=[xT_.r()])
                        else:
                            p.op("dve", lambda e, j=j, psb=psb, xT_=xT_: e.tensor_copy(
                                out=xT_[:, :, j * 128:(j + 1) * 128], in_=psb.rearrange("p (k n) -> p k n", k=8)),
                                r=[bk.r()], w=[xT_.r()])

                def emit_GL(n, fc):
                    e_ = n // NBLK
                    wg_, wl_ = wgb[e_ % 2], wlb[e_ % 2]
                    xT_, aT = xT[n % 2], aT2[n % 2]
                    j = fc % 2
                    pg = PD[j]
                    for (w_, c0) in ((wg_, 0), (wl_, 512)):
                        for kc in range(8):
                            p.op("pe", lambda e, kc=kc, w_=w_, c0=c0, pg=pg: e.matmul(
                                pg[:, c0:c0 + 512], lhsT=w_[:, kc, fc * 128:(fc + 1) * 128], rhs=xT_[:, kc, :],
                                start=(kc == 0), stop=(kc == 7)), r=[w_.r(), xT_.r()], w=[pg.r()])
                    p.op("dve", lambda e: e.tensor_scalar(
                        out=gg[j][:], in0=pg[:, 0:512], scalar1=bg[:, fc, e_:e_ + 1], scalar2=7.0, op0=ALU.add, op1=ALU.min),
                        r=[pg.r(), bg.r()], w=[gg[j].r()])
                    p.op("act", lambda e: e.activation(out=sg[j][:], in_=gg[j][:], func=AF.Sigmoid, scale=1.702),
                         r=[gg[j].r()], w=[sg[j].r()])
                    p.op("dve", lambda e: e.tensor_scalar(
                        out=l1[j][:], in0=pg[:, 512:1024], scalar1=bl[:, fc, e_:e_ + 1], scalar2=7.0, op0=ALU.add, op1=ALU.min),
                        r=[pg.r(), bl.r()], w=[l1[j].r()])
                    p.op("dve", lambda e: e.tensor_scalar(
                        out=l1[j][:], in0=l1[j][:], scalar1=-7.0, scalar2=1.0, op0=ALU.max, op1=ALU.add),
                        r=[l1[j].r()], w=[l1[j].r()])
                    p.op(POOLENG, lambda e: e.tensor_tensor(out=t1[j][:], in0=gg[j][:], in1=sg[j][:], op=ALU.mult),
                         r=[gg[j].r(), sg[j].r()], w=[t1[j].r()])
                    p.op("dve", lambda e: e.tensor_tensor(out=aT[:, fc, :], in0=t1[j][:], in1=l1[j][:], op=ALU.mult),
                         r=[t1[j].r(), l1[j].r()], w=[aT.r(fc)])

                def emit_DOWN(n):
                    e_, blk = divmod(n, NBLK)
                    wd_, bd_, aT = wdb[e_ % 2], bdb[e_ % 2], aT2[n % 2]
                    row0 = e_ * CAP + blk * 512
                    for ti in range(4):
                        ycnt[0] += 1
                        ys_ = ysb[ycnt[0] % 2]
                        for half in range(2):
                            bk = nbank()
                            for fc in range(8):
                                p.op("pe", lambda e, fc=fc, half=half, ti=ti, bk=bk: e.matmul(
                                    bk.ap(), lhsT=aT[:, fc, ti * 128:(ti + 1) * 128],
                                    rhs=wd_[:, fc, half * 512:(half + 1) * 512], start=(fc == 0), stop=(fc == 7)),
                                    r=[aT.r(fc), wd_.r()], w=[bk.r()])
                            p.op("dve", lambda e, half=half, bk=bk, ys_=ys_: e.tensor_tensor(
                                out=ys_[:, half * 512:(half + 1) * 512], in0=bk.ap(), in1=bd_[:, half * 512:(half + 1) * 512], op=ALU.add),
                                r=[bk.r(), bd_.r()], w=[ys_.r()])
                        p.dma("sp", ye_d[row0 + ti * 128:row0 + (ti + 1) * 128, :], ys_[:], r=[ys_.r()], w=[], sem="o")

                emit_T(0)
                for n in range(NTOT):
                    if n % NBLK == 0:
                        load_w(n // NBLK)
                    emit_GL(n, 0)
                    emit_GL(n, 1)
                    if n > 0:
                        emit_DOWN(n - 1)
                    for fc in range(2, 6):
                        emit_GL(n, fc)
                    if n + 1 < NTOT:
                        emit_T(n + 1)
                    emit_GL(n, 6)
                    emit_GL(n, 7)
                emit_DOWN(NTOT - 1)
            p.barrier()
            with ExitStack() as L:
                yk = [[sb(L, "yk%d_%d" % (i, k), [128, D]) for k in range(4)] for i in range(2)]
                acc = [sb(L, "acc%d" % i, [128, D]) for i in range(2)]
                xe1 = [sb(L, "xe1%d" % i, [128, D]) for i in range(2)]
                jk = sb(L, "jkF", [128, D], BF16)
                st = [sb(L, "stF%d" % i, [128, 4]) for i in range(2)]
                Gf = sb(L, "GfF", [128, D])
                for gt in range(nb * NT):
                    b, t = divmod(gt, NT)
                    i = gt % 2
                    if t == 0:
                        p.dma("sp", Gf[:], gf_d[b], r=[gfres[b]], w=[Gf.r()], sem="x")
                    for k in range(4):
                        p.idma(out=yk[i][k][:], out_offset=None, in_=ye_d,
                               in_offset=bass.IndirectOffsetOnAxis(ap=RIi[:, gt, k:k + 1], axis=0), bounds=NE * CAP - 1,
                               r=[RIi.r(gt)], w=[yk[i][k].r()], sem="ga")
                    p.dma("sp", xe1[i][:], out_d[b, t * 128:(t + 1) * 128, :], r=[x1res[b][t]], w=[xe1[i].r()], sem="x")
                    a_ = acc[i]
                    p.op("dve", lambda e, a_=a_, i=i, gt=gt: e.tensor_scalar_mul(out=a_[:], in0=yk[i][0][:], scalar1=RIw[:, gt, 0:1]),
                         r=[yk[i][0].r(), RIw.r(gt)], w=[a_.r()])
                    for k in range(1, 4):
                        p.op("dve", lambda e, a_=a_, i=i, gt=gt, k=k: e.scalar_tensor_tensor(
                            out=a_[:], in0=yk[i][k][:], scalar=RIw[:, gt, k:k + 1], in1=a_[:], op0=ALU.mult, op1=ALU.add),
                            r=[yk[i][k].r(), RIw.r(gt), a_.r()], w=[a_.r()])
                    if b == 0 and "ffn" in dbg_d:
                        dump("ffn", a_[:], a_.r(), dst=dbg_d["ffn"][t])
                    rstd_of(a_[:], [a_.r()], jk, st[i])
                    p.op("dve", lambda e, a_=a_, i=i: e.scalar_tensor_tensor(
                        out=a_[:], in0=a_[:], scalar=st[i][:, 1:2], in1=Gf[:], op0=ALU.mult, op1=ALU.mult),
                        r=[a_.r(), st[i].r(), Gf.r()], w=[a_.r()])
                    p.op("dve", lambda e, a_=a_, i=i: e.tensor_tensor(out=xe1[i][:], in0=xe1[i][:], in1=a_[:], op=ALU.add),
                         r=[xe1[i].r(), a_.r()], w=[xe1[i].r()])
                    p.dma("sp", out_d[b, t * 128:(t + 1) * 128, :], xe1[i][:], r=[xe1[i].r()], w=[x1res[b][t]], sem="o")
            p.barrier()

        p.barrier()
    return nc, p


def _host_consts():
    c = {}
    c["ident"] = np.eye(128, dtype=np.float32)
    j = np.arange(128)
    c["trif"] = (j[:, None] <= j[None, :]).astype(np.float32)
    c["trib"] = (j[:, None] >= j[None, :]).astype(np.float32)
    c["tris"] = (j[:, None] < j[None, :]).astype(np.float32)
    c["ecap"] = np.ascontiguousarray(np.broadcast_to((np.arange(NE) * CAP).astype(np.float32)[None, :], (128, NE)))
    t = np.arange(S)
    row = (t // 64).astype(np.float32)
    col = (t % 64).astype(np.float32)
    inv_freq = (np.float32(10000.0) ** (-np.arange(16, dtype=np.float32) / np.float32(16))).astype(np.float32)
    cos = np.zeros((128, S), np.float32)
    sin = np.zeros((128, S), np.float32)
    for pp in range(128):
        d = pp % 64
        pos = row if d < 32 else col
        dd = d % 32
        f = dd % 16
        sign = -1.0 if dd < 16 else 1.0
        ang = (pos * inv_freq[f]).astype(np.float32)
        cos[pp] = np.cos(ang)
        sin[pp] = sign * np.sin(ang)
    c["ropecos"] = cos
    c["ropesin"] = sin
    return c


def _na_bias_table(rpb):
    reps = [0, 1, 5, 14, 15]
    kk = np.arange(128)
    krl = kk // 64
    kc = kk % 64
    qq = np.arange(128)
    qrl = qq // 64
    qc = qq % 64
    cs = np.clip(qc - 8, 0, 48)
    tab = np.full((8, 128, 5, 5, 128), NEG, np.float32)
    for ci, i in enumerate(reps):
        js = int(np.clip(i - 2, 0, 11))
        for s in range(5):
            j = js + s
            kr = 2 * j + krl
            r = 2 * i + qrl
            rs = np.clip(r - 4, 0, 24)
            vr = (kr[:, None] >= rs[None, :]) & (kr[:, None] < rs[None, :] + 8)
            vc = (kc[:, None] >= cs[None, :]) & (kc[:, None] < cs[None, :] + 16)
            valid = vr & vc
            dr = np.clip(kr[:, None] - r[None, :] + 7, 0, 14)
            dc = np.clip(kc[:, None] - qc[None, :] + 15, 0, 30)
            vals = rpb[:, dr, dc]
            tab[:, :, ci, s, :] = np.where(valid[None], vals, np.float32(NEG))
    return np.ascontiguousarray(tab.reshape(8, 128, 5 * 640))


def _prep_inputs(inp, nb=NB, ncores=8):
    f = lambda a: np.ascontiguousarray(np.asarray(a, dtype=np.float32))
    consts = _host_consts()
    w_in = f(inp["w_in"][0])
    d = np.arange(64)
    partner = np.where((d % 32) < 16, d + 16, d - 16)
    permq = np.concatenate([C_MLQ + h * 64 + partner for h in range(4)])
    permk = np.concatenate([C_MLK + h * 64 + partner for h in range(4)])
    w_qkp = np.ascontiguousarray(np.concatenate([w_in[:, permq], w_in[:, permk]], axis=1))
    shared = dict(
        w_ada=f(inp["w_ada"][0]), b_ada=f(inp["b_ada"][0]).reshape(1, -1),
        g4=np.ascontiguousarray(np.stack([f(inp["g_mix_pre"][0]), f(inp["g_mix_post"][0]),
                                          f(inp["g_ffn_pre"][0]), f(inp["g_ffn_post"][0])])),
        w_in=w_in, w_qkp=w_qkp, b_mg=f(inp["b_mlstm_gates"][0]).reshape(1, 16),
        nab=_na_bias_table(f(inp["rpb"][0])), g_head=f(inp["g_mlstm_head"][0]).reshape(1, 512),
        w_bna=f(inp["w_branch_na"][0]), w_bml=f(inp["w_branch_ml"][0]), w_out=f(inp["w_out"][0]),
        w_router=f(inp["w_router"][0]), b_router=f(inp["b_router"][0]).reshape(1, NE),
        w_gate=f(inp["w_gate"][0]), w_lin=f(inp["w_lin"][0]), w_down=f(inp["w_down"][0]),
        bgT=np.ascontiguousarray(f(inp["b_gate"][0]).reshape(NE, 8, 128).transpose(2, 1, 0)),
        blT=np.ascontiguousarray(f(inp["b_lin"][0]).reshape(NE, 8, 128).transpose(2, 1, 0)),
        b_down=f(inp["b_down"][0]), **consts)
    x = f(inp["x"]); ctx = f(inp["ctx"]); c = f(inp["c"]); cc = f(inp["c_ctx"])
    maps = []
    for k in range(ncores):
        sl = slice(k * nb, (k + 1) * nb)
        c5 = np.zeros((5, D), np.float32)
        c5[:nb] = c[sl]
        c5[4] = cc
        cT = np.ascontiguousarray(c5.reshape(5, 8, 128).transpose(2, 1, 0))
        m = dict(shared)
        m.update(x=np.ascontiguousarray(x[sl]), ctx=np.ascontiguousarray(ctx[sl]), cT=cT)
        maps.append(m)
    return maps


def kernel(**inputs):
    maps = _prep_inputs(inputs)
    nc, _ = build()
    res = run_bass_kernel_spmd(nc, maps, core_ids=list(range(8)))
    return np.concatenate([r["out"] for r in res.results], axis=0).astype(np.float32)
```
